# Optimizing a Trainium2 kernel written in Bass

```python
import math
import jax, jax.numpy as jnp
from jax import lax
import numpy as np

D_MODEL = 2048
BATCH = 4
SEQ = 8192
DEPTH = 1

CTX_LEN = 256
GRID_W = 64

F_GROUPS = 4
F_GROUP_DIM = 256
F_WIDTH = F_GROUPS * F_GROUP_DIM
S_GROUP_DIM = 16
S_GROUPS = 32
S_WIDTH = S_GROUPS * S_GROUP_DIM
S_STATE = 64
DT_MIN = 1e-3
DT_MAX = 1e-1
N_DIRECTIONS = 2
N_BRANCH = 2
IN_WIDTH = F_WIDTH + S_WIDTH + N_BRANCH * D_MODEL
N_EXPERT_GROUPS = 4
EXPERTS_PER_GROUP = 8
N_EXPERTS = N_EXPERT_GROUPS * EXPERTS_PER_GROUP
TOP_K = 2
D_EXPERT = 1024
ROW_BLOCK = 128
N_MOD = 6
LN_EPS = 1e-5
DEEPNORM_ALPHA = (2.0 * DEPTH) ** 0.25
DEEPNORM_BETA = (8.0 * DEPTH) ** -0.25

kernel_name = "hybrid_fourier_s5_hmoe_dit"

F32 = jnp.float32


def layer_norm(x, gain=None, bias=None):
    xf = x.astype(F32)
    mu = xf.mean(-1, keepdims=True)
    var = jnp.square(xf - mu).mean(-1, keepdims=True)
    y = (xf - mu) * lax.rsqrt(var + LN_EPS)
    if gain is not None:
        y = y * gain.astype(F32) + bias.astype(F32)
    return y.astype(x.dtype)


def modulate(x, shift, scale):
    return layer_norm(x) * (1 + scale[:, None]) + shift[:, None]


def fourier_mix(u, pos_shape):
    b = u.shape[0]
    uf = u.astype(F32).reshape((b,) + pos_shape + (F_GROUPS, F_GROUP_DIM))
    axes = tuple(range(1, 1 + len(pos_shape))) + (uf.ndim - 1,)
    y = jnp.fft.fftn(uf, axes=axes, norm="ortho").real
    return y.reshape(u.shape).astype(u.dtype)


def _linear_recurrence(left, right):
    a_l, b_l = left
    a_r, b_r = right
    return a_r * a_l, a_r * b_l + b_r


def s5_states(u, lam_re, lam_im, log_dt, b_re, b_im, s0):
    lam = lax.complex(lam_re.astype(F32), lam_im.astype(F32))
    dt = jnp.exp(log_dt.astype(F32))[:, None]
    lam_bar = jnp.exp(lam * dt)
    b_cplx = lax.complex(b_re.astype(F32), b_im.astype(F32))
    b_bar = ((lam_bar - 1.0) / lam)[:, :, None] * b_cplx
    bu = jnp.einsum("gph,blgh->blgp", b_bar, u.astype(jnp.complex64))
    if s0 is not None:
        bu = bu.at[:, 0].add(lam_bar * s0)
    a = jnp.broadcast_to(lam_bar, bu.shape)
    _, states = lax.associative_scan(_linear_recurrence, (a, bu), axis=1)
    return states


def s5_readout(states, c_re, c_im):
    c_cplx = lax.complex(c_re.astype(F32), c_im.astype(F32))
    return jnp.einsum("ghp,blgp->blgh", c_cplx, states).real


def s5_glu(y, w_glu, b_glu):
    y = jax.nn.gelu(y.reshape(y.shape[0], y.shape[1], S_WIDTH))
    return y * jax.nn.sigmoid(y @ w_glu.astype(F32) + b_glu.astype(F32))


def merge_branches(f_out, s_out, z_f, z_s, w_f_proj, w_s_proj, w_out):
    m = jax.nn.sigmoid(z_f) * (f_out @ w_f_proj) + jax.nn.sigmoid(z_s) * (s_out @ w_s_proj)
    return m @ w_out


def hierarchical_route(h, w_group, b_group, w_expert, b_expert):
    t = h.shape[0]
    g_prob = jax.nn.softmax((h @ w_group + b_group).astype(F32), axis=-1)
    g_top, g_idx = lax.top_k(g_prob, 1)
    e_logits = (h @ w_expert + b_expert).astype(F32).reshape(t, N_EXPERT_GROUPS, EXPERTS_PER_GROUP)
    e_logits = jnp.take_along_axis(e_logits, g_idx[:, :, None], axis=1)[:, 0]
    e_top, e_local = lax.top_k(jax.nn.softmax(e_logits, axis=-1), TOP_K)
    weights = g_top * e_top / e_top.sum(-1, keepdims=True)
    experts = g_idx * EXPERTS_PER_GROUP + e_local
    return experts, weights


def expert_dispatch(h, experts, weights, w1, w3, w2):
    t, d = h.shape
    n_assign = t * TOP_K
    e_flat = experts.reshape(-1)
    w_flat = weights.reshape(-1)
    tok_flat = jnp.repeat(jnp.arange(t, dtype=jnp.int32), TOP_K)
    counts = jnp.bincount(e_flat, length=N_EXPERTS)
    padded = (counts + ROW_BLOCK - 1) // ROW_BLOCK * ROW_BLOCK
    start = jnp.cumsum(counts) - counts
    pend = jnp.cumsum(padded)
    pstart = pend - padded
    order = jnp.argsort(e_flat)
    e_sorted = e_flat[order]
    dest = pstart[e_sorted] + jnp.arange(n_assign) - start[e_sorted]
    n_blocks = -(-(n_assign + N_EXPERTS * (ROW_BLOCK - 1)) // ROW_BLOCK)
    n_rows = n_blocks * ROW_BLOCK
    row_tok = jnp.full((n_rows,), t, jnp.int32).at[dest].set(tok_flat[order])
    row_w = jnp.zeros((n_rows,), F32).at[dest].set(w_flat[order])
    block_exp = jnp.minimum(
        jnp.searchsorted(pend, jnp.arange(n_blocks) * ROW_BLOCK, side="right"), N_EXPERTS - 1)
    h_pad = jnp.concatenate([h, jnp.zeros((1, d), h.dtype)], axis=0)
    xb = h_pad[row_tok].reshape(n_blocks, ROW_BLOCK, d)

    def run_block(args):
        xblk, e = args
        return (jax.nn.silu(xblk @ w1[e]) * (xblk @ w3[e])) @ w2[e]

    yb = lax.map(run_block, (xb, block_exp)).reshape(n_rows, d)
    out = jnp.zeros_like(h_pad).at[row_tok].add(yb * row_w[:, None].astype(yb.dtype))
    return out[:t]


def hierarchical_moe(h, w_group, b_group, w_expert, b_expert, w1, w3, w2):
    experts, weights = hierarchical_route(h, w_group, b_group, w_expert, b_expert)
    return expert_dispatch(h, experts, weights, w1, w3, w2)


def setup_inputs(seed: int = 0) -> dict:
    key = jax.random.key(seed)
    ks = jax.random.split(key, 32)
    nrm = jax.random.normal
    L, D = DEPTH, D_MODEL
    G, P, H = S_GROUPS, S_STATE, S_GROUP_DIM
    lam_im0 = jnp.pi * jnp.arange(P, dtype=F32)
    return {
        "x": nrm(ks[0], (BATCH, SEQ, D), F32),
        "c": nrm(ks[1], (BATCH, D), F32),
        "ctx": nrm(ks[2], (BATCH, CTX_LEN, D), F32),
        "c_ctx": nrm(ks[3], (D,), F32),
        "w_ada": nrm(ks[4], (L, D, N_MOD * D), F32) * D ** -0.5,
        "b_ada": 0.01 * nrm(ks[5], (L, N_MOD * D), F32),
        "w_in": nrm(ks[6], (L, D, IN_WIDTH), F32) * D ** -0.5,
        "w_f_proj": nrm(ks[7], (L, F_WIDTH, D), F32) * F_WIDTH ** -0.5,
        "lam_re": -0.5 * (1.0 + 0.01 * nrm(ks[8], (L, N_DIRECTIONS, G, P), F32)),
        "lam_im": lam_im0 + 0.01 * nrm(ks[9], (L, N_DIRECTIONS, G, P), F32),
        "log_dt": jax.random.uniform(ks[10], (L, N_DIRECTIONS, G), F32,
                                     math.log(DT_MIN), math.log(DT_MAX)),
        "b_re": nrm(ks[11], (L, N_DIRECTIONS, G, P, H), F32) * (2.0 * H) ** -0.5,
        "b_im": nrm(ks[12], (L, N_DIRECTIONS, G, P, H), F32) * (2.0 * H) ** -0.5,
        "c_re": nrm(ks[13], (L, N_DIRECTIONS, G, H, P), F32) * (2.0 * P) ** -0.5,
        "c_im": nrm(ks[14], (L, N_DIRECTIONS, G, H, P), F32) * (2.0 * P) ** -0.5,
        "d_skip": nrm(ks[15], (L, G, H), F32),
        "w_glu": nrm(ks[16], (L, S_WIDTH, S_WIDTH), F32) * S_WIDTH ** -0.5,
        "b_glu": 0.01 * nrm(ks[17], (L, S_WIDTH), F32),
        "w_s_proj": nrm(ks[18], (L, S_WIDTH, D), F32) * S_WIDTH ** -0.5,
        "w_out": nrm(ks[19], (L, D, D), F32) * D ** -0.5 * DEEPNORM_BETA,
        "ln1_g": 1.0 + 0.01 * nrm(ks[20], (L, D), F32),
        "ln1_b": 0.01 * nrm(ks[21], (L, D), F32),
        "w_group": nrm(ks[22], (L, D, N_EXPERT_GROUPS), F32) * D ** -0.5,
        "b_group": 0.01 * nrm(ks[23], (L, N_EXPERT_GROUPS), F32),
        "w_expert": nrm(ks[24], (L, D, N_EXPERTS), F32) * D ** -0.5,
        "b_expert": 0.01 * nrm(ks[25], (L, N_EXPERTS), F32),
        "w1": nrm(ks[26], (L, N_EXPERTS, D, D_EXPERT), F32) * D ** -0.5,
        "w3": nrm(ks[27], (L, N_EXPERTS, D, D_EXPERT), F32) * D ** -0.5,
        "w2": nrm(ks[28], (L, N_EXPERTS, D_EXPERT, D), F32) * D_EXPERT ** -0.5 * DEEPNORM_BETA,
        "ln2_g": 1.0 + 0.01 * nrm(ks[29], (L, D), F32),
        "ln2_b": 0.01 * nrm(ks[30], (L, D), F32),
    }


def reference(x, c, ctx, c_ctx, w_ada, b_ada, w_in, w_f_proj, lam_re, lam_im, log_dt,
              b_re, b_im, c_re, c_im, d_skip, w_glu, b_glu, w_s_proj, w_out,
              ln1_g, ln1_b, w_group, b_group, w_expert, b_expert, w1, w3, w2,
              ln2_g, ln2_b):
    bsz, seq, _ = x.shape
    ctx_len = ctx.shape[1]
    rows = seq // GRID_W
    ctx_x = ctx
    cut = (F_WIDTH, F_WIDTH + S_WIDTH, F_WIDTH + S_WIDTH + D_MODEL)
    for l in range(DEPTH):
        last = l == DEPTH - 1
        mod = jnp.split(jax.nn.silu(c) @ w_ada[l] + b_ada[l], N_MOD, axis=-1)
        mod_c = jnp.split(jax.nn.silu(c_ctx)[None] @ w_ada[l] + b_ada[l], N_MOD, axis=-1)
        ssm_dir = [(lam_re[l, d], lam_im[l, d], log_dt[l, d], b_re[l, d], b_im[l, d])
                   for d in range(N_DIRECTIONS)]

        h = modulate(x, mod[0], mod[1])
        u_f, u_s, z_f, z_s = jnp.split(h @ w_in[l], cut, axis=-1)
        hc = modulate(ctx_x, mod_c[0], mod_c[1])
        if last:
            uc_s = hc @ w_in[l, :, F_WIDTH:F_WIDTH + S_WIDTH]
        else:
            uc_f, uc_s, zc_f, zc_s = jnp.split(hc @ w_in[l], cut, axis=-1)
        uc_s = uc_s.astype(F32).reshape(bsz, ctx_len, S_GROUPS, S_GROUP_DIM)
        sc_f = s5_states(uc_s, *ssm_dir[0], None)
        sc_b = s5_states(uc_s[:, ::-1], *ssm_dir[1], None)
        u_s = u_s.astype(F32).reshape(bsz, seq, S_GROUPS, S_GROUP_DIM)
        s_f = s5_states(u_s, *ssm_dir[0], sc_f[:, -1])
        s_b = s5_states(u_s[:, ::-1], *ssm_dir[1], sc_b[:, -1])[:, ::-1]
        y_s = (s5_readout(s_f, c_re[l, 0], c_im[l, 0]) + s5_readout(s_b, c_re[l, 1], c_im[l, 1])
               + d_skip[l] * u_s)
        s_out = s5_glu(y_s, w_glu[l], b_glu[l]).astype(x.dtype)
        f_out = fourier_mix(u_f, (rows, GRID_W))
        mix = merge_branches(f_out, s_out, z_f, z_s, w_f_proj[l], w_s_proj[l], w_out[l])
        x_mid = layer_norm(DEEPNORM_ALPHA * x + mod[2][:, None] * mix, ln1_g[l], ln1_b[l])
        if not last:
            yc_s = (s5_readout(sc_f, c_re[l, 0], c_im[l, 0])
                    + s5_readout(sc_b[:, ::-1], c_re[l, 1], c_im[l, 1]) + d_skip[l] * uc_s)
            sc_out = s5_glu(yc_s, w_glu[l], b_glu[l]).astype(x.dtype)
            fc_out = fourier_mix(uc_f, (ctx_len,))
            mix_c = merge_branches(fc_out, sc_out, zc_f, zc_s, w_f_proj[l], w_s_proj[l], w_out[l])
            ctx_x = layer_norm(DEEPNORM_ALPHA * ctx_x + mod_c[2][:, None] * mix_c, ln1_g[l], ln1_b[l])
        x = x_mid

        h2 = modulate(x, mod[3], mod[4]).reshape(bsz * seq, D_MODEL)
        moe_args = (w_group[l], b_group[l], w_expert[l], b_expert[l], w1[l], w3[l], w2[l])
        if last:
            y2 = hierarchical_moe(h2, *moe_args).reshape(bsz, seq, D_MODEL)
        else:
            h2c = modulate(ctx_x, mod_c[3], mod_c[4]).reshape(bsz * ctx_len, D_MODEL)
            y_all = hierarchical_moe(jnp.concatenate([h2, h2c], axis=0), *moe_args)
            y2 = y_all[:bsz * seq].reshape(bsz, seq, D_MODEL)
            y2c = y_all[bsz * seq:].reshape(bsz, ctx_len, D_MODEL)
            ctx_x = layer_norm(DEEPNORM_ALPHA * ctx_x + mod_c[5][:, None] * y2c, ln2_g[l], ln2_b[l])
        x = layer_norm(DEEPNORM_ALPHA * x + mod[5][:, None] * y2, ln2_g[l], ln2_b[l])
    return x
```

```python
import os
from contextlib import ExitStack
import numpy as np
import ml_dtypes
import concourse.bass as bass
import concourse.mybir as mybir
from concourse.bass_utils import run_bass_kernel_spmd

F32 = mybir.dt.float32
BF16 = mybir.dt.bfloat16
I32 = mybir.dt.int32
ALU = mybir.AluOpType
AF = mybir.ActivationFunctionType
AX = mybir.AxisListType
NPBF = ml_dtypes.bfloat16

D = 2048
TOWN = 4096
TALL = 8192
ALPHA = 2.0 ** 0.25
EPS = 1e-5
PI = float(np.pi)


class Buf:
    __slots__ = ("name", "w", "r")

    def __init__(self, name=""):
        self.name = name
        self.w = None
        self.r = []


class Flow:
    def __init__(self, nc, n_dma_sems=24):
        self.nc = nc
        self.engs = {"pe": nc.tensor, "dve": nc.vector, "act": nc.scalar, "pool": nc.gpsimd, "sp": nc.sync}
        self.sem = {k: nc.alloc_semaphore("s_" + k) for k in self.engs}
        self.cnt = {k: 0 for k in self.engs}
        self.waited = {k: {} for k in self.engs}
        self.pending = {k: [] for k in self.engs}
        self.dsems = [nc.alloc_semaphore("d%d" % i) for i in range(n_dma_sems)]
        self.dcnt = [0] * n_dma_sems
        self.dnext = 0
        self.semobj = {}
        for k, s in self.sem.items():
            self.semobj[id(s)] = s
        for s in self.dsems:
            self.semobj[id(s)] = s

    def _need(self, reads, writes):
        need = {}

        def add(p):
            if p is None:
                return
            s, v = p
            k = id(s)
            if need.get(k, 0) < v:
                need[k] = v
        for b in reads:
            add(b.w)
        for b in writes:
            add(b.w)
            for p in b.r:
                add(p)
        return need

    def _emit_waits(self, e, need):
        eng = self.engs[e]
        own = id(self.sem[e])
        for k, v in need.items():
            if k == own and e == "pe":
                continue
            if self.waited[e].get(k, 0) >= v:
                continue
            eng.wait_ge(self.semobj[k], v)
            self.waited[e][k] = v

    def _commit(self, reads, writes, tag):
        for b in writes:
            b.w = tag
            b.r = []
        for b in reads:
            if b.w is not tag:
                b.r.append(tag)
                if len(b.r) > 48:
                    best = {}
                    for (s, v) in b.r:
                        if best.get(id(s), (None, 0))[1] < v:
                            best[id(s)] = (s, v)
                    b.r = list(best.values())

    def op(self, e, fn, reads=(), writes=(), signal=True):
        reads = list(reads)
        writes = list(writes)
        need = self._need(reads, writes)
        self._emit_waits(e, need)
        ins = fn(self.engs[e])
        if signal:
            self.cnt[e] += 1
            ins.then_inc(self.sem[e], 1)
            tag = (self.sem[e], self.cnt[e])
            pr = [b for (b, w) in self.pending[e] if not w] + reads
            pw = [b for (b, w) in self.pending[e] if w] + writes
            self.pending[e] = []
            self._commit(pr, pw, tag)
        else:
            assert e == "pe"
            for b in reads:
                self.pending[e].append((b, False))
            for b in writes:
                self.pending[e].append((b, True))
        return ins

    def dma(self, q, fn, reads=(), writes=()):
        reads = list(reads)
        writes = list(writes)
        need = self._need(reads, writes)
        i = self.dnext
        self.dnext = (self.dnext + 1) % len(self.dsems)
        s = self.dsems[i]
        if self.dcnt[i] > 0:
            need[id(s)] = max(need.get(id(s), 0), self.dcnt[i])
        self._emit_waits(q, need)
        ins = fn(self.engs[q])
        self.dcnt[i] += 16
        ins.then_inc(s, 16)
        tag = (s, self.dcnt[i])
        self._commit(reads, writes, tag)
        return ins

    def barrier(self):
        need = {}
        for k, s in self.sem.items():
            if self.cnt[k] > 0:
                need[id(s)] = self.cnt[k]
        for i, s in enumerate(self.dsems):
            if self.dcnt[i] > 0:
                need[id(s)] = self.dcnt[i]
        for e in self.engs:
            assert not self.pending[e]
            self._emit_waits(e, dict(need))

    def finish(self, bufs, e="sp"):
        need = self._need(bufs, [])
        self._emit_waits(e, need)


def host_consts(half):
    c = {}
    c["ident_bf"] = np.eye(128, dtype=np.float32).astype(NPBF)
    c["ident_f"] = np.eye(128, dtype=np.float32)
    rv = np.arange(128)
    rt = (rv + 64 * half) % 128
    kt = np.arange(64) + 64 * half
    ang = 2 * np.pi * np.outer(rt, kt) / 128.0
    s = 1.0 / np.sqrt(128.0)
    c["rowdft"] = np.concatenate([np.cos(ang) * s, -np.sin(ang) * s], axis=1).astype(NPBF)
    ch = np.arange(256)
    ang = 2 * np.pi * np.outer(ch, ch) / 256.0
    C = np.cos(ang) / 16.0
    S = np.sin(ang) / 16.0
    c["chA"] = np.concatenate([C, -S], axis=1).reshape(2, 128, 512).transpose(1, 0, 2).copy().astype(NPBF)
    c["chB"] = np.concatenate([S, C], axis=1).reshape(2, 128, 512).transpose(1, 0, 2).copy().astype(NPBF)
    cc = np.arange(64)
    ang = 2 * np.pi * np.outer(cc, cc) / 64.0
    CC = np.zeros((128, 128), np.float32)
    CS = np.zeros((128, 128), np.float32)
    CC[0:64, 0:64] = np.cos(ang) / 8.0
    CS[0:64, 0:64] = np.sin(ang) / 8.0
    c["colC"] = CC.astype(NPBF)
    c["colS"] = CS.astype(NPBF)
    c["halfcol"] = np.full((128, 1), float(half), np.float32)
    c["jrow"] = np.tile(np.arange(9, dtype=np.float32)[None, :], (128, 1))
    c["crow"] = np.tile(np.arange(544, dtype=np.float32)[None, :], (128, 1))
    m = np.zeros((4, 32, 4, 32), np.float32)
    for q in range(4):
        m[q, :, q, :] = 1.0
    c["blkmask"] = m.reshape(128, 128)
    tri = (np.arange(128)[:, None] < np.arange(128)[None, :]).astype(np.float32)
    c["tri"] = tri.astype(NPBF)
    c["ones_bf"] = np.ones((128, 128), np.float32).astype(NPBF)
    c["thr"] = np.tile((128.0 * np.arange(32, dtype=np.float32))[None, :], (128, 1))
    c["blkrow"] = np.tile((1.0 * np.arange(96, dtype=np.float32))[None, :], (128, 1))
    c["pidx"] = np.arange(128, dtype=np.float32).reshape(128, 1)
    c["pg4"] = (4.0 * np.arange(128, dtype=np.float32)[:, None] + np.arange(4, dtype=np.float32)[None, :])
    tk = (np.arange(128)[:, None] + 128 * np.arange(32)[None, :]).astype(np.int32)
    c["tokid"] = np.concatenate([tk, tk + TOWN], axis=1).astype(np.int32)
    return c


CONST_SPECS = {
    "ident_bf": ([128, 128], BF16), "ident_f": ([128, 128], F32), "rowdft": ([128, 128], BF16),
    "chA": ([128, 2, 512], BF16), "chB": ([128, 2, 512], BF16), "colC": ([128, 128], BF16), "colS": ([128, 128], BF16),
    "halfcol": ([128, 1], F32), "jrow": ([128, 9], F32), "crow": ([128, 544], F32), "blkmask": ([128, 128], F32),
    "tri": ([128, 128], BF16), "ones_bf": ([128, 128], BF16), "thr": ([128, 32], F32), "blkrow": ([128, 96], F32),
    "pidx": ([128, 1], F32), "tokid": ([128, 64], I32), "pg4": ([128, 4], F32),
}


def host_layout(inp, b, half):
    m = {}
    xb = inp["x"][b]
    m["x"] = np.ascontiguousarray(np.concatenate([xb[half * TOWN:(half + 1) * TOWN], xb[(1 - half) * TOWN:(2 - half) * TOWN]], axis=0))
    m["ctx"] = np.ascontiguousarray(inp["ctx"][b])
    cv = np.stack([inp["c"][b], inp["c_ctx"]], axis=-1)
    m["cvT"] = np.ascontiguousarray(cv.reshape(16, 128, 2).transpose(1, 0, 2))
    m["w_ada"] = inp["w_ada"][0]
    m["b_adaT"] = np.ascontiguousarray(inp["b_ada"][0].reshape(96, 128).T)
    m["w_in"] = inp["w_in"][0]
    m["w_f_proj"] = inp["w_f_proj"][0]
    m["w_s_proj"] = inp["w_s_proj"][0]
    m["w_out"] = inp["w_out"][0]
    m["w_glu"] = inp["w_glu"][0]
    m["b_gluT"] = np.ascontiguousarray(inp["b_glu"][0].reshape(4, 128).T)
    m["lnrows"] = np.ascontiguousarray(np.stack([inp["ln1_g"][0], inp["ln1_b"][0], inp["ln2_g"][0], inp["ln2_b"][0]], axis=0))

    def pd(a):
        return np.ascontiguousarray(a.reshape(2, 16, 2, 64).transpose(2, 3, 0, 1).reshape(128, 32))
    m["lam_re"] = pd(inp["lam_re"][0])
    m["lam_im"] = pd(inp["lam_im"][0])
    m["log_dt"] = pd(np.broadcast_to(inp["log_dt"][0][:, :, None], (2, 32, 64)))

    def bpad(a):
        o = np.zeros((2, 64, 2, 16, 2, 16), np.float32)
        a6 = a.reshape(2, 16, 2, 64, 16)
        for g2 in range(2):
            o[g2, :, :, :, g2, :] = a6[:, :, g2].transpose(2, 0, 1, 3)
        return np.ascontiguousarray(o.reshape(128, 32, 32))
    m["b_re"] = bpad(inp["b_re"][0])
    m["b_im"] = bpad(inp["b_im"][0])

    def cpad(a):
        o = np.zeros((2, 64, 2, 16, 2, 16), np.float32)
        a6 = a.reshape(2, 16, 2, 16, 64)
        for g2 in range(2):
            o[g2, :, :, :, g2, :] = a6[:, :, g2].transpose(3, 0, 1, 2)
        return np.ascontiguousarray(o.reshape(128, 32, 32))
    m["c_re"] = cpad(inp["c_re"][0])
    m["c_im"] = cpad(inp["c_im"][0])
    m["d_skipT"] = np.ascontiguousarray(inp["d_skip"][0].reshape(4, 128).T)
    wr = np.concatenate([inp["w_group"][0], inp["w_expert"][0]], axis=1)
    m["w_rt"] = np.ascontiguousarray(wr.reshape(16, 128, 36).transpose(1, 0, 2))
    m["b_rt"] = np.ascontiguousarray(np.concatenate([inp["b_group"][0], inp["b_expert"][0]])[None, :])
    m["w1"] = inp["w1"][0]
    m["w3"] = inp["w3"][0]
    m["w2"] = inp["w2"][0]
    m.update(host_consts(half))
    return m


IN_SPECS = {
    "x": ([TALL, D], F32), "ctx": ([256, D], F32), "cvT": ([128, 16, 2], F32), "w_ada": ([D, 6 * D], F32),
    "b_adaT": ([128, 96], F32), "w_in": ([D, 5632], F32), "w_f_proj": ([1024, D], F32), "w_s_proj": ([512, D], F32),
    "w_out": ([D, D], F32), "w_glu": ([512, 512], F32), "b_gluT": ([128, 4], F32), "lnrows": ([4, D], F32),
    "lam_re": ([128, 32], F32), "lam_im": ([128, 32], F32), "log_dt": ([128, 32], F32),
    "b_re": ([128, 32, 32], F32), "b_im": ([128, 32, 32], F32), "c_re": ([128, 32, 32], F32), "c_im": ([128, 32, 32], F32),
    "d_skipT": ([128, 4], F32), "w_rt": ([128, 16, 36], F32), "b_rt": ([1, 36], F32),
    "w1": ([32, D, 1024], F32), "w3": ([32, D, 1024], F32), "w2": ([32, 1024, D], F32),
}


def build(stop_after=None, dbg=()):
    nc = bass.Bass("TRN2", target_bir_lowering=False)
    F = Flow(nc)
    I = {}
    early = stop_after in ("P0", "A1", "A2", "A3", "A4", "R")
    for k, (shp, dt) in list(IN_SPECS.items()) + list(CONST_SPECS.items()):
        if early and k in ("w1", "w2", "w3"):
            continue
        I[k] = nc.dram_tensor(k, shp, dt, kind="ExternalInput").ap()
    out_ap = nc.dram_tensor("out", [TOWN, D], F32, kind="ExternalOutput").ap()
    b_out = Buf("out")

    def scratch(name, shape, dt):
        kind = "ExternalOutput" if name in dbg else "Internal"
        return nc.dram_tensor(name, shape, dt, kind=kind).ap()

    _n = [0]

    def sb(shape, dt, name=None):
        _n[0] += 1
        return nc.alloc_sbuf_tensor(name or ("t%d" % _n[0]), shape, dt)

    psT = [nc.alloc_psum_tensor("psT%d" % i, [128, 1024], BF16) for i in range(2)]
    bpsT = [Buf("psT%d" % i) for i in range(2)]
    ps = [nc.alloc_psum_tensor("ps%d" % i, [128, 512], F32) for i in range(6)]
    bps = [Buf("ps%d" % i) for i in range(6)]

    def load_const(name, q="sp"):
        shp, dt = CONST_SPECS[name]
        t = sb(shp, dt, "c_" + name)
        b = Buf(name)
        F.dma(q, lambda e: e.dma_start(out=t[:], in_=I[name]), writes=[b])
        return t, b
    ident_bf, b_identbf = load_const("ident_bf")
    ident_f, b_identf = load_const("ident_f")
    halfcol, b_half = load_const("halfcol")
    epscol = sb([128, 1], F32, "epscol")
    b_eps = Buf("eps")
    F.op("dve", lambda e: e.memset(epscol[:], EPS), writes=[b_eps])

    modc = sb([128, 96], F32, "modc")
    modx = sb([128, 32], F32, "modx")
    b_modc, b_modx = Buf("modc"), Buf("modx")
    gates_d = scratch("gates_d", [6, D], F32)
    b_gates = Buf("gates_d")
    with nc.sbuf_tensor("cc", [128, 16, 2], F32) as cc, nc.sbuf_tensor("badaT", [128, 96], F32) as badaT, \
            nc.sbuf_tensor("wada0", [128, 2048], F32) as wa0, nc.sbuf_tensor("wada1", [128, 2048], F32) as wa1, \
            nc.sbuf_tensor("wada2", [128, 2048], F32) as wa2, nc.sbuf_tensor("gcol", [128, 6, 16], F32) as gcol:
        b_cc, b_bada = Buf("cc"), Buf("bada")
        F.dma("sp", lambda e: e.dma_start(out=cc[:], in_=I["cvT"]), writes=[b_cc])
        F.dma("sp", lambda e: e.dma_start(out=badaT[:], in_=I["b_adaT"]), writes=[b_bada])
        F.op("act", lambda e: e.activation(out=cc[:], in_=cc[:], func=AF.Silu), reads=[b_cc], writes=[b_cc])
        was = [wa0, wa1, wa2]
        bwas = [Buf("wa%d" % i) for i in range(3)]
        pacc = ps[0]
        pv = pacc[:, 0:192].rearrange("p (m t) -> p m t", t=2)
        for m in range(96):
            wt = was[m % 3]
            bw = bwas[m % 3]
            ncol = 2 if m < 32 else 1
            F.dma("sp" if m % 2 else "act", lambda e, wt=wt, m=m: e.dma_start(
                out=wt[:].rearrange("p (k c) -> p k c", k=16)[:, :, 0:128],
                in_=I["w_ada"][:, m * 128:(m + 1) * 128].rearrange("(k p) c -> p k c", p=128)), writes=[bw])
            for k in range(16):
                F.op("pe", lambda e, wt=wt, m=m, k=k: e.matmul(
                    pv[:, m, :], lhsT=wt[:, k * 64:k * 64 + 128] if False else wt[:].rearrange("p (k c) -> p k c", k=16)[:, k, 0:128],
                    rhs=cc[:, k, :], start=(k == 0), stop=(k == 15)),
                    reads=[bw, b_cc], writes=[bps[0]], signal=(k == 15))
        F.op("dve", lambda e: e.tensor_tensor(out=modc[:], in0=pv[:, :, 0], in1=badaT[:], op=ALU.add),
             reads=[bps[0], b_bada], writes=[b_modc])
        F.op("dve", lambda e: e.tensor_tensor(out=modx[:], in0=pv[:, 0:32, 1], in1=badaT[:, 0:32], op=ALU.add),
             reads=[bps[0], b_bada], writes=[b_modx])
        F.op("dve", lambda e: e.tensor_scalar(out=modc[:, 16:32], in0=modc[:, 16:32], scalar1=1.0, scalar2=None, op0=ALU.add),
             reads=[b_modc], writes=[b_modc])
        F.op("dve", lambda e: e.tensor_scalar(out=modc[:, 64:80], in0=modc[:, 64:80], scalar1=1.0, scalar2=None, op0=ALU.add),
             reads=[b_modc], writes=[b_modc])
        F.op("dve", lambda e: e.tensor_scalar(out=modx[:, 16:32], in0=modx[:, 16:32], scalar1=1.0, scalar2=None, op0=ALU.add),
             reads=[b_modx], writes=[b_modx])
        b_gcol = Buf("gcol")
        F.op("dve", lambda e: e.tensor_copy(out=gcol[:].rearrange("p g j -> p (g j)"), in_=modc[:]), reads=[b_modc], writes=[b_gcol])
        F.dma("sp", lambda e: e.dma_start(out=gates_d.rearrange("g (j p) -> p g j", p=128), in_=gcol[:],
                                          allow_slow_non_contiguous=True), reads=[b_gcol], writes=[b_gates])
        if "modc_d" in dbg:
            md = scratch("modc_d", [128, 96], F32)
            F.dma("sp", lambda e: e.dma_start(out=md, in_=modc[:]), reads=[b_modc], writes=[Buf()])
        F.barrier()
    sh1, sc1p, sh2, sc2p = modc[:, 0:16], modc[:, 16:32], modc[:, 48:64], modc[:, 64:80]
    shx, scxp = modx[:, 0:16], modx[:, 16:32]
    if stop_after == "P0":
        F.barrier()
        return nc

    Wmix_d = scratch("Wmix_d", [16, 128, 44, 128], BF16)
    Wo_d = scratch("Wo_d", [128, 16, D], BF16)
    b_Wmix, b_Wo = Buf("Wmix_d"), Buf("Wo_d")
    Wmix_v = Wmix_d.rearrange("oc p k c -> p oc k c")
    wp_jobs = []
    for k in range(16):
        wp_jobs.append((I["w_in"][k * 128:(k + 1) * 128, 1536:3584], Wmix_v[:, :, k, :]))
        wp_jobs.append((I["w_in"][k * 128:(k + 1) * 128, 3584:5632], Wmix_v[:, :, 16 + k, :]))
    for k in range(8):
        wp_jobs.append((I["w_f_proj"][k * 128:(k + 1) * 128, :], Wmix_v[:, :, 32 + k, :]))
    for k in range(4):
        wp_jobs.append((I["w_s_proj"][k * 128:(k + 1) * 128, :], Wmix_v[:, :, 40 + k, :]))
    F1d = scratch("F1d", [8, 128, 64, 128], BF16)
    b_F1d = Buf("F1d")
    es_us = ExitStack()
    usT = es_us.enter_context(nc.sbuf_tensor("usT", [128, 4, 8704], BF16))
    b_usT = Buf("usT")

    def ln_stats(xt, bx, st, mv, rs, nmr, bst):
        for i in range(4):
            F.op("dve", lambda e, i=i: e.bn_stats(out=st[:, 6 * i:6 * i + 6], in_=xt[:, i * 512:(i + 1) * 512]), reads=[bx], writes=[bst])
        F.op("dve", lambda e: e.bn_aggr(out=mv[:], in_=st[:]), reads=[bst], writes=[bst])
        F.op("act", lambda e: e.activation(out=rs[:], in_=mv[:, 1:2], func=AF.Sqrt, bias=epscol[:, 0:1], scale=1.0),
             reads=[bst, b_eps], writes=[bst])
        F.op("dve", lambda e: e.reciprocal(out=rs[:], in_=rs[:]), reads=[bst], writes=[bst])
        F.op("dve", lambda e: e.scalar_tensor_tensor(out=nmr[:], in0=mv[:, 0:1], scalar=-1.0, in1=rs[:], op0=ALU.mult, op1=ALU.mult),
             reads=[bst], writes=[bst])

    with nc.sbuf_tensor("Win", [128, 16, 1536], BF16) as Win, nc.sbuf_tensor("s_rowdft", [128, 128], BF16) as rowdft, \
            nc.sbuf_tensor("xt0", [128, D], F32) as xt0, nc.sbuf_tensor("xt1", [128, D], F32) as xt1, \
            nc.sbuf_tensor("xn0", [128, D], BF16) as xn0, nc.sbuf_tensor("xn1", [128, D], BF16) as xn1, \
            nc.sbuf_tensor("hT0", [128, 16, 128], BF16) as hT0, nc.sbuf_tensor("hT1", [128, 16, 128], BF16) as hT1, \
            nc.sbuf_tensor("uf0", [128, 1024], BF16) as uf0, nc.sbuf_tensor("uf1", [128, 1024], BF16) as uf1, \
            nc.sbuf_tensor("f10", [128, 8, 128], BF16) as f10, nc.sbuf_tensor("f11", [128, 8, 128], BF16) as f11, \
            nc.sbuf_tensor("st0", [128, 24], F32) as st0, nc.sbuf_tensor("st1", [128, 24], F32) as st1, \
            nc.sbuf_tensor("sm0", [128, 4], F32) as sm0, nc.sbuf_tensor("sm1", [128, 4], F32) as sm1:
        b_Win = Buf("Win")
        b_rd = Buf("rowdft")
        es_wp = ExitStack()
        F.dma("sp", lambda e: e.dma_start(out=rowdft[:], in_=I["rowdft"]), writes=[b_rd])
        for kg in range(4):
            F.dma("pool", lambda e, kg=kg: e.dma_start(
                out=Win[:, kg * 4:(kg + 1) * 4, :],
                in_=I["w_in"][kg * 512:(kg + 1) * 512, 0:1536].rearrange("(k p) n -> p k n", p=128)), writes=[b_Win])
        xts, xns, hTs, ufs, f1s, sts, sms = [xt0, xt1], [xn0, xn1], [hT0, hT1], [uf0, uf1], [f10, f11], [st0, st1], [sm0, sm1]
        bxt, bxn, bhT, buf_, bf1, bst = ([Buf("xt%d" % i) for i in range(2)], [Buf("xn%d" % i) for i in range(2)],
                                         [Buf("hT%d" % i) for i in range(2)], [Buf("uf%d" % i) for i in range(2)],
                                         [Buf("f1%d" % i) for i in range(2)], [Buf("st%d" % i) for i in range(2)])
        xcol = I["x"].rearrange("(r c) d -> c r d", c=64)
        ntile = 64 + 2

        def S1(ti):
            p = ti % 2
            xt, xn, st, sm = xts[p], xns[p], sts[p], sms[p]
            src = xcol[ti] if ti < 64 else I["ctx"][(ti - 64) * 128:(ti - 63) * 128, :]
            F.dma("sp", lambda e: e.dma_start(out=xt[:], in_=src), writes=[bxt[p]])
            mv, rs, nmr = sm[:, 0:2], sm[:, 2:3], sm[:, 3:4]
            ln_stats(xt, bxt[p], st, mv, rs, nmr, bst[p])
            F.op("act", lambda e: e.activation(out=xn[:], in_=xt[:], func=AF.Identity, bias=nmr, scale=rs),
                 reads=[bxt[p], bst[p]], writes=[bxn[p]])

        def S2pe(ti):
            p = ti % 2
            for hb in range(2):
                for jj in range(8):
                    j = hb * 8 + jj
                    F.op("pe", lambda e: e.transpose(out=psT[hb][:, jj * 128:(jj + 1) * 128], in_=xns[p][:, j * 128:(j + 1) * 128], identity=ident_bf[:]),
                         reads=[bxn[p], b_identbf], writes=[bpsT[hb]], signal=(jj == 7))

        def S2ev(ti):
            p = ti % 2
            hT = hTs[p]
            scp, shf, bmod = (sc1p, sh1, b_modc) if ti < 64 else (scxp, shx, b_modx)
            for hb in range(2):
                for jj in range(8):
                    j = hb * 8 + jj
                    if jj % 2 == 0:
                        F.op("act", lambda e: e.activation(out=hT[:, j, :], in_=psT[hb][:, jj * 128:(jj + 1) * 128], func=AF.Identity,
                                                           bias=shf[:, j:j + 1], scale=scp[:, j:j + 1]), reads=[bpsT[hb], bmod], writes=[bhT[p]])
                    else:
                        F.op("dve", lambda e: e.tensor_scalar(out=hT[:, j, :], in0=psT[hb][:, jj * 128:(jj + 1) * 128], scalar1=scp[:, j:j + 1],
                                                              scalar2=shf[:, j:j + 1], op0=ALU.mult, op1=ALU.add), reads=[bpsT[hb], bmod], writes=[bhT[p]])

        def S3pe(ti):
            p = ti % 2
            hT = hTs[p]
            for m in range(4):
                for k in range(16):
                    F.op("pe", lambda e: e.matmul(ps[2][:, m * 128:(m + 1) * 128], lhsT=Win[:, k, 1024 + m * 128:1024 + (m + 1) * 128], rhs=hT[:, k, :],
                                                  start=(k == 0), stop=(k == 15)), reads=[b_Win, bhT[p]], writes=[bps[2]], signal=(k == 15 and m == 3))
            if ti < 64:
                for n in range(2):
                    for k in range(16):
                        F.op("pe", lambda e: e.matmul(ps[n][:], lhsT=hT[:, k, :], rhs=Win[:, k, n * 512:(n + 1) * 512], start=(k == 0), stop=(k == 15)),
                             reads=[b_Win, bhT[p]], writes=[bps[n]], signal=(k == 15))

        def S3ev(ti):
            p = ti % 2
            uf = ufs[p]
            pc = ps[2][:].rearrange("p (m t) -> p m t", m=4)
            if ti < 64:
                c = ti
                F.op("act", lambda e: e.activation(out=usT[:, :, c:4096:64], in_=pc[:, :, 0:64], func=AF.Copy), reads=[bps[2]], writes=[b_usT])
                F.op("dve", lambda e: e.tensor_copy(out=usT[:, :, 4352 + c:8448:64], in_=pc[:, :, 64:128]), reads=[bps[2]], writes=[b_usT])
                F.op("act", lambda e: e.activation(out=uf[:, 0:512], in_=ps[0][:], func=AF.Copy), reads=[bps[0]], writes=[buf_[p]])
                F.op("dve", lambda e: e.tensor_copy(out=uf[:, 512:1024], in_=ps[1][:]), reads=[bps[1]], writes=[buf_[p]])
            else:
                t0 = (ti - 64) * 128
                F.op("act", lambda e: e.activation(out=usT[:, :, 4096 + t0:4096 + t0 + 128], in_=pc, func=AF.Copy), reads=[bps[2]], writes=[b_usT])
                F.op("dve", lambda e: e.tensor_copy(out=usT[:, :, 8448 + t0:8448 + t0 + 128], in_=pc), reads=[bps[2]], writes=[b_usT])

        def S4pe(ti):
            p = ti % 2
            for m in range(8):
                F.op("pe", lambda e: e.matmul(ps[3 + m // 4][:, (m % 4) * 128:(m % 4 + 1) * 128], lhsT=ufs[p][:, m * 128:(m + 1) * 128], rhs=rowdft[:],
                                              start=True, stop=True), reads=[buf_[p], b_rd], writes=[bps[3 + m // 4]], signal=(m % 4 == 3))

        def S4ev(ti):
            p = ti % 2
            f1 = f1s[p]
            F.op("act", lambda e: e.activation(out=f1[:, 0:4, :], in_=ps[3][:].rearrange("p (m t) -> p m t", m=4), func=AF.Copy), reads=[bps[3]], writes=[bf1[p]])
            F.op("dve", lambda e: e.tensor_copy(out=f1[:, 4:8, :], in_=ps[4][:].rearrange("p (m t) -> p m t", m=4)), reads=[bps[4]], writes=[bf1[p]])
            F.dma("sp", lambda e: e.dma_start(out=F1d.rearrange("m ch c k -> ch m c k")[:, :, ti, :], in_=f1[:]), reads=[bf1[p]], writes=[b_F1d])

        wpf = [es_wp.enter_context(nc.sbuf_tensor("wpA%d" % i, [128, 2048], F32)) for i in range(2)]
        wpb = [es_wp.enter_context(nc.sbuf_tensor("wpB%d" % i, [128, 2048], BF16)) for i in range(2)]
        bwpf = [Buf("wpA%d" % i) for i in range(2)]
        bwpb = [Buf("wpB%d" % i) for i in range(2)]

        def wprep_piece(ji):
            src, dst = wp_jobs[ji]
            p = ji % 2
            F.dma("pool", lambda e: e.dma_start(out=wpf[p][:], in_=src), writes=[bwpf[p]])
            F.op("pool", lambda e: e.tensor_copy(out=wpb[p][:], in_=wpf[p][:]), reads=[bwpf[p]], writes=[bwpb[p]])
            F.dma("pool", lambda e: e.dma_start(out=dst, in_=wpb[p][:].rearrange("p (oc c) -> p oc c", c=128)), reads=[bwpb[p]], writes=[b_Wmix])

        ok = lambda t: 0 <= t < ntile
        S1(0)
        S1(1)
        S2pe(0)
        S2ev(0)
        for n in range(0, ntile + 1):
            if ok(n + 2):
                S1(n + 2)
            if ok(n + 1):
                S2pe(n + 1)
            if ok(n):
                S3pe(n)
            if ok(n - 1) and n - 1 < 64:
                S4pe(n - 1)
            if ok(n + 1):
                S2ev(n + 1)
            if ok(n):
                S3ev(n)
            if ok(n - 1) and n - 1 < 64:
                S4ev(n - 1)
            if n < len(wp_jobs):
                wprep_piece(n)
        if "usT_d" in dbg:
            ud = scratch("usT_d", [128, 4, 8704], BF16)
            F.dma("sp", lambda e: e.dma_start(out=ud, in_=usT[:]), reads=[b_usT], writes=[Buf()])
        F.barrier()
        es_wp.close()
    if stop_after == "A1":
        F.barrier()
        return nc

    fT_d = scratch("fT_d", [8, 128, TOWN], BF16)
    b_fTd = Buf("fT_d")
    with nc.sbuf_tensor("s_chA", [128, 2, 512], BF16) as chA, nc.sbuf_tensor("s_chB", [128, 2, 512], BF16) as chB, \
            nc.sbuf_tensor("s_colC", [128, 128], BF16) as colC, nc.sbuf_tensor("s_colS", [128, 128], BF16) as colS, \
            nc.sbuf_tensor("F1s0", [128, 2, 64, 128], BF16) as F1s0, nc.sbuf_tensor("F1s1", [128, 2, 64, 128], BF16) as F1s1, \
            nc.sbuf_tensor("G0", [128, 512], BF16) as G0, nc.sbuf_tensor("G1", [128, 512], BF16) as G1, \
            nc.sbuf_tensor("fo0", [128, 2, TOWN], BF16) as fo0, nc.sbuf_tensor("fo1", [128, 2, TOWN], BF16) as fo1:
        b_tabs = Buf("dfttabs")
        for t, nm in ((chA, "chA"), (chB, "chB"), (colC, "colC"), (colS, "colS")):
            F.dma("sp", lambda e, t=t, nm=nm: e.dma_start(out=t[:], in_=I[nm]), writes=[b_tabs])
        F1ss, Gs, fos = [F1s0, F1s1], [G0, G1], [fo0, fo1]
        bF1s, bG, bfo = [Buf("F1s0"), Buf("F1s1")], [Buf("G0"), Buf("G1")], [Buf("fo0"), Buf("fo1")]
        for gi in range(4):
            F1s, fo = F1ss[gi % 2], fos[gi % 2]
            for kc in range(2):
                F.dma("sp" if kc == 0 else "act", lambda e, F1s=F1s, kc=kc, gi=gi: e.dma_start(out=F1s[:, kc, :, :], in_=F1d[2 * gi + kc]),
                      reads=[b_F1d], writes=[bF1s[gi % 2]])
            for kr in range(64):
                G = Gs[kr % 2]
                pa = ps[kr % 2]
                bpa = bps[kr % 2]
                n = 0
                for kc in range(2):
                    for (off, tab) in ((0, chA), (64, chB)):
                        F.op("pe", lambda e, F1s=F1s, kc=kc, off=off, tab=tab, kr=kr, pa=pa, n=n: e.matmul(
                            pa[0:64, :], lhsT=F1s[:, kc, :, off + kr], rhs=tab[:, kc, :], start=(n == 0), stop=(n == 3)),
                            reads=[bF1s[gi % 2], b_tabs], writes=[bpa], signal=(n == 3))
                        n += 1
                if kr % 2 == 0:
                    F.op("act", lambda e, G=G, pa=pa: e.activation(out=G[0:64, :], in_=pa[0:64, :], func=AF.Copy), reads=[bpa], writes=[bG[kr % 2]])
                else:
                    F.op("dve", lambda e, G=G, pa=pa: e.tensor_copy(out=G[0:64, :], in_=pa[0:64, :]), reads=[bpa], writes=[bG[kr % 2]])
                g4 = kr // 4
                pb = ps[2 + g4 % 2]
                bpb = bps[2 + g4 % 2]
                for q in range(2):
                    c0 = (q * 4 + kr % 4) * 64
                    F.op("pe", lambda e, G=G, q=q, pb=pb, c0=c0: e.matmul(pb[:, c0:c0 + 64], lhsT=G[0:64, q * 128:(q + 1) * 128], rhs=colC[0:64, 0:64],
                                                                 start=True, stop=False), reads=[bG[kr % 2], b_tabs], writes=[bpb], signal=False)
                    F.op("pe", lambda e, G=G, q=q, pb=pb, c0=c0: e.matmul(pb[:, c0:c0 + 64], lhsT=G[0:64, 256 + q * 128:256 + (q + 1) * 128], rhs=colS[0:64, 0:64],
                                                                 start=False, stop=True), reads=[bG[kr % 2], b_tabs], writes=[bpb], signal=(q == 1))
                if kr % 4 == 3:
                    kr0 = kr - 3
                    if g4 % 2 == 0:
                        F.op("dve", lambda e, fo=fo, pb=pb, kr0=kr0: e.tensor_copy(out=fo[:, :, kr0 * 64:(kr0 + 4) * 64],
                                                                           in_=pb[:].rearrange("p (q t) -> p q t", q=2)),
                             reads=[bpb], writes=[bfo[gi % 2]])
                    else:
                        F.op("act", lambda e, fo=fo, pb=pb, kr0=kr0: e.activation(out=fo[:, :, kr0 * 64:(kr0 + 4) * 64],
                                                                            in_=pb[:].rearrange("p (q t) -> p q t", q=2), func=AF.Copy),
                             reads=[bpb], writes=[bfo[gi % 2]])
            F.dma("sp", lambda e, fo=fo, gi=gi: e.dma_start(out=fT_d[2 * gi:2 * gi + 2].rearrange("q p t -> p q t"), in_=fo[:]),
                  reads=[bfo[gi % 2]], writes=[b_fTd])
        F.barrier()
    if stop_after == "A2":
        F.barrier()
        return nc

    sT_d = scratch("sT_d", [4, 128, TOWN], BF16)
    b_sTd = Buf("sT_d")
    gT_d = scratch("gT_d", [4, 128, TOWN], BF16)
    b_gTd = Buf("gT_d")
    yT_dbg = scratch("yT_d", [4, 8, 128, 512], F32) if "yT_d" in dbg else None
    PIS = 3.141592
    with ExitStack() as es_a3:
        par = es_a3.enter_context(nc.sbuf_tensor("s5par", [128, 32 * 12], F32))
        pw = es_a3.enter_context(nc.sbuf_tensor("s5pw", [128, 32 * 9 * 8], F32))
        pwi = es_a3.enter_context(nc.sbuf_tensor("s5pi", [128, 32 * 9], I32))
        Bt = es_a3.enter_context(nc.sbuf_tensor("s5b", [128, 2, 1024], F32))
        Ct = es_a3.enter_context(nc.sbuf_tensor("s5c", [128, 2, 1024], F32))
        Btmp = es_a3.enter_context(nc.sbuf_tensor("s5bt", [128, 2, 1024], F32))
        jrow = es_a3.enter_context(nc.sbuf_tensor("s5jrow", [128, 9], F32))
        crow = es_a3.enter_context(nc.sbuf_tensor("s5crow", [128, 544], F32))
        blkmask = es_a3.enter_context(nc.sbuf_tensor("s5mask", [128, 128], F32))
        dsk = es_a3.enter_context(nc.sbuf_tensor("s5dsk", [128, 4], F32))
        Wt = es_a3.enter_context(nc.sbuf_tensor("s5W", [128, 2, 1056], F32))
        QWt = es_a3.enter_context(nc.sbuf_tensor("s5QW", [128, 2 * 8 * 2 * 128], BF16))
        KWt = es_a3.enter_context(nc.sbuf_tensor("s5KW", [128, 2 * 8 * 128], BF16))
        NWt = es_a3.enter_context(nc.sbuf_tensor("s5NW", [128, 2 * 2 * 1024], BF16))
        tab = es_a3.enter_context(nc.sbuf_tensor("s5tab", [128, 2, 544], F32))
        tf = es_a3.enter_context(nc.sbuf_tensor("s5tf", [128, 544], F32))
        ti_ = es_a3.enter_context(nc.sbuf_tensor("s5ti", [128, 544], I32))
        dm = es_a3.enter_context(nc.sbuf_tensor("s5d", [128, 6, 544], F32))
        cr_ = es_a3.enter_context(nc.sbuf_tensor("s5cr", [128, 16], F32))
        St = es_a3.enter_context(nc.sbuf_tensor("s5S", [128, 8 * 2 * 520], BF16))
        yt = es_a3.enter_context(nc.sbuf_tensor("s5y", [128, 2, 512], F32))
        gt = es_a3.enter_context(nc.sbuf_tensor("s5g", [128, TOWN], BF16))
        sgt = es_a3.enter_context(nc.sbuf_tensor("s5sg", [128, 512], F32))
        b_par, b_W, b_QW, b_KW, b_NW = Buf("par"), Buf("W"), Buf("QW"), Buf("KW"), Buf("NW")
        Zt = Wt
        dmflat = dm[:].rearrange("p a c -> p (a c)")
        b_Z = b_W
        b_tmp = b_W
        b_tab, b_S, b_y, b_g, b_gl = Buf("tab"), Buf("S"), [Buf("y0"), Buf("y1")], Buf("g"), Buf("gl")
        dq = ["sp", "act"]
        lre, lim, ldt = par[:, 0:32], par[:, 32:64], par[:, 64:96]
        F.dma("sp", lambda e: e.dma_start(out=lre, in_=I["lam_re"]), writes=[b_par])
        F.dma("sp", lambda e: e.dma_start(out=lim, in_=I["lam_im"]), writes=[b_par])
        F.dma("sp", lambda e: e.dma_start(out=ldt, in_=I["log_dt"]), writes=[b_par])
        F.dma("sp", lambda e: e.dma_start(out=Bt[:, 0, :], in_=I["b_re"].rearrange("p a b -> p (a b)")), writes=[b_par])
        F.dma("sp", lambda e: e.dma_start(out=Bt[:, 1, :], in_=I["b_im"].rearrange("p a b -> p (a b)")), writes=[b_par])
        F.dma("act", lambda e: e.dma_start(out=Ct[:, 0, :], in_=I["c_re"].rearrange("p a b -> p (a b)")), writes=[b_par])
        F.dma("act", lambda e: e.dma_start(out=Ct[:, 1, :], in_=I["c_im"].rearrange("p a b -> p (a b)")), writes=[b_par])
        F.dma("sp", lambda e: e.dma_start(out=jrow[:], in_=I["jrow"]), writes=[b_par])
        F.dma("sp", lambda e: e.dma_start(out=crow[:], in_=I["crow"]), writes=[b_par])
        F.dma("sp", lambda e: e.dma_start(out=blkmask[:], in_=I["blkmask"]), writes=[b_par])
        F.dma("sp", lambda e: e.dma_start(out=dsk[:], in_=I["d_skipT"]), writes=[b_par])

        def V(fn, reads, writes):
            F.op("dve", fn, reads=reads, writes=writes)

        def A(fn, reads, writes):
            F.op("act", fn, reads=reads, writes=writes)
        P_ = [b_par]
        dtc, aa, th = par[:, 96:128], par[:, 128:160], par[:, 160:192]
        A(lambda e: e.activation(out=dtc, in_=ldt, func=AF.Exp), P_, P_)
        V(lambda e: e.tensor_tensor(out=aa, in0=lre, in1=dtc, op=ALU.mult), P_, P_)
        V(lambda e: e.tensor_tensor(out=th, in0=lim, in1=dtc, op=ALU.mult), P_, P_)
        def p3(i):
            return pw[:, i * 288:(i + 1) * 288].rearrange("p (a j) -> p a j", j=9)
        MAG, ANG, RED, SIN, COS, PR, PI_, TMP = [p3(i) for i in range(8)]
        pwi3 = pwi[:].rearrange("p (a j) -> p a j", j=9)
        jb = jrow[:].unsqueeze(1).to_broadcast([128, 32, 9])
        V(lambda e: e.tensor_tensor(out=MAG, in0=aa.unsqueeze(2).to_broadcast([128, 32, 9]), in1=jb, op=ALU.mult), P_, P_)
        A(lambda e: e.activation(out=MAG, in_=MAG, func=AF.Exp), P_, P_)
        V(lambda e: e.tensor_tensor(out=ANG, in0=th.unsqueeze(2).to_broadcast([128, 32, 9]), in1=jb, op=ALU.mult), P_, P_)

        def range_reduce(dst, src, tmpf, tmpi, shift, R, Wr):
            V(lambda e: e.tensor_scalar(out=tmpf, in0=src, scalar1=shift, scalar2=1.0 / (2 * PI), op0=ALU.add, op1=ALU.mult), R, Wr)
            V(lambda e: e.tensor_copy(out=tmpi, in_=tmpf), Wr, Wr)
            V(lambda e: e.tensor_copy(out=tmpf, in_=tmpi), Wr, Wr)
            V(lambda e: e.scalar_tensor_tensor(out=tmpf, in0=tmpf, scalar=-2 * PI, in1=src, op0=ALU.mult, op1=ALU.add), R + Wr, Wr)
            V(lambda e: e.tensor_scalar(out=dst, in0=tmpf, scalar1=shift, scalar2=-PIS, op0=ALU.add, op1=ALU.max), Wr, Wr)
            V(lambda e: e.tensor_scalar(out=dst, in0=dst, scalar1=PIS, scalar2=None, op0=ALU.min), Wr, Wr)
        range_reduce(RED, ANG, TMP, pwi3, 0.0, P_, P_)
        A(lambda e: e.activation(out=SIN, in_=RED, func=AF.Sin), P_, P_)
        range_reduce(COS, ANG, TMP, pwi3, PI / 2, P_, P_)
        A(lambda e: e.activation(out=COS, in_=COS, func=AF.Sin), P_, P_)
        V(lambda e: e.tensor_tensor(out=PR, in0=MAG, in1=COS, op=ALU.mult), P_, P_)
        V(lambda e: e.tensor_tensor(out=PI_, in0=MAG, in1=SIN, op=ALU.mult), P_, P_)
        nr, ni, den, cr, ci, t1c, t2c = [par[:, 192 + 32 * i:224 + 32 * i] for i in range(6)] + [par[:, 352:384]]
        V(lambda e: e.tensor_scalar(out=nr, in0=PR[:, :, 1], scalar1=-1.0, scalar2=None, op0=ALU.add), P_, P_)
        V(lambda e: e.tensor_copy(out=ni, in_=PI_[:, :, 1]), P_, P_)
        V(lambda e: e.tensor_tensor(out=den, in0=lre, in1=lre, op=ALU.mult), P_, P_)
        V(lambda e: e.tensor_tensor(out=t1c, in0=lim, in1=lim, op=ALU.mult), P_, P_)
        V(lambda e: e.tensor_tensor(out=den, in0=den, in1=t1c, op=ALU.add), P_, P_)
        V(lambda e: e.reciprocal(out=den, in_=den), P_, P_)
        V(lambda e: e.tensor_tensor(out=cr, in0=nr, in1=lre, op=ALU.mult), P_, P_)
        V(lambda e: e.tensor_tensor(out=t1c, in0=ni, in1=lim, op=ALU.mult), P_, P_)
        V(lambda e: e.tensor_tensor(out=cr, in0=cr, in1=t1c, op=ALU.add), P_, P_)
        V(lambda e: e.tensor_tensor(out=cr, in0=cr, in1=den, op=ALU.mult), P_, P_)
        V(lambda e: e.tensor_tensor(out=ci, in0=ni, in1=lre, op=ALU.mult), P_, P_)
        V(lambda e: e.tensor_tensor(out=t1c, in0=nr, in1=lim, op=ALU.mult), P_, P_)
        V(lambda e: e.tensor_tensor(out=ci, in0=ci, in1=t1c, op=ALU.subtract), P_, P_)
        V(lambda e: e.tensor_tensor(out=ci, in0=ci, in1=den, op=ALU.mult), P_, P_)
        B3 = lambda i: Bt[:, i, :].rearrange("p (a h) -> p a h", h=32)
        T3 = lambda i: Btmp[:, i, :].rearrange("p (a h) -> p a h", h=32)
        crb = cr.unsqueeze(2).to_broadcast([128, 32, 32])
        cib = ci.unsqueeze(2).to_broadcast([128, 32, 32])
        V(lambda e: e.tensor_tensor(out=T3(0), in0=B3(0), in1=crb, op=ALU.mult), P_, P_)
        V(lambda e: e.tensor_tensor(out=T3(1), in0=B3(1), in1=cib, op=ALU.mult), P_, P_)
        V(lambda e: e.tensor_tensor(out=T3(0), in0=T3(0), in1=T3(1), op=ALU.subtract), P_, P_)
        V(lambda e: e.tensor_tensor(out=T3(1), in0=B3(1), in1=crb, op=ALU.mult), P_, P_)
        V(lambda e: e.tensor_tensor(out=B3(1), in0=B3(0), in1=cib, op=ALU.mult), P_, P_)
        V(lambda e: e.tensor_tensor(out=T3(1), in0=T3(1), in1=B3(1), op=ALU.add), P_, P_)
        BB = Btmp
        CN = Bt[:, 0, :]
        V(lambda e: e.tensor_scalar(out=CN, in0=Ct[:, 1, :], scalar1=-1.0, scalar2=None, op0=ALU.mult), P_, P_)

        def bview(t2d, pd0):
            return t2d[:, pd0 * 32:(pd0 + 4) * 32].rearrange("p (q h) -> p q h", q=4).unsqueeze(1).to_broadcast([128, 8, 4, 32])

        def pview(T, pd0, j0):
            return T[:, pd0:pd0 + 4, j0:j0 + 8].rearrange("p q j -> p j q").unsqueeze(3).to_broadcast([128, 8, 4, 32])

        for blk in range(4):
            for d in range(2):
                pd0 = d * 16 + blk * 4
                W4 = lambda i: Wt[:, i, 0:1024].rearrange("p (j q h) -> p j q h", j=8, q=4)
                X4 = lambda i: dmflat[:, i * 1024:(i + 1) * 1024].rearrange("p (j q h) -> p j q h", j=8, q=4)
                RW = [b_par, b_W]
                V(lambda e: e.tensor_tensor(out=W4(0), in0=bview(BB[:, 0, :], pd0), in1=pview(PR, pd0, 0), op=ALU.mult), [b_par], [b_W])
                V(lambda e: e.tensor_tensor(out=X4(0), in0=bview(BB[:, 1, :], pd0), in1=pview(PI_, pd0, 0), op=ALU.mult), [b_par], [b_W])
                V(lambda e: e.tensor_tensor(out=W4(0), in0=W4(0), in1=X4(0), op=ALU.subtract), RW, [b_W])
                V(lambda e: e.tensor_tensor(out=W4(1), in0=bview(BB[:, 1, :], pd0), in1=pview(PR, pd0, 0), op=ALU.mult), [b_par], [b_W])
                V(lambda e: e.tensor_tensor(out=X4(1), in0=bview(BB[:, 0, :], pd0), in1=pview(PI_, pd0, 0), op=ALU.mult), [b_par], [b_W])
                V(lambda e: e.tensor_tensor(out=W4(1), in0=W4(1), in1=X4(1), op=ALU.add), RW, [b_W])
                for reim in range(2):
                    for jh in range(2):
                        pst = ps[(reim * 2 + jh) % 4]
                        bpst = bps[(reim * 2 + jh) % 4]
                        for jj in range(4):
                            j = jh * 4 + jj
                            F.op("pe", lambda e: e.transpose(out=pst[:, jj * 128:(jj + 1) * 128], in_=Wt[:, reim, j * 128:(j + 1) * 128], identity=ident_f[:]),
                                 reads=[b_W, b_identf], writes=[bpst], signal=(jj == 3))
                        dst = QWt[:].rearrange("p (d j r c) -> p d j r c", d=2, j=8, r=2)[:, d, jh * 4:(jh + 1) * 4, reim, :]
                        A(lambda e: e.activation(out=dst, in_=pst[:].rearrange("p (j c) -> p j c", j=4), func=AF.Copy), [bpst], [b_QW])
                for jh in range(2):
                    pst = ps[4 + jh]
                    bpst = bps[4 + jh]
                    for jj in range(4):
                        j = jh * 4 + jj
                        F.op("pe", lambda e: e.matmul(pst[:, jj * 128:(jj + 1) * 128], lhsT=Wt[:, 0, j * 128:(j + 1) * 128],
                                                      rhs=Ct[:, 0, pd0 * 32:(pd0 + 4) * 32], start=True, stop=False),
                             reads=[b_W, b_par], writes=[bpst], signal=False)
                        F.op("pe", lambda e: e.matmul(pst[:, jj * 128:(jj + 1) * 128], lhsT=Wt[:, 1, j * 128:(j + 1) * 128],
                                                      rhs=CN[:, pd0 * 32:(pd0 + 4) * 32], start=False, stop=True),
                             reads=[b_W, b_par], writes=[bpst], signal=(jj == 3))
                    dst = KWt[:].rearrange("p (d j c) -> p d j c", d=2, j=8)[:, d, jh * 4:(jh + 1) * 4, :]
                    V(lambda e: e.tensor_tensor(out=dst, in0=pst[:].rearrange("p (j c) -> p j c", j=4),
                                                in1=blkmask[:].unsqueeze(1).to_broadcast([128, 4, 128]), op=ALU.mult), [bpst, b_par], [b_KW])
                if d == 0:
                    k00 = KWt[:, 0:128]
                    V(lambda e: e.scalar_tensor_tensor(out=k00, in0=ident_f[:], scalar=dsk[:, blk:blk + 1], in1=k00, op0=ALU.mult, op1=ALU.add),
                      [b_identf, b_par, b_KW], [b_KW])
                N4 = lambda r: NWt[:].rearrange("p (d r x) -> p d r x", d=2, r=2)[:, d, r, :].rearrange("p (j q h) -> p j q h", j=8, q=4)
                V(lambda e: e.tensor_tensor(out=X4(0), in0=bview(Ct[:, 0, :], pd0), in1=pview(PR, pd0, 1), op=ALU.mult), [b_par, b_W], [b_W])
                V(lambda e: e.tensor_tensor(out=X4(1), in0=bview(CN, pd0), in1=pview(PI_, pd0, 1), op=ALU.mult), [b_par, b_W], [b_W])
                V(lambda e: e.tensor_tensor(out=N4(0), in0=X4(0), in1=X4(1), op=ALU.add), [b_W], [b_NW])
                V(lambda e: e.tensor_tensor(out=X4(0), in0=bview(Ct[:, 0, :], pd0), in1=pview(PI_, pd0, 1), op=ALU.mult), [b_par, b_W], [b_W])
                V(lambda e: e.tensor_tensor(out=X4(1), in0=bview(CN, pd0), in1=pview(PR, pd0, 1), op=ALU.mult), [b_par, b_W], [b_W])
                V(lambda e: e.tensor_tensor(out=N4(1), in0=X4(1), in1=X4(0), op=ALU.subtract), [b_W], [b_NW])
            QW5 = QWt[:].rearrange("p (d j r c) -> p d j r c", d=2, j=8, r=2)
            KW4 = KWt[:].rearrange("p (d j c) -> p d j c", d=2, j=8)
            NW6 = NWt[:].rearrange("p (d r j q h) -> p d r j q h", d=2, r=2, j=8, q=4)
            S5v = St[:].rearrange("p (a r c) -> p a r c", a=8, r=2)
            for d in range(2):
                for q in range(4):
                    pdl = d * 4 + q
                    pd = d * 16 + blk * 4 + q
                    rows = slice(32 * q, 32 * q + 32)
                    if d == 0:
                        segs = [(4096, 512, 0, False), (8192, 32, 512, False), (0, 512, 544, False)]
                    else:
                        segs = [(4352, 512, 32, True), (8448, 32, 0, True), (0, 512, 544, True)]
                    for reim in range(2):
                        for gi_, (base, L, zo, rev) in enumerate(segs):
                            pz = ps[(reim * 3 + gi_) % 6]
                            bpz = bps[(reim * 3 + gi_) % 6]
                            for s_ in range(8):
                                j = 7 - s_ if d == 0 else s_
                                F.op("pe", lambda e: e.matmul(pz[:, 0:L], lhsT=QW5[rows, d, j, reim, :],
                                                              rhs=usT[rows, blk, base + s_:base + s_ + 8 * (L - 1) + 1:8],
                                                              start=(s_ == 0), stop=(s_ == 7), tile_position=(32 * q, 0)),
                                     reads=[b_QW, b_usT], writes=[bpz], signal=(s_ == 7))
                            zs = Zt[:, reim, zo:zo + L]
                            if rev:
                                zs = zs[:, ::-1]
                            if reim == 0:
                                A(lambda e: e.activation(out=zs, in_=pz[:, 0:L], func=AF.Copy), [bpz], [b_Z])
                            else:
                                V(lambda e: e.tensor_copy(out=zs, in_=pz[:, 0:L]), [bpz], [b_Z])
                    r8, th8, c8, s8 = MAG[:, pd, 8:9], RED[:, pd, 8:9], COS[:, pd, 8:9], SIN[:, pd, 8:9]
                    TB = [b_tab]
                    V(lambda e: e.tensor_scalar(out=dm[:, 0, :], in0=crow[:], scalar1=th8, scalar2=None, op0=ALU.mult), [b_par, b_tmp], [b_tmp])
                    range_reduce(tab[:, 0, :], dm[:, 0, :], tf[:], ti_[:], 0.0, [b_tmp], [b_tab])
                    A(lambda e: e.activation(out=tab[:, 0, :], in_=tab[:, 0, :], func=AF.Sin), TB, TB)
                    range_reduce(tab[:, 1, :], dm[:, 0, :], tf[:], ti_[:], PI / 2, [b_tmp], [b_tab])
                    A(lambda e: e.activation(out=tab[:, 1, :], in_=tab[:, 1, :], func=AF.Sin), TB, TB)
                    sinT, cosT = tab[:, 0, :], tab[:, 1, :]
                    TM = [b_tmp]

                    def demod(zo, L):
                        zr, zi = Zt[:, 0, zo:zo + L], Zt[:, 1, zo:zo + L]
                        V(lambda e: e.tensor_tensor(out=dm[:, 0, 0:L], in0=zr, in1=cosT[:, 0:L], op=ALU.mult), [b_Z, b_tab, b_tmp], TM)
                        V(lambda e: e.tensor_tensor(out=dm[:, 1, 0:L], in0=zi, in1=sinT[:, 0:L], op=ALU.mult), [b_Z, b_tab, b_tmp], TM)
                        V(lambda e: e.tensor_tensor(out=dm[:, 2, 0:L], in0=dm[:, 0, 0:L], in1=dm[:, 1, 0:L], op=ALU.add), TM, TM)
                        V(lambda e: e.tensor_tensor(out=dm[:, 0, 0:L], in0=zi, in1=cosT[:, 0:L], op=ALU.mult), [b_Z, b_tab, b_tmp], TM)
                        V(lambda e: e.tensor_tensor(out=dm[:, 1, 0:L], in0=zr, in1=sinT[:, 0:L], op=ALU.mult), [b_Z, b_tab, b_tmp], TM)
                        V(lambda e: e.tensor_tensor(out=dm[:, 3, 0:L], in0=dm[:, 0, 0:L], in1=dm[:, 1, 0:L], op=ALU.subtract), TM, TM)

                    def scan(L, ire, iim):
                        V(lambda e: e.tensor_tensor_scan(out=dm[:, 4, 0:L], data0=r8.to_broadcast([128, L]), data1=dm[:, 2, 0:L],
                                                         initial=ire, op0=ALU.mult, op1=ALU.add), [b_par, b_tmp], TM)
                        V(lambda e: e.tensor_tensor_scan(out=dm[:, 5, 0:L], data0=r8.to_broadcast([128, L]), data1=dm[:, 3, 0:L],
                                                         initial=iim, op0=ALU.mult, op1=ALU.add), [b_par, b_tmp], TM)
                    demod(0, 544)
                    scan(544, 0.0, 0.0)
                    sre2, sim2 = dm[:, 4, 31:544:512], dm[:, 5, 31:544:512]
                    cs2, sn2 = tab[:, 1, 31:544:512], tab[:, 0, 31:544:512]
                    C_ = lambda i, n=1: cr_[:, i:i + n]
                    V(lambda e: e.tensor_tensor(out=C_(4, 2), in0=sre2, in1=cs2, op=ALU.mult), [b_tmp, b_tab], TM)
                    V(lambda e: e.tensor_tensor(out=C_(6, 2), in0=sim2, in1=sn2, op=ALU.mult), [b_tmp, b_tab], TM)
                    V(lambda e: e.tensor_tensor(out=C_(0, 2), in0=C_(4, 2), in1=C_(6, 2), op=ALU.subtract), TM, TM)
                    V(lambda e: e.tensor_tensor(out=C_(4, 2), in0=sim2, in1=cs2, op=ALU.mult), [b_tmp, b_tab], TM)
                    V(lambda e: e.tensor_tensor(out=C_(6, 2), in0=sre2, in1=sn2, op=ALU.mult), [b_tmp, b_tab], TM)
                    V(lambda e: e.tensor_tensor(out=C_(2, 2), in0=C_(4, 2), in1=C_(6, 2), op=ALU.add), TM, TM)
                    for (o, src) in ((8, 0), (9, 2)):
                        a_, b__ = (C_(src), C_(src + 1)) if d == 0 else (C_(src + 1), C_(src))
                        V(lambda e: e.tensor_tensor(out=C_(4), in0=b__, in1=a_, op=ALU.subtract), TM, TM)
                        V(lambda e: e.scalar_tensor_tensor(out=C_(o), in0=C_(4), scalar=halfcol[:, 0:1], in1=a_, op0=ALU.mult, op1=ALU.add),
                          [b_tmp, b_half], TM)
                    V(lambda e: e.tensor_tensor(out=C_(4), in0=C_(9), in1=s8, op=ALU.mult), [b_tmp, b_par], TM)
                    V(lambda e: e.scalar_tensor_tensor(out=C_(10), in0=C_(8), scalar=c8, in1=C_(4), op0=ALU.mult, op1=ALU.subtract), [b_tmp, b_par], TM)
                    V(lambda e: e.tensor_tensor(out=C_(4), in0=C_(9), in1=c8, op=ALU.mult), [b_tmp, b_par], TM)
                    V(lambda e: e.scalar_tensor_tensor(out=C_(11), in0=C_(8), scalar=s8, in1=C_(4), op0=ALU.mult, op1=ALU.add), [b_tmp, b_par], TM)
                    ccol = 0 if d == 0 else 512
                    V(lambda e: e.tensor_copy(out=S5v[:, pdl, 0, ccol:ccol + 1], in_=C_(8)), TM, [b_S])
                    V(lambda e: e.tensor_copy(out=S5v[:, pdl, 1, ccol:ccol + 1], in_=C_(9)), TM, [b_S])
                    demod(544, 512)
                    scan(512, C_(10), C_(11))
                    if d == 0:
                        ore, oim = S5v[:, pdl, 0, 1:513], S5v[:, pdl, 1, 1:513]
                    else:
                        ore, oim = S5v[:, pdl, 0, 0:512][:, ::-1], S5v[:, pdl, 1, 0:512][:, ::-1]
                    V(lambda e: e.tensor_tensor(out=dm[:, 0, 0:512], in0=dm[:, 4, 0:512], in1=cosT[:, 0:512], op=ALU.mult), [b_tmp, b_tab], TM)
                    V(lambda e: e.tensor_tensor(out=dm[:, 1, 0:512], in0=dm[:, 5, 0:512], in1=sinT[:, 0:512], op=ALU.mult), [b_tmp, b_tab], TM)
                    V(lambda e: e.tensor_tensor(out=ore, in0=dm[:, 0, 0:512], in1=dm[:, 1, 0:512], op=ALU.subtract), TM, [b_S])
                    V(lambda e: e.tensor_tensor(out=dm[:, 0, 0:512], in0=dm[:, 5, 0:512], in1=cosT[:, 0:512], op=ALU.mult), [b_tmp, b_tab], TM)
                    V(lambda e: e.tensor_tensor(out=dm[:, 1, 0:512], in0=dm[:, 4, 0:512], in1=sinT[:, 0:512], op=ALU.mult), [b_tmp, b_tab], TM)
                    V(lambda e: e.tensor_tensor(out=oim, in0=dm[:, 0, 0:512], in1=dm[:, 1, 0:512], op=ALU.add), TM, [b_S])
            for s_ in range(8):
                py = ps[s_ % 2]
                bpy = bps[s_ % 2]
                mms = []
                for j in range(s_ + 1):
                    mms.append((KW4[:, 0, j, :], usT[:, blk, s_ - j:s_ - j + 4089:8], None, [b_KW, b_usT]))
                for j in range(8 - s_):
                    mms.append((KW4[:, 1, j, :], usT[:, blk, s_ + j:s_ + j + 4089:8], None, [b_KW, b_usT]))
                for q in range(4):
                    mms.append((NW6[:, 0, 0, s_, q, :], S5v[:, q, 0, 0:512], q, [b_NW, b_S]))
                    mms.append((NW6[:, 0, 1, s_, q, :], S5v[:, q, 1, 0:512], q, [b_NW, b_S]))
                    mms.append((NW6[:, 1, 0, 7 - s_, q, :], S5v[:, 4 + q, 0, 1:513], q, [b_NW, b_S]))
                    mms.append((NW6[:, 1, 1, 7 - s_, q, :], S5v[:, 4 + q, 1, 1:513], q, [b_NW, b_S]))
                for i_, (lh, rh, q, rd) in enumerate(mms):
                    first, last = i_ == 0, i_ == len(mms) - 1
                    if q is None:
                        F.op("pe", lambda e: e.matmul(py[:], lhsT=lh, rhs=rh, start=first, stop=last), reads=rd, writes=[bpy], signal=last)
                    else:
                        F.op("pe", lambda e: e.matmul(py[32 * q:32 * q + 32, :], lhsT=lh, rhs=rh, start=first, stop=last, tile_position=(0, 32 * q)),
                             reads=rd, writes=[bpy], signal=last)
                y = yt[:, s_ % 2, :]
                by = b_y[s_ % 2]
                A(lambda e: e.activation(out=y, in_=py[:], func=AF.Copy), [bpy], [by])
                if yT_dbg is not None:
                    F.dma("sp", lambda e: e.dma_start(out=yT_dbg[blk, s_], in_=y), reads=[by], writes=[Buf()])
                V(lambda e: e.tensor_tensor(out=sgt[:], in0=y, in1=y, op=ALU.mult), [by], TM)
                V(lambda e: e.tensor_scalar(out=sgt[:], in0=sgt[:], scalar1=0.044715, scalar2=1.0, op0=ALU.mult, op1=ALU.add), TM, TM)
                V(lambda e: e.tensor_tensor(out=sgt[:], in0=sgt[:], in1=y, op=ALU.mult), [by, b_tmp], TM)
                A(lambda e: e.activation(out=sgt[:], in_=sgt[:], func=AF.Sigmoid, scale=1.5957691216057308), TM, TM)
                V(lambda e: e.tensor_tensor(out=gt[:, s_:4096:8], in0=y, in1=sgt[:], op=ALU.mult), [by, b_tmp], [b_g])
            F.dma("sp", lambda e: e.dma_start(out=gT_d[blk], in_=gt[:]), reads=[b_g], writes=[b_gTd])
        F.barrier()
    es_us.close()
    with nc.sbuf_tensor("s5wg", [128, 4, 512], BF16) as wglu, nc.sbuf_tensor("s5bg", [128, 4], F32) as bglu, \
            nc.sbuf_tensor("s5gl", [128, 4, 512], BF16) as gl, nc.sbuf_tensor("s5sg2", [128, 512], F32) as sgt, \
            nc.sbuf_tensor("s5so", [128, 2, 512], BF16) as sot:
        b_par, b_gl, b_tmp = Buf("par2"), Buf("gl"), Buf("tmp2")

        def V(fn, reads, writes):
            F.op("dve", fn, reads=reads, writes=writes)

        def A(fn, reads, writes):
            F.op("act", fn, reads=reads, writes=writes)
        F.dma("sp", lambda e: e.dma_start(out=bglu[:], in_=I["b_gluT"]), writes=[b_par])
        F.dma("pool", lambda e: e.dma_start(out=wglu[:], in_=I["w_glu"].rearrange("(k p) n -> p k n", p=128)), writes=[b_par])
        b_so = [Buf("so0"), Buf("so1")]
        for n in range(8):
            F.dma("sp", lambda e: e.dma_start(out=gl[:], in_=gT_d[:, :, n * 512:(n + 1) * 512].rearrange("k p t -> p k t")), reads=[b_gTd], writes=[b_gl])
            for m in range(4):
                pg = ps[m % 2]
                bpg = bps[m % 2]
                for k in range(4):
                    F.op("pe", lambda e: e.matmul(pg[:], lhsT=wglu[:, k, m * 128:(m + 1) * 128], rhs=gl[:, k, :], start=(k == 0), stop=(k == 3)),
                         reads=[b_par, b_gl], writes=[bpg], signal=(k == 3))
                A(lambda e: e.activation(out=sgt[:], in_=pg[:], func=AF.Sigmoid, bias=bglu[:, m:m + 1], scale=1.0), [bpg, b_par, b_tmp], [b_tmp])
                so = sot[:, m % 2, :]
                V(lambda e: e.tensor_tensor(out=so, in0=gl[:, m, :], in1=sgt[:], op=ALU.mult), [b_gl, b_tmp], [b_so[m % 2]])
                F.dma("act", lambda e: e.dma_start(out=sT_d[m][:, n * 512:(n + 1) * 512], in_=so), reads=[b_so[m % 2]], writes=[b_sTd])
        F.barrier()
    if stop_after == "A3":
        F.barrier()
        return nc

    with ExitStack() as es:
        T = lambda nm, shp, dt: es.enter_context(nc.sbuf_tensor(nm, shp, dt))
        wf = [T("wpf%d" % i, [128, 4096], F32) for i in range(2)]
        wb = [T("wpb%d" % i, [128, 4096], BF16) for i in range(2)]
        bwf = [Buf("wpf%d" % i) for i in range(2)]
        bwb = [Buf("wpb%d" % i) for i in range(2)]
        jobs = []
        for k in range(16):
            jobs.append((I["w_out"][k * 128:(k + 1) * 128, :], 2048, [(Wo_d[:, k, :], 0, 2048)], b_Wo))
        g1w = T("wp_g1", [128, D], F32)
        bg1w = Buf("wp_g1")
        F.dma("sp", lambda e: e.dma_start(out=g1w[:], in_=gates_d[2:3, :].partition_broadcast(128)), reads=[b_gates], writes=[bg1w])
        for ji, (src, n, dsts, bd) in enumerate(jobs):
            p = ji % 2
            F.dma("sp", lambda e: e.dma_start(out=wf[p][:, 0:n], in_=src), writes=[bwf[p]])
            eng = ("act", "dve", "pool")[ji % 3]
            if bd is b_Wo:
                F.op("dve", lambda e: e.tensor_tensor(out=wb[p][:, 0:n], in0=wf[p][:, 0:n], in1=g1w[:, 0:n], op=ALU.mult), reads=[bwf[p], bg1w], writes=[bwb[p]])
            elif eng == "act":
                F.op("act", lambda e: e.activation(out=wb[p][:, 0:n], in_=wf[p][:, 0:n], func=AF.Copy), reads=[bwf[p]], writes=[bwb[p]])
            else:
                F.op(eng, lambda e: e.tensor_copy(out=wb[p][:, 0:n], in_=wf[p][:, 0:n]), reads=[bwf[p]], writes=[bwb[p]])
            for (dst, a_, b__) in dsts:
                if len(dst.shape) == 3:
                    F.dma("act", lambda e: e.dma_start(out=dst, in_=wb[p][:, a_:b__].rearrange("p (oc c) -> p oc c", c=128)), reads=[bwb[p]], writes=[bd])
                else:
                    F.dma("act", lambda e: e.dma_start(out=dst, in_=wb[p][:, a_:b__]), reads=[bwb[p]], writes=[bd])
        F.barrier()

    xmid_d = scratch("xmid_d", [TOWN, D], F32)
    xn2_d = scratch("xn2_d", [TOWN, D], BF16)
    b_xmid, b_xn2 = Buf("xmid_d"), Buf("xn2_d")
    lg_all = sb([128, 32, 36], F32, "lg_all")
    b_lg = Buf("lg_all")
    with ExitStack() as es:
        T = lambda nm, shp, dt: es.enter_context(nc.sbuf_tensor(nm, shp, dt))
        vt = [T("a4v%d" % i, [128, D], F32) for i in range(4)]
        bvt = [Buf("a4v%d" % i) for i in range(4)]
        xn = [T("a4xn%d" % i, [128, D], BF16) for i in range(2)]
        bxn = [Buf("a4xn%d" % i) for i in range(2)]
        xf = T("a4xf", [128, D], F32)
        bxf = Buf("a4xf")
        hTbs = [T("a4hT%d" % i, [128, 16, 512], BF16) for i in range(2)]
        bhTs = [Buf("a4hT%d" % i) for i in range(2)]
        fTb, sTb = T("a4fT", [128, 8, 512], BF16), T("a4sT", [128, 4, 512], BF16)
        bfs = Buf("a4fs")
        wm = [T("a4wm%d" % i, [128, 44, 128], BF16) for i in range(2)]
        bwm = [Buf("a4wm%d" % i) for i in range(2)]
        mT = T("a4mT", [128, 16, 512], BF16)
        bmT = Buf("a4mT")
        wo = [T("a4wo%d" % i, [128, 16, 256], BF16) for i in range(2)]
        xn2b = T("a4xn2b", [128, D], BF16)
        bxn2b = Buf("xn2b")
        bwo = [Buf("a4wo%d" % i) for i in range(2)]
        sA, sB = T("a4sA", [128, 512], F32), T("a4sB", [128, 512], F32)
        bsA, bsB = Buf("sA"), Buf("sB")
        lngb, lnbb = T("a4lg", [128, D], F32), T("a4lb", [128, D], F32)
        bbc = Buf("a4bc")
        h2T = T("a4h2T", [128, 16, 128], F32)
        bh2T = Buf("a4h2T")
        wr = T("a4wr", [128, 16, 36], F32)
        brt = T("a4brt", [128, 36], F32)
        bwr = Buf("a4wr")
        st = [T("a4st%d" % i, [128, 24], F32) for i in range(2)]
        sm = [T("a4sm%d" % i, [128, 4], F32) for i in range(2)]
        bst = [Buf("a4st%d" % i) for i in range(2)]
        F.dma("sp", lambda e: e.dma_start(out=lngb[:], in_=I["lnrows"][0:1, :].partition_broadcast(128)), writes=[bbc])
        F.dma("sp", lambda e: e.dma_start(out=lnbb[:], in_=I["lnrows"][1:2, :].partition_broadcast(128)), writes=[bbc])
        F.dma("sp", lambda e: e.dma_start(out=wr[:], in_=I["w_rt"]), writes=[bwr])
        F.dma("sp", lambda e: e.dma_start(out=brt[:], in_=I["b_rt"].partition_broadcast(128)), writes=[bwr])
        stc = [0]
        xa = [T("a4xa%d" % i, [128, D], F32) for i in range(2)]
        bxa = [Buf("a4xa%d" % i) for i in range(2)]

        def stage_a0(tb):
            t0 = tb * 512
            F.dma("act", lambda e: e.dma_start(out=fTb[:], in_=fT_d[:, :, t0:t0 + 512].rearrange("k p t -> p k t")), reads=[b_fTd], writes=[bfs])
            F.dma("act", lambda e: e.dma_start(out=sTb[:], in_=sT_d[:, :, t0:t0 + 512].rearrange("k p t -> p k t")), reads=[b_sTd], writes=[bfs])

        def stage_a1(tb, i):
            r0 = tb * 512 + i * 128
            xx, bxx = xa[i % 2], bxa[i % 2]
            F.dma("sp", lambda e: e.dma_start(out=xx[:], in_=I["x"][r0:r0 + 128, :]), writes=[bxx])
            p = i % 2
            mv, rs, nmr = sma[p][:, 0:2], sma[p][:, 2:3], sma[p][:, 3:4]
            ln_stats(xx, bxx, sta[p], mv, rs, nmr, bsta[p])
            F.op("act", lambda e: e.activation(out=xn[p][:], in_=xx[:], func=AF.Identity, bias=nmr, scale=rs),
                 reads=[bxx, bsta[p]], writes=[bxn[p]])

        def stage_a2(tb, i):
            p = i % 2
            hTb, bhT = hTbs[tb % 2], bhTs[tb % 2]
            for hb in range(2):
                for jj in range(8):
                    j = hb * 8 + jj
                    F.op("pe", lambda e: e.transpose(out=psT[hb][:, jj * 128:(jj + 1) * 128], in_=xn[p][:, j * 128:(j + 1) * 128], identity=ident_bf[:]),
                         reads=[bxn[p], b_identbf], writes=[bpsT[hb]], signal=(jj == 7))
                for jj in range(8):
                    j = hb * 8 + jj
                    if jj % 2 == 0:
                        F.op("act", lambda e: e.activation(out=hTb[:, j, i * 128:(i + 1) * 128], in_=psT[hb][:, jj * 128:(jj + 1) * 128], func=AF.Identity,
                                                           bias=sh1[:, j:j + 1], scale=sc1p[:, j:j + 1]), reads=[bpsT[hb], b_modc], writes=[bhT])
                    else:
                        F.op("dve", lambda e: e.tensor_scalar(out=hTb[:, j, i * 128:(i + 1) * 128], in0=psT[hb][:, jj * 128:(jj + 1) * 128],
                                                              scalar1=sc1p[:, j:j + 1], scalar2=sh1[:, j:j + 1], op0=ALU.mult, op1=ALU.add),
                             reads=[bpsT[hb], b_modc], writes=[bhT])

        def stage_b(tb, oc):
            w = wm[oc % 2]
            bw = bwm[oc % 2]
            hTb, bhT = hTbs[tb % 2], bhTs[tb % 2]
            F.dma("sp", lambda e: e.dma_start(out=w[:], in_=Wmix_d[oc]), reads=[b_Wmix], writes=[bw])
            for k in range(16):
                F.op("pe", lambda e: e.matmul(ps[0][:], lhsT=w[:, k, :], rhs=hTb[:, k, :], start=(k == 0), stop=(k == 15)),
                     reads=[bw, bhT], writes=[bps[0]], signal=(k == 15))
            for k in range(16):
                F.op("pe", lambda e: e.matmul(ps[1][:], lhsT=w[:, 16 + k, :], rhs=hTb[:, k, :], start=(k == 0), stop=(k == 15)),
                     reads=[bw, bhT], writes=[bps[1]], signal=(k == 15))
            for k in range(8):
                F.op("pe", lambda e: e.matmul(ps[2][:], lhsT=w[:, 32 + k, :], rhs=fTb[:, k, :], start=(k == 0), stop=(k == 7)),
                     reads=[bw, bfs], writes=[bps[2]], signal=(k == 7))
            for k in range(4):
                F.op("pe", lambda e: e.matmul(ps[3][:], lhsT=w[:, 40 + k, :], rhs=sTb[:, k, :], start=(k == 0), stop=(k == 3)),
                     reads=[bw, bfs], writes=[bps[3]], signal=(k == 3))
            F.op("act", lambda e: e.activation(out=sA[:], in_=ps[0][:], func=AF.Sigmoid), reads=[bps[0]], writes=[bsA])
            F.op("act", lambda e: e.activation(out=sB[:], in_=ps[1][:], func=AF.Sigmoid), reads=[bps[1]], writes=[bsB])
            F.op("dve", lambda e: e.tensor_tensor(out=sA[:], in0=sA[:], in1=ps[2][:], op=ALU.mult), reads=[bsA, bps[2]], writes=[bsA])
            F.op("dve", lambda e: e.tensor_tensor(out=sB[:], in0=sB[:], in1=ps[3][:], op=ALU.mult), reads=[bsB, bps[3]], writes=[bsB])
            F.op("dve", lambda e: e.tensor_tensor(out=mT[:, oc, :], in0=sA[:], in1=sB[:], op=ALU.add), reads=[bsA, bsB], writes=[bmT])

        def stage_c(tb):
            t0 = tb * 512
            for i in range(4):
                r0 = t0 + i * 128
                F.dma("act", lambda e: e.dma_start(out=vt[i][:], in_=I["x"][r0:r0 + 128, :]), writes=[bvt[i]])
            for n in range(8):
                wv = wo[n % 2]
                bwv = bwo[n % 2]
                F.dma("sp", lambda e: e.dma_start(out=wv[:], in_=Wo_d[:, :, n * 256:(n + 1) * 256]), reads=[b_Wo], writes=[bwv])
                for i in range(4):
                    pp = ps[4 + i % 2]
                    bpp = bps[4 + i % 2]
                    for mc in range(16):
                        F.op("pe", lambda e: e.matmul(pp[:, 0:256], lhsT=mT[:, mc, i * 128:(i + 1) * 128], rhs=wv[:, mc, :], start=(mc == 0), stop=(mc == 15)),
                             reads=[bmT, bwv], writes=[bpp], signal=(mc == 15))
                    F.op("dve", lambda e: e.scalar_tensor_tensor(out=vt[i][:, n * 256:(n + 1) * 256], in0=vt[i][:, n * 256:(n + 1) * 256], scalar=ALPHA,
                                                                 in1=pp[:, 0:256], op0=ALU.mult, op1=ALU.add), reads=[bvt[i], bpp], writes=[bvt[i]])

        def stage_d1(tb, i):
            r0 = tb * 512 + i * 128
            p = i % 2
            mv, rs, nmr = sm[p][:, 0:2], sm[p][:, 2:3], sm[p][:, 3:4]
            ln_stats(vt[i], bvt[i], st[p], mv, rs, nmr, bst[p])
            F.op("act", lambda e: e.activation(out=vt[i][:], in_=vt[i][:], func=AF.Identity, bias=nmr, scale=rs), reads=[bvt[i], bst[p]], writes=[bvt[i]])
            F.op("pool", lambda e: e.tensor_tensor(out=vt[i][:], in0=vt[i][:], in1=lngb[:], op=ALU.mult), reads=[bvt[i], bbc], writes=[bvt[i]])
            F.op("pool", lambda e: e.tensor_tensor(out=vt[i][:], in0=vt[i][:], in1=lnbb[:], op=ALU.add), reads=[bvt[i], bbc], writes=[bvt[i]])
            F.dma("pool", lambda e: e.dma_start(out=xmid_d[r0:r0 + 128, :], in_=vt[i][:]), reads=[bvt[i]], writes=[b_xmid])
            ln_stats(vt[i], bvt[i], st[p], mv, rs, nmr, bst[p])
            F.op("act", lambda e: e.activation(out=xf[:], in_=vt[i][:], func=AF.Identity, bias=nmr, scale=rs), reads=[bvt[i], bst[p]], writes=[bxf])
            F.op("pool", lambda e: e.tensor_copy(out=xn2b[:], in_=xf[:]), reads=[bxf], writes=[bxn2b])
            F.dma("pool", lambda e: e.dma_start(out=xn2_d[r0:r0 + 128, :], in_=xn2b[:]), reads=[bxn2b], writes=[b_xn2])

        def stage_d2(tb, i):
            tix = tb * 4 + i
            for g4 in range(4):
                pt = ps[4 + g4 % 2]
                bpt = bps[4 + g4 % 2]
                for jj in range(4):
                    j = g4 * 4 + jj
                    F.op("pe", lambda e: e.transpose(out=pt[:, jj * 128:(jj + 1) * 128], in_=xf[:, j * 128:(j + 1) * 128], identity=ident_f[:]),
                         reads=[bxf, b_identf], writes=[bpt], signal=(jj == 3))
                for jj in range(4):
                    j = g4 * 4 + jj
                    if jj % 2 == 0:
                        F.op("act", lambda e: e.activation(out=h2T[:, j, :], in_=pt[:, jj * 128:(jj + 1) * 128], func=AF.Identity,
                                                           bias=sh2[:, j:j + 1], scale=sc2p[:, j:j + 1]), reads=[bpt, b_modc], writes=[bh2T])
                    else:
                        F.op("dve", lambda e: e.tensor_scalar(out=h2T[:, j, :], in0=pt[:, jj * 128:(jj + 1) * 128],
                                                              scalar1=sc2p[:, j:j + 1], scalar2=sh2[:, j:j + 1], op0=ALU.mult, op1=ALU.add),
                             reads=[bpt, b_modc], writes=[bh2T])
            for k in range(16):
                F.op("pe", lambda e: e.matmul(ps[4][:, 0:36], lhsT=h2T[:, k, :], rhs=wr[:, k, :], start=(k == 0), stop=(k == 15)),
                     reads=[bh2T, bwr], writes=[bps[4]], signal=(k == 15))
            F.op("dve", lambda e: e.tensor_tensor(out=lg_all[:, tix, :], in0=ps[4][:, 0:36], in1=brt[:], op=ALU.add), reads=[bps[4], bwr], writes=[b_lg])

        sta = [T("a4sta%d" % i, [128, 24], F32) for i in range(2)]
        sma = [T("a4sma%d" % i, [128, 4], F32) for i in range(2)]
        bsta = [Buf("a4sta%d" % i) for i in range(2)]
        for i in range(4):
            stage_a1(0, i)
            stage_a2(0, i)
        for tb in range(9):
            if tb < 8:
                stage_a0(tb)
            for oc in range(16):
                i4, r4 = oc // 4, oc % 4
                if r4 == 0 and tb >= 1:
                    stage_d1(tb - 1, i4)
                if r4 == 1 and tb + 1 < 8:
                    stage_a1(tb + 1, i4)
                if tb < 8:
                    stage_b(tb, oc)
                if r4 == 2 and tb + 1 < 8:
                    stage_a2(tb + 1, i4)
                if r4 == 3 and tb >= 1:
                    stage_d2(tb - 1, i4)
            if tb < 8:
                stage_c(tb)
        if "lg_d" in dbg:
            ld = scratch("lg_d", [128, 32, 36], F32)
            F.dma("sp", lambda e: e.dma_start(out=ld, in_=lg_all[:]), reads=[b_lg], writes=[Buf()])
        F.barrier()
    if stop_after == "A4":
        F.barrier()
        return nc

    NBLK = 96
    NROW = NBLK * 128
    rowinfo_d = scratch("rowinfo_d", [NROW, 1], I32)
    roww_d = scratch("roww_d", [NROW, 1], F32)
    ybuf_d = scratch("ybuf_d", [2 * TOWN, D], BF16)
    b_rowinfo, b_roww, b_ybuf = Buf("rowinfo_d"), Buf("roww_d"), Buf("ybuf_d")
    idxw = sb([128, NBLK, 4], I32, "idxw")
    b_bexp = Buf("bexp")
    with ExitStack() as es:
        T = lambda nm, shp, dt: es.enter_context(nc.sbuf_tensor(nm, shp, dt))
        R_ = [Buf("rt")]
        V = lambda fn, rd=R_, wr=R_: F.op("dve", fn, reads=rd, writes=wr)
        A = lambda fn, rd=R_, wr=R_: F.op("act", fn, reads=rd, writes=wr)
        tri, ones_bf = T("r_tri", [128, 128], BF16), T("r_ones", [128, 128], BF16)
        thr, blkrow = T("r_thr", [128, 32], F32), T("r_blkrow", [128, NBLK], F32)
        tokid = T("r_tokid", [128, 64], I32)
        for t_, nm in ((tri, "tri"), (ones_bf, "ones_bf"), (thr, "thr"), (blkrow, "blkrow"), (tokid, "tokid")):
            F.dma("sp", lambda e: e.dma_start(out=t_[:], in_=I[nm]), writes=R_)
        gmax, gsum, gtop = T("r_gmax", [128, 32], F32), T("r_gsum", [128, 32], F32), T("r_gtop", [128, 32], F32)
        ohg, exg = T("r_ohg", [128, 32, 4], F32), T("r_exg", [128, 32, 4], F32)
        msk = T("r_msk", [128, 32, 32], F32)
        m8 = T("r_m8", [128, 32, 8], F32)
        oh = [T("r_oh%d" % k, [128, 32, 32], F32) for k in range(2)]
        cntb = T("r_cnt", [128, 32, 32], BF16)
        pf = T("r_pf", [128, 32, 32], F32)
        tot, nbv, pendb, pst_ = T("r_tot", [128, 32], F32), T("r_nb", [128, 32], F32), T("r_pend", [128, 32], F32), T("r_pst", [128, 32], F32)
        cmp_ = T("r_cmp", [128, NBLK, 32], F32)
        onesf = T("r_onesf", [128, 32], F32)
        wk = [T("r_w%d" % k, [128, 32], F32) for k in range(2)]
        dst = [T("r_dst%d" % k, [128, 32], F32) for k in range(2)]
        dsti = [T("r_dsti%d" % k, [128, 32], I32) for k in range(2)]
        bef, bef2 = T("r_bef", [128, NBLK], F32), T("r_bef2", [128, NBLK], F32)
        oobt = T("r_oob", [128, NBLK], I32)
        lgp, lep = lg_all[:, :, 0:4], lg_all[:, :, 4:36]
        V(lambda e: e.tensor_reduce(out=gmax[:], in_=lgp, axis=AX.X, op=ALU.max), [b_lg], R_)
        V(lambda e: e.tensor_tensor(out=exg[:], in0=lgp, in1=gmax[:].unsqueeze(2).to_broadcast([128, 32, 4]), op=ALU.subtract), [b_lg] + R_, R_)
        V(lambda e: e.tensor_single_scalar(out=ohg[:], in_=exg[:], scalar=0.0, op=ALU.is_ge))
        A(lambda e: e.activation(out=exg[:], in_=exg[:], func=AF.Exp))
        V(lambda e: e.tensor_reduce(out=gsum[:], in_=exg[:], axis=AX.X, op=ALU.add))
        V(lambda e: e.reciprocal(out=gtop[:], in_=gsum[:]))
        V(lambda e: e.tensor_scalar(out=ohg[:], in0=ohg[:], scalar1=-1.0, scalar2=1e30, op0=ALU.add, op1=ALU.mult))
        V(lambda e: e.tensor_tensor(out=msk[:].rearrange("p t (g x) -> p t g x", g=4), in0=lep.rearrange("p t (g x) -> p t g x", g=4),
                                    in1=ohg[:].unsqueeze(3).to_broadcast([128, 32, 4, 8]), op=ALU.add), [b_lg] + R_, R_)
        for j in range(32):
            V(lambda e: e.max(out=m8[:, j, :], in_=msk[:, j, :]))
        for k in range(2):
            V(lambda e: e.tensor_tensor(out=oh[k][:], in0=msk[:], in1=m8[:, :, k:k + 1].to_broadcast([128, 32, 32]), op=ALU.is_equal))
        V(lambda e: e.tensor_tensor(out=wk[1][:], in0=m8[:, :, 1], in1=m8[:, :, 0], op=ALU.subtract))
        A(lambda e: e.activation(out=wk[1][:], in_=wk[1][:], func=AF.Exp))
        V(lambda e: e.tensor_scalar(out=wk[1][:], in0=wk[1][:], scalar1=1.0, scalar2=None, op0=ALU.add))
        V(lambda e: e.reciprocal(out=wk[1][:], in_=wk[1][:]))
        V(lambda e: e.tensor_tensor(out=wk[0][:], in0=gtop[:], in1=wk[1][:], op=ALU.mult))
        V(lambda e: e.tensor_tensor(out=wk[1][:], in0=gtop[:], in1=wk[0][:], op=ALU.subtract))
        V(lambda e: e.tensor_tensor(out=cntb[:], in0=oh[0][:], in1=oh[1][:], op=ALU.add))
        for hb in range(2):
            pp = ps[hb]
            for jj in range(16):
                j = hb * 16 + jj
                n_mm = 1 + j
                F.op("pe", lambda e: e.matmul(pp[:, jj * 32:(jj + 1) * 32], lhsT=tri[:], rhs=cntb[:, j, :], start=True, stop=(n_mm == 1)),
                     reads=R_, writes=[bps[hb]], signal=(n_mm == 1 and jj == 15))
                for j2 in range(j):
                    F.op("pe", lambda e: e.matmul(pp[:, jj * 32:(jj + 1) * 32], lhsT=ones_bf[:], rhs=cntb[:, j2, :], start=False, stop=(j2 == j - 1)),
                         reads=R_, writes=[bps[hb]], signal=(j2 == j - 1 and jj == 15))
            V(lambda e: e.tensor_copy(out=pf[:, hb * 16:(hb + 1) * 16, :], in_=pp[:].rearrange("p (t x) -> p t x", t=16)), [bps[hb]] + R_, R_)
        for j in range(32):
            F.op("pe", lambda e: e.matmul(ps[2][:, 0:32], lhsT=ones_bf[:], rhs=cntb[:, j, :], start=(j == 0), stop=(j == 31)),
                 reads=R_, writes=[bps[2]], signal=(j == 31))
        V(lambda e: e.tensor_copy(out=tot[:], in_=ps[2][:, 0:32]), [bps[2]] + R_, R_)
        V(lambda e: e.tensor_tensor(out=cmp_[:, 0:32, :], in0=tot[:].unsqueeze(2).to_broadcast([128, 32, 32]),
                                    in1=thr[:].unsqueeze(1).to_broadcast([128, 32, 32]), op=ALU.is_gt))
        V(lambda e: e.tensor_reduce(out=nbv[:], in_=cmp_[:, 0:32, :], axis=AX.X, op=ALU.add))
        V(lambda e: e.memset(onesf[:], 1.0))
        V(lambda e: e.tensor_tensor_scan(out=pendb[:], data0=onesf[:], data1=nbv[:], initial=0.0, op0=ALU.mult, op1=ALU.add))
        V(lambda e: e.tensor_tensor(out=pst_[:], in0=pendb[:], in1=nbv[:], op=ALU.subtract))
        V(lambda e: e.tensor_scalar(out=pst_[:], in0=pst_[:], scalar1=128.0, scalar2=None, op0=ALU.mult))
        V(lambda e: e.tensor_tensor(out=pf[:], in0=pf[:], in1=pst_[:].unsqueeze(1).to_broadcast([128, 32, 32]), op=ALU.add))
        for k in range(2):
            V(lambda e: e.tensor_tensor(out=oh[k][:], in0=oh[k][:], in1=pf[:], op=ALU.mult))
            V(lambda e: e.tensor_reduce(out=dst[k][:], in_=oh[k][:], axis=AX.X, op=ALU.add))
            V(lambda e: e.tensor_copy(out=dsti[k][:], in_=dst[k][:]))
        V(lambda e: e.tensor_tensor(out=cmp_[:], in0=pendb[:].unsqueeze(1).to_broadcast([128, NBLK, 32]),
                                    in1=blkrow[:].unsqueeze(2).to_broadcast([128, NBLK, 32]), op=ALU.is_le))
        V(lambda e: e.tensor_reduce(out=bef[:], in_=cmp_[:], axis=AX.X, op=ALU.add))
        V(lambda e: e.tensor_scalar(out=bef[:], in0=bef[:], scalar1=31.0, scalar2=None, op0=ALU.min))
        V(lambda e: e.memset(bef2[:], 1.0))
        V(lambda e: e.tensor_tensor(out=bef2[:, 1:NBLK], in0=bef[:, 1:NBLK], in1=bef[:, 0:NBLK - 1], op=ALU.not_equal))
        pg4 = T("r_pg4", [128, 4], F32)
        idxf = T("r_idxf", [128, NBLK, 4], F32)
        F.dma("sp", lambda e: e.dma_start(out=pg4[:], in_=I["pg4"]), writes=R_)
        V(lambda e: e.tensor_scalar(out=bef[:], in0=bef[:], scalar1=-64.0, scalar2=None, op0=ALU.add))
        V(lambda e: e.tensor_tensor(out=bef[:], in0=bef[:], in1=bef2[:], op=ALU.mult))
        V(lambda e: e.tensor_scalar(out=bef[:], in0=bef[:], scalar1=64.0, scalar2=512.0, op0=ALU.add, op1=ALU.mult))
        V(lambda e: e.tensor_tensor(out=idxf[:], in0=bef[:].unsqueeze(2).to_broadcast([128, NBLK, 4]),
                                    in1=pg4[:].unsqueeze(1).to_broadcast([128, NBLK, 4]), op=ALU.add))
        V(lambda e: e.tensor_copy(out=idxw[:], in_=idxf[:]), R_, [b_bexp])
        V(lambda e: e.memset(oobt[:], 1 << 20))
        F.dma("sp", lambda e: e.dma_start(out=rowinfo_d.rearrange("(p a) o -> p (a o)", p=128), in_=oobt[:]), reads=R_, writes=[b_rowinfo])
        F.dma("sp", lambda e: e.dma_start(out=roww_d.rearrange("(p a) o -> p (a o)", p=128), in_=bef[:]), reads=R_, writes=[b_roww])
        for k in range(2):
            for j in range(32):
                F.dma("pool", lambda e: e.indirect_dma_start(out=rowinfo_d, out_offset=bass.IndirectOffsetOnAxis(ap=dsti[k][:, j:j + 1], axis=0),
                                                             in_=tokid[:, k * 32 + j:k * 32 + j + 1], in_offset=None), reads=R_, writes=[b_rowinfo])
                F.dma("pool", lambda e: e.indirect_dma_start(out=roww_d, out_offset=bass.IndirectOffsetOnAxis(ap=dsti[k][:, j:j + 1], axis=0),
                                                             in_=wk[k][:, j:j + 1], in_offset=None), reads=R_, writes=[b_roww])
        if "rt_d" in dbg:
            rd = scratch("rt_d", [128, 6, 32], F32)
            rt = T("r_dbg", [128, 6, 32], F32)
            V(lambda e: e.tensor_copy(out=rt[:, 0, :], in_=dst[0][:]))
            V(lambda e: e.tensor_copy(out=rt[:, 1, :], in_=dst[1][:]))
            V(lambda e: e.tensor_copy(out=rt[:, 2, :], in_=wk[0][:]))
            V(lambda e: e.tensor_copy(out=rt[:, 3, :], in_=wk[1][:]))
            V(lambda e: e.tensor_copy(out=rt[:, 4, :], in_=tot[:]))
            V(lambda e: e.tensor_copy(out=rt[:, 5, :], in_=bef[:, 0:32]))
            F.dma("sp", lambda e: e.dma_start(out=rd, in_=rt[:]), reads=R_, writes=[Buf()])
        F.barrier()
    if stop_after == "R":
        F.barrier()
        return nc

    with ExitStack() as es:
        T = lambda nm, shp, dt: es.enter_context(nc.sbuf_tensor(nm, shp, dt))
        W1, W3, W2 = T("m_w1", [128, 16, 1024], BF16), T("m_w3", [128, 16, 1024], BF16), T("m_w2", [128, 8, D], BF16)
        bW1 = [Buf("w1_%d" % i) for i in range(4)]
        bW3 = [Buf("w3_%d" % i) for i in range(4)]
        bW2 = [Buf("w2_%d" % i) for i in range(4)]
        X = [T("m_x%d" % i, [128, D], BF16) for i in range(2)]
        XT = [T("m_xt%d" % i, [128, 16, 128], BF16) for i in range(2)]
        sl = [T("m_sl%d" % i, [128, 512], F32) for i in range(2)]
        h1 = [T("m_h1%d" % i, [128, 1024], BF16) for i in range(2)]
        h1T = [T("m_h1T%d" % i, [128, 8, 128], BF16) for i in range(2)]
        ysb = [T("m_y%d" % i, [128, D], BF16) for i in range(2)]
        ri = [T("m_ri%d" % i, [128, 2], I32) for i in range(2)]
        rw = [T("m_rw%d" % i, [128, 1], F32) for i in range(2)]
        bX, bXT, bsl, bh1, bh1T, bys, bri = ([Buf("mx%d" % i) for i in range(2)], [Buf("mxt%d" % i) for i in range(2)], [Buf("msl%d" % i) for i in range(2)],
                                             [Buf("mh1%d" % i) for i in range(2)], [Buf("mh1T%d" % i) for i in range(2)], [Buf("my%d" % i) for i in range(2)],
                                             [Buf("mri%d" % i) for i in range(2)])
        w1v = I["w1"].rearrange("e (p g k) n -> (e p g) (k n)", p=128, g=4, k=4)
        w3v = I["w3"].rearrange("e (p g k) n -> (e p g) (k n)", p=128, g=4, k=4)
        w2v = I["w2"].rearrange("e (p g k) n -> (e p g) (k n)", p=128, g=4, k=2)
        sc2k, sh2k = T("m_sc2k", [128, 16], F32), T("m_sh2k", [128, 16], F32)
        b_m2k = Buf("m2k")
        F.dma("sp", lambda e: e.dma_start(out=sh2k[:], in_=gates_d[3].rearrange("(p k) -> p k", k=16)), reads=[b_gates], writes=[b_m2k])
        F.dma("sp", lambda e: e.dma_start(out=sc2k[:], in_=gates_d[4].rearrange("(p k) -> p k", k=16)), reads=[b_gates], writes=[b_m2k])

        rb_w = nc.gpsimd.alloc_register("rb_w")
        nc.gpsimd.reg_mov(rb_w, 32 * 512 - 1)
        rb_y = nc.gpsimd.alloc_register("rb_y")
        nc.gpsimd.reg_mov(rb_y, 2 * TOWN - 1)

        def wload(i, dst_ap, src, g, bw):
            F.dma("pool", lambda e: e.indirect_dma_start(out=dst_ap, out_offset=None, in_=src,
                                                         in_offset=bass.IndirectOffsetOnAxis(ap=idxw[:, i, g:g + 1], axis=0),
                                                         bounds_check=rb_w, oob_is_err=False), reads=[b_bexp], writes=[bw])
        def rows(i):
            p = i % 2
            F.dma("sp", lambda e: e.dma_start(out=ri[p][:, 0:1], in_=rowinfo_d[i * 128:(i + 1) * 128, :]), reads=[b_rowinfo], writes=[bri[p]])
            F.op("dve", lambda e: e.tensor_single_scalar(out=ri[p][:, 1:2], in_=ri[p][:, 0:1], scalar=4095, op=ALU.bitwise_and), reads=[bri[p]], writes=[bri[p]])
            F.dma("pool", lambda e: e.indirect_dma_start(out=X[p][:], out_offset=None, in_=xn2_d, in_offset=bass.IndirectOffsetOnAxis(ap=ri[p][:, 1:2], axis=0)),
                  reads=[bri[p], b_xn2], writes=[bX[p]])

        def T1(i):
            p = i % 2
            for hb in range(2):
                for jj in range(8):
                    j = hb * 8 + jj
                    F.op("pe", lambda e: e.transpose(out=psT[hb][:, jj * 128:(jj + 1) * 128], in_=X[p][:, j:D:16], identity=ident_bf[:]),
                         reads=[bX[p], b_identbf], writes=[bpsT[hb]], signal=(jj == 7))

        def E1(i):
            p = i % 2
            for hb in range(2):
                for jj in range(8):
                    j = hb * 8 + jj
                    if jj % 2 == 0:
                        F.op("act", lambda e: e.activation(out=XT[p][:, j, :], in_=psT[hb][:, jj * 128:(jj + 1) * 128], func=AF.Identity,
                                                           bias=sh2k[:, j:j + 1], scale=sc2k[:, j:j + 1]), reads=[bpsT[hb], b_m2k], writes=[bXT[p]])
                    else:
                        F.op("dve", lambda e: e.tensor_scalar(out=XT[p][:, j, :], in0=psT[hb][:, jj * 128:(jj + 1) * 128],
                                                              scalar1=sc2k[:, j:j + 1], scalar2=sh2k[:, j:j + 1], op0=ALU.mult, op1=ALU.add),
                             reads=[bpsT[hb], b_m2k], writes=[bXT[p]])

        def W13(i):
            for kg in range(4):
                wload(i, W1[:, 4 * kg:4 * kg + 4, :].rearrange("p k n -> p (k n)"), w1v, kg, bW1[kg])
                wload(i, W3[:, 4 * kg:4 * kg + 4, :].rearrange("p k n -> p (k n)"), w3v, kg, bW3[kg])

        def W2l(i):
            for kg in range(4):
                wload(i, W2[:, 2 * kg:2 * kg + 2, :].rearrange("p k n -> p (k n)"), w2v, kg, bW2[kg])

        def H(i):
            p = i % 2
            for k in range(16):
                for n in range(2):
                    F.op("pe", lambda e: e.matmul(ps[2 * n][:], lhsT=XT[p][:, k, :], rhs=W1[:, k, n * 512:(n + 1) * 512], start=(k == 0), stop=(k == 15)),
                         reads=[bXT[p], bW1[k // 4]], writes=[bps[2 * n]], signal=(k % 4 == 3))
                    F.op("pe", lambda e: e.matmul(ps[2 * n + 1][:], lhsT=XT[p][:, k, :], rhs=W3[:, k, n * 512:(n + 1) * 512], start=(k == 0), stop=(k == 15)),
                         reads=[bXT[p], bW3[k // 4]], writes=[bps[2 * n + 1]], signal=(k % 4 == 3))
            for n in range(2):
                F.op("act", lambda e: e.activation(out=sl[n][:], in_=ps[2 * n][:], func=AF.Silu), reads=[bps[2 * n]], writes=[bsl[n]])
                F.op("dve", lambda e: e.tensor_tensor(out=h1[p][:, n * 512:(n + 1) * 512], in0=sl[n][:], in1=ps[2 * n + 1][:], op=ALU.mult),
                     reads=[bsl[n], bps[2 * n + 1]], writes=[bh1[p]])

        ps5b = ps[5][:].bitcast(BF16)

        def T2(i):
            p = i % 2
            for jj in range(8):
                F.op("pe", lambda e: e.transpose(out=ps5b[:, jj * 128:(jj + 1) * 128], in_=h1[p][:, jj:1024:8], identity=ident_bf[:]),
                     reads=[bh1[p], b_identbf], writes=[bps[5]], signal=(jj == 7))
            F.op("act", lambda e: e.activation(out=h1T[p][:, 0:4, :], in_=ps5b[:, 0:512].rearrange("p (k t) -> p k t", k=4), func=AF.Copy),
                 reads=[bps[5]], writes=[bh1T[p]])
            F.op("dve", lambda e: e.tensor_copy(out=h1T[p][:, 4:8, :], in_=ps5b[:, 512:1024].rearrange("p (k t) -> p k t", k=4)),
                 reads=[bps[5]], writes=[bh1T[p]])

        def Y(i):
            p = i % 2
            for n in range(4):
                py = ps[4 + n % 2]
                for k in range(8):
                    F.op("pe", lambda e: e.matmul(py[:], lhsT=h1T[p][:, k, :], rhs=W2[:, k, n * 512:(n + 1) * 512], start=(k == 0), stop=(k == 7)),
                         reads=[bh1T[p], bW2[k // 2]], writes=[bps[4 + n % 2]], signal=(k == 7))
                if n % 2 == 0:
                    F.op("act", lambda e: e.activation(out=ysb[p][:, n * 512:(n + 1) * 512], in_=py[:], func=AF.Identity, scale=rw3[i % 3][:, 0:1]),
                         reads=[bps[4 + n % 2], bri3[i % 3]], writes=[bys[p]])
                else:
                    F.op("dve", lambda e: e.tensor_scalar(out=ysb[p][:, n * 512:(n + 1) * 512], in0=py[:], scalar1=rw3[i % 3][:, 0:1], scalar2=None, op0=ALU.mult),
                         reads=[bps[4 + n % 2], bri3[i % 3]], writes=[bys[p]])

        def SC(i):
            p = i % 2
            F.dma("pool", lambda e: e.indirect_dma_start(out=ybuf_d, out_offset=bass.IndirectOffsetOnAxis(ap=ri3[i % 3][:, 0:1], axis=0), in_=ysb[p][:], in_offset=None,
                                                         bounds_check=rb_y, oob_is_err=False), reads=[bys[p], bri3[i % 3]], writes=[b_ybuf])
        ri3 = [T("m_ri3%d" % k, [128, 1], I32) for k in range(3)]
        bri3 = [Buf("mri3%d" % k) for k in range(3)]
        rw3 = [T("m_rw3%d" % k, [128, 1], F32) for k in range(3)]

        def rows3(i):
            F.dma("sp", lambda e: e.dma_start(out=ri3[i % 3][:], in_=rowinfo_d[i * 128:(i + 1) * 128, :]), reads=[b_rowinfo], writes=[bri3[i % 3]])
            F.dma("sp", lambda e: e.dma_start(out=rw3[i % 3][:], in_=roww_d[i * 128:(i + 1) * 128, :]), reads=[b_roww], writes=[bri3[i % 3]])
        W13(0)
        W2l(0)
        rows(0)
        rows3(0)
        rows(1)
        rows3(1)
        T1(0)
        E1(0)
        for i in range(NBLK):
            if i + 2 < NBLK:
                rows3(i + 2)
                rows(i + 2)
            H(i)
            if i + 1 < NBLK:
                W13(i + 1)
                T1(i + 1)
            T2(i)
            if i + 1 < NBLK:
                E1(i + 1)
            Y(i)
            if i + 1 < NBLK:
                W2l(i + 1)
            SC(i)
        F.barrier()
    if stop_after == "MOE":
        F.barrier()
        return nc

    with ExitStack() as es:
        T = lambda nm, shp, dt: es.enter_context(nc.sbuf_tensor(nm, shp, dt))
        g2b, lg2, lb2 = T("f_g2", [128, D], F32), T("f_lg", [128, D], F32), T("f_lb", [128, D], F32)
        bbc = Buf("f_bc")
        F.dma("sp", lambda e: e.dma_start(out=g2b[:], in_=gates_d[5:6, :].partition_broadcast(128)), reads=[b_gates], writes=[bbc])
        F.dma("sp", lambda e: e.dma_start(out=lg2[:], in_=I["lnrows"][2:3, :].partition_broadcast(128)), writes=[bbc])
        F.dma("sp", lambda e: e.dma_start(out=lb2[:], in_=I["lnrows"][3:4, :].partition_broadcast(128)), writes=[bbc])
        NB_ = 4
        xm = [T("f_xm%d" % i, [128, D], F32) for i in range(NB_)]
        y0 = [T("f_y0%d" % i, [128, D], BF16) for i in range(NB_)]
        y1 = [T("f_y1%d" % i, [128, D], BF16) for i in range(NB_)]
        ys = [T("f_ys%d" % i, [128, D], F32) for i in range(2)]
        bys_ = [Buf("fys%d" % i) for i in range(2)]
        st = [T("f_st%d" % i, [128, 24], F32) for i in range(NB_)]
        sm = [T("f_sm%d" % i, [128, 4], F32) for i in range(NB_)]
        bxm, by0, by1, bst = ([Buf("fxm%d" % i) for i in range(NB_)], [Buf("fy0%d" % i) for i in range(NB_)], [Buf("fy1%d" % i) for i in range(NB_)],
                              [Buf("fst%d" % i) for i in range(NB_)])
        def f_loads(ti):
            p = ti % NB_
            r0 = ti * 128
            F.dma("sp", lambda e: e.dma_start(out=xm[p][:], in_=xmid_d[r0:r0 + 128, :]), reads=[b_xmid], writes=[bxm[p]])
            F.dma("sp", lambda e: e.dma_start(out=y0[p][:], in_=ybuf_d[r0:r0 + 128, :]), reads=[b_ybuf], writes=[by0[p]])
            F.dma("sp", lambda e: e.dma_start(out=y1[p][:], in_=ybuf_d[TOWN + r0:TOWN + r0 + 128, :]), reads=[b_ybuf], writes=[by1[p]])
        for ti in range(3):
            f_loads(ti)
        for ti in range(32):
            p = ti % NB_
            r0 = ti * 128
            if ti + 3 < 32:
                f_loads(ti + 3)
            q = ti % 2
            F.op("pool", lambda e: e.tensor_tensor(out=ys[q][:], in0=y0[p][:], in1=y1[p][:], op=ALU.add), reads=[by0[p], by1[p]], writes=[bys_[q]])
            F.op("pool", lambda e: e.tensor_tensor(out=ys[q][:], in0=ys[q][:], in1=g2b[:], op=ALU.mult), reads=[bys_[q], bbc], writes=[bys_[q]])
            F.op("dve", lambda e: e.scalar_tensor_tensor(out=xm[p][:], in0=xm[p][:], scalar=ALPHA, in1=ys[q][:], op0=ALU.mult, op1=ALU.add),
                 reads=[bxm[p], bys_[q]], writes=[bxm[p]])
            mv, rs, nmr = sm[p][:, 0:2], sm[p][:, 2:3], sm[p][:, 3:4]
            ln_stats(xm[p], bxm[p], st[p], mv, rs, nmr, bst[p])
            F.op("act", lambda e: e.activation(out=xm[p][:], in_=xm[p][:], func=AF.Identity, bias=nmr, scale=rs), reads=[bxm[p], bst[p]], writes=[bxm[p]])
            F.op("dve", lambda e: e.tensor_tensor(out=xm[p][:], in0=xm[p][:], in1=lg2[:], op=ALU.mult), reads=[bxm[p], bbc], writes=[bxm[p]])
            F.op("dve", lambda e: e.tensor_tensor(out=xm[p][:], in0=xm[p][:], in1=lb2[:], op=ALU.add), reads=[bxm[p], bbc], writes=[bxm[p]])
            F.dma("act", lambda e: e.dma_start(out=out_ap[r0:r0 + 128, :], in_=xm[p][:]), reads=[bxm[p]], writes=[b_out])
        F.barrier()
    F.finish([b_out], "sp")
    return nc


def kernel(**inputs):
    inp = {k: np.asarray(v) for k, v in inputs.items()}
    nc = build()
    in_maps = []
    for core in range(8):
        b, half = core // 2, core % 2
        in_maps.append(host_layout(inp, b, half))
    res = run_bass_kernel_spmd(nc, in_maps, core_ids=list(range(8)))
    out = np.zeros((4, TALL, D), np.float32)
    for core in range(8):
        b, half = core // 2, core % 2
        out[b, half * TOWN:(half + 1) * TOWN] = res.results[core]["out"]
    return out
```

```python
import os
from contextlib import ExitStack
import numpy as np
import ml_dtypes
import concourse.bass as bass
import concourse.mybir as mybir
from concourse.bass_utils import run_bass_kernel_spmd

F32 = mybir.dt.float32
BF16 = mybir.dt.bfloat16
I32 = mybir.dt.int32
ALU = mybir.AluOpType
AF = mybir.ActivationFunctionType
AX = mybir.AxisListType
NPBF = ml_dtypes.bfloat16

D = 2048
TOWN = 4096
TALL = 8192
ALPHA = 2.0 ** 0.25
EPS = 1e-5
PI = float(np.pi)


class Buf:
    __slots__ = ("name", "w", "r")

    def __init__(self, name=""):
        self.name = name
        self.w = None
        self.r = []


class Flow:
    def __init__(self, nc, n_dma_sems=24):
        self.nc = nc
        self.engs = {"pe": nc.tensor, "dve": nc.vector, "act": nc.scalar, "pool": nc.gpsimd, "sp": nc.sync}
        self.sem = {k: nc.alloc_semaphore("s_" + k) for k in self.engs}
        self.cnt = {k: 0 for k in self.engs}
        self.waited = {k: {} for k in self.engs}
        self.pending = {k: [] for k in self.engs}
        self.dsems = [nc.alloc_semaphore("d%d" % i) for i in range(n_dma_sems)]
        self.dcnt = [0] * n_dma_sems
        self.dnext = 0
        self.semobj = {}
        for k, s in self.sem.items():
            self.semobj[id(s)] = s
        for s in self.dsems:
            self.semobj[id(s)] = s

    def _need(self, reads, writes):
        need = {}

        def add(p):
            if p is None:
                return
            s, v = p
            k = id(s)
            if need.get(k, 0) < v:
                need[k] = v
        for b in reads:
            add(b.w)
        for b in writes:
            add(b.w)
            for p in b.r:
                add(p)
        return need

    def _emit_waits(self, e, need):
        eng = self.engs[e]
        own = id(self.sem[e])
        for k, v in need.items():
            if k == own and e == "pe":
                continue
            if self.waited[e].get(k, 0) >= v:
                continue
            eng.wait_ge(self.semobj[k], v)
            self.waited[e][k] = v

    def _commit(self, reads, writes, tag):
        for b in writes:
            b.w = tag
            b.r = []
        for b in reads:
            if b.w is not tag:
                b.r.append(tag)
                if len(b.r) > 48:
                    best = {}
                    for (s, v) in b.r:
                        if best.get(id(s), (None, 0))[1] < v:
                            best[id(s)] = (s, v)
                    b.r = list(best.values())

    def op(self, e, fn, reads=(), writes=(), signal=True):
        reads = list(reads)
        writes = list(writes)
        need = self._need(reads, writes)
        self._emit_waits(e, need)
        ins = fn(self.engs[e])
        if signal:
            self.cnt[e] += 1
            ins.then_inc(self.sem[e], 1)
            tag = (self.sem[e], self.cnt[e])
            pr = [b for (b, w) in self.pending[e] if not w] + reads
            pw = [b for (b, w) in self.pending[e] if w] + writes
            self.pending[e] = []
            self._commit(pr, pw, tag)
        else:
            assert e == "pe"
            for b in reads:
                self.pending[e].append((b, False))
            for b in writes:
                self.pending[e].append((b, True))
        return ins

    def dma(self, q, fn, reads=(), writes=()):
        reads = list(reads)
        writes = list(writes)
        need = self._need(reads, writes)
        i = self.dnext
        self.dnext = (self.dnext + 1) % len(self.dsems)
        s = self.dsems[i]
        if self.dcnt[i] > 0:
            need[id(s)] = max(need.get(id(s), 0), self.dcnt[i])
        self._emit_waits(q, need)
        ins = fn(self.engs[q])
        self.dcnt[i] += 16
        ins.then_inc(s, 16)
        tag = (s, self.dcnt[i])
        self._commit(reads, writes, tag)
        return ins

    def barrier(self):
        need = {}
        for k, s in self.sem.items():
            if self.cnt[k] > 0:
                need[id(s)] = self.cnt[k]
        for i, s in enumerate(self.dsems):
            if self.dcnt[i] > 0:
                need[id(s)] = self.dcnt[i]
        for e in self.engs:
            assert not self.pending[e]
            self._emit_waits(e, dict(need))

    def finish(self, bufs, e="sp"):
        need = self._need(bufs, [])
        self._emit_waits(e, need)


def host_consts(half):
    c = {}
    c["ident_bf"] = np.eye(128, dtype=np.float32).astype(NPBF)
    c["ident_f"] = np.eye(128, dtype=np.float32)
    rv = np.arange(128)
    rt = (rv + 64 * half) % 128
    kt = np.arange(64) + 64 * half
    ang = 2 * np.pi * np.outer(rt, kt) / 128.0
    s = 1.0 / np.sqrt(128.0)
    c["rowdft"] = np.concatenate([np.cos(ang) * s, -np.sin(ang) * s], axis=1).astype(NPBF)
    ch = np.arange(256)
    ang = 2 * np.pi * np.outer(ch, ch) / 256.0
    C = np.cos(ang) / 16.0
    S = np.sin(ang) / 16.0
    c["chA"] = np.concatenate([C, -S], axis=1).reshape(2, 128, 512).transpose(1, 0, 2).copy().astype(NPBF)
    c["chB"] = np.concatenate([S, C], axis=1).reshape(2, 128, 512).transpose(1, 0, 2).copy().astype(NPBF)
    cc = np.arange(64)
    ang = 2 * np.pi * np.outer(cc, cc) / 64.0
    CC = np.zeros((128, 128), np.float32)
    CS = np.zeros((128, 128), np.float32)
    CC[0:64, 0:64] = np.cos(ang) / 8.0
    CS[0:64, 0:64] = np.sin(ang) / 8.0
    c["colC"] = CC.astype(NPBF)
    c["colS"] = CS.astype(NPBF)
    c["halfcol"] = np.full((128, 1), float(half), np.float32)
    c["jrow"] = np.tile(np.arange(9, dtype=np.float32)[None, :], (128, 1))
    c["crow"] = np.tile(np.arange(544, dtype=np.float32)[None, :], (128, 1))
    m = np.zeros((4, 32, 4, 32), np.float32)
    for q in range(4):
        m[q, :, q, :] = 1.0
    c["blkmask"] = m.reshape(128, 128)
    tri = (np.arange(128)[:, None] < np.arange(128)[None, :]).astype(np.float32)
    c["tri"] = tri.astype(NPBF)
    c["ones_bf"] = np.ones((128, 128), np.float32).astype(NPBF)
    c["thr"] = np.tile((128.0 * np.arange(32, dtype=np.float32))[None, :], (128, 1))
    c["blkrow"] = np.tile((1.0 * np.arange(96, dtype=np.float32))[None, :], (128, 1))
    c["pidx"] = np.arange(128, dtype=np.float32).reshape(128, 1)
    c["pg4"] = (4.0 * np.arange(128, dtype=np.float32)[:, None] + np.arange(4, dtype=np.float32)[None, :])
    tk = (np.arange(128)[:, None] + 128 * np.arange(32)[None, :]).astype(np.int32)
    c["tokid"] = np.concatenate([tk, tk + TOWN], axis=1).astype(np.int32)
    return c


CONST_SPECS = {
    "ident_bf": ([128, 128], BF16), "ident_f": ([128, 128], F32), "rowdft": ([128, 128], BF16),
    "chA": ([128, 2, 512], BF16), "chB": ([128, 2, 512], BF16), "colC": ([128, 128], BF16), "colS": ([128, 128], BF16),
    "halfcol": ([128, 1], F32), "jrow": ([128, 9], F32), "crow": ([128, 544], F32), "blkmask": ([128, 128], F32),
    "tri": ([128, 128], BF16), "ones_bf": ([128, 128], BF16), "thr": ([128, 32], F32), "blkrow": ([128, 96], F32),
    "pidx": ([128, 1], F32), "tokid": ([128, 64], I32), "pg4": ([128, 4], F32),
}


def host_layout(inp, b, half):
    m = {}
    xb = inp["x"][b]
    m["x"] = np.ascontiguousarray(np.concatenate([xb[half * TOWN:(half + 1) * TOWN], xb[(1 - half) * TOWN:(2 - half) * TOWN]], axis=0))
    m["ctx"] = np.ascontiguousarray(inp["ctx"][b])
    cv = np.stack([inp["c"][b], inp["c_ctx"]], axis=-1)
    m["cvT"] = np.ascontiguousarray(cv.reshape(16, 128, 2).transpose(1, 0, 2))
    m["w_ada"] = inp["w_ada"][0]
    m["b_adaT"] = np.ascontiguousarray(inp["b_ada"][0].reshape(96, 128).T)
    m["w_in"] = inp["w_in"][0]
    m["w_f_proj"] = inp["w_f_proj"][0]
    m["w_s_proj"] = inp["w_s_proj"][0]
    m["w_out"] = inp["w_out"][0]
    m["w_glu"] = inp["w_glu"][0]
    m["b_gluT"] = np.ascontiguousarray(inp["b_glu"][0].reshape(4, 128).T)
    m["lnrows"] = np.ascontiguousarray(np.stack([inp["ln1_g"][0], inp["ln1_b"][0], inp["ln2_g"][0], inp["ln2_b"][0]], axis=0))

    def pd(a):
        return np.ascontiguousarray(a.reshape(2, 16, 2, 64).transpose(2, 3, 0, 1).reshape(128, 32))
    m["lam_re"] = pd(inp["lam_re"][0])
    m["lam_im"] = pd(inp["lam_im"][0])
    m["log_dt"] = pd(np.broadcast_to(inp["log_dt"][0][:, :, None], (2, 32, 64)))

    def bpad(a):
        o = np.zeros((2, 64, 2, 16, 2, 16), np.float32)
        a6 = a.reshape(2, 16, 2, 64, 16)
        for g2 in range(2):
            o[g2, :, :, :, g2, :] = a6[:, :, g2].transpose(2, 0, 1, 3)
        return np.ascontiguousarray(o.reshape(128, 32, 32))
    m["b_re"] = bpad(inp["b_re"][0])
    m["b_im"] = bpad(inp["b_im"][0])

    def cpad(a):
        o = np.zeros((2, 64, 2, 16, 2, 16), np.float32)
        a6 = a.reshape(2, 16, 2, 16, 64)
        for g2 in range(2):
            o[g2, :, :, :, g2, :] = a6[:, :, g2].transpose(3, 0, 1, 2)
        return np.ascontiguousarray(o.reshape(128, 32, 32))
    m["c_re"] = cpad(inp["c_re"][0])
    m["c_im"] = cpad(inp["c_im"][0])
    m["d_skipT"] = np.ascontiguousarray(inp["d_skip"][0].reshape(4, 128).T)
    wr = np.concatenate([inp["w_group"][0], inp["w_expert"][0]], axis=1)
    m["w_rt"] = np.ascontiguousarray(wr.reshape(16, 128, 36).transpose(1, 0, 2))
    m["b_rt"] = np.ascontiguousarray(np.concatenate([inp["b_group"][0], inp["b_expert"][0]])[None, :])
    m["w1"] = inp["w1"][0]
    m["w3"] = inp["w3"][0]
    m["w2"] = inp["w2"][0]
    m.update(host_consts(half))
    return m


IN_SPECS = {
    "x": ([TALL, D], F32), "ctx": ([256, D], F32), "cvT": ([128, 16, 2], F32), "w_ada": ([D, 6 * D], F32),
    "b_adaT": ([128, 96], F32), "w_in": ([D, 5632], F32), "w_f_proj": ([1024, D], F32), "w_s_proj": ([512, D], F32),
    "w_out": ([D, D], F32), "w_glu": ([512, 512], F32), "b_gluT": ([128, 4], F32), "lnrows": ([4, D], F32),
    "lam_re": ([128, 32], F32), "lam_im": ([128, 32], F32), "log_dt": ([128, 32], F32),
    "b_re": ([128, 32, 32], F32), "b_im": ([128, 32, 32], F32), "c_re": ([128, 32, 32], F32), "c_im": ([128, 32, 32], F32),
    "d_skipT": ([128, 4], F32), "w_rt": ([128, 16, 36], F32), "b_rt": ([1, 36], F32),
    "w1": ([32, D, 1024], F32), "w3": ([32, D, 1024], F32), "w2": ([32, 1024, D], F32),
}


def build(stop_after=None, dbg=()):
    nc = bass.Bass("TRN2", target_bir_lowering=False)
    F = Flow(nc)
    I = {}
    early = stop_after in ("P0", "A1", "A2", "A3", "A4", "R")
    for k, (shp, dt) in list(IN_SPECS.items()) + list(CONST_SPECS.items()):
        if early and k in ("w1", "w2", "w3"):
            continue
        I[k] = nc.dram_tensor(k, shp, dt, kind="ExternalInput").ap()
    out_ap = nc.dram_tensor("out", [TOWN, D], F32, kind="ExternalOutput").ap()
    b_out = Buf("out")

    def scratch(name, shape, dt):
        kind = "ExternalOutput" if name in dbg else "Internal"
        return nc.dram_tensor(name, shape, dt, kind=kind).ap()

    _n = [0]

    def sb(shape, dt, name=None):
        _n[0] += 1
        return nc.alloc_sbuf_tensor(name or ("t%d" % _n[0]), shape, dt)

    psT = [nc.alloc_psum_tensor("psT%d" % i, [128, 1024], BF16) for i in range(2)]
    bpsT = [Buf("psT%d" % i) for i in range(2)]
    ps = [nc.alloc_psum_tensor("ps%d" % i, [128, 512], F32) for i in range(6)]
    bps = [Buf("ps%d" % i) for i in range(6)]

    def load_const(name, q="sp"):
        shp, dt = CONST_SPECS[name]
        t = sb(shp, dt, "c_" + name)
        b = Buf(name)
        F.dma(q, lambda e: e.dma_start(out=t[:], in_=I[name]), writes=[b])
        return t, b
    ident_bf, b_identbf = load_const("ident_bf")
    ident_f, b_identf = load_const("ident_f")
    halfcol, b_half = load_const("halfcol")
    epscol = sb([128, 1], F32, "epscol")
    b_eps = Buf("eps")
    F.op("dve", lambda e: e.memset(epscol[:], EPS), writes=[b_eps])

    modc = sb([128, 96], F32, "modc")
    modx = sb([128, 32], F32, "modx")
    b_modc, b_modx = Buf("modc"), Buf("modx")
    gates_d = scratch("gates_d", [6, D], F32)
    b_gates = Buf("gates_d")
    with nc.sbuf_tensor("cc", [128, 16, 2], F32) as cc, nc.sbuf_tensor("badaT", [128, 96], F32) as badaT, \
            nc.sbuf_tensor("gcol", [128, 6, 16], F32) as gcol, ExitStack() as es_p0:
        b_cc, b_bada = Buf("cc"), Buf("bada")
        F.dma("sp", lambda e: e.dma_start(out=cc[:], in_=I["cvT"]), writes=[b_cc])
        F.dma("sp", lambda e: e.dma_start(out=badaT[:], in_=I["b_adaT"]), writes=[b_bada])
        F.op("act", lambda e: e.activation(out=cc[:], in_=cc[:], func=AF.Silu), reads=[b_cc], writes=[b_cc])
        rowbuf = es_p0.enter_context(nc.sbuf_tensor("p0row", [2, 6 * D], F32))
        b_row = Buf("p0row")
        NWB = 6
        wts = [es_p0.enter_context(nc.sbuf_tensor("p0w%d" % i, [128, 2048], F32)) for i in range(NWB)]
        bwts = [Buf("p0w%d" % i) for i in range(NWB)]
        it = 0
        for cg in range(6):
            for k in range(16):
                wt, bw = wts[it % NWB], bwts[it % NWB]
                F.dma("sp" if it % 2 else "act", lambda e: e.dma_start(out=wt[:], in_=I["w_ada"][k * 128:(k + 1) * 128, cg * 2048:(cg + 1) * 2048]), writes=[bw])
                it += 1
                for nt in range(4):
                    F.op("pe", lambda e: e.matmul(ps[nt][0:2, :], lhsT=cc[:, k, :], rhs=wt[:, nt * 512:(nt + 1) * 512], start=(k == 0), stop=(k == 15)),
                         reads=[bw, b_cc], writes=[bps[nt]], signal=(nt == 3 or k == 15))
            for nt in range(4):
                c0 = cg * 2048 + nt * 512
                if nt % 2 == 0:
                    F.op("act", lambda e: e.activation(out=rowbuf[0:2, c0:c0 + 512], in_=ps[nt][0:2, :], func=AF.Copy), reads=[bps[nt]], writes=[b_row])
                else:
                    F.op("dve", lambda e: e.tensor_copy(out=rowbuf[0:2, c0:c0 + 512], in_=ps[nt][0:2, :]), reads=[bps[nt]], writes=[b_row])
        pacc = ps[4]
        pv = pacc[:, 0:192].rearrange("p (m t) -> p m t", t=2)
        for m in range(96):
            F.op("pe", lambda e: e.transpose(out=pv[:, m, :], in_=rowbuf[0:2, m * 128:(m + 1) * 128], identity=ident_f[0:2, 0:2]),
                 reads=[b_row, b_identf], writes=[bps[4]], signal=(m == 95))
        F.op("dve", lambda e: e.tensor_tensor(out=modc[:], in0=pv[:, :, 0], in1=badaT[:], op=ALU.add),
             reads=[bps[4], b_bada], writes=[b_modc])
        F.op("dve", lambda e: e.tensor_tensor(out=modx[:], in0=pv[:, 0:32, 1], in1=badaT[:, 0:32], op=ALU.add),
             reads=[bps[4], b_bada], writes=[b_modx])
        F.op("dve", lambda e: e.tensor_scalar(out=modc[:, 16:32], in0=modc[:, 16:32], scalar1=1.0, scalar2=None, op0=ALU.add),
             reads=[b_modc], writes=[b_modc])
        F.op("dve", lambda e: e.tensor_scalar(out=modc[:, 64:80], in0=modc[:, 64:80], scalar1=1.0, scalar2=None, op0=ALU.add),
             reads=[b_modc], writes=[b_modc])
        F.op("dve", lambda e: e.tensor_scalar(out=modx[:, 16:32], in0=modx[:, 16:32], scalar1=1.0, scalar2=None, op0=ALU.add),
             reads=[b_modx], writes=[b_modx])
        b_gcol = Buf("gcol")
        F.op("dve", lambda e: e.tensor_copy(out=gcol[:].rearrange("p g j -> p (g j)"), in_=modc[:]), reads=[b_modc], writes=[b_gcol])
        F.dma("sp", lambda e: e.dma_start(out=gates_d.rearrange("g (j p) -> p g j", p=128), in_=gcol[:],
                                          allow_slow_non_contiguous=True), reads=[b_gcol], writes=[b_gates])
        if "modc_d" in dbg:
            md = scratch("modc_d", [128, 96], F32)
            F.dma("sp", lambda e: e.dma_start(out=md, in_=modc[:]), reads=[b_modc], writes=[Buf()])
        F.barrier()
    sh1, sc1p, sh2, sc2p = modc[:, 0:16], modc[:, 16:32], modc[:, 48:64], modc[:, 64:80]
    shx, scxp = modx[:, 0:16], modx[:, 16:32]
    if stop_after == "P0":
        F.barrier()
        return nc

    Wmix_d = scratch("Wmix_d", [16, 128, 44, 128], BF16)
    Wo_d = scratch("Wo_d", [128, 16, D], BF16)
    b_Wmix, b_Wo = Buf("Wmix_d"), Buf("Wo_d")
    Wmix_v = Wmix_d.rearrange("oc p k c -> p oc k c")
    wp_jobs = []
    for k in range(16):
        wp_jobs.append((I["w_in"][k * 128:(k + 1) * 128, 1536:3584], Wmix_v[:, :, k, :]))
        wp_jobs.append((I["w_in"][k * 128:(k + 1) * 128, 3584:5632], Wmix_v[:, :, 16 + k, :]))
    for k in range(8):
        wp_jobs.append((I["w_f_proj"][k * 128:(k + 1) * 128, :], Wmix_v[:, :, 32 + k, :]))
    for k in range(4):
        wp_jobs.append((I["w_s_proj"][k * 128:(k + 1) * 128, :], Wmix_v[:, :, 40 + k, :]))
    F1d = scratch("F1d", [8, 128, 64, 128], BF16)
    b_F1d = Buf("F1d")
    es_us = ExitStack()
    usT = es_us.enter_context(nc.sbuf_tensor("usT", [128, 4, 8704], BF16))
    b_usT = Buf("usT")

    def ln_stats(xt, bx, st, mv, rs, nmr, bst):
        for i in range(4):
            F.op("dve", lambda e, i=i: e.bn_stats(out=st[:, 6 * i:6 * i + 6], in_=xt[:, i * 512:(i + 1) * 512]), reads=[bx], writes=[bst])
        F.op("dve", lambda e: e.bn_aggr(out=mv[:], in_=st[:]), reads=[bst], writes=[bst])
        F.op("act", lambda e: e.activation(out=rs[:], in_=mv[:, 1:2], func=AF.Sqrt, bias=epscol[:, 0:1], scale=1.0),
             reads=[bst, b_eps], writes=[bst])
        F.op("dve", lambda e: e.reciprocal(out=rs[:], in_=rs[:]), reads=[bst], writes=[bst])
        F.op("dve", lambda e: e.scalar_tensor_tensor(out=nmr[:], in0=mv[:, 0:1], scalar=-1.0, in1=rs[:], op0=ALU.mult, op1=ALU.mult),
             reads=[bst], writes=[bst])

    with nc.sbuf_tensor("Win", [128, 16, 1536], BF16) as Win, nc.sbuf_tensor("s_rowdft", [128, 128], BF16) as rowdft, \
            nc.sbuf_tensor("xt0", [128, D], F32) as xt0, nc.sbuf_tensor("xt1", [128, D], F32) as xt1, \
            nc.sbuf_tensor("xn0", [128, D], BF16) as xn0, nc.sbuf_tensor("xn1", [128, D], BF16) as xn1, \
            nc.sbuf_tensor("hT0", [128, 16, 128], BF16) as hT0, nc.sbuf_tensor("hT1", [128, 16, 128], BF16) as hT1, \
            nc.sbuf_tensor("uf0", [128, 1024], BF16) as uf0, nc.sbuf_tensor("uf1", [128, 1024], BF16) as uf1, \
            nc.sbuf_tensor("f10", [128, 8, 128], BF16) as f10, nc.sbuf_tensor("f11", [128, 8, 128], BF16) as f11, \
            nc.sbuf_tensor("st0", [128, 24], F32) as st0, nc.sbuf_tensor("st1", [128, 24], F32) as st1, \
            nc.sbuf_tensor("sm0", [128, 4], F32) as sm0, nc.sbuf_tensor("sm1", [128, 4], F32) as sm1:
        b_Win = Buf("Win")
        b_rd = Buf("rowdft")
        es_wp = ExitStack()
        F.dma("sp", lambda e: e.dma_start(out=rowdft[:], in_=I["rowdft"]), writes=[b_rd])
        for kg in range(4):
            F.dma("pool", lambda e, kg=kg: e.dma_start(
                out=Win[:, kg * 4:(kg + 1) * 4, :],
                in_=I["w_in"][kg * 512:(kg + 1) * 512, 0:1536].rearrange("(k p) n -> p k n", p=128)), writes=[b_Win])
        xts, xns, hTs, ufs, f1s, sts, sms = [xt0, xt1], [xn0, xn1], [hT0, hT1], [uf0, uf1], [f10, f11], [st0, st1], [sm0, sm1]
        bxt, bxn, bhT, buf_, bf1, bst = ([Buf("xt%d" % i) for i in range(2)], [Buf("xn%d" % i) for i in range(2)],
                                         [Buf("hT%d" % i) for i in range(2)], [Buf("uf%d" % i) for i in range(2)],
                                         [Buf("f1%d" % i) for i in range(2)], [Buf("st%d" % i) for i in range(2)])
        xcol = I["x"].rearrange("(r c) d -> c r d", c=64)
        ntile = 64 + 2

        def S1(ti):
            p = ti % 2
            xt, xn, st, sm = xts[p], xns[p], sts[p], sms[p]
            src = xcol[ti] if ti < 64 else I["ctx"][(ti - 64) * 128:(ti - 63) * 128, :]
            F.dma("sp", lambda e: e.dma_start(out=xt[:], in_=src), writes=[bxt[p]])
            mv, rs, nmr = sm[:, 0:2], sm[:, 2:3], sm[:, 3:4]
            ln_stats(xt, bxt[p], st, mv, rs, nmr, bst[p])
            F.op("act", lambda e: e.activation(out=xn[:], in_=xt[:], func=AF.Identity, bias=nmr, scale=rs),
                 reads=[bxt[p], bst[p]], writes=[bxn[p]])

        def S2pe(ti):
            p = ti % 2
            for hb in range(2):
                for jj in range(8):
                    j = hb * 8 + jj
                    F.op("pe", lambda e: e.transpose(out=psT[hb][:, jj * 128:(jj + 1) * 128], in_=xns[p][:, j * 128:(j + 1) * 128], identity=ident_bf[:]),
                         reads=[bxn[p], b_identbf], writes=[bpsT[hb]], signal=(jj == 7))

        def S2ev(ti):
            p = ti % 2
            hT = hTs[p]
            scp, shf, bmod = (sc1p, sh1, b_modc) if ti < 64 else (scxp, shx, b_modx)
            for hb in range(2):
                for jj in range(8):
                    j = hb * 8 + jj
                    if jj % 2 == 0:
                        F.op("act", lambda e: e.activation(out=hT[:, j, :], in_=psT[hb][:, jj * 128:(jj + 1) * 128], func=AF.Identity,
                                                           bias=shf[:, j:j + 1], scale=scp[:, j:j + 1]), reads=[bpsT[hb], bmod], writes=[bhT[p]])
                    else:
                        F.op("dve", lambda e: e.tensor_scalar(out=hT[:, j, :], in0=psT[hb][:, jj * 128:(jj + 1) * 128], scalar1=scp[:, j:j + 1],
                                                              scalar2=shf[:, j:j + 1], op0=ALU.mult, op1=ALU.add), reads=[bpsT[hb], bmod], writes=[bhT[p]])

        def S3pe(ti):
            p = ti % 2
            hT = hTs[p]
            for m in range(4):
                for k in range(16):
                    F.op("pe", lambda e: e.matmul(ps[2][:, m * 128:(m + 1) * 128], lhsT=Win[:, k, 1024 + m * 128:1024 + (m + 1) * 128], rhs=hT[:, k, :],
                                                  start=(k == 0), stop=(k == 15)), reads=[b_Win, bhT[p]], writes=[bps[2]], signal=(k == 15 and m == 3))
            if ti < 64:
                for n in range(2):
                    for k in range(16):
                        F.op("pe", lambda e: e.matmul(ps[n][:], lhsT=hT[:, k, :], rhs=Win[:, k, n * 512:(n + 1) * 512], start=(k == 0), stop=(k == 15)),
                             reads=[b_Win, bhT[p]], writes=[bps[n]], signal=(k == 15))

        def S3ev(ti):
            p = ti % 2
            uf = ufs[p]
            pc = ps[2][:].rearrange("p (m t) -> p m t", m=4)
            if ti < 64:
                c = ti
                F.op("act", lambda e: e.activation(out=usT[:, :, c:4096:64], in_=pc[:, :, 0:64], func=AF.Copy), reads=[bps[2]], writes=[b_usT])
                F.op("dve", lambda e: e.tensor_copy(out=usT[:, :, 4352 + c:8448:64], in_=pc[:, :, 64:128]), reads=[bps[2]], writes=[b_usT])
                F.op("act", lambda e: e.activation(out=uf[:, 0:512], in_=ps[0][:], func=AF.Copy), reads=[bps[0]], writes=[buf_[p]])
                F.op("dve", lambda e: e.tensor_copy(out=uf[:, 512:1024], in_=ps[1][:]), reads=[bps[1]], writes=[buf_[p]])
            else:
                t0 = (ti - 64) * 128
                F.op("act", lambda e: e.activation(out=usT[:, :, 4096 + t0:4096 + t0 + 128], in_=pc, func=AF.Copy), reads=[bps[2]], writes=[b_usT])
                F.op("dve", lambda e: e.tensor_copy(out=usT[:, :, 8448 + t0:8448 + t0 + 128], in_=pc), reads=[bps[2]], writes=[b_usT])

        def S4pe(ti):
            p = ti % 2
            for m in range(8):
                F.op("pe", lambda e: e.matmul(ps[3 + m // 4][:, (m % 4) * 128:(m % 4 + 1) * 128], lhsT=ufs[p][:, m * 128:(m + 1) * 128], rhs=rowdft[:],
                                              start=True, stop=True), reads=[buf_[p], b_rd], writes=[bps[3 + m // 4]], signal=(m % 4 == 3))

        def S4ev(ti):
            p = ti % 2
            f1 = f1s[p]
            F.op("act", lambda e: e.activation(out=f1[:, 0:4, :], in_=ps[3][:].rearrange("p (m t) -> p m t", m=4), func=AF.Copy), reads=[bps[3]], writes=[bf1[p]])
            F.op("dve", lambda e: e.tensor_copy(out=f1[:, 4:8, :], in_=ps[4][:].rearrange("p (m t) -> p m t", m=4)), reads=[bps[4]], writes=[bf1[p]])
            F.dma("sp", lambda e: e.dma_start(out=F1d.rearrange("m ch c k -> ch m c k")[:, :, ti, :], in_=f1[:]), reads=[bf1[p]], writes=[b_F1d])

        wpf = [es_wp.enter_context(nc.sbuf_tensor("wpA%d" % i, [128, 2048], F32)) for i in range(2)]
        wpb = [es_wp.enter_context(nc.sbuf_tensor("wpB%d" % i, [128, 2048], BF16)) for i in range(2)]
        bwpf = [Buf("wpA%d" % i) for i in range(2)]
        bwpb = [Buf("wpB%d" % i) for i in range(2)]

        def wprep_piece(ji):
            src, dst = wp_jobs[ji]
            p = ji % 2
            F.dma("pool", lambda e: e.dma_start(out=wpf[p][:], in_=src), writes=[bwpf[p]])
            F.op("pool", lambda e: e.tensor_copy(out=wpb[p][:], in_=wpf[p][:]), reads=[bwpf[p]], writes=[bwpb[p]])
            F.dma("pool", lambda e: e.dma_start(out=dst, in_=wpb[p][:].rearrange("p (oc c) -> p oc c", c=128)), reads=[bwpb[p]], writes=[b_Wmix])

        ok = lambda t: 0 <= t < ntile
        S1(0)
        S1(1)
        S2pe(0)
        S2ev(0)
        for n in range(0, ntile + 1):
            if ok(n + 2):
                S1(n + 2)
            if ok(n + 1):
                S2pe(n + 1)
            if ok(n):
                S3pe(n)
            if ok(n - 1) and n - 1 < 64:
                S4pe(n - 1)
            if ok(n + 1):
                S2ev(n + 1)
            if ok(n):
                S3ev(n)
            if ok(n - 1) and n - 1 < 64:
                S4ev(n - 1)
            if n < len(wp_jobs):
                wprep_piece(n)
        if "usT_d" in dbg:
            ud = scratch("usT_d", [128, 4, 8704], BF16)
            F.dma("sp", lambda e: e.dma_start(out=ud, in_=usT[:]), reads=[b_usT], writes=[Buf()])
        F.barrier()
        es_wp.close()
    if stop_after == "A1":
        F.barrier()
        return nc

    fT_d = scratch("fT_d", [8, 128, TOWN], BF16)
    b_fTd = Buf("fT_d")
    with nc.sbuf_tensor("s_chA", [128, 2, 512], BF16) as chA, nc.sbuf_tensor("s_chB", [128, 2, 512], BF16) as chB, \
            nc.sbuf_tensor("s_colC", [128, 128], BF16) as colC, nc.sbuf_tensor("s_colS", [128, 128], BF16) as colS, \
            nc.sbuf_tensor("F1s0", [128, 2, 64, 128], BF16) as F1s0, nc.sbuf_tensor("F1s1", [128, 2, 64, 128], BF16) as F1s1, \
            nc.sbuf_tensor("G0", [128, 512], BF16) as G0, nc.sbuf_tensor("G1", [128, 512], BF16) as G1, \
            nc.sbuf_tensor("fo0", [128, 2, TOWN], BF16) as fo0, nc.sbuf_tensor("fo1", [128, 2, TOWN], BF16) as fo1:
        b_tabs = Buf("dfttabs")
        for t, nm in ((chA, "chA"), (chB, "chB"), (colC, "colC"), (colS, "colS")):
            F.dma("sp", lambda e, t=t, nm=nm: e.dma_start(out=t[:], in_=I[nm]), writes=[b_tabs])
        F1ss, Gs, fos = [F1s0, F1s1], [G0, G1], [fo0, fo1]
        bF1s, bG, bfo = [Buf("F1s0"), Buf("F1s1")], [Buf("G0"), Buf("G1")], [Buf("fo0"), Buf("fo1")]
        for gi in range(4):
            F1s, fo = F1ss[gi % 2], fos[gi % 2]
            for kc in range(2):
                F.dma("sp" if kc == 0 else "act", lambda e, F1s=F1s, kc=kc, gi=gi: e.dma_start(out=F1s[:, kc, :, :], in_=F1d[2 * gi + kc]),
                      reads=[b_F1d], writes=[bF1s[gi % 2]])
            def step2(kr):
                pa, bpa = ps[kr % 2], bps[kr % 2]
                n = 0
                for kc in range(2):
                    for (off, tab) in ((0, chA), (64, chB)):
                        F.op("pe", lambda e: e.matmul(pa[0:64, :], lhsT=F1s[:, kc, :, off + kr], rhs=tab[:, kc, :], start=(n == 0), stop=(n == 3)),
                             reads=[bF1s[gi % 2], b_tabs], writes=[bpa], signal=(n == 3))
                        n += 1
                G = Gs[kr % 2]
                if kr % 2 == 0:
                    F.op("act", lambda e: e.activation(out=G[0:64, :], in_=pa[0:64, :], func=AF.Copy), reads=[bpa], writes=[bG[kr % 2]])
                else:
                    F.op("dve", lambda e: e.tensor_copy(out=G[0:64, :], in_=pa[0:64, :]), reads=[bpa], writes=[bG[kr % 2]])

            def step3(kr):
                G = Gs[kr % 2]
                g4 = kr // 4
                pb, bpb = ps[2 + g4 % 2], bps[2 + g4 % 2]
                for q in range(2):
                    c0 = (q * 4 + kr % 4) * 64
                    F.op("pe", lambda e: e.matmul(pb[:, c0:c0 + 64], lhsT=G[0:64, q * 128:(q + 1) * 128], rhs=colC[0:64, 0:64],
                                                  start=True, stop=False), reads=[bG[kr % 2], b_tabs], writes=[bpb], signal=False)
                    F.op("pe", lambda e: e.matmul(pb[:, c0:c0 + 64], lhsT=G[0:64, 256 + q * 128:256 + (q + 1) * 128], rhs=colS[0:64, 0:64],
                                                  start=False, stop=True), reads=[bG[kr % 2], b_tabs], writes=[bpb], signal=(q == 1))
                if kr % 4 == 3:
                    kr0 = kr - 3
                    if g4 % 2 == 0:
                        F.op("dve", lambda e: e.tensor_copy(out=fo[:, :, kr0 * 64:(kr0 + 4) * 64], in_=pb[:].rearrange("p (q t) -> p q t", q=2)),
                             reads=[bpb], writes=[bfo[gi % 2]])
                    else:
                        F.op("act", lambda e: e.activation(out=fo[:, :, kr0 * 64:(kr0 + 4) * 64], in_=pb[:].rearrange("p (q t) -> p q t", q=2), func=AF.Copy),
                             reads=[bpb], writes=[bfo[gi % 2]])
            step2(0)
            for kr in range(64):
                if kr + 1 < 64:
                    step2(kr + 1)
                step3(kr)
            F.dma("sp", lambda e, fo=fo, gi=gi: e.dma_start(out=fT_d[2 * gi:2 * gi + 2].rearrange("q p t -> p q t"), in_=fo[:]),
                  reads=[bfo[gi % 2]], writes=[b_fTd])
        F.barrier()
    if stop_after == "A2":
        F.barrier()
        return nc

    sT_d = scratch("sT_d", [4, 128, TOWN], BF16)
    b_sTd = Buf("sT_d")
    gT_d = scratch("gT_d", [4, 128, TOWN], BF16)
    b_gTd = Buf("gT_d")
    yT_dbg = scratch("yT_d", [4, 8, 128, 512], F32) if "yT_d" in dbg else None
    PIS = 3.141592
    with ExitStack() as es_a3:
        par = es_a3.enter_context(nc.sbuf_tensor("s5par", [128, 32 * 12], F32))
        pw = es_a3.enter_context(nc.sbuf_tensor("s5pw", [128, 32 * 9 * 8], F32))
        pwi = es_a3.enter_context(nc.sbuf_tensor("s5pi", [128, 32 * 9], I32))
        Bt = es_a3.enter_context(nc.sbuf_tensor("s5b", [128, 2, 1024], F32))
        Ct = es_a3.enter_context(nc.sbuf_tensor("s5c", [128, 2, 1024], F32))
        Btmp = es_a3.enter_context(nc.sbuf_tensor("s5bt", [128, 2, 1024], F32))
        jrow = es_a3.enter_context(nc.sbuf_tensor("s5jrow", [128, 9], F32))
        crow = es_a3.enter_context(nc.sbuf_tensor("s5crow", [128, 544], F32))
        blkmask = es_a3.enter_context(nc.sbuf_tensor("s5mask", [128, 128], F32))
        dsk = es_a3.enter_context(nc.sbuf_tensor("s5dsk", [128, 4], F32))
        Wt = es_a3.enter_context(nc.sbuf_tensor("s5W", [128, 2, 1056], F32))
        QWt = es_a3.enter_context(nc.sbuf_tensor("s5QW", [128, 2 * 8 * 2 * 128], BF16))
        KWt = es_a3.enter_context(nc.sbuf_tensor("s5KW", [128, 2 * 8 * 128], BF16))
        NWt = es_a3.enter_context(nc.sbuf_tensor("s5NW", [128, 2 * 2 * 1024], BF16))
        tab = es_a3.enter_context(nc.sbuf_tensor("s5tab", [128, 2, 544], F32))
        tf = es_a3.enter_context(nc.sbuf_tensor("s5tf", [128, 544], F32))
        ti_ = es_a3.enter_context(nc.sbuf_tensor("s5ti", [128, 544], I32))
        dm = es_a3.enter_context(nc.sbuf_tensor("s5d", [128, 6, 544], F32))
        cr_ = es_a3.enter_context(nc.sbuf_tensor("s5cr", [128, 16], F32))
        St = es_a3.enter_context(nc.sbuf_tensor("s5S", [128, 8 * 2 * 520], BF16))
        yt = es_a3.enter_context(nc.sbuf_tensor("s5y", [128, 2, 512], F32))
        gt = es_a3.enter_context(nc.sbuf_tensor("s5g", [128, TOWN], BF16))
        sgt = es_a3.enter_context(nc.sbuf_tensor("s5sg", [128, 512], F32))
        b_par, b_W, b_QW, b_KW, b_NW = Buf("par"), Buf("W"), Buf("QW"), Buf("KW"), Buf("NW")
        Zt = Wt
        dmflat = dm[:].rearrange("p a c -> p (a c)")
        b_Z = b_W
        b_tmp = b_W
        b_tab, b_S, b_y, b_g, b_gl = Buf("tab"), Buf("S"), [Buf("y0"), Buf("y1")], Buf("g"), Buf("gl")
        dq = ["sp", "act"]
        lre, lim, ldt = par[:, 0:32], par[:, 32:64], par[:, 64:96]
        F.dma("sp", lambda e: e.dma_start(out=lre, in_=I["lam_re"]), writes=[b_par])
        F.dma("sp", lambda e: e.dma_start(out=lim, in_=I["lam_im"]), writes=[b_par])
        F.dma("sp", lambda e: e.dma_start(out=ldt, in_=I["log_dt"]), writes=[b_par])
        F.dma("sp", lambda e: e.dma_start(out=Bt[:, 0, :], in_=I["b_re"].rearrange("p a b -> p (a b)")), writes=[b_par])
        F.dma("sp", lambda e: e.dma_start(out=Bt[:, 1, :], in_=I["b_im"].rearrange("p a b -> p (a b)")), writes=[b_par])
        F.dma("act", lambda e: e.dma_start(out=Ct[:, 0, :], in_=I["c_re"].rearrange("p a b -> p (a b)")), writes=[b_par])
        F.dma("act", lambda e: e.dma_start(out=Ct[:, 1, :], in_=I["c_im"].rearrange("p a b -> p (a b)")), writes=[b_par])
        F.dma("sp", lambda e: e.dma_start(out=jrow[:], in_=I["jrow"]), writes=[b_par])
        F.dma("sp", lambda e: e.dma_start(out=crow[:], in_=I["crow"]), writes=[b_par])
        F.dma("sp", lambda e: e.dma_start(out=blkmask[:], in_=I["blkmask"]), writes=[b_par])
        F.dma("sp", lambda e: e.dma_start(out=dsk[:], in_=I["d_skipT"]), writes=[b_par])

        def V(fn, reads, writes):
            F.op("dve", fn, reads=reads, writes=writes)

        def A(fn, reads, writes):
            F.op("act", fn, reads=reads, writes=writes)
        P_ = [b_par]
        dtc, aa, th = par[:, 96:128], par[:, 128:160], par[:, 160:192]
        A(lambda e: e.activation(out=dtc, in_=ldt, func=AF.Exp), P_, P_)
        V(lambda e: e.tensor_tensor(out=aa, in0=lre, in1=dtc, op=ALU.mult), P_, P_)
        V(lambda e: e.tensor_tensor(out=th, in0=lim, in1=dtc, op=ALU.mult), P_, P_)
        def p3(i):
            return pw[:, i * 288:(i + 1) * 288].rearrange("p (a j) -> p a j", j=9)
        MAG, ANG, RED, SIN, COS, PR, PI_, TMP = [p3(i) for i in range(8)]
        pwi3 = pwi[:].rearrange("p (a j) -> p a j", j=9)
        jb = jrow[:].unsqueeze(1).to_broadcast([128, 32, 9])
        V(lambda e: e.tensor_tensor(out=MAG, in0=aa.unsqueeze(2).to_broadcast([128, 32, 9]), in1=jb, op=ALU.mult), P_, P_)
        A(lambda e: e.activation(out=MAG, in_=MAG, func=AF.Exp), P_, P_)
        V(lambda e: e.tensor_tensor(out=ANG, in0=th.unsqueeze(2).to_broadcast([128, 32, 9]), in1=jb, op=ALU.mult), P_, P_)

        def range_reduce(dst, src, tmpf, tmpi, shift, R, Wr):
            V(lambda e: e.tensor_scalar(out=tmpf, in0=src, scalar1=shift, scalar2=1.0 / (2 * PI), op0=ALU.add, op1=ALU.mult), R, Wr)
            V(lambda e: e.tensor_copy(out=tmpi, in_=tmpf), Wr, Wr)
            V(lambda e: e.tensor_copy(out=tmpf, in_=tmpi), Wr, Wr)
            V(lambda e: e.scalar_tensor_tensor(out=tmpf, in0=tmpf, scalar=-2 * PI, in1=src, op0=ALU.mult, op1=ALU.add), R + Wr, Wr)
            V(lambda e: e.tensor_scalar(out=dst, in0=tmpf, scalar1=shift, scalar2=-PIS, op0=ALU.add, op1=ALU.max), Wr, Wr)
            V(lambda e: e.tensor_scalar(out=dst, in0=dst, scalar1=PIS, scalar2=None, op0=ALU.min), Wr, Wr)
        range_reduce(RED, ANG, TMP, pwi3, 0.0, P_, P_)
        A(lambda e: e.activation(out=SIN, in_=RED, func=AF.Sin), P_, P_)
        range_reduce(COS, ANG, TMP, pwi3, PI / 2, P_, P_)
        A(lambda e: e.activation(out=COS, in_=COS, func=AF.Sin), P_, P_)
        V(lambda e: e.tensor_tensor(out=PR, in0=MAG, in1=COS, op=ALU.mult), P_, P_)
        V(lambda e: e.tensor_tensor(out=PI_, in0=MAG, in1=SIN, op=ALU.mult), P_, P_)
        nr, ni, den, cr, ci, t1c, t2c = [par[:, 192 + 32 * i:224 + 32 * i] for i in range(6)] + [par[:, 352:384]]
        V(lambda e: e.tensor_scalar(out=nr, in0=PR[:, :, 1], scalar1=-1.0, scalar2=None, op0=ALU.add), P_, P_)
        V(lambda e: e.tensor_copy(out=ni, in_=PI_[:, :, 1]), P_, P_)
        V(lambda e: e.tensor_tensor(out=den, in0=lre, in1=lre, op=ALU.mult), P_, P_)
        V(lambda e: e.tensor_tensor(out=t1c, in0=lim, in1=lim, op=ALU.mult), P_, P_)
        V(lambda e: e.tensor_tensor(out=den, in0=den, in1=t1c, op=ALU.add), P_, P_)
        V(lambda e: e.reciprocal(out=den, in_=den), P_, P_)
        V(lambda e: e.tensor_tensor(out=cr, in0=nr, in1=lre, op=ALU.mult), P_, P_)
        V(lambda e: e.tensor_tensor(out=t1c, in0=ni, in1=lim, op=ALU.mult), P_, P_)
        V(lambda e: e.tensor_tensor(out=cr, in0=cr, in1=t1c, op=ALU.add), P_, P_)
        V(lambda e: e.tensor_tensor(out=cr, in0=cr, in1=den, op=ALU.mult), P_, P_)
        V(lambda e: e.tensor_tensor(out=ci, in0=ni, in1=lre, op=ALU.mult), P_, P_)
        V(lambda e: e.tensor_tensor(out=t1c, in0=nr, in1=lim, op=ALU.mult), P_, P_)
        V(lambda e: e.tensor_tensor(out=ci, in0=ci, in1=t1c, op=ALU.subtract), P_, P_)
        V(lambda e: e.tensor_tensor(out=ci, in0=ci, in1=den, op=ALU.mult), P_, P_)
        B3 = lambda i: Bt[:, i, :].rearrange("p (a h) -> p a h", h=32)
        T3 = lambda i: Btmp[:, i, :].rearrange("p (a h) -> p a h", h=32)
        crb = cr.unsqueeze(2).to_broadcast([128, 32, 32])
        cib = ci.unsqueeze(2).to_broadcast([128, 32, 32])
        V(lambda e: e.tensor_tensor(out=T3(0), in0=B3(0), in1=crb, op=ALU.mult), P_, P_)
        V(lambda e: e.tensor_tensor(out=T3(1), in0=B3(1), in1=cib, op=ALU.mult), P_, P_)
        V(lambda e: e.tensor_tensor(out=T3(0), in0=T3(0), in1=T3(1), op=ALU.subtract), P_, P_)
        V(lambda e: e.tensor_tensor(out=T3(1), in0=B3(1), in1=crb, op=ALU.mult), P_, P_)
        V(lambda e: e.tensor_tensor(out=B3(1), in0=B3(0), in1=cib, op=ALU.mult), P_, P_)
        V(lambda e: e.tensor_tensor(out=T3(1), in0=T3(1), in1=B3(1), op=ALU.add), P_, P_)
        BB = Btmp
        CN = Bt[:, 0, :]
        V(lambda e: e.tensor_scalar(out=CN, in0=Ct[:, 1, :], scalar1=-1.0, scalar2=None, op0=ALU.mult), P_, P_)

        def bview(t2d, pd0):
            return t2d[:, pd0 * 32:(pd0 + 4) * 32].rearrange("p (q h) -> p q h", q=4).unsqueeze(1).to_broadcast([128, 8, 4, 32])

        def pview(T, pd0, j0):
            return T[:, pd0:pd0 + 4, j0:j0 + 8].rearrange("p q j -> p j q").unsqueeze(3).to_broadcast([128, 8, 4, 32])

        for blk in range(4):
            for d in range(2):
                pd0 = d * 16 + blk * 4
                W4 = lambda i: Wt[:, i, 0:1024].rearrange("p (j q h) -> p j q h", j=8, q=4)
                X4 = lambda i: dmflat[:, i * 1024:(i + 1) * 1024].rearrange("p (j q h) -> p j q h", j=8, q=4)
                RW = [b_par, b_W]
                V(lambda e: e.tensor_tensor(out=W4(0), in0=bview(BB[:, 0, :], pd0), in1=pview(PR, pd0, 0), op=ALU.mult), [b_par], [b_W])
                V(lambda e: e.tensor_tensor(out=X4(0), in0=bview(BB[:, 1, :], pd0), in1=pview(PI_, pd0, 0), op=ALU.mult), [b_par], [b_W])
                V(lambda e: e.tensor_tensor(out=W4(0), in0=W4(0), in1=X4(0), op=ALU.subtract), RW, [b_W])
                V(lambda e: e.tensor_tensor(out=W4(1), in0=bview(BB[:, 1, :], pd0), in1=pview(PR, pd0, 0), op=ALU.mult), [b_par], [b_W])
                V(lambda e: e.tensor_tensor(out=X4(1), in0=bview(BB[:, 0, :], pd0), in1=pview(PI_, pd0, 0), op=ALU.mult), [b_par], [b_W])
                V(lambda e: e.tensor_tensor(out=W4(1), in0=W4(1), in1=X4(1), op=ALU.add), RW, [b_W])
                for reim in range(2):
                    for jh in range(2):
                        pst = ps[(reim * 2 + jh) % 4]
                        bpst = bps[(reim * 2 + jh) % 4]
                        for jj in range(4):
                            j = jh * 4 + jj
                            F.op("pe", lambda e: e.transpose(out=pst[:, jj * 128:(jj + 1) * 128], in_=Wt[:, reim, j * 128:(j + 1) * 128], identity=ident_f[:]),
                                 reads=[b_W, b_identf], writes=[bpst], signal=(jj == 3))
                        dst = QWt[:].rearrange("p (d j r c) -> p d j r c", d=2, j=8, r=2)[:, d, jh * 4:(jh + 1) * 4, reim, :]
                        A(lambda e: e.activation(out=dst, in_=pst[:].rearrange("p (j c) -> p j c", j=4), func=AF.Copy), [bpst], [b_QW])
                for jh in range(2):
                    pst = ps[4 + jh]
                    bpst = bps[4 + jh]
                    for jj in range(4):
                        j = jh * 4 + jj
                        F.op("pe", lambda e: e.matmul(pst[:, jj * 128:(jj + 1) * 128], lhsT=Wt[:, 0, j * 128:(j + 1) * 128],
                                                      rhs=Ct[:, 0, pd0 * 32:(pd0 + 4) * 32], start=True, stop=False),
                             reads=[b_W, b_par], writes=[bpst], signal=False)
                        F.op("pe", lambda e: e.matmul(pst[:, jj * 128:(jj + 1) * 128], lhsT=Wt[:, 1, j * 128:(j + 1) * 128],
                                                      rhs=CN[:, pd0 * 32:(pd0 + 4) * 32], start=False, stop=True),
                             reads=[b_W, b_par], writes=[bpst], signal=(jj == 3))
                    dst = KWt[:].rearrange("p (d j c) -> p d j c", d=2, j=8)[:, d, jh * 4:(jh + 1) * 4, :]
                    V(lambda e: e.tensor_tensor(out=dst, in0=pst[:].rearrange("p (j c) -> p j c", j=4),
                                                in1=blkmask[:].unsqueeze(1).to_broadcast([128, 4, 128]), op=ALU.mult), [bpst, b_par], [b_KW])
                if d == 0:
                    k00 = KWt[:, 0:128]
                    V(lambda e: e.scalar_tensor_tensor(out=k00, in0=ident_f[:], scalar=dsk[:, blk:blk + 1], in1=k00, op0=ALU.mult, op1=ALU.add),
                      [b_identf, b_par, b_KW], [b_KW])
                N4 = lambda r: NWt[:].rearrange("p (d r x) -> p d r x", d=2, r=2)[:, d, r, :].rearrange("p (j q h) -> p j q h", j=8, q=4)
                V(lambda e: e.tensor_tensor(out=X4(0), in0=bview(Ct[:, 0, :], pd0), in1=pview(PR, pd0, 1), op=ALU.mult), [b_par, b_W], [b_W])
                V(lambda e: e.tensor_tensor(out=X4(1), in0=bview(CN, pd0), in1=pview(PI_, pd0, 1), op=ALU.mult), [b_par, b_W], [b_W])
                V(lambda e: e.tensor_tensor(out=N4(0), in0=X4(0), in1=X4(1), op=ALU.add), [b_W], [b_NW])
                V(lambda e: e.tensor_tensor(out=X4(0), in0=bview(Ct[:, 0, :], pd0), in1=pview(PI_, pd0, 1), op=ALU.mult), [b_par, b_W], [b_W])
                V(lambda e: e.tensor_tensor(out=X4(1), in0=bview(CN, pd0), in1=pview(PR, pd0, 1), op=ALU.mult), [b_par, b_W], [b_W])
                V(lambda e: e.tensor_tensor(out=N4(1), in0=X4(1), in1=X4(0), op=ALU.subtract), [b_W], [b_NW])
            QW5 = QWt[:].rearrange("p (d j r c) -> p d j r c", d=2, j=8, r=2)
            KW4 = KWt[:].rearrange("p (d j c) -> p d j c", d=2, j=8)
            NW6 = NWt[:].rearrange("p (d r j q h) -> p d r j q h", d=2, r=2, j=8, q=4)
            S5v = St[:].rearrange("p (a r c) -> p a r c", a=8, r=2)
            for d in range(2):
                for q in range(4):
                    pdl = d * 4 + q
                    pd = d * 16 + blk * 4 + q
                    rows = slice(32 * q, 32 * q + 32)
                    if d == 0:
                        segs = [(4096, 512, 0, False), (8192, 32, 512, False), (0, 512, 544, False)]
                    else:
                        segs = [(4352, 512, 32, True), (8448, 32, 0, True), (0, 512, 544, True)]
                    for reim in range(2):
                        for gi_, (base, L, zo, rev) in enumerate(segs):
                            pz = ps[(reim * 3 + gi_) % 6]
                            bpz = bps[(reim * 3 + gi_) % 6]
                            for s_ in range(8):
                                j = 7 - s_ if d == 0 else s_
                                F.op("pe", lambda e: e.matmul(pz[:, 0:L], lhsT=QW5[rows, d, j, reim, :],
                                                              rhs=usT[rows, blk, base + s_:base + s_ + 8 * (L - 1) + 1:8],
                                                              start=(s_ == 0), stop=(s_ == 7), tile_position=(32 * q, 0)),
                                     reads=[b_QW, b_usT], writes=[bpz], signal=(s_ == 7))
                            zs = Zt[:, reim, zo:zo + L]
                            if rev:
                                zs = zs[:, ::-1]
                            if reim == 0:
                                A(lambda e: e.activation(out=zs, in_=pz[:, 0:L], func=AF.Copy), [bpz], [b_Z])
                            else:
                                V(lambda e: e.tensor_copy(out=zs, in_=pz[:, 0:L]), [bpz], [b_Z])
                    r8, th8, c8, s8 = MAG[:, pd, 8:9], RED[:, pd, 8:9], COS[:, pd, 8:9], SIN[:, pd, 8:9]
                    TB = [b_tab]
                    V(lambda e: e.tensor_scalar(out=dm[:, 0, :], in0=crow[:], scalar1=th8, scalar2=None, op0=ALU.mult), [b_par, b_tmp], [b_tmp])
                    range_reduce(tab[:, 0, :], dm[:, 0, :], tf[:], ti_[:], 0.0, [b_tmp], [b_tab])
                    A(lambda e: e.activation(out=tab[:, 0, :], in_=tab[:, 0, :], func=AF.Sin), TB, TB)
                    range_reduce(tab[:, 1, :], dm[:, 0, :], tf[:], ti_[:], PI / 2, [b_tmp], [b_tab])
                    A(lambda e: e.activation(out=tab[:, 1, :], in_=tab[:, 1, :], func=AF.Sin), TB, TB)
                    sinT, cosT = tab[:, 0, :], tab[:, 1, :]
                    TM = [b_tmp]

                    def demod(zo, L):
                        zr, zi = Zt[:, 0, zo:zo + L], Zt[:, 1, zo:zo + L]
                        V(lambda e: e.tensor_tensor(out=dm[:, 0, 0:L], in0=zr, in1=cosT[:, 0:L], op=ALU.mult), [b_Z, b_tab, b_tmp], TM)
                        V(lambda e: e.tensor_tensor(out=dm[:, 1, 0:L], in0=zi, in1=sinT[:, 0:L], op=ALU.mult), [b_Z, b_tab, b_tmp], TM)
                        V(lambda e: e.tensor_tensor(out=dm[:, 2, 0:L], in0=dm[:, 0, 0:L], in1=dm[:, 1, 0:L], op=ALU.add), TM, TM)
                        V(lambda e: e.tensor_tensor(out=dm[:, 0, 0:L], in0=zi, in1=cosT[:, 0:L], op=ALU.mult), [b_Z, b_tab, b_tmp], TM)
                        V(lambda e: e.tensor_tensor(out=dm[:, 1, 0:L], in0=zr, in1=sinT[:, 0:L], op=ALU.mult), [b_Z, b_tab, b_tmp], TM)
                        V(lambda e: e.tensor_tensor(out=dm[:, 3, 0:L], in0=dm[:, 0, 0:L], in1=dm[:, 1, 0:L], op=ALU.subtract), TM, TM)

                    def scan(L, ire, iim):
                        V(lambda e: e.tensor_tensor_scan(out=dm[:, 4, 0:L], data0=r8.to_broadcast([128, L]), data1=dm[:, 2, 0:L],
                                                         initial=ire, op0=ALU.mult, op1=ALU.add), [b_par, b_tmp], TM)
                        V(lambda e: e.tensor_tensor_scan(out=dm[:, 5, 0:L], data0=r8.to_broadcast([128, L]), data1=dm[:, 3, 0:L],
                                                         initial=iim, op0=ALU.mult, op1=ALU.add), [b_par, b_tmp], TM)
                    demod(0, 544)
                    scan(544, 0.0, 0.0)
                    sre2, sim2 = dm[:, 4, 31:544:512], dm[:, 5, 31:544:512]
                    cs2, sn2 = tab[:, 1, 31:544:512], tab[:, 0, 31:544:512]
                    C_ = lambda i, n=1: cr_[:, i:i + n]
                    V(lambda e: e.tensor_tensor(out=C_(4, 2), in0=sre2, in1=cs2, op=ALU.mult), [b_tmp, b_tab], TM)
                    V(lambda e: e.tensor_tensor(out=C_(6, 2), in0=sim2, in1=sn2, op=ALU.mult), [b_tmp, b_tab], TM)
                    V(lambda e: e.tensor_tensor(out=C_(0, 2), in0=C_(4, 2), in1=C_(6, 2), op=ALU.subtract), TM, TM)
                    V(lambda e: e.tensor_tensor(out=C_(4, 2), in0=sim2, in1=cs2, op=ALU.mult), [b_tmp, b_tab], TM)
                    V(lambda e: e.tensor_tensor(out=C_(6, 2), in0=sre2, in1=sn2, op=ALU.mult), [b_tmp, b_tab], TM)
                    V(lambda e: e.tensor_tensor(out=C_(2, 2), in0=C_(4, 2), in1=C_(6, 2), op=ALU.add), TM, TM)
                    for (o, src) in ((8, 0), (9, 2)):
                        a_, b__ = (C_(src), C_(src + 1)) if d == 0 else (C_(src + 1), C_(src))
                        V(lambda e: e.tensor_tensor(out=C_(4), in0=b__, in1=a_, op=ALU.subtract), TM, TM)
                        V(lambda e: e.scalar_tensor_tensor(out=C_(o), in0=C_(4), scalar=halfcol[:, 0:1], in1=a_, op0=ALU.mult, op1=ALU.add),
                          [b_tmp, b_half], TM)
                    V(lambda e: e.tensor_tensor(out=C_(4), in0=C_(9), in1=s8, op=ALU.mult), [b_tmp, b_par], TM)
                    V(lambda e: e.scalar_tensor_tensor(out=C_(10), in0=C_(8), scalar=c8, in1=C_(4), op0=ALU.mult, op1=ALU.subtract), [b_tmp, b_par], TM)
                    V(lambda e: e.tensor_tensor(out=C_(4), in0=C_(9), in1=c8, op=ALU.mult), [b_tmp, b_par], TM)
                    V(lambda e: e.scalar_tensor_tensor(out=C_(11), in0=C_(8), scalar=s8, in1=C_(4), op0=ALU.mult, op1=ALU.add), [b_tmp, b_par], TM)
                    ccol = 0 if d == 0 else 512
                    V(lambda e: e.tensor_copy(out=S5v[:, pdl, 0, ccol:ccol + 1], in_=C_(8)), TM, [b_S])
                    V(lambda e: e.tensor_copy(out=S5v[:, pdl, 1, ccol:ccol + 1], in_=C_(9)), TM, [b_S])
                    demod(544, 512)
                    scan(512, C_(10), C_(11))
                    if d == 0:
                        ore, oim = S5v[:, pdl, 0, 1:513], S5v[:, pdl, 1, 1:513]
                    else:
                        ore, oim = S5v[:, pdl, 0, 0:512][:, ::-1], S5v[:, pdl, 1, 0:512][:, ::-1]
                    V(lambda e: e.tensor_tensor(out=dm[:, 0, 0:512], in0=dm[:, 4, 0:512], in1=cosT[:, 0:512], op=ALU.mult), [b_tmp, b_tab], TM)
                    V(lambda e: e.tensor_tensor(out=dm[:, 1, 0:512], in0=dm[:, 5, 0:512], in1=sinT[:, 0:512], op=ALU.mult), [b_tmp, b_tab], TM)
                    V(lambda e: e.tensor_tensor(out=ore, in0=dm[:, 0, 0:512], in1=dm[:, 1, 0:512], op=ALU.subtract), TM, [b_S])
                    V(lambda e: e.tensor_tensor(out=dm[:, 0, 0:512], in0=dm[:, 5, 0:512], in1=cosT[:, 0:512], op=ALU.mult), [b_tmp, b_tab], TM)
                    V(lambda e: e.tensor_tensor(out=dm[:, 1, 0:512], in0=dm[:, 4, 0:512], in1=sinT[:, 0:512], op=ALU.mult), [b_tmp, b_tab], TM)
                    V(lambda e: e.tensor_tensor(out=oim, in0=dm[:, 0, 0:512], in1=dm[:, 1, 0:512], op=ALU.add), TM, [b_S])
            for s_ in range(8):
                py = ps[s_ % 2]
                bpy = bps[s_ % 2]
                mms = []
                for j in range(s_ + 1):
                    mms.append((KW4[:, 0, j, :], usT[:, blk, s_ - j:s_ - j + 4089:8], None, [b_KW, b_usT]))
                for j in range(8 - s_):
                    mms.append((KW4[:, 1, j, :], usT[:, blk, s_ + j:s_ + j + 4089:8], None, [b_KW, b_usT]))
                for q in range(4):
                    mms.append((NW6[:, 0, 0, s_, q, :], S5v[:, q, 0, 0:512], q, [b_NW, b_S]))
                    mms.append((NW6[:, 0, 1, s_, q, :], S5v[:, q, 1, 0:512], q, [b_NW, b_S]))
                    mms.append((NW6[:, 1, 0, 7 - s_, q, :], S5v[:, 4 + q, 0, 1:513], q, [b_NW, b_S]))
                    mms.append((NW6[:, 1, 1, 7 - s_, q, :], S5v[:, 4 + q, 1, 1:513], q, [b_NW, b_S]))
                for i_, (lh, rh, q, rd) in enumerate(mms):
                    first, last = i_ == 0, i_ == len(mms) - 1
                    if q is None:
                        F.op("pe", lambda e: e.matmul(py[:], lhsT=lh, rhs=rh, start=first, stop=last), reads=rd, writes=[bpy], signal=last)
                    else:
                        F.op("pe", lambda e: e.matmul(py[32 * q:32 * q + 32, :], lhsT=lh, rhs=rh, start=first, stop=last, tile_position=(0, 32 * q)),
                             reads=rd, writes=[bpy], signal=last)
                y = yt[:, s_ % 2, :]
                by = b_y[s_ % 2]
                A(lambda e: e.activation(out=y, in_=py[:], func=AF.Copy), [bpy], [by])
                if yT_dbg is not None:
                    F.dma("sp", lambda e: e.dma_start(out=yT_dbg[blk, s_], in_=y), reads=[by], writes=[Buf()])
                V(lambda e: e.tensor_tensor(out=sgt[:], in0=y, in1=y, op=ALU.mult), [by], TM)
                V(lambda e: e.tensor_scalar(out=sgt[:], in0=sgt[:], scalar1=0.044715, scalar2=1.0, op0=ALU.mult, op1=ALU.add), TM, TM)
                V(lambda e: e.tensor_tensor(out=sgt[:], in0=sgt[:], in1=y, op=ALU.mult), [by, b_tmp], TM)
                A(lambda e: e.activation(out=sgt[:], in_=sgt[:], func=AF.Sigmoid, scale=1.5957691216057308), TM, TM)
                V(lambda e: e.tensor_tensor(out=gt[:, s_:4096:8], in0=y, in1=sgt[:], op=ALU.mult), [by, b_tmp], [b_g])
            F.dma("sp", lambda e: e.dma_start(out=gT_d[blk], in_=gt[:]), reads=[b_g], writes=[b_gTd])
        F.barrier()
    es_us.close()
    with nc.sbuf_tensor("s5wg", [128, 4, 512], BF16) as wglu, nc.sbuf_tensor("s5bg", [128, 4], F32) as bglu, \
            nc.sbuf_tensor("s5gl", [128, 4, 512], BF16) as gl, nc.sbuf_tensor("s5sg2", [128, 512], F32) as sgt, \
            nc.sbuf_tensor("s5so", [128, 2, 512], BF16) as sot:
        b_par, b_gl, b_tmp = Buf("par2"), Buf("gl"), Buf("tmp2")

        def V(fn, reads, writes):
            F.op("dve", fn, reads=reads, writes=writes)

        def A(fn, reads, writes):
            F.op("act", fn, reads=reads, writes=writes)
        F.dma("sp", lambda e: e.dma_start(out=bglu[:], in_=I["b_gluT"]), writes=[b_par])
        F.dma("pool", lambda e: e.dma_start(out=wglu[:], in_=I["w_glu"].rearrange("(k p) n -> p k n", p=128)), writes=[b_par])
        b_so = [Buf("so0"), Buf("so1")]
        for n in range(8):
            F.dma("sp", lambda e: e.dma_start(out=gl[:], in_=gT_d[:, :, n * 512:(n + 1) * 512].rearrange("k p t -> p k t")), reads=[b_gTd], writes=[b_gl])
            for m in range(4):
                pg = ps[m % 2]
                bpg = bps[m % 2]
                for k in range(4):
                    F.op("pe", lambda e: e.matmul(pg[:], lhsT=wglu[:, k, m * 128:(m + 1) * 128], rhs=gl[:, k, :], start=(k == 0), stop=(k == 3)),
                         reads=[b_par, b_gl], writes=[bpg], signal=(k == 3))
                A(lambda e: e.activation(out=sgt[:], in_=pg[:], func=AF.Sigmoid, bias=bglu[:, m:m + 1], scale=1.0), [bpg, b_par, b_tmp], [b_tmp])
                so = sot[:, m % 2, :]
                V(lambda e: e.tensor_tensor(out=so, in0=gl[:, m, :], in1=sgt[:], op=ALU.mult), [b_gl, b_tmp], [b_so[m % 2]])
                F.dma("act", lambda e: e.dma_start(out=sT_d[m][:, n * 512:(n + 1) * 512], in_=so), reads=[b_so[m % 2]], writes=[b_sTd])
        F.barrier()
    if stop_after == "A3":
        F.barrier()
        return nc

    with ExitStack() as es:
        T = lambda nm, shp, dt: es.enter_context(nc.sbuf_tensor(nm, shp, dt))
        wf = [T("wpf%d" % i, [128, 4096], F32) for i in range(2)]
        wb = [T("wpb%d" % i, [128, 4096], BF16) for i in range(2)]
        bwf = [Buf("wpf%d" % i) for i in range(2)]
        bwb = [Buf("wpb%d" % i) for i in range(2)]
        jobs = []
        for k in range(16):
            jobs.append((I["w_out"][k * 128:(k + 1) * 128, :], 2048, [(Wo_d[:, k, :], 0, 2048)], b_Wo))
        g1w = T("wp_g1", [128, D], F32)
        bg1w = Buf("wp_g1")
        F.dma("sp", lambda e: e.dma_start(out=g1w[:], in_=gates_d[2:3, :].partition_broadcast(128)), reads=[b_gates], writes=[bg1w])
        for ji, (src, n, dsts, bd) in enumerate(jobs):
            p = ji % 2
            F.dma("sp", lambda e: e.dma_start(out=wf[p][:, 0:n], in_=src), writes=[bwf[p]])
            eng = ("act", "dve", "pool")[ji % 3]
            if bd is b_Wo:
                F.op("dve", lambda e: e.tensor_tensor(out=wb[p][:, 0:n], in0=wf[p][:, 0:n], in1=g1w[:, 0:n], op=ALU.mult), reads=[bwf[p], bg1w], writes=[bwb[p]])
            elif eng == "act":
                F.op("act", lambda e: e.activation(out=wb[p][:, 0:n], in_=wf[p][:, 0:n], func=AF.Copy), reads=[bwf[p]], writes=[bwb[p]])
            else:
                F.op(eng, lambda e: e.tensor_copy(out=wb[p][:, 0:n], in_=wf[p][:, 0:n]), reads=[bwf[p]], writes=[bwb[p]])
            for (dst, a_, b__) in dsts:
                if len(dst.shape) == 3:
                    F.dma("act", lambda e: e.dma_start(out=dst, in_=wb[p][:, a_:b__].rearrange("p (oc c) -> p oc c", c=128)), reads=[bwb[p]], writes=[bd])
                else:
                    F.dma("act", lambda e: e.dma_start(out=dst, in_=wb[p][:, a_:b__]), reads=[bwb[p]], writes=[bd])
        F.barrier()

    xmid_d = scratch("xmid_d", [TOWN, D], F32)
    xn2_d = scratch("xn2_d", [TOWN, D], BF16)
    b_xmid, b_xn2 = Buf("xmid_d"), Buf("xn2_d")
    lg_all = sb([128, 32, 36], F32, "lg_all")
    b_lg = Buf("lg_all")
    with ExitStack() as es:
        T = lambda nm, shp, dt: es.enter_context(nc.sbuf_tensor(nm, shp, dt))
        vt = [T("a4v%d" % i, [128, D], F32) for i in range(4)]
        bvt = [Buf("a4v%d" % i) for i in range(4)]
        xn = [T("a4xn%d" % i, [128, D], BF16) for i in range(2)]
        bxn = [Buf("a4xn%d" % i) for i in range(2)]
        xf = T("a4xf", [128, D], F32)
        bxf = Buf("a4xf")
        hTbs = [T("a4hT%d" % i, [128, 16, 512], BF16) for i in range(2)]
        bhTs = [Buf("a4hT%d" % i) for i in range(2)]
        fTb, sTb = T("a4fT", [128, 8, 512], BF16), T("a4sT", [128, 4, 512], BF16)
        bfs = Buf("a4fs")
        wm = [T("a4wm%d" % i, [128, 44, 128], BF16) for i in range(2)]
        bwm = [Buf("a4wm%d" % i) for i in range(2)]
        mT = T("a4mT", [128, 16, 512], BF16)
        bmT = Buf("a4mT")
        wo = [T("a4wo%d" % i, [128, 16, 256], BF16) for i in range(2)]
        xn2b = T("a4xn2b", [128, D], BF16)
        bxn2b = Buf("xn2b")
        bwo = [Buf("a4wo%d" % i) for i in range(2)]
        sA, sB = T("a4sA", [128, 512], F32), T("a4sB", [128, 512], F32)
        bsA, bsB = Buf("sA"), Buf("sB")
        lngb, lnbb = T("a4lg", [128, D], F32), T("a4lb", [128, D], F32)
        bbc = Buf("a4bc")
        h2T = T("a4h2T", [128, 16, 128], F32)
        bh2T = Buf("a4h2T")
        wr = T("a4wr", [128, 16, 36], F32)
        brt = T("a4brt", [128, 36], F32)
        bwr = Buf("a4wr")
        st = [T("a4st%d" % i, [128, 24], F32) for i in range(2)]
        sm = [T("a4sm%d" % i, [128, 4], F32) for i in range(2)]
        bst = [Buf("a4st%d" % i) for i in range(2)]
        F.dma("sp", lambda e: e.dma_start(out=lngb[:], in_=I["lnrows"][0:1, :].partition_broadcast(128)), writes=[bbc])
        F.dma("sp", lambda e: e.dma_start(out=lnbb[:], in_=I["lnrows"][1:2, :].partition_broadcast(128)), writes=[bbc])
        F.dma("sp", lambda e: e.dma_start(out=wr[:], in_=I["w_rt"]), writes=[bwr])
        F.dma("sp", lambda e: e.dma_start(out=brt[:], in_=I["b_rt"].partition_broadcast(128)), writes=[bwr])
        stc = [0]
        xa = [T("a4xa%d" % i, [128, D], F32) for i in range(2)]
        bxa = [Buf("a4xa%d" % i) for i in range(2)]

        def stage_a0(tb):
            t0 = tb * 512
            F.dma("act", lambda e: e.dma_start(out=fTb[:], in_=fT_d[:, :, t0:t0 + 512].rearrange("k p t -> p k t")), reads=[b_fTd], writes=[bfs])
            F.dma("act", lambda e: e.dma_start(out=sTb[:], in_=sT_d[:, :, t0:t0 + 512].rearrange("k p t -> p k t")), reads=[b_sTd], writes=[bfs])

        def stage_a1(tb, i):
            r0 = tb * 512 + i * 128
            xx, bxx = xa[i % 2], bxa[i % 2]
            F.dma("sp", lambda e: e.dma_start(out=xx[:], in_=I["x"][r0:r0 + 128, :]), writes=[bxx])
            p = i % 2
            mv, rs, nmr = sma[p][:, 0:2], sma[p][:, 2:3], sma[p][:, 3:4]
            ln_stats(xx, bxx, sta[p], mv, rs, nmr, bsta[p])
            F.op("act", lambda e: e.activation(out=xn[p][:], in_=xx[:], func=AF.Identity, bias=nmr, scale=rs),
                 reads=[bxx, bsta[p]], writes=[bxn[p]])

        def stage_a2(tb, i):
            p = i % 2
            hTb, bhT = hTbs[tb % 2], bhTs[tb % 2]
            for hb in range(2):
                for jj in range(8):
                    j = hb * 8 + jj
                    F.op("pe", lambda e: e.transpose(out=psT[hb][:, jj * 128:(jj + 1) * 128], in_=xn[p][:, j * 128:(j + 1) * 128], identity=ident_bf[:]),
                         reads=[bxn[p], b_identbf], writes=[bpsT[hb]], signal=(jj == 7))
                for jj in range(8):
                    j = hb * 8 + jj
                    if jj % 2 == 0:
                        F.op("act", lambda e: e.activation(out=hTb[:, j, i * 128:(i + 1) * 128], in_=psT[hb][:, jj * 128:(jj + 1) * 128], func=AF.Identity,
                                                           bias=sh1[:, j:j + 1], scale=sc1p[:, j:j + 1]), reads=[bpsT[hb], b_modc], writes=[bhT])
                    else:
                        F.op("dve", lambda e: e.tensor_scalar(out=hTb[:, j, i * 128:(i + 1) * 128], in0=psT[hb][:, jj * 128:(jj + 1) * 128],
                                                              scalar1=sc1p[:, j:j + 1], scalar2=sh1[:, j:j + 1], op0=ALU.mult, op1=ALU.add),
                             reads=[bpsT[hb], b_modc], writes=[bhT])

        def stage_b(tb, oc):
            w = wm[oc % 2]
            bw = bwm[oc % 2]
            hTb, bhT = hTbs[tb % 2], bhTs[tb % 2]
            F.dma("sp", lambda e: e.dma_start(out=w[:], in_=Wmix_d[oc]), reads=[b_Wmix], writes=[bw])
            for k in range(16):
                F.op("pe", lambda e: e.matmul(ps[0][:], lhsT=w[:, k, :], rhs=hTb[:, k, :], start=(k == 0), stop=(k == 15)),
                     reads=[bw, bhT], writes=[bps[0]], signal=(k == 15))
            for k in range(16):
                F.op("pe", lambda e: e.matmul(ps[1][:], lhsT=w[:, 16 + k, :], rhs=hTb[:, k, :], start=(k == 0), stop=(k == 15)),
                     reads=[bw, bhT], writes=[bps[1]], signal=(k == 15))
            for k in range(8):
                F.op("pe", lambda e: e.matmul(ps[2][:], lhsT=w[:, 32 + k, :], rhs=fTb[:, k, :], start=(k == 0), stop=(k == 7)),
                     reads=[bw, bfs], writes=[bps[2]], signal=(k == 7))
            for k in range(4):
                F.op("pe", lambda e: e.matmul(ps[3][:], lhsT=w[:, 40 + k, :], rhs=sTb[:, k, :], start=(k == 0), stop=(k == 3)),
                     reads=[bw, bfs], writes=[bps[3]], signal=(k == 3))
            F.op("act", lambda e: e.activation(out=sA[:], in_=ps[0][:], func=AF.Sigmoid), reads=[bps[0]], writes=[bsA])
            F.op("act", lambda e: e.activation(out=sB[:], in_=ps[1][:], func=AF.Sigmoid), reads=[bps[1]], writes=[bsB])
            F.op("dve", lambda e: e.tensor_tensor(out=sA[:], in0=sA[:], in1=ps[2][:], op=ALU.mult), reads=[bsA, bps[2]], writes=[bsA])
            F.op("dve", lambda e: e.tensor_tensor(out=sB[:], in0=sB[:], in1=ps[3][:], op=ALU.mult), reads=[bsB, bps[3]], writes=[bsB])
            F.op("dve", lambda e: e.tensor_tensor(out=mT[:, oc, :], in0=sA[:], in1=sB[:], op=ALU.add), reads=[bsA, bsB], writes=[bmT])

        def stage_c(tb):
            t0 = tb * 512
            for i in range(4):
                r0 = t0 + i * 128
                F.dma("act", lambda e: e.dma_start(out=vt[i][:], in_=I["x"][r0:r0 + 128, :]), writes=[bvt[i]])
            for n in range(8):
                wv = wo[n % 2]
                bwv = bwo[n % 2]
                F.dma("sp", lambda e: e.dma_start(out=wv[:], in_=Wo_d[:, :, n * 256:(n + 1) * 256]), reads=[b_Wo], writes=[bwv])
                for i in range(4):
                    pp = ps[4 + i % 2]
                    bpp = bps[4 + i % 2]
                    for mc in range(16):
                        F.op("pe", lambda e: e.matmul(pp[:, 0:256], lhsT=mT[:, mc, i * 128:(i + 1) * 128], rhs=wv[:, mc, :], start=(mc == 0), stop=(mc == 15)),
                             reads=[bmT, bwv], writes=[bpp], signal=(mc == 15))
                    F.op("dve", lambda e: e.scalar_tensor_tensor(out=vt[i][:, n * 256:(n + 1) * 256], in0=vt[i][:, n * 256:(n + 1) * 256], scalar=ALPHA,
                                                                 in1=pp[:, 0:256], op0=ALU.mult, op1=ALU.add), reads=[bvt[i], bpp], writes=[bvt[i]])

        def stage_d1(tb, i):
            r0 = tb * 512 + i * 128
            p = i % 2
            mv, rs, nmr = sm[p][:, 0:2], sm[p][:, 2:3], sm[p][:, 3:4]
            ln_stats(vt[i], bvt[i], st[p], mv, rs, nmr, bst[p])
            F.op("act", lambda e: e.activation(out=vt[i][:], in_=vt[i][:], func=AF.Identity, bias=nmr, scale=rs), reads=[bvt[i], bst[p]], writes=[bvt[i]])
            F.op("pool", lambda e: e.tensor_tensor(out=vt[i][:], in0=vt[i][:], in1=lngb[:], op=ALU.mult), reads=[bvt[i], bbc], writes=[bvt[i]])
            F.op("pool", lambda e: e.tensor_tensor(out=vt[i][:], in0=vt[i][:], in1=lnbb[:], op=ALU.add), reads=[bvt[i], bbc], writes=[bvt[i]])
            F.dma("pool", lambda e: e.dma_start(out=xmid_d[r0:r0 + 128, :], in_=vt[i][:]), reads=[bvt[i]], writes=[b_xmid])
            ln_stats(vt[i], bvt[i], st[p], mv, rs, nmr, bst[p])
            F.op("act", lambda e: e.activation(out=xf[:], in_=vt[i][:], func=AF.Identity, bias=nmr, scale=rs), reads=[bvt[i], bst[p]], writes=[bxf])
            F.op("pool", lambda e: e.tensor_copy(out=xn2b[:], in_=xf[:]), reads=[bxf], writes=[bxn2b])
            F.dma("pool", lambda e: e.dma_start(out=xn2_d[r0:r0 + 128, :], in_=xn2b[:]), reads=[bxn2b], writes=[b_xn2])

        def stage_d2(tb, i):
            tix = tb * 4 + i
            for g4 in range(4):
                pt = ps[4 + g4 % 2]
                bpt = bps[4 + g4 % 2]
                for jj in range(4):
                    j = g4 * 4 + jj
                    F.op("pe", lambda e: e.transpose(out=pt[:, jj * 128:(jj + 1) * 128], in_=xf[:, j * 128:(j + 1) * 128], identity=ident_f[:]),
                         reads=[bxf, b_identf], writes=[bpt], signal=(jj == 3))
                for jj in range(4):
                    j = g4 * 4 + jj
                    if jj % 2 == 0:
                        F.op("act", lambda e: e.activation(out=h2T[:, j, :], in_=pt[:, jj * 128:(jj + 1) * 128], func=AF.Identity,
                                                           bias=sh2[:, j:j + 1], scale=sc2p[:, j:j + 1]), reads=[bpt, b_modc], writes=[bh2T])
                    else:
                        F.op("dve", lambda e: e.tensor_scalar(out=h2T[:, j, :], in0=pt[:, jj * 128:(jj + 1) * 128],
                                                              scalar1=sc2p[:, j:j + 1], scalar2=sh2[:, j:j + 1], op0=ALU.mult, op1=ALU.add),
                             reads=[bpt, b_modc], writes=[bh2T])
            for k in range(16):
                F.op("pe", lambda e: e.matmul(ps[4][:, 0:36], lhsT=h2T[:, k, :], rhs=wr[:, k, :], start=(k == 0), stop=(k == 15)),
                     reads=[bh2T, bwr], writes=[bps[4]], signal=(k == 15))
            F.op("dve", lambda e: e.tensor_tensor(out=lg_all[:, tix, :], in0=ps[4][:, 0:36], in1=brt[:], op=ALU.add), reads=[bps[4], bwr], writes=[b_lg])

        sta = [T("a4sta%d" % i, [128, 24], F32) for i in range(2)]
        sma = [T("a4sma%d" % i, [128, 4], F32) for i in range(2)]
        bsta = [Buf("a4sta%d" % i) for i in range(2)]
        for i in range(4):
            stage_a1(0, i)
            stage_a2(0, i)
        for tb in range(9):
            if tb < 8:
                stage_a0(tb)
            for oc in range(16):
                i4, r4 = oc // 4, oc % 4
                if r4 == 0 and tb >= 1:
                    stage_d1(tb - 1, i4)
                if r4 == 1 and tb + 1 < 8:
                    stage_a1(tb + 1, i4)
                if tb < 8:
                    stage_b(tb, oc)
                if r4 == 2 and tb + 1 < 8:
                    stage_a2(tb + 1, i4)
                if r4 == 3 and tb >= 1:
                    stage_d2(tb - 1, i4)
            if tb < 8:
                stage_c(tb)
        if "lg_d" in dbg:
            ld = scratch("lg_d", [128, 32, 36], F32)
            F.dma("sp", lambda e: e.dma_start(out=ld, in_=lg_all[:]), reads=[b_lg], writes=[Buf()])
        F.barrier()
    if stop_after == "A4":
        F.barrier()
        return nc

    NBLK = 96
    NROW = NBLK * 128
    rowinfo_d = scratch("rowinfo_d", [NROW, 1], I32)
    roww_d = scratch("roww_d", [NROW, 1], F32)
    ybuf_d = scratch("ybuf_d", [2 * TOWN, D], BF16)
    b_rowinfo, b_roww, b_ybuf = Buf("rowinfo_d"), Buf("roww_d"), Buf("ybuf_d")
    idxw = sb([128, NBLK, 4], I32, "idxw")
    b_bexp = Buf("bexp")
    with ExitStack() as es:
        T = lambda nm, shp, dt: es.enter_context(nc.sbuf_tensor(nm, shp, dt))
        R_ = [Buf("rt")]
        V = lambda fn, rd=R_, wr=R_: F.op("dve", fn, reads=rd, writes=wr)
        A = lambda fn, rd=R_, wr=R_: F.op("act", fn, reads=rd, writes=wr)
        tri, ones_bf = T("r_tri", [128, 128], BF16), T("r_ones", [128, 128], BF16)
        thr, blkrow = T("r_thr", [128, 32], F32), T("r_blkrow", [128, NBLK], F32)
        tokid = T("r_tokid", [128, 64], I32)
        for t_, nm in ((tri, "tri"), (ones_bf, "ones_bf"), (thr, "thr"), (blkrow, "blkrow"), (tokid, "tokid")):
            F.dma("sp", lambda e: e.dma_start(out=t_[:], in_=I[nm]), writes=R_)
        gmax, gsum, gtop = T("r_gmax", [128, 32], F32), T("r_gsum", [128, 32], F32), T("r_gtop", [128, 32], F32)
        ohg, exg = T("r_ohg", [128, 32, 4], F32), T("r_exg", [128, 32, 4], F32)
        msk = T("r_msk", [128, 32, 32], F32)
        m8 = T("r_m8", [128, 32, 8], F32)
        oh = [T("r_oh%d" % k, [128, 32, 32], F32) for k in range(2)]
        cntb = T("r_cnt", [128, 32, 32], BF16)
        pf = T("r_pf", [128, 32, 32], F32)
        tot, nbv, pendb, pst_ = T("r_tot", [128, 32], F32), T("r_nb", [128, 32], F32), T("r_pend", [128, 32], F32), T("r_pst", [128, 32], F32)
        cmp_ = T("r_cmp", [128, NBLK, 32], F32)
        onesf = T("r_onesf", [128, 32], F32)
        wk = [T("r_w%d" % k, [128, 32], F32) for k in range(2)]
        dst = [T("r_dst%d" % k, [128, 32], F32) for k in range(2)]
        dsti = [T("r_dsti%d" % k, [128, 32], I32) for k in range(2)]
        bef, bef2 = T("r_bef", [128, NBLK], F32), T("r_bef2", [128, NBLK], F32)
        oobt = T("r_oob", [128, NBLK], I32)
        lgp, lep = lg_all[:, :, 0:4], lg_all[:, :, 4:36]
        V(lambda e: e.tensor_reduce(out=gmax[:], in_=lgp, axis=AX.X, op=ALU.max), [b_lg], R_)
        V(lambda e: e.tensor_tensor(out=exg[:], in0=lgp, in1=gmax[:].unsqueeze(2).to_broadcast([128, 32, 4]), op=ALU.subtract), [b_lg] + R_, R_)
        V(lambda e: e.tensor_single_scalar(out=ohg[:], in_=exg[:], scalar=0.0, op=ALU.is_ge))
        A(lambda e: e.activation(out=exg[:], in_=exg[:], func=AF.Exp))
        V(lambda e: e.tensor_reduce(out=gsum[:], in_=exg[:], axis=AX.X, op=ALU.add))
        V(lambda e: e.reciprocal(out=gtop[:], in_=gsum[:]))
        V(lambda e: e.tensor_scalar(out=ohg[:], in0=ohg[:], scalar1=-1.0, scalar2=1e30, op0=ALU.add, op1=ALU.mult))
        V(lambda e: e.tensor_tensor(out=msk[:].rearrange("p t (g x) -> p t g x", g=4), in0=lep.rearrange("p t (g x) -> p t g x", g=4),
                                    in1=ohg[:].unsqueeze(3).to_broadcast([128, 32, 4, 8]), op=ALU.add), [b_lg] + R_, R_)
        for j in range(32):
            V(lambda e: e.max(out=m8[:, j, :], in_=msk[:, j, :]))
        for k in range(2):
            V(lambda e: e.tensor_tensor(out=oh[k][:], in0=msk[:], in1=m8[:, :, k:k + 1].to_broadcast([128, 32, 32]), op=ALU.is_equal))
        V(lambda e: e.tensor_tensor(out=wk[1][:], in0=m8[:, :, 1], in1=m8[:, :, 0], op=ALU.subtract))
        A(lambda e: e.activation(out=wk[1][:], in_=wk[1][:], func=AF.Exp))
        V(lambda e: e.tensor_scalar(out=wk[1][:], in0=wk[1][:], scalar1=1.0, scalar2=None, op0=ALU.add))
        V(lambda e: e.reciprocal(out=wk[1][:], in_=wk[1][:]))
        V(lambda e: e.tensor_tensor(out=wk[0][:], in0=gtop[:], in1=wk[1][:], op=ALU.mult))
        V(lambda e: e.tensor_tensor(out=wk[1][:], in0=gtop[:], in1=wk[0][:], op=ALU.subtract))
        V(lambda e: e.tensor_tensor(out=cntb[:], in0=oh[0][:], in1=oh[1][:], op=ALU.add))
        for hb in range(2):
            pp = ps[hb]
            for jj in range(16):
                j = hb * 16 + jj
                n_mm = 1 + j
                F.op("pe", lambda e: e.matmul(pp[:, jj * 32:(jj + 1) * 32], lhsT=tri[:], rhs=cntb[:, j, :], start=True, stop=(n_mm == 1)),
                     reads=R_, writes=[bps[hb]], signal=(n_mm == 1 and jj == 15))
                for j2 in range(j):
                    F.op("pe", lambda e: e.matmul(pp[:, jj * 32:(jj + 1) * 32], lhsT=ones_bf[:], rhs=cntb[:, j2, :], start=False, stop=(j2 == j - 1)),
                         reads=R_, writes=[bps[hb]], signal=(j2 == j - 1 and jj == 15))
            V(lambda e: e.tensor_copy(out=pf[:, hb * 16:(hb + 1) * 16, :], in_=pp[:].rearrange("p (t x) -> p t x", t=16)), [bps[hb]] + R_, R_)
        for j in range(32):
            F.op("pe", lambda e: e.matmul(ps[2][:, 0:32], lhsT=ones_bf[:], rhs=cntb[:, j, :], start=(j == 0), stop=(j == 31)),
                 reads=R_, writes=[bps[2]], signal=(j == 31))
        V(lambda e: e.tensor_copy(out=tot[:], in_=ps[2][:, 0:32]), [bps[2]] + R_, R_)
        V(lambda e: e.tensor_tensor(out=cmp_[:, 0:32, :], in0=tot[:].unsqueeze(2).to_broadcast([128, 32, 32]),
                                    in1=thr[:].unsqueeze(1).to_broadcast([128, 32, 32]), op=ALU.is_gt))
        V(lambda e: e.tensor_reduce(out=nbv[:], in_=cmp_[:, 0:32, :], axis=AX.X, op=ALU.add))
        V(lambda e: e.memset(onesf[:], 1.0))
        V(lambda e: e.tensor_tensor_scan(out=pendb[:], data0=onesf[:], data1=nbv[:], initial=0.0, op0=ALU.mult, op1=ALU.add))
        V(lambda e: e.tensor_tensor(out=pst_[:], in0=pendb[:], in1=nbv[:], op=ALU.subtract))
        V(lambda e: e.tensor_scalar(out=pst_[:], in0=pst_[:], scalar1=128.0, scalar2=None, op0=ALU.mult))
        V(lambda e: e.tensor_tensor(out=pf[:], in0=pf[:], in1=pst_[:].unsqueeze(1).to_broadcast([128, 32, 32]), op=ALU.add))
        for k in range(2):
            V(lambda e: e.tensor_tensor(out=oh[k][:], in0=oh[k][:], in1=pf[:], op=ALU.mult))
            V(lambda e: e.tensor_reduce(out=dst[k][:], in_=oh[k][:], axis=AX.X, op=ALU.add))
            V(lambda e: e.tensor_copy(out=dsti[k][:], in_=dst[k][:]))
        V(lambda e: e.tensor_tensor(out=cmp_[:], in0=pendb[:].unsqueeze(1).to_broadcast([128, NBLK, 32]),
                                    in1=blkrow[:].unsqueeze(2).to_broadcast([128, NBLK, 32]), op=ALU.is_le))
        V(lambda e: e.tensor_reduce(out=bef[:], in_=cmp_[:], axis=AX.X, op=ALU.add))
        V(lambda e: e.tensor_scalar(out=bef[:], in0=bef[:], scalar1=31.0, scalar2=None, op0=ALU.min))
        V(lambda e: e.memset(bef2[:], 1.0))
        V(lambda e: e.tensor_tensor(out=bef2[:, 1:NBLK], in0=bef[:, 1:NBLK], in1=bef[:, 0:NBLK - 1], op=ALU.not_equal))
        pg4 = T("r_pg4", [128, 4], F32)
        idxf = T("r_idxf", [128, NBLK, 4], F32)
        F.dma("sp", lambda e: e.dma_start(out=pg4[:], in_=I["pg4"]), writes=R_)
        V(lambda e: e.tensor_scalar(out=bef[:], in0=bef[:], scalar1=-64.0, scalar2=None, op0=ALU.add))
        V(lambda e: e.tensor_tensor(out=bef[:], in0=bef[:], in1=bef2[:], op=ALU.mult))
        V(lambda e: e.tensor_scalar(out=bef[:], in0=bef[:], scalar1=64.0, scalar2=512.0, op0=ALU.add, op1=ALU.mult))
        V(lambda e: e.tensor_tensor(out=idxf[:], in0=bef[:].unsqueeze(2).to_broadcast([128, NBLK, 4]),
                                    in1=pg4[:].unsqueeze(1).to_broadcast([128, NBLK, 4]), op=ALU.add))
        V(lambda e: e.tensor_copy(out=idxw[:], in_=idxf[:]), R_, [b_bexp])
        V(lambda e: e.memset(oobt[:], 1 << 20))
        F.dma("sp", lambda e: e.dma_start(out=rowinfo_d.rearrange("(p a) o -> p (a o)", p=128), in_=oobt[:]), reads=R_, writes=[b_rowinfo])
        F.dma("sp", lambda e: e.dma_start(out=roww_d.rearrange("(p a) o -> p (a o)", p=128), in_=bef[:]), reads=R_, writes=[b_roww])
        for k in range(2):
            for j in range(32):
                F.dma("pool", lambda e: e.indirect_dma_start(out=rowinfo_d, out_offset=bass.IndirectOffsetOnAxis(ap=dsti[k][:, j:j + 1], axis=0),
                                                             in_=tokid[:, k * 32 + j:k * 32 + j + 1], in_offset=None), reads=R_, writes=[b_rowinfo])
                F.dma("pool", lambda e: e.indirect_dma_start(out=roww_d, out_offset=bass.IndirectOffsetOnAxis(ap=dsti[k][:, j:j + 1], axis=0),
                                                             in_=wk[k][:, j:j + 1], in_offset=None), reads=R_, writes=[b_roww])
        if "rt_d" in dbg:
            rd = scratch("rt_d", [128, 6, 32], F32)
            rt = T("r_dbg", [128, 6, 32], F32)
            V(lambda e: e.tensor_copy(out=rt[:, 0, :], in_=dst[0][:]))
            V(lambda e: e.tensor_copy(out=rt[:, 1, :], in_=dst[1][:]))
            V(lambda e: e.tensor_copy(out=rt[:, 2, :], in_=wk[0][:]))
            V(lambda e: e.tensor_copy(out=rt[:, 3, :], in_=wk[1][:]))
            V(lambda e: e.tensor_copy(out=rt[:, 4, :], in_=tot[:]))
            V(lambda e: e.tensor_copy(out=rt[:, 5, :], in_=bef[:, 0:32]))
            F.dma("sp", lambda e: e.dma_start(out=rd, in_=rt[:]), reads=R_, writes=[Buf()])
        F.barrier()
    if stop_after == "R":
        F.barrier()
        return nc

    with ExitStack() as es:
        T = lambda nm, shp, dt: es.enter_context(nc.sbuf_tensor(nm, shp, dt))
        W1, W3, W2 = T("m_w1", [128, 16, 1024], BF16), T("m_w3", [128, 16, 1024], BF16), T("m_w2", [128, 8, D], BF16)
        bW1 = [Buf("w1_%d" % i) for i in range(4)]
        bW3 = [Buf("w3_%d" % i) for i in range(4)]
        bW2 = [Buf("w2_%d" % i) for i in range(4)]
        X = [T("m_x%d" % i, [128, D], BF16) for i in range(2)]
        XT = [T("m_xt%d" % i, [128, 16, 128], BF16) for i in range(2)]
        sl = [T("m_sl%d" % i, [128, 512], F32) for i in range(2)]
        h1 = [T("m_h1%d" % i, [128, 1024], BF16) for i in range(2)]
        h1T = [T("m_h1T%d" % i, [128, 8, 128], BF16) for i in range(2)]
        ysb = [T("m_y%d" % i, [128, D], BF16) for i in range(2)]
        ri = [T("m_ri%d" % i, [128, 2], I32) for i in range(2)]
        rw = [T("m_rw%d" % i, [128, 1], F32) for i in range(2)]
        bX, bXT, bsl, bh1, bh1T, bys, bri = ([Buf("mx%d" % i) for i in range(2)], [Buf("mxt%d" % i) for i in range(2)], [Buf("msl%d" % i) for i in range(2)],
                                             [Buf("mh1%d" % i) for i in range(2)], [Buf("mh1T%d" % i) for i in range(2)], [Buf("my%d" % i) for i in range(2)],
                                             [Buf("mri%d" % i) for i in range(2)])
        w1v = I["w1"].rearrange("e (p g k) n -> (e p g) (k n)", p=128, g=4, k=4)
        w3v = I["w3"].rearrange("e (p g k) n -> (e p g) (k n)", p=128, g=4, k=4)
        w2v = I["w2"].rearrange("e (p g k) n -> (e p g) (k n)", p=128, g=4, k=2)
        sc2k, sh2k = T("m_sc2k", [128, 16], F32), T("m_sh2k", [128, 16], F32)
        b_m2k = Buf("m2k")
        F.dma("sp", lambda e: e.dma_start(out=sh2k[:], in_=gates_d[3].rearrange("(p k) -> p k", k=16)), reads=[b_gates], writes=[b_m2k])
        F.dma("sp", lambda e: e.dma_start(out=sc2k[:], in_=gates_d[4].rearrange("(p k) -> p k", k=16)), reads=[b_gates], writes=[b_m2k])

        rb_w = nc.gpsimd.alloc_register("rb_w")
        nc.gpsimd.reg_mov(rb_w, 32 * 512 - 1)
        rb_y = nc.gpsimd.alloc_register("rb_y")
        nc.gpsimd.reg_mov(rb_y, 2 * TOWN - 1)

        def wload(i, dst_ap, src, g, bw):
            F.dma("pool", lambda e: e.indirect_dma_start(out=dst_ap, out_offset=None, in_=src,
                                                         in_offset=bass.IndirectOffsetOnAxis(ap=idxw[:, i, g:g + 1], axis=0),
                                                         bounds_check=rb_w, oob_is_err=False), reads=[b_bexp], writes=[bw])
        def rows(i):
            p = i % 2
            F.dma("sp", lambda e: e.dma_start(out=ri[p][:, 0:1], in_=rowinfo_d[i * 128:(i + 1) * 128, :]), reads=[b_rowinfo], writes=[bri[p]])
            F.op("dve", lambda e: e.tensor_single_scalar(out=ri[p][:, 1:2], in_=ri[p][:, 0:1], scalar=4095, op=ALU.bitwise_and), reads=[bri[p]], writes=[bri[p]])
            F.dma("pool", lambda e: e.indirect_dma_start(out=X[p][:], out_offset=None, in_=xn2_d, in_offset=bass.IndirectOffsetOnAxis(ap=ri[p][:, 1:2], axis=0)),
                  reads=[bri[p], b_xn2], writes=[bX[p]])

        def T1(i):
            p = i % 2
            for hb in range(2):
                for jj in range(8):
                    j = hb * 8 + jj
                    F.op("pe", lambda e: e.transpose(out=psT[hb][:, jj * 128:(jj + 1) * 128], in_=X[p][:, j:D:16], identity=ident_bf[:]),
                         reads=[bX[p], b_identbf], writes=[bpsT[hb]], signal=(jj == 7))

        def E1(i):
            p = i % 2
            for hb in range(2):
                for jj in range(8):
                    j = hb * 8 + jj
                    if jj % 2 == 0:
                        F.op("act", lambda e: e.activation(out=XT[p][:, j, :], in_=psT[hb][:, jj * 128:(jj + 1) * 128], func=AF.Identity,
                                                           bias=sh2k[:, j:j + 1], scale=sc2k[:, j:j + 1]), reads=[bpsT[hb], b_m2k], writes=[bXT[p]])
                    else:
                        F.op("dve", lambda e: e.tensor_scalar(out=XT[p][:, j, :], in0=psT[hb][:, jj * 128:(jj + 1) * 128],
                                                              scalar1=sc2k[:, j:j + 1], scalar2=sh2k[:, j:j + 1], op0=ALU.mult, op1=ALU.add),
                             reads=[bpsT[hb], b_m2k], writes=[bXT[p]])

        def W13(i):
            for kg in range(4):
                wload(i, W1[:, 4 * kg:4 * kg + 4, :].rearrange("p k n -> p (k n)"), w1v, kg, bW1[kg])
                wload(i, W3[:, 4 * kg:4 * kg + 4, :].rearrange("p k n -> p (k n)"), w3v, kg, bW3[kg])

        def W2l(i):
            for kg in range(4):
                wload(i, W2[:, 2 * kg:2 * kg + 2, :].rearrange("p k n -> p (k n)"), w2v, kg, bW2[kg])

        def H(i):
            p = i % 2
            for k in range(16):
                for n in range(2):
                    F.op("pe", lambda e: e.matmul(ps[2 * n][:], lhsT=XT[p][:, k, :], rhs=W1[:, k, n * 512:(n + 1) * 512], start=(k == 0), stop=(k == 15)),
                         reads=[bXT[p], bW1[k // 4]], writes=[bps[2 * n]], signal=(k % 4 == 3))
                    F.op("pe", lambda e: e.matmul(ps[2 * n + 1][:], lhsT=XT[p][:, k, :], rhs=W3[:, k, n * 512:(n + 1) * 512], start=(k == 0), stop=(k == 15)),
                         reads=[bXT[p], bW3[k // 4]], writes=[bps[2 * n + 1]], signal=(k % 4 == 3))
            for n in range(2):
                F.op("act", lambda e: e.activation(out=sl[n][:], in_=ps[2 * n][:], func=AF.Silu), reads=[bps[2 * n]], writes=[bsl[n]])
                F.op("dve", lambda e: e.tensor_tensor(out=h1[p][:, n * 512:(n + 1) * 512], in0=sl[n][:], in1=ps[2 * n + 1][:], op=ALU.mult),
                     reads=[bsl[n], bps[2 * n + 1]], writes=[bh1[p]])

        ps5b = ps[5][:].bitcast(BF16)

        def T2(i):
            p = i % 2
            for jj in range(8):
                F.op("pe", lambda e: e.transpose(out=ps5b[:, jj * 128:(jj + 1) * 128], in_=h1[p][:, jj:1024:8], identity=ident_bf[:]),
                     reads=[bh1[p], b_identbf], writes=[bps[5]], signal=(jj == 7))
            F.op("act", lambda e: e.activation(out=h1T[p][:, 0:4, :], in_=ps5b[:, 0:512].rearrange("p (k t) -> p k t", k=4), func=AF.Copy),
                 reads=[bps[5]], writes=[bh1T[p]])
            F.op("dve", lambda e: e.tensor_copy(out=h1T[p][:, 4:8, :], in_=ps5b[:, 512:1024].rearrange("p (k t) -> p k t", k=4)),
                 reads=[bps[5]], writes=[bh1T[p]])

        def Y(i):
            p = i % 2
            for n in range(4):
                py = ps[4 + n % 2]
                for k in range(8):
                    F.op("pe", lambda e: e.matmul(py[:], lhsT=h1T[p][:, k, :], rhs=W2[:, k, n * 512:(n + 1) * 512], start=(k == 0), stop=(k == 7)),
                         reads=[bh1T[p], bW2[k // 2]], writes=[bps[4 + n % 2]], signal=(k == 7))
                if n % 2 == 0:
                    F.op("act", lambda e: e.activation(out=ysb[p][:, n * 512:(n + 1) * 512], in_=py[:], func=AF.Identity, scale=rw3[i % 3][:, 0:1]),
                         reads=[bps[4 + n % 2], bri3[i % 3]], writes=[bys[p]])
                else:
                    F.op("dve", lambda e: e.tensor_scalar(out=ysb[p][:, n * 512:(n + 1) * 512], in0=py[:], scalar1=rw3[i % 3][:, 0:1], scalar2=None, op0=ALU.mult),
                         reads=[bps[4 + n % 2], bri3[i % 3]], writes=[bys[p]])

        def SC(i):
            p = i % 2
            F.dma("pool", lambda e: e.indirect_dma_start(out=ybuf_d, out_offset=bass.IndirectOffsetOnAxis(ap=ri3[i % 3][:, 0:1], axis=0), in_=ysb[p][:], in_offset=None,
                                                         bounds_check=rb_y, oob_is_err=False), reads=[bys[p], bri3[i % 3]], writes=[b_ybuf])
        ri3 = [T("m_ri3%d" % k, [128, 1], I32) for k in range(3)]
        bri3 = [Buf("mri3%d" % k) for k in range(3)]
        rw3 = [T("m_rw3%d" % k, [128, 1], F32) for k in range(3)]

        def rows3(i):
            F.dma("sp", lambda e: e.dma_start(out=ri3[i % 3][:], in_=rowinfo_d[i * 128:(i + 1) * 128, :]), reads=[b_rowinfo], writes=[bri3[i % 3]])
            F.dma("sp", lambda e: e.dma_start(out=rw3[i % 3][:], in_=roww_d[i * 128:(i + 1) * 128, :]), reads=[b_roww], writes=[bri3[i % 3]])
        W13(0)
        W2l(0)
        rows(0)
        rows3(0)
        rows(1)
        rows3(1)
        T1(0)
        E1(0)
        for i in range(NBLK):
            if i + 2 < NBLK:
                rows3(i + 2)
                rows(i + 2)
            H(i)
            if i + 1 < NBLK:
                W13(i + 1)
                T1(i + 1)
            T2(i)
            if i + 1 < NBLK:
                E1(i + 1)
            Y(i)
            if i + 1 < NBLK:
                W2l(i + 1)
            SC(i)
        F.barrier()
    if stop_after == "MOE":
        F.barrier()
        return nc

    with ExitStack() as es:
        T = lambda nm, shp, dt: es.enter_context(nc.sbuf_tensor(nm, shp, dt))
        g2b, lg2, lb2 = T("f_g2", [128, D], F32), T("f_lg", [128, D], F32), T("f_lb", [128, D], F32)
        bbc = Buf("f_bc")
        F.dma("sp", lambda e: e.dma_start(out=g2b[:], in_=gates_d[5:6, :].partition_broadcast(128)), reads=[b_gates], writes=[bbc])
        F.dma("sp", lambda e: e.dma_start(out=lg2[:], in_=I["lnrows"][2:3, :].partition_broadcast(128)), writes=[bbc])
        F.dma("sp", lambda e: e.dma_start(out=lb2[:], in_=I["lnrows"][3:4, :].partition_broadcast(128)), writes=[bbc])
        NB_ = 4
        xm = [T("f_xm%d" % i, [128, D], F32) for i in range(NB_)]
        y0 = [T("f_y0%d" % i, [128, D], BF16) for i in range(NB_)]
        y1 = [T("f_y1%d" % i, [128, D], BF16) for i in range(NB_)]
        ys = [T("f_ys%d" % i, [128, D], F32) for i in range(2)]
        bys_ = [Buf("fys%d" % i) for i in range(2)]
        st = [T("f_st%d" % i, [128, 24], F32) for i in range(NB_)]
        sm = [T("f_sm%d" % i, [128, 4], F32) for i in range(NB_)]
        bxm, by0, by1, bst = ([Buf("fxm%d" % i) for i in range(NB_)], [Buf("fy0%d" % i) for i in range(NB_)], [Buf("fy1%d" % i) for i in range(NB_)],
                              [Buf("fst%d" % i) for i in range(NB_)])
        def f_loads(ti):
            p = ti % NB_
            r0 = ti * 128
            F.dma("sp", lambda e: e.dma_start(out=xm[p][:], in_=xmid_d[r0:r0 + 128, :]), reads=[b_xmid], writes=[bxm[p]])
            F.dma("sp", lambda e: e.dma_start(out=y0[p][:], in_=ybuf_d[r0:r0 + 128, :]), reads=[b_ybuf], writes=[by0[p]])
            F.dma("sp", lambda e: e.dma_start(out=y1[p][:], in_=ybuf_d[TOWN + r0:TOWN + r0 + 128, :]), reads=[b_ybuf], writes=[by1[p]])
        for ti in range(3):
            f_loads(ti)
        for ti in range(32):
            p = ti % NB_
            r0 = ti * 128
            if ti + 3 < 32:
                f_loads(ti + 3)
            q = ti % 2
            F.op("pool", lambda e: e.tensor_tensor(out=ys[q][:], in0=y0[p][:], in1=y1[p][:], op=ALU.add), reads=[by0[p], by1[p]], writes=[bys_[q]])
            F.op("pool", lambda e: e.tensor_tensor(out=ys[q][:], in0=ys[q][:], in1=g2b[:], op=ALU.mult), reads=[bys_[q], bbc], writes=[bys_[q]])
            F.op("dve", lambda e: e.scalar_tensor_tensor(out=xm[p][:], in0=xm[p][:], scalar=ALPHA, in1=ys[q][:], op0=ALU.mult, op1=ALU.add),
                 reads=[bxm[p], bys_[q]], writes=[bxm[p]])
            mv, rs, nmr = sm[p][:, 0:2], sm[p][:, 2:3], sm[p][:, 3:4]
            ln_stats(xm[p], bxm[p], st[p], mv, rs, nmr, bst[p])
            F.op("act", lambda e: e.activation(out=xm[p][:], in_=xm[p][:], func=AF.Identity, bias=nmr, scale=rs), reads=[bxm[p], bst[p]], writes=[bxm[p]])
            F.op("dve", lambda e: e.tensor_tensor(out=xm[p][:], in0=xm[p][:], in1=lg2[:], op=ALU.mult), reads=[bxm[p], bbc], writes=[bxm[p]])
            F.op("dve", lambda e: e.tensor_tensor(out=xm[p][:], in0=xm[p][:], in1=lb2[:], op=ALU.add), reads=[bxm[p], bbc], writes=[bxm[p]])
            F.dma("act", lambda e: e.dma_start(out=out_ap[r0:r0 + 128, :], in_=xm[p][:]), reads=[bxm[p]], writes=[b_out])
        F.barrier()
    F.finish([b_out], "sp")
    return nc


def kernel(**inputs):
    inp = {k: np.asarray(v) for k, v in inputs.items()}
    nc = build()
    in_maps = []
    for core in range(8):
        b, half = core // 2, core % 2
        in_maps.append(host_layout(inp, b, half))
    res = run_bass_kernel_spmd(nc, in_maps, core_ids=list(range(8)))
    out = np.zeros((4, TALL, D), np.float32)
    for core in range(8):
        b, half = core // 2, core % 2
        out[b, half * TOWN:(half + 1) * TOWN] = res.results[core]["out"]
    return out
```

```python
import os
from contextlib import ExitStack
import numpy as np
import ml_dtypes
import concourse.bass as bass
import concourse.mybir as mybir
from concourse.bass_utils import run_bass_kernel_spmd

F32 = mybir.dt.float32
BF16 = mybir.dt.bfloat16
I32 = mybir.dt.int32
ALU = mybir.AluOpType
AF = mybir.ActivationFunctionType
AX = mybir.AxisListType
NPBF = ml_dtypes.bfloat16

D = 2048
TOWN = 4096
TALL = 8192
ALPHA = 2.0 ** 0.25
EPS = 1e-5
PI = float(np.pi)


class Buf:
    __slots__ = ("name", "w", "r")

    def __init__(self, name=""):
        self.name = name
        self.w = None
        self.r = []


class Flow:
    def __init__(self, nc, n_dma_sems=24):
        self.nc = nc
        self.engs = {"pe": nc.tensor, "dve": nc.vector, "act": nc.scalar, "pool": nc.gpsimd, "sp": nc.sync}
        self.sem = {k: nc.alloc_semaphore("s_" + k) for k in self.engs}
        self.cnt = {k: 0 for k in self.engs}
        self.waited = {k: {} for k in self.engs}
        self.pending = {k: [] for k in self.engs}
        self.dsems = [nc.alloc_semaphore("d%d" % i) for i in range(n_dma_sems)]
        self.dcnt = [0] * n_dma_sems
        self.dnext = 0
        self.semobj = {}
        for k, s in self.sem.items():
            self.semobj[id(s)] = s
        for s in self.dsems:
            self.semobj[id(s)] = s

    def _need(self, reads, writes):
        need = {}

        def add(p):
            if p is None:
                return
            s, v = p
            k = id(s)
            if need.get(k, 0) < v:
                need[k] = v
        for b in reads:
            add(b.w)
        for b in writes:
            add(b.w)
            for p in b.r:
                add(p)
        return need

    def _emit_waits(self, e, need):
        eng = self.engs[e]
        own = id(self.sem[e])
        for k, v in need.items():
            if k == own and e == "pe":
                continue
            if self.waited[e].get(k, 0) >= v:
                continue
            eng.wait_ge(self.semobj[k], v)
            self.waited[e][k] = v

    def _commit(self, reads, writes, tag):
        for b in writes:
            b.w = tag
            b.r = []
        for b in reads:
            if b.w is not tag:
                b.r.append(tag)
                if len(b.r) > 48:
                    best = {}
                    for (s, v) in b.r:
                        if best.get(id(s), (None, 0))[1] < v:
                            best[id(s)] = (s, v)
                    b.r = list(best.values())

    def op(self, e, fn, reads=(), writes=(), signal=True):
        reads = list(reads)
        writes = list(writes)
        need = self._need(reads, writes)
        self._emit_waits(e, need)
        ins = fn(self.engs[e])
        if signal:
            self.cnt[e] += 1
            ins.then_inc(self.sem[e], 1)
            tag = (self.sem[e], self.cnt[e])
            pr = [b for (b, w) in self.pending[e] if not w] + reads
            pw = [b for (b, w) in self.pending[e] if w] + writes
            self.pending[e] = []
            self._commit(pr, pw, tag)
        else:
            assert e == "pe"
            for b in reads:
                self.pending[e].append((b, False))
            for b in writes:
                self.pending[e].append((b, True))
        return ins

    def dma(self, q, fn, reads=(), writes=()):
        reads = list(reads)
        writes = list(writes)
        need = self._need(reads, writes)
        i = self.dnext
        self.dnext = (self.dnext + 1) % len(self.dsems)
        s = self.dsems[i]
        if self.dcnt[i] > 0:
            need[id(s)] = max(need.get(id(s), 0), self.dcnt[i])
        self._emit_waits(q, need)
        ins = fn(self.engs[q])
        self.dcnt[i] += 16
        ins.then_inc(s, 16)
        tag = (s, self.dcnt[i])
        self._commit(reads, writes, tag)
        return ins

    def barrier(self):
        need = {}
        for k, s in self.sem.items():
            if self.cnt[k] > 0:
                need[id(s)] = self.cnt[k]
        for i, s in enumerate(self.dsems):
            if self.dcnt[i] > 0:
                need[id(s)] = self.dcnt[i]
        for e in self.engs:
            assert not self.pending[e]
            self._emit_waits(e, dict(need))

    def finish(self, bufs, e="sp"):
        need = self._need(bufs, [])
        self._emit_waits(e, need)


def host_consts(half):
    c = {}
    c["ident_bf"] = np.eye(128, dtype=np.float32).astype(NPBF)
    c["ident_f"] = np.eye(128, dtype=np.float32)
    rv = np.arange(128)
    rt = (rv + 64 * half) % 128
    kt = np.arange(64) + 64 * half
    ang = 2 * np.pi * np.outer(rt, kt) / 128.0
    s = 1.0 / np.sqrt(128.0)
    c["rowdft"] = np.concatenate([np.cos(ang) * s, -np.sin(ang) * s], axis=1).astype(NPBF)
    ch = np.arange(256)
    ang = 2 * np.pi * np.outer(ch, ch) / 256.0
    C = np.cos(ang) / 16.0
    S = np.sin(ang) / 16.0
    c["chA"] = np.concatenate([C, -S], axis=1).reshape(2, 128, 512).transpose(1, 0, 2).copy().astype(NPBF)
    c["chB"] = np.concatenate([S, C], axis=1).reshape(2, 128, 512).transpose(1, 0, 2).copy().astype(NPBF)
    cc = np.arange(64)
    ang = 2 * np.pi * np.outer(cc, cc) / 64.0
    CC = np.zeros((128, 128), np.float32)
    CS = np.zeros((128, 128), np.float32)
    CC[0:64, 0:64] = np.cos(ang) / 8.0
    CS[0:64, 0:64] = np.sin(ang) / 8.0
    c["colC"] = CC.astype(NPBF)
    c["colS"] = CS.astype(NPBF)
    c["halfcol"] = np.full((128, 1), float(half), np.float32)
    c["jrow"] = np.tile(np.arange(9, dtype=np.float32)[None, :], (128, 1))
    c["crow"] = np.tile(np.arange(544, dtype=np.float32)[None, :], (128, 1))
    m = np.zeros((4, 32, 4, 32), np.float32)
    for q in range(4):
        m[q, :, q, :] = 1.0
    c["blkmask"] = m.reshape(128, 128)
    tri = (np.arange(128)[:, None] < np.arange(128)[None, :]).astype(np.float32)
    c["tri"] = tri.astype(NPBF)
    c["ones_bf"] = np.ones((128, 128), np.float32).astype(NPBF)
    c["thr"] = np.tile((128.0 * np.arange(32, dtype=np.float32))[None, :], (128, 1))
    c["blkrow"] = np.tile((1.0 * np.arange(96, dtype=np.float32))[None, :], (128, 1))
    c["pidx"] = np.arange(128, dtype=np.float32).reshape(128, 1)
    c["pg4"] = (4.0 * np.arange(128, dtype=np.float32)[:, None] + np.arange(4, dtype=np.float32)[None, :])
    tk = (np.arange(128)[:, None] + 128 * np.arange(32)[None, :]).astype(np.int32)
    c["tokid"] = np.concatenate([tk, tk + TOWN], axis=1).astype(np.int32)
    return c


CONST_SPECS = {
    "ident_bf": ([128, 128], BF16), "ident_f": ([128, 128], F32), "rowdft": ([128, 128], BF16),
    "chA": ([128, 2, 512], BF16), "chB": ([128, 2, 512], BF16), "colC": ([128, 128], BF16), "colS": ([128, 128], BF16),
    "halfcol": ([128, 1], F32), "jrow": ([128, 9], F32), "crow": ([128, 544], F32), "blkmask": ([128, 128], F32),
    "tri": ([128, 128], BF16), "ones_bf": ([128, 128], BF16), "thr": ([128, 32], F32), "blkrow": ([128, 96], F32),
    "pidx": ([128, 1], F32), "tokid": ([128, 64], I32), "pg4": ([128, 4], F32),
}


def host_layout(inp, b, half):
    m = {}
    xb = inp["x"][b]
    m["x"] = np.ascontiguousarray(np.concatenate([xb[half * TOWN:(half + 1) * TOWN], xb[(1 - half) * TOWN:(2 - half) * TOWN]], axis=0))
    m["ctx"] = np.ascontiguousarray(inp["ctx"][b])
    cv = np.stack([inp["c"][b], inp["c_ctx"]], axis=-1)
    m["cvT"] = np.ascontiguousarray(cv.reshape(16, 128, 2).transpose(1, 0, 2))
    m["w_ada"] = inp["w_ada"][0]
    m["b_adaT"] = np.ascontiguousarray(inp["b_ada"][0].reshape(96, 128).T)
    m["w_in"] = inp["w_in"][0]
    m["w_f_proj"] = inp["w_f_proj"][0]
    m["w_s_proj"] = inp["w_s_proj"][0]
    m["w_out"] = inp["w_out"][0]
    m["w_glu"] = inp["w_glu"][0]
    m["b_gluT"] = np.ascontiguousarray(inp["b_glu"][0].reshape(4, 128).T)
    m["lnrows"] = np.ascontiguousarray(np.stack([inp["ln1_g"][0], inp["ln1_b"][0], inp["ln2_g"][0], inp["ln2_b"][0]], axis=0))

    def pd(a):
        return np.ascontiguousarray(a.reshape(2, 16, 2, 64).transpose(2, 3, 0, 1).reshape(128, 32))
    m["lam_re"] = pd(inp["lam_re"][0])
    m["lam_im"] = pd(inp["lam_im"][0])
    m["log_dt"] = pd(np.broadcast_to(inp["log_dt"][0][:, :, None], (2, 32, 64)))

    def bpad(a):
        o = np.zeros((2, 64, 2, 16, 2, 16), np.float32)
        a6 = a.reshape(2, 16, 2, 64, 16)
        for g2 in range(2):
            o[g2, :, :, :, g2, :] = a6[:, :, g2].transpose(2, 0, 1, 3)
        return np.ascontiguousarray(o.reshape(128, 32, 32))
    m["b_re"] = bpad(inp["b_re"][0])
    m["b_im"] = bpad(inp["b_im"][0])

    def cpad(a):
        o = np.zeros((2, 64, 2, 16, 2, 16), np.float32)
        a6 = a.reshape(2, 16, 2, 16, 64)
        for g2 in range(2):
            o[g2, :, :, :, g2, :] = a6[:, :, g2].transpose(3, 0, 1, 2)
        return np.ascontiguousarray(o.reshape(128, 32, 32))
    m["c_re"] = cpad(inp["c_re"][0])
    m["c_im"] = cpad(inp["c_im"][0])
    m["d_skipT"] = np.ascontiguousarray(inp["d_skip"][0].reshape(4, 128).T)
    wr = np.concatenate([inp["w_group"][0], inp["w_expert"][0]], axis=1)
    m["w_rt"] = np.ascontiguousarray(wr.reshape(16, 128, 36).transpose(1, 0, 2))
    m["b_rt"] = np.ascontiguousarray(np.concatenate([inp["b_group"][0], inp["b_expert"][0]])[None, :])
    m["w1"] = inp["w1"][0]
    m["w3"] = inp["w3"][0]
    m["w2"] = inp["w2"][0]
    m.update(host_consts(half))
    return m


IN_SPECS = {
    "x": ([TALL, D], F32), "ctx": ([256, D], F32), "cvT": ([128, 16, 2], F32), "w_ada": ([D, 6 * D], F32),
    "b_adaT": ([128, 96], F32), "w_in": ([D, 5632], F32), "w_f_proj": ([1024, D], F32), "w_s_proj": ([512, D], F32),
    "w_out": ([D, D], F32), "w_glu": ([512, 512], F32), "b_gluT": ([128, 4], F32), "lnrows": ([4, D], F32),
    "lam_re": ([128, 32], F32), "lam_im": ([128, 32], F32), "log_dt": ([128, 32], F32),
    "b_re": ([128, 32, 32], F32), "b_im": ([128, 32, 32], F32), "c_re": ([128, 32, 32], F32), "c_im": ([128, 32, 32], F32),
    "d_skipT": ([128, 4], F32), "w_rt": ([128, 16, 36], F32), "b_rt": ([1, 36], F32),
    "w1": ([32, D, 1024], F32), "w3": ([32, D, 1024], F32), "w2": ([32, 1024, D], F32),
}


def build(stop_after=None, dbg=()):
    nc = bass.Bass("TRN2", target_bir_lowering=False)
    F = Flow(nc)
    I = {}
    early = stop_after in ("P0", "A1", "A2", "A3", "A4", "R")
    for k, (shp, dt) in list(IN_SPECS.items()) + list(CONST_SPECS.items()):
        if early and k in ("w1", "w2", "w3"):
            continue
        I[k] = nc.dram_tensor(k, shp, dt, kind="ExternalInput").ap()
    out_ap = nc.dram_tensor("out", [TOWN, D], F32, kind="ExternalOutput").ap()
    b_out = Buf("out")

    def scratch(name, shape, dt):
        kind = "ExternalOutput" if name in dbg else "Internal"
        return nc.dram_tensor(name, shape, dt, kind=kind).ap()

    _n = [0]

    def sb(shape, dt, name=None):
        _n[0] += 1
        return nc.alloc_sbuf_tensor(name or ("t%d" % _n[0]), shape, dt)

    psT = [nc.alloc_psum_tensor("psT%d" % i, [128, 1024], BF16) for i in range(2)]
    bpsT = [Buf("psT%d" % i) for i in range(2)]
    ps = [nc.alloc_psum_tensor("ps%d" % i, [128, 512], F32) for i in range(6)]
    bps = [Buf("ps%d" % i) for i in range(6)]

    def load_const(name, q="sp"):
        shp, dt = CONST_SPECS[name]
        t = sb(shp, dt, "c_" + name)
        b = Buf(name)
        F.dma(q, lambda e: e.dma_start(out=t[:], in_=I[name]), writes=[b])
        return t, b
    ident_bf, b_identbf = load_const("ident_bf")
    ident_f, b_identf = load_const("ident_f")
    halfcol, b_half = load_const("halfcol")
    epscol = sb([128, 1], F32, "epscol")
    b_eps = Buf("eps")
    F.op("dve", lambda e: e.memset(epscol[:], EPS), writes=[b_eps])

    modc = sb([128, 96], F32, "modc")
    modx = sb([128, 32], F32, "modx")
    b_modc, b_modx = Buf("modc"), Buf("modx")
    gates_d = scratch("gates_d", [6, D], F32)
    b_gates = Buf("gates_d")
    with nc.sbuf_tensor("cc", [128, 16, 2], F32) as cc, nc.sbuf_tensor("badaT", [128, 96], F32) as badaT, \
            nc.sbuf_tensor("gcol", [128, 6, 16], F32) as gcol, ExitStack() as es_p0:
        b_cc, b_bada = Buf("cc"), Buf("bada")
        F.dma("sp", lambda e: e.dma_start(out=cc[:], in_=I["cvT"]), writes=[b_cc])
        F.dma("sp", lambda e: e.dma_start(out=badaT[:], in_=I["b_adaT"]), writes=[b_bada])
        F.op("act", lambda e: e.activation(out=cc[:], in_=cc[:], func=AF.Silu), reads=[b_cc], writes=[b_cc])
        rowbuf = es_p0.enter_context(nc.sbuf_tensor("p0row", [2, 6 * D], F32))
        b_row = Buf("p0row")
        NWB = 6
        wts = [es_p0.enter_context(nc.sbuf_tensor("p0w%d" % i, [128, 2048], F32)) for i in range(NWB)]
        bwts = [Buf("p0w%d" % i) for i in range(NWB)]
        it = 0
        for cg in range(6):
            for k in range(16):
                wt, bw = wts[it % NWB], bwts[it % NWB]
                F.dma("sp" if it % 2 else "act", lambda e: e.dma_start(out=wt[:], in_=I["w_ada"][k * 128:(k + 1) * 128, cg * 2048:(cg + 1) * 2048]), writes=[bw])
                it += 1
                for nt in range(4):
                    F.op("pe", lambda e: e.matmul(ps[nt][0:2, :], lhsT=cc[:, k, :], rhs=wt[:, nt * 512:(nt + 1) * 512], start=(k == 0), stop=(k == 15)),
                         reads=[bw, b_cc], writes=[bps[nt]], signal=(nt == 3 or k == 15))
            for nt in range(4):
                c0 = cg * 2048 + nt * 512
                if nt % 2 == 0:
                    F.op("act", lambda e: e.activation(out=rowbuf[0:2, c0:c0 + 512], in_=ps[nt][0:2, :], func=AF.Copy), reads=[bps[nt]], writes=[b_row])
                else:
                    F.op("dve", lambda e: e.tensor_copy(out=rowbuf[0:2, c0:c0 + 512], in_=ps[nt][0:2, :]), reads=[bps[nt]], writes=[b_row])
        pacc = ps[4]
        pv = pacc[:, 0:192].rearrange("p (m t) -> p m t", t=2)
        for m in range(96):
            F.op("pe", lambda e: e.transpose(out=pv[:, m, :], in_=rowbuf[0:2, m * 128:(m + 1) * 128], identity=ident_f[0:2, 0:2]),
                 reads=[b_row, b_identf], writes=[bps[4]], signal=(m == 95))
        F.op("dve", lambda e: e.tensor_tensor(out=modc[:], in0=pv[:, :, 0], in1=badaT[:], op=ALU.add),
             reads=[bps[4], b_bada], writes=[b_modc])
        F.op("dve", lambda e: e.tensor_tensor(out=modx[:], in0=pv[:, 0:32, 1], in1=badaT[:, 0:32], op=ALU.add),
             reads=[bps[4], b_bada], writes=[b_modx])
        F.op("dve", lambda e: e.tensor_scalar(out=modc[:, 16:32], in0=modc[:, 16:32], scalar1=1.0, scalar2=None, op0=ALU.add),
             reads=[b_modc], writes=[b_modc])
        F.op("dve", lambda e: e.tensor_scalar(out=modc[:, 64:80], in0=modc[:, 64:80], scalar1=1.0, scalar2=None, op0=ALU.add),
             reads=[b_modc], writes=[b_modc])
        F.op("dve", lambda e: e.tensor_scalar(out=modx[:, 16:32], in0=modx[:, 16:32], scalar1=1.0, scalar2=None, op0=ALU.add),
             reads=[b_modx], writes=[b_modx])
        b_gcol = Buf("gcol")
        F.op("dve", lambda e: e.tensor_copy(out=gcol[:].rearrange("p g j -> p (g j)"), in_=modc[:]), reads=[b_modc], writes=[b_gcol])
        F.dma("sp", lambda e: e.dma_start(out=gates_d.rearrange("g (j p) -> p g j", p=128), in_=gcol[:],
                                          allow_slow_non_contiguous=True), reads=[b_gcol], writes=[b_gates])
        if "modc_d" in dbg:
            md = scratch("modc_d", [128, 96], F32)
            F.dma("sp", lambda e: e.dma_start(out=md, in_=modc[:]), reads=[b_modc], writes=[Buf()])
        F.barrier()
    sh1, sc1p, sh2, sc2p = modc[:, 0:16], modc[:, 16:32], modc[:, 48:64], modc[:, 64:80]
    shx, scxp = modx[:, 0:16], modx[:, 16:32]
    if stop_after == "P0":
        F.barrier()
        return nc

    Wmix_d = scratch("Wmix_d", [16, 128, 44, 128], BF16)
    Wo_d = scratch("Wo_d", [8, 128, 16, 256], BF16)
    b_Wmix, b_Wo = Buf("Wmix_d"), Buf("Wo_d")
    Wmix_v = Wmix_d.rearrange("oc p k c -> p oc k c")
    wp_jobs = []
    for k in range(16):
        wp_jobs.append((I["w_in"][k * 128:(k + 1) * 128, 1536:3584], Wmix_v[:, :, k, :]))
        wp_jobs.append((I["w_in"][k * 128:(k + 1) * 128, 3584:5632], Wmix_v[:, :, 16 + k, :]))
    for k in range(8):
        wp_jobs.append((I["w_f_proj"][k * 128:(k + 1) * 128, :], Wmix_v[:, :, 32 + k, :]))
    for k in range(4):
        wp_jobs.append((I["w_s_proj"][k * 128:(k + 1) * 128, :], Wmix_v[:, :, 40 + k, :]))
    F1d = scratch("F1d", [8, 128, 64, 128], BF16)
    b_F1d = Buf("F1d")
    es_us = ExitStack()
    usT = es_us.enter_context(nc.sbuf_tensor("usT", [128, 4, 8704], BF16))
    b_usT = Buf("usT")

    def ln_stats(xt, bx, st, mv, rs, nmr, bst):
        for i in range(4):
            F.op("dve", lambda e, i=i: e.bn_stats(out=st[:, 6 * i:6 * i + 6], in_=xt[:, i * 512:(i + 1) * 512]), reads=[bx], writes=[bst])
        F.op("dve", lambda e: e.bn_aggr(out=mv[:], in_=st[:]), reads=[bst], writes=[bst])
        F.op("act", lambda e: e.activation(out=rs[:], in_=mv[:, 1:2], func=AF.Sqrt, bias=epscol[:, 0:1], scale=1.0),
             reads=[bst, b_eps], writes=[bst])
        F.op("dve", lambda e: e.reciprocal(out=rs[:], in_=rs[:]), reads=[bst], writes=[bst])
        F.op("dve", lambda e: e.scalar_tensor_tensor(out=nmr[:], in0=mv[:, 0:1], scalar=-1.0, in1=rs[:], op0=ALU.mult, op1=ALU.mult),
             reads=[bst], writes=[bst])

    with nc.sbuf_tensor("Win", [128, 16, 1536], BF16) as Win, nc.sbuf_tensor("s_rowdft", [128, 128], BF16) as rowdft, \
            nc.sbuf_tensor("xt0", [128, D], F32) as xt0, nc.sbuf_tensor("xt1", [128, D], F32) as xt1, \
            nc.sbuf_tensor("xn0", [128, D], BF16) as xn0, nc.sbuf_tensor("xn1", [128, D], BF16) as xn1, \
            nc.sbuf_tensor("hT0", [128, 16, 128], BF16) as hT0, nc.sbuf_tensor("hT1", [128, 16, 128], BF16) as hT1, \
            nc.sbuf_tensor("uf0", [128, 1024], BF16) as uf0, nc.sbuf_tensor("uf1", [128, 1024], BF16) as uf1, \
            nc.sbuf_tensor("f10", [128, 8, 128], BF16) as f10, nc.sbuf_tensor("f11", [128, 8, 128], BF16) as f11, \
            nc.sbuf_tensor("st0", [128, 24], F32) as st0, nc.sbuf_tensor("st1", [128, 24], F32) as st1, \
            nc.sbuf_tensor("sm0", [128, 4], F32) as sm0, nc.sbuf_tensor("sm1", [128, 4], F32) as sm1:
        b_Win = Buf("Win")
        b_rd = Buf("rowdft")
        es_wp = ExitStack()
        F.dma("sp", lambda e: e.dma_start(out=rowdft[:], in_=I["rowdft"]), writes=[b_rd])
        for kg in range(4):
            F.dma("pool", lambda e, kg=kg: e.dma_start(
                out=Win[:, kg * 4:(kg + 1) * 4, :],
                in_=I["w_in"][kg * 512:(kg + 1) * 512, 0:1536].rearrange("(k p) n -> p k n", p=128)), writes=[b_Win])
        xts, xns, hTs, ufs, f1s, sts, sms = [xt0, xt1], [xn0, xn1], [hT0, hT1], [uf0, uf1], [f10, f11], [st0, st1], [sm0, sm1]
        bxt, bxn, bhT, buf_, bf1, bst = ([Buf("xt%d" % i) for i in range(2)], [Buf("xn%d" % i) for i in range(2)],
                                         [Buf("hT%d" % i) for i in range(2)], [Buf("uf%d" % i) for i in range(2)],
                                         [Buf("f1%d" % i) for i in range(2)], [Buf("st%d" % i) for i in range(2)])
        xcol = I["x"].rearrange("(r c) d -> c r d", c=64)
        ntile = 64 + 2

        def S1(ti):
            p = ti % 2
            xt, xn, st, sm = xts[p], xns[p], sts[p], sms[p]
            src = xcol[ti] if ti < 64 else I["ctx"][(ti - 64) * 128:(ti - 63) * 128, :]
            F.dma("sp", lambda e: e.dma_start(out=xt[:], in_=src), writes=[bxt[p]])
            mv, rs, nmr = sm[:, 0:2], sm[:, 2:3], sm[:, 3:4]
            ln_stats(xt, bxt[p], st, mv, rs, nmr, bst[p])
            F.op("act", lambda e: e.activation(out=xn[:], in_=xt[:], func=AF.Identity, bias=nmr, scale=rs),
                 reads=[bxt[p], bst[p]], writes=[bxn[p]])

        def S2pe(ti):
            p = ti % 2
            for hb in range(2):
                for jj in range(8):
                    j = hb * 8 + jj
                    F.op("pe", lambda e: e.transpose(out=psT[hb][:, jj * 128:(jj + 1) * 128], in_=xns[p][:, j * 128:(j + 1) * 128], identity=ident_bf[:]),
                         reads=[bxn[p], b_identbf], writes=[bpsT[hb]], signal=(jj == 7))

        def S2ev(ti):
            p = ti % 2
            hT = hTs[p]
            scp, shf, bmod = (sc1p, sh1, b_modc) if ti < 64 else (scxp, shx, b_modx)
            for hb in range(2):
                for jj in range(8):
                    j = hb * 8 + jj
                    if jj % 2 == 0:
                        F.op("act", lambda e: e.activation(out=hT[:, j, :], in_=psT[hb][:, jj * 128:(jj + 1) * 128], func=AF.Identity,
                                                           bias=shf[:, j:j + 1], scale=scp[:, j:j + 1]), reads=[bpsT[hb], bmod], writes=[bhT[p]])
                    else:
                        F.op("dve", lambda e: e.tensor_scalar(out=hT[:, j, :], in0=psT[hb][:, jj * 128:(jj + 1) * 128], scalar1=scp[:, j:j + 1],
                                                              scalar2=shf[:, j:j + 1], op0=ALU.mult, op1=ALU.add), reads=[bpsT[hb], bmod], writes=[bhT[p]])

        def S3pe(ti):
            p = ti % 2
            hT = hTs[p]
            for m in range(4):
                for k in range(16):
                    F.op("pe", lambda e: e.matmul(ps[2][:, m * 128:(m + 1) * 128], lhsT=Win[:, k, 1024 + m * 128:1024 + (m + 1) * 128], rhs=hT[:, k, :],
                                                  start=(k == 0), stop=(k == 15)), reads=[b_Win, bhT[p]], writes=[bps[2]], signal=(k == 15 and m == 3))
            if ti < 64:
                for n in range(2):
                    for k in range(16):
                        F.op("pe", lambda e: e.matmul(ps[n][:], lhsT=hT[:, k, :], rhs=Win[:, k, n * 512:(n + 1) * 512], start=(k == 0), stop=(k == 15)),
                             reads=[b_Win, bhT[p]], writes=[bps[n]], signal=(k == 15))

        def S3ev(ti):
            p = ti % 2
            uf = ufs[p]
            pc = ps[2][:].rearrange("p (m t) -> p m t", m=4)
            if ti < 64:
                c = ti
                F.op("act", lambda e: e.activation(out=usT[:, :, c:4096:64], in_=pc[:, :, 0:64], func=AF.Copy), reads=[bps[2]], writes=[b_usT])
                F.op("dve", lambda e: e.tensor_copy(out=usT[:, :, 4352 + c:8448:64], in_=pc[:, :, 64:128]), reads=[bps[2]], writes=[b_usT])
                F.op("act", lambda e: e.activation(out=uf[:, 0:512], in_=ps[0][:], func=AF.Copy), reads=[bps[0]], writes=[buf_[p]])
                F.op("dve", lambda e: e.tensor_copy(out=uf[:, 512:1024], in_=ps[1][:]), reads=[bps[1]], writes=[buf_[p]])
            else:
                t0 = (ti - 64) * 128
                F.op("act", lambda e: e.activation(out=usT[:, :, 4096 + t0:4096 + t0 + 128], in_=pc, func=AF.Copy), reads=[bps[2]], writes=[b_usT])
                F.op("dve", lambda e: e.tensor_copy(out=usT[:, :, 8448 + t0:8448 + t0 + 128], in_=pc), reads=[bps[2]], writes=[b_usT])

        def S4pe(ti):
            p = ti % 2
            for m in range(8):
                F.op("pe", lambda e: e.matmul(ps[3 + m // 4][:, (m % 4) * 128:(m % 4 + 1) * 128], lhsT=ufs[p][:, m * 128:(m + 1) * 128], rhs=rowdft[:],
                                              start=True, stop=True), reads=[buf_[p], b_rd], writes=[bps[3 + m // 4]], signal=(m % 4 == 3))

        def S4ev(ti):
            p = ti % 2
            f1 = f1s[p]
            F.op("act", lambda e: e.activation(out=f1[:, 0:4, :], in_=ps[3][:].rearrange("p (m t) -> p m t", m=4), func=AF.Copy), reads=[bps[3]], writes=[bf1[p]])
            F.op("dve", lambda e: e.tensor_copy(out=f1[:, 4:8, :], in_=ps[4][:].rearrange("p (m t) -> p m t", m=4)), reads=[bps[4]], writes=[bf1[p]])
            F.dma("sp", lambda e: e.dma_start(out=F1d.rearrange("m ch c k -> ch m c k")[:, :, ti, :], in_=f1[:]), reads=[bf1[p]], writes=[b_F1d])

        wpf = [es_wp.enter_context(nc.sbuf_tensor("wpA%d" % i, [128, 2048], F32)) for i in range(2)]
        wpb = [es_wp.enter_context(nc.sbuf_tensor("wpB%d" % i, [128, 2048], BF16)) for i in range(2)]
        bwpf = [Buf("wpA%d" % i) for i in range(2)]
        bwpb = [Buf("wpB%d" % i) for i in range(2)]

        def wprep_piece(ji):
            src, dst = wp_jobs[ji]
            p = ji % 2
            F.dma("pool", lambda e: e.dma_start(out=wpf[p][:], in_=src), writes=[bwpf[p]])
            F.op("pool", lambda e: e.tensor_copy(out=wpb[p][:], in_=wpf[p][:]), reads=[bwpf[p]], writes=[bwpb[p]])
            F.dma("pool", lambda e: e.dma_start(out=dst, in_=wpb[p][:].rearrange("p (oc c) -> p oc c", c=128)), reads=[bwpb[p]], writes=[b_Wmix])

        ok = lambda t: 0 <= t < ntile
        S1(0)
        S1(1)
        S2pe(0)
        S2ev(0)
        for n in range(0, ntile + 1):
            if ok(n + 2):
                S1(n + 2)
            if ok(n + 1):
                S2pe(n + 1)
            if ok(n):
                S3pe(n)
            if ok(n - 1) and n - 1 < 64:
                S4pe(n - 1)
            if ok(n + 1):
                S2ev(n + 1)
            if ok(n):
                S3ev(n)
            if ok(n - 1) and n - 1 < 64:
                S4ev(n - 1)
            if n < len(wp_jobs):
                wprep_piece(n)
        if "usT_d" in dbg:
            ud = scratch("usT_d", [128, 4, 8704], BF16)
            F.dma("sp", lambda e: e.dma_start(out=ud, in_=usT[:]), reads=[b_usT], writes=[Buf()])
        F.barrier()
        es_wp.close()
    if stop_after == "A1":
        F.barrier()
        return nc

    fT_d = scratch("fT_d", [8, 128, TOWN], BF16)
    b_fTd = Buf("fT_d")
    with nc.sbuf_tensor("s_chA", [128, 2, 512], BF16) as chA, nc.sbuf_tensor("s_chB", [128, 2, 512], BF16) as chB, \
            nc.sbuf_tensor("s_colC", [128, 128], BF16) as colC, nc.sbuf_tensor("s_colS", [128, 128], BF16) as colS, \
            nc.sbuf_tensor("F1s0", [128, 2, 64, 128], BF16) as F1s0, nc.sbuf_tensor("F1s1", [128, 2, 64, 128], BF16) as F1s1, \
            nc.sbuf_tensor("G0", [128, 512], BF16) as G0, nc.sbuf_tensor("G1", [128, 512], BF16) as G1, \
            nc.sbuf_tensor("fo0", [128, 2, TOWN], BF16) as fo0, nc.sbuf_tensor("fo1", [128, 2, TOWN], BF16) as fo1:
        b_tabs = Buf("dfttabs")
        for t, nm in ((chA, "chA"), (chB, "chB"), (colC, "colC"), (colS, "colS")):
            F.dma("sp", lambda e, t=t, nm=nm: e.dma_start(out=t[:], in_=I[nm]), writes=[b_tabs])
        F1ss, Gs, fos = [F1s0, F1s1], [G0, G1], [fo0, fo1]
        bF1s, bG, bfo = [Buf("F1s0"), Buf("F1s1")], [Buf("G0"), Buf("G1")], [Buf("fo0"), Buf("fo1")]
        for gi in range(4):
            F1s, fo = F1ss[gi % 2], fos[gi % 2]
            for kc in range(2):
                F.dma("sp" if kc == 0 else "act", lambda e, F1s=F1s, kc=kc, gi=gi: e.dma_start(out=F1s[:, kc, :, :], in_=F1d[2 * gi + kc]),
                      reads=[b_F1d], writes=[bF1s[gi % 2]])
            def step2(kr):
                pa, bpa = ps[kr % 2], bps[kr % 2]
                n = 0
                for kc in range(2):
                    for (off, tab) in ((0, chA), (64, chB)):
                        F.op("pe", lambda e: e.matmul(pa[0:64, :], lhsT=F1s[:, kc, :, off + kr], rhs=tab[:, kc, :], start=(n == 0), stop=(n == 3)),
                             reads=[bF1s[gi % 2], b_tabs], writes=[bpa], signal=(n == 3))
                        n += 1
                G = Gs[kr % 2]
                if kr % 2 == 0:
                    F.op("act", lambda e: e.activation(out=G[0:64, :], in_=pa[0:64, :], func=AF.Copy), reads=[bpa], writes=[bG[kr % 2]])
                else:
                    F.op("dve", lambda e: e.tensor_copy(out=G[0:64, :], in_=pa[0:64, :]), reads=[bpa], writes=[bG[kr % 2]])

            def step3(kr):
                G = Gs[kr % 2]
                g4 = kr // 4
                pb, bpb = ps[2 + g4 % 2], bps[2 + g4 % 2]
                for q in range(2):
                    c0 = (q * 4 + kr % 4) * 64
                    F.op("pe", lambda e: e.matmul(pb[:, c0:c0 + 64], lhsT=G[0:64, q * 128:(q + 1) * 128], rhs=colC[0:64, 0:64],
                                                  start=True, stop=False), reads=[bG[kr % 2], b_tabs], writes=[bpb], signal=False)
                    F.op("pe", lambda e: e.matmul(pb[:, c0:c0 + 64], lhsT=G[0:64, 256 + q * 128:256 + (q + 1) * 128], rhs=colS[0:64, 0:64],
                                                  start=False, stop=True), reads=[bG[kr % 2], b_tabs], writes=[bpb], signal=(q == 1))
                if kr % 4 == 3:
                    kr0 = kr - 3
                    if g4 % 2 == 0:
                        F.op("dve", lambda e: e.tensor_copy(out=fo[:, :, kr0 * 64:(kr0 + 4) * 64], in_=pb[:].rearrange("p (q t) -> p q t", q=2)),
                             reads=[bpb], writes=[bfo[gi % 2]])
                    else:
                        F.op("act", lambda e: e.activation(out=fo[:, :, kr0 * 64:(kr0 + 4) * 64], in_=pb[:].rearrange("p (q t) -> p q t", q=2), func=AF.Copy),
                             reads=[bpb], writes=[bfo[gi % 2]])
            step2(0)
            for kr in range(64):
                if kr + 1 < 64:
                    step2(kr + 1)
                step3(kr)
            F.dma("sp", lambda e, fo=fo, gi=gi: e.dma_start(out=fT_d[2 * gi:2 * gi + 2].rearrange("q p t -> p q t"), in_=fo[:]),
                  reads=[bfo[gi % 2]], writes=[b_fTd])
        F.barrier()
    if stop_after == "A2":
        F.barrier()
        return nc

    sT_d = scratch("sT_d", [4, 128, TOWN], BF16)
    b_sTd = Buf("sT_d")
    gT_d = scratch("gT_d", [4, 128, TOWN], BF16)
    b_gTd = Buf("gT_d")
    yT_dbg = scratch("yT_d", [4, 8, 128, 512], F32) if "yT_d" in dbg else None
    PIS = 3.141592
    with ExitStack() as es_a3:
        par = es_a3.enter_context(nc.sbuf_tensor("s5par", [128, 32 * 12], F32))
        pw = es_a3.enter_context(nc.sbuf_tensor("s5pw", [128, 32 * 9 * 8], F32))
        pwi = es_a3.enter_context(nc.sbuf_tensor("s5pi", [128, 32 * 9], I32))
        Bt = es_a3.enter_context(nc.sbuf_tensor("s5b", [128, 2, 1024], F32))
        Ct = es_a3.enter_context(nc.sbuf_tensor("s5c", [128, 2, 1024], F32))
        Btmp = es_a3.enter_context(nc.sbuf_tensor("s5bt", [128, 2, 1024], F32))
        jrow = es_a3.enter_context(nc.sbuf_tensor("s5jrow", [128, 9], F32))
        crow = es_a3.enter_context(nc.sbuf_tensor("s5crow", [128, 544], F32))
        blkmask = es_a3.enter_context(nc.sbuf_tensor("s5mask", [128, 128], F32))
        dsk = es_a3.enter_context(nc.sbuf_tensor("s5dsk", [128, 4], F32))
        Wt = es_a3.enter_context(nc.sbuf_tensor("s5W", [128, 2, 1056], F32))
        QWt = es_a3.enter_context(nc.sbuf_tensor("s5QW", [128, 2 * 8 * 2 * 128], BF16))
        KWt = es_a3.enter_context(nc.sbuf_tensor("s5KW", [128, 2 * 8 * 128], BF16))
        NWt = es_a3.enter_context(nc.sbuf_tensor("s5NW", [128, 2 * 2 * 1024], BF16))
        tab = es_a3.enter_context(nc.sbuf_tensor("s5tab", [128, 2, 544], F32))
        tf = es_a3.enter_context(nc.sbuf_tensor("s5tf", [128, 544], F32))
        ti_ = es_a3.enter_context(nc.sbuf_tensor("s5ti", [128, 544], I32))
        dm = es_a3.enter_context(nc.sbuf_tensor("s5d", [128, 6, 544], F32))
        cr_ = es_a3.enter_context(nc.sbuf_tensor("s5cr", [128, 16], F32))
        St = es_a3.enter_context(nc.sbuf_tensor("s5S", [128, 8 * 2 * 520], BF16))
        yt = es_a3.enter_context(nc.sbuf_tensor("s5y", [128, 2, 512], F32))
        gt = es_a3.enter_context(nc.sbuf_tensor("s5g", [128, TOWN], BF16))
        sgt = es_a3.enter_context(nc.sbuf_tensor("s5sg", [128, 512], F32))
        b_par, b_W, b_QW, b_KW, b_NW = Buf("par"), Buf("W"), Buf("QW"), Buf("KW"), Buf("NW")
        Zt = Wt
        dmflat = dm[:].rearrange("p a c -> p (a c)")
        b_Z = b_W
        b_tmp = b_W
        b_tab, b_S, b_y, b_g, b_gl = Buf("tab"), Buf("S"), [Buf("y0"), Buf("y1")], Buf("g"), Buf("gl")
        dq = ["sp", "act"]
        lre, lim, ldt = par[:, 0:32], par[:, 32:64], par[:, 64:96]
        F.dma("sp", lambda e: e.dma_start(out=lre, in_=I["lam_re"]), writes=[b_par])
        F.dma("sp", lambda e: e.dma_start(out=lim, in_=I["lam_im"]), writes=[b_par])
        F.dma("sp", lambda e: e.dma_start(out=ldt, in_=I["log_dt"]), writes=[b_par])
        F.dma("sp", lambda e: e.dma_start(out=Bt[:, 0, :], in_=I["b_re"].rearrange("p a b -> p (a b)")), writes=[b_par])
        F.dma("sp", lambda e: e.dma_start(out=Bt[:, 1, :], in_=I["b_im"].rearrange("p a b -> p (a b)")), writes=[b_par])
        F.dma("act", lambda e: e.dma_start(out=Ct[:, 0, :], in_=I["c_re"].rearrange("p a b -> p (a b)")), writes=[b_par])
        F.dma("act", lambda e: e.dma_start(out=Ct[:, 1, :], in_=I["c_im"].rearrange("p a b -> p (a b)")), writes=[b_par])
        F.dma("sp", lambda e: e.dma_start(out=jrow[:], in_=I["jrow"]), writes=[b_par])
        F.dma("sp", lambda e: e.dma_start(out=crow[:], in_=I["crow"]), writes=[b_par])
        F.dma("sp", lambda e: e.dma_start(out=blkmask[:], in_=I["blkmask"]), writes=[b_par])
        F.dma("sp", lambda e: e.dma_start(out=dsk[:], in_=I["d_skipT"]), writes=[b_par])

        def V(fn, reads, writes):
            F.op("dve", fn, reads=reads, writes=writes)

        def A(fn, reads, writes):
            F.op("act", fn, reads=reads, writes=writes)
        P_ = [b_par]
        dtc, aa, th = par[:, 96:128], par[:, 128:160], par[:, 160:192]
        A(lambda e: e.activation(out=dtc, in_=ldt, func=AF.Exp), P_, P_)
        V(lambda e: e.tensor_tensor(out=aa, in0=lre, in1=dtc, op=ALU.mult), P_, P_)
        V(lambda e: e.tensor_tensor(out=th, in0=lim, in1=dtc, op=ALU.mult), P_, P_)
        def p3(i):
            return pw[:, i * 288:(i + 1) * 288].rearrange("p (a j) -> p a j", j=9)
        MAG, ANG, RED, SIN, COS, PR, PI_, TMP = [p3(i) for i in range(8)]
        pwi3 = pwi[:].rearrange("p (a j) -> p a j", j=9)
        jb = jrow[:].unsqueeze(1).to_broadcast([128, 32, 9])
        V(lambda e: e.tensor_tensor(out=MAG, in0=aa.unsqueeze(2).to_broadcast([128, 32, 9]), in1=jb, op=ALU.mult), P_, P_)
        A(lambda e: e.activation(out=MAG, in_=MAG, func=AF.Exp), P_, P_)
        V(lambda e: e.tensor_tensor(out=ANG, in0=th.unsqueeze(2).to_broadcast([128, 32, 9]), in1=jb, op=ALU.mult), P_, P_)

        def range_reduce(dst, src, tmpf, tmpi, shift, R, Wr):
            V(lambda e: e.tensor_scalar(out=tmpf, in0=src, scalar1=shift, scalar2=1.0 / (2 * PI), op0=ALU.add, op1=ALU.mult), R, Wr)
            V(lambda e: e.tensor_copy(out=tmpi, in_=tmpf), Wr, Wr)
            V(lambda e: e.tensor_copy(out=tmpf, in_=tmpi), Wr, Wr)
            V(lambda e: e.scalar_tensor_tensor(out=tmpf, in0=tmpf, scalar=-2 * PI, in1=src, op0=ALU.mult, op1=ALU.add), R + Wr, Wr)
            V(lambda e: e.tensor_scalar(out=dst, in0=tmpf, scalar1=shift, scalar2=-PIS, op0=ALU.add, op1=ALU.max), Wr, Wr)
            V(lambda e: e.tensor_scalar(out=dst, in0=dst, scalar1=PIS, scalar2=None, op0=ALU.min), Wr, Wr)
        range_reduce(RED, ANG, TMP, pwi3, 0.0, P_, P_)
        A(lambda e: e.activation(out=SIN, in_=RED, func=AF.Sin), P_, P_)
        range_reduce(COS, ANG, TMP, pwi3, PI / 2, P_, P_)
        A(lambda e: e.activation(out=COS, in_=COS, func=AF.Sin), P_, P_)
        V(lambda e: e.tensor_tensor(out=PR, in0=MAG, in1=COS, op=ALU.mult), P_, P_)
        V(lambda e: e.tensor_tensor(out=PI_, in0=MAG, in1=SIN, op=ALU.mult), P_, P_)
        nr, ni, den, cr, ci, t1c, t2c = [par[:, 192 + 32 * i:224 + 32 * i] for i in range(6)] + [par[:, 352:384]]
        V(lambda e: e.tensor_scalar(out=nr, in0=PR[:, :, 1], scalar1=-1.0, scalar2=None, op0=ALU.add), P_, P_)
        V(lambda e: e.tensor_copy(out=ni, in_=PI_[:, :, 1]), P_, P_)
        V(lambda e: e.tensor_tensor(out=den, in0=lre, in1=lre, op=ALU.mult), P_, P_)
        V(lambda e: e.tensor_tensor(out=t1c, in0=lim, in1=lim, op=ALU.mult), P_, P_)
        V(lambda e: e.tensor_tensor(out=den, in0=den, in1=t1c, op=ALU.add), P_, P_)
        V(lambda e: e.reciprocal(out=den, in_=den), P_, P_)
        V(lambda e: e.tensor_tensor(out=cr, in0=nr, in1=lre, op=ALU.mult), P_, P_)
        V(lambda e: e.tensor_tensor(out=t1c, in0=ni, in1=lim, op=ALU.mult), P_, P_)
        V(lambda e: e.tensor_tensor(out=cr, in0=cr, in1=t1c, op=ALU.add), P_, P_)
        V(lambda e: e.tensor_tensor(out=cr, in0=cr, in1=den, op=ALU.mult), P_, P_)
        V(lambda e: e.tensor_tensor(out=ci, in0=ni, in1=lre, op=ALU.mult), P_, P_)
        V(lambda e: e.tensor_tensor(out=t1c, in0=nr, in1=lim, op=ALU.mult), P_, P_)
        V(lambda e: e.tensor_tensor(out=ci, in0=ci, in1=t1c, op=ALU.subtract), P_, P_)
        V(lambda e: e.tensor_tensor(out=ci, in0=ci, in1=den, op=ALU.mult), P_, P_)
        B3 = lambda i: Bt[:, i, :].rearrange("p (a h) -> p a h", h=32)
        T3 = lambda i: Btmp[:, i, :].rearrange("p (a h) -> p a h", h=32)
        crb = cr.unsqueeze(2).to_broadcast([128, 32, 32])
        cib = ci.unsqueeze(2).to_broadcast([128, 32, 32])
        V(lambda e: e.tensor_tensor(out=T3(0), in0=B3(0), in1=crb, op=ALU.mult), P_, P_)
        V(lambda e: e.tensor_tensor(out=T3(1), in0=B3(1), in1=cib, op=ALU.mult), P_, P_)
        V(lambda e: e.tensor_tensor(out=T3(0), in0=T3(0), in1=T3(1), op=ALU.subtract), P_, P_)
        V(lambda e: e.tensor_tensor(out=T3(1), in0=B3(1), in1=crb, op=ALU.mult), P_, P_)
        V(lambda e: e.tensor_tensor(out=B3(1), in0=B3(0), in1=cib, op=ALU.mult), P_, P_)
        V(lambda e: e.tensor_tensor(out=T3(1), in0=T3(1), in1=B3(1), op=ALU.add), P_, P_)
        BB = Btmp
        CN = Bt[:, 0, :]
        V(lambda e: e.tensor_scalar(out=CN, in0=Ct[:, 1, :], scalar1=-1.0, scalar2=None, op0=ALU.mult), P_, P_)

        def bview(t2d, pd0):
            return t2d[:, pd0 * 32:(pd0 + 4) * 32].rearrange("p (q h) -> p q h", q=4).unsqueeze(1).to_broadcast([128, 8, 4, 32])

        def pview(T, pd0, j0):
            return T[:, pd0:pd0 + 4, j0:j0 + 8].rearrange("p q j -> p j q").unsqueeze(3).to_broadcast([128, 8, 4, 32])

        for blk in range(4):
            for d in range(2):
                pd0 = d * 16 + blk * 4
                W4 = lambda i: Wt[:, i, 0:1024].rearrange("p (j q h) -> p j q h", j=8, q=4)
                X4 = lambda i: dmflat[:, i * 1024:(i + 1) * 1024].rearrange("p (j q h) -> p j q h", j=8, q=4)
                RW = [b_par, b_W]
                V(lambda e: e.tensor_tensor(out=W4(0), in0=bview(BB[:, 0, :], pd0), in1=pview(PR, pd0, 0), op=ALU.mult), [b_par], [b_W])
                V(lambda e: e.tensor_tensor(out=X4(0), in0=bview(BB[:, 1, :], pd0), in1=pview(PI_, pd0, 0), op=ALU.mult), [b_par], [b_W])
                V(lambda e: e.tensor_tensor(out=W4(0), in0=W4(0), in1=X4(0), op=ALU.subtract), RW, [b_W])
                V(lambda e: e.tensor_tensor(out=W4(1), in0=bview(BB[:, 1, :], pd0), in1=pview(PR, pd0, 0), op=ALU.mult), [b_par], [b_W])
                V(lambda e: e.tensor_tensor(out=X4(1), in0=bview(BB[:, 0, :], pd0), in1=pview(PI_, pd0, 0), op=ALU.mult), [b_par], [b_W])
                V(lambda e: e.tensor_tensor(out=W4(1), in0=W4(1), in1=X4(1), op=ALU.add), RW, [b_W])
                for reim in range(2):
                    for jh in range(2):
                        pst = ps[(reim * 2 + jh) % 4]
                        bpst = bps[(reim * 2 + jh) % 4]
                        for jj in range(4):
                            j = jh * 4 + jj
                            F.op("pe", lambda e: e.transpose(out=pst[:, jj * 128:(jj + 1) * 128], in_=Wt[:, reim, j * 128:(j + 1) * 128], identity=ident_f[:]),
                                 reads=[b_W, b_identf], writes=[bpst], signal=(jj == 3))
                        dst = QWt[:].rearrange("p (d j r c) -> p d j r c", d=2, j=8, r=2)[:, d, jh * 4:(jh + 1) * 4, reim, :]
                        A(lambda e: e.activation(out=dst, in_=pst[:].rearrange("p (j c) -> p j c", j=4), func=AF.Copy), [bpst], [b_QW])
                for jh in range(2):
                    pst = ps[4 + jh]
                    bpst = bps[4 + jh]
                    for jj in range(4):
                        j = jh * 4 + jj
                        F.op("pe", lambda e: e.matmul(pst[:, jj * 128:(jj + 1) * 128], lhsT=Wt[:, 0, j * 128:(j + 1) * 128],
                                                      rhs=Ct[:, 0, pd0 * 32:(pd0 + 4) * 32], start=True, stop=False),
                             reads=[b_W, b_par], writes=[bpst], signal=False)
                        F.op("pe", lambda e: e.matmul(pst[:, jj * 128:(jj + 1) * 128], lhsT=Wt[:, 1, j * 128:(j + 1) * 128],
                                                      rhs=CN[:, pd0 * 32:(pd0 + 4) * 32], start=False, stop=True),
                             reads=[b_W, b_par], writes=[bpst], signal=(jj == 3))
                    dst = KWt[:].rearrange("p (d j c) -> p d j c", d=2, j=8)[:, d, jh * 4:(jh + 1) * 4, :]
                    V(lambda e: e.tensor_tensor(out=dst, in0=pst[:].rearrange("p (j c) -> p j c", j=4),
                                                in1=blkmask[:].unsqueeze(1).to_broadcast([128, 4, 128]), op=ALU.mult), [bpst, b_par], [b_KW])
                if d == 0:
                    k00 = KWt[:, 0:128]
                    V(lambda e: e.scalar_tensor_tensor(out=k00, in0=ident_f[:], scalar=dsk[:, blk:blk + 1], in1=k00, op0=ALU.mult, op1=ALU.add),
                      [b_identf, b_par, b_KW], [b_KW])
                N4 = lambda r: NWt[:].rearrange("p (d r x) -> p d r x", d=2, r=2)[:, d, r, :].rearrange("p (j q h) -> p j q h", j=8, q=4)
                V(lambda e: e.tensor_tensor(out=X4(0), in0=bview(Ct[:, 0, :], pd0), in1=pview(PR, pd0, 1), op=ALU.mult), [b_par, b_W], [b_W])
                V(lambda e: e.tensor_tensor(out=X4(1), in0=bview(CN, pd0), in1=pview(PI_, pd0, 1), op=ALU.mult), [b_par, b_W], [b_W])
                V(lambda e: e.tensor_tensor(out=N4(0), in0=X4(0), in1=X4(1), op=ALU.add), [b_W], [b_NW])
                V(lambda e: e.tensor_tensor(out=X4(0), in0=bview(Ct[:, 0, :], pd0), in1=pview(PI_, pd0, 1), op=ALU.mult), [b_par, b_W], [b_W])
                V(lambda e: e.tensor_tensor(out=X4(1), in0=bview(CN, pd0), in1=pview(PR, pd0, 1), op=ALU.mult), [b_par, b_W], [b_W])
                V(lambda e: e.tensor_tensor(out=N4(1), in0=X4(1), in1=X4(0), op=ALU.subtract), [b_W], [b_NW])
            QW5 = QWt[:].rearrange("p (d j r c) -> p d j r c", d=2, j=8, r=2)
            KW4 = KWt[:].rearrange("p (d j c) -> p d j c", d=2, j=8)
            NW6 = NWt[:].rearrange("p (d r j q h) -> p d r j q h", d=2, r=2, j=8, q=4)
            S5v = St[:].rearrange("p (a r c) -> p a r c", a=8, r=2)
            for d in range(2):
                for q in range(4):
                    pdl = d * 4 + q
                    pd = d * 16 + blk * 4 + q
                    rows = slice(32 * q, 32 * q + 32)
                    if d == 0:
                        segs = [(4096, 512, 0, False), (8192, 32, 512, False), (0, 512, 544, False)]
                    else:
                        segs = [(4352, 512, 32, True), (8448, 32, 0, True), (0, 512, 544, True)]
                    for reim in range(2):
                        for gi_, (base, L, zo, rev) in enumerate(segs):
                            pz = ps[(reim * 3 + gi_) % 6]
                            bpz = bps[(reim * 3 + gi_) % 6]
                            for s_ in range(8):
                                j = 7 - s_ if d == 0 else s_
                                F.op("pe", lambda e: e.matmul(pz[:, 0:L], lhsT=QW5[rows, d, j, reim, :],
                                                              rhs=usT[rows, blk, base + s_:base + s_ + 8 * (L - 1) + 1:8],
                                                              start=(s_ == 0), stop=(s_ == 7), tile_position=(32 * q, 0)),
                                     reads=[b_QW, b_usT], writes=[bpz], signal=(s_ == 7))
                            zs = Zt[:, reim, zo:zo + L]
                            if rev:
                                zs = zs[:, ::-1]
                            if reim == 0:
                                A(lambda e: e.activation(out=zs, in_=pz[:, 0:L], func=AF.Copy), [bpz], [b_Z])
                            else:
                                V(lambda e: e.tensor_copy(out=zs, in_=pz[:, 0:L]), [bpz], [b_Z])
                    r8, th8, c8, s8 = MAG[:, pd, 8:9], RED[:, pd, 8:9], COS[:, pd, 8:9], SIN[:, pd, 8:9]
                    TB = [b_tab]
                    V(lambda e: e.tensor_scalar(out=dm[:, 0, :], in0=crow[:], scalar1=th8, scalar2=None, op0=ALU.mult), [b_par, b_tmp], [b_tmp])
                    range_reduce(tab[:, 0, :], dm[:, 0, :], tf[:], ti_[:], 0.0, [b_tmp], [b_tab])
                    A(lambda e: e.activation(out=tab[:, 0, :], in_=tab[:, 0, :], func=AF.Sin), TB, TB)
                    range_reduce(tab[:, 1, :], dm[:, 0, :], tf[:], ti_[:], PI / 2, [b_tmp], [b_tab])
                    A(lambda e: e.activation(out=tab[:, 1, :], in_=tab[:, 1, :], func=AF.Sin), TB, TB)
                    sinT, cosT = tab[:, 0, :], tab[:, 1, :]
                    TM = [b_tmp]

                    def demod(zo, L):
                        zr, zi = Zt[:, 0, zo:zo + L], Zt[:, 1, zo:zo + L]
                        V(lambda e: e.tensor_tensor(out=dm[:, 0, 0:L], in0=zr, in1=cosT[:, 0:L], op=ALU.mult), [b_Z, b_tab, b_tmp], TM)
                        V(lambda e: e.tensor_tensor(out=dm[:, 1, 0:L], in0=zi, in1=sinT[:, 0:L], op=ALU.mult), [b_Z, b_tab, b_tmp], TM)
                        V(lambda e: e.tensor_tensor(out=dm[:, 2, 0:L], in0=dm[:, 0, 0:L], in1=dm[:, 1, 0:L], op=ALU.add), TM, TM)
                        V(lambda e: e.tensor_tensor(out=dm[:, 0, 0:L], in0=zi, in1=cosT[:, 0:L], op=ALU.mult), [b_Z, b_tab, b_tmp], TM)
                        V(lambda e: e.tensor_tensor(out=dm[:, 1, 0:L], in0=zr, in1=sinT[:, 0:L], op=ALU.mult), [b_Z, b_tab, b_tmp], TM)
                        V(lambda e: e.tensor_tensor(out=dm[:, 3, 0:L], in0=dm[:, 0, 0:L], in1=dm[:, 1, 0:L], op=ALU.subtract), TM, TM)

                    def scan(L, ire, iim):
                        V(lambda e: e.tensor_tensor_scan(out=dm[:, 4, 0:L], data0=r8.to_broadcast([128, L]), data1=dm[:, 2, 0:L],
                                                         initial=ire, op0=ALU.mult, op1=ALU.add), [b_par, b_tmp], TM)
                        V(lambda e: e.tensor_tensor_scan(out=dm[:, 5, 0:L], data0=r8.to_broadcast([128, L]), data1=dm[:, 3, 0:L],
                                                         initial=iim, op0=ALU.mult, op1=ALU.add), [b_par, b_tmp], TM)
                    demod(0, 544)
                    scan(544, 0.0, 0.0)
                    sre2, sim2 = dm[:, 4, 31:544:512], dm[:, 5, 31:544:512]
                    cs2, sn2 = tab[:, 1, 31:544:512], tab[:, 0, 31:544:512]
                    C_ = lambda i, n=1: cr_[:, i:i + n]
                    V(lambda e: e.tensor_tensor(out=C_(4, 2), in0=sre2, in1=cs2, op=ALU.mult), [b_tmp, b_tab], TM)
                    V(lambda e: e.tensor_tensor(out=C_(6, 2), in0=sim2, in1=sn2, op=ALU.mult), [b_tmp, b_tab], TM)
                    V(lambda e: e.tensor_tensor(out=C_(0, 2), in0=C_(4, 2), in1=C_(6, 2), op=ALU.subtract), TM, TM)
                    V(lambda e: e.tensor_tensor(out=C_(4, 2), in0=sim2, in1=cs2, op=ALU.mult), [b_tmp, b_tab], TM)
                    V(lambda e: e.tensor_tensor(out=C_(6, 2), in0=sre2, in1=sn2, op=ALU.mult), [b_tmp, b_tab], TM)
                    V(lambda e: e.tensor_tensor(out=C_(2, 2), in0=C_(4, 2), in1=C_(6, 2), op=ALU.add), TM, TM)
                    for (o, src) in ((8, 0), (9, 2)):
                        a_, b__ = (C_(src), C_(src + 1)) if d == 0 else (C_(src + 1), C_(src))
                        V(lambda e: e.tensor_tensor(out=C_(4), in0=b__, in1=a_, op=ALU.subtract), TM, TM)
                        V(lambda e: e.scalar_tensor_tensor(out=C_(o), in0=C_(4), scalar=halfcol[:, 0:1], in1=a_, op0=ALU.mult, op1=ALU.add),
                          [b_tmp, b_half], TM)
                    V(lambda e: e.tensor_tensor(out=C_(4), in0=C_(9), in1=s8, op=ALU.mult), [b_tmp, b_par], TM)
                    V(lambda e: e.scalar_tensor_tensor(out=C_(10), in0=C_(8), scalar=c8, in1=C_(4), op0=ALU.mult, op1=ALU.subtract), [b_tmp, b_par], TM)
                    V(lambda e: e.tensor_tensor(out=C_(4), in0=C_(9), in1=c8, op=ALU.mult), [b_tmp, b_par], TM)
                    V(lambda e: e.scalar_tensor_tensor(out=C_(11), in0=C_(8), scalar=s8, in1=C_(4), op0=ALU.mult, op1=ALU.add), [b_tmp, b_par], TM)
                    ccol = 0 if d == 0 else 512
                    V(lambda e: e.tensor_copy(out=S5v[:, pdl, 0, ccol:ccol + 1], in_=C_(8)), TM, [b_S])
                    V(lambda e: e.tensor_copy(out=S5v[:, pdl, 1, ccol:ccol + 1], in_=C_(9)), TM, [b_S])
                    demod(544, 512)
                    scan(512, C_(10), C_(11))
                    if d == 0:
                        ore, oim = S5v[:, pdl, 0, 1:513], S5v[:, pdl, 1, 1:513]
                    else:
                        ore, oim = S5v[:, pdl, 0, 0:512][:, ::-1], S5v[:, pdl, 1, 0:512][:, ::-1]
                    V(lambda e: e.tensor_tensor(out=dm[:, 0, 0:512], in0=dm[:, 4, 0:512], in1=cosT[:, 0:512], op=ALU.mult), [b_tmp, b_tab], TM)
                    V(lambda e: e.tensor_tensor(out=dm[:, 1, 0:512], in0=dm[:, 5, 0:512], in1=sinT[:, 0:512], op=ALU.mult), [b_tmp, b_tab], TM)
                    V(lambda e: e.tensor_tensor(out=ore, in0=dm[:, 0, 0:512], in1=dm[:, 1, 0:512], op=ALU.subtract), TM, [b_S])
                    V(lambda e: e.tensor_tensor(out=dm[:, 0, 0:512], in0=dm[:, 5, 0:512], in1=cosT[:, 0:512], op=ALU.mult), [b_tmp, b_tab], TM)
                    V(lambda e: e.tensor_tensor(out=dm[:, 1, 0:512], in0=dm[:, 4, 0:512], in1=sinT[:, 0:512], op=ALU.mult), [b_tmp, b_tab], TM)
                    V(lambda e: e.tensor_tensor(out=oim, in0=dm[:, 0, 0:512], in1=dm[:, 1, 0:512], op=ALU.add), TM, [b_S])
            for s_ in range(8):
                py = ps[s_ % 2]
                bpy = bps[s_ % 2]
                mms = []
                for j in range(s_ + 1):
                    mms.append((KW4[:, 0, j, :], usT[:, blk, s_ - j:s_ - j + 4089:8], None, [b_KW, b_usT]))
                for j in range(8 - s_):
                    mms.append((KW4[:, 1, j, :], usT[:, blk, s_ + j:s_ + j + 4089:8], None, [b_KW, b_usT]))
                for q in range(4):
                    mms.append((NW6[:, 0, 0, s_, q, :], S5v[:, q, 0, 0:512], q, [b_NW, b_S]))
                    mms.append((NW6[:, 0, 1, s_, q, :], S5v[:, q, 1, 0:512], q, [b_NW, b_S]))
                    mms.append((NW6[:, 1, 0, 7 - s_, q, :], S5v[:, 4 + q, 0, 1:513], q, [b_NW, b_S]))
                    mms.append((NW6[:, 1, 1, 7 - s_, q, :], S5v[:, 4 + q, 1, 1:513], q, [b_NW, b_S]))
                for i_, (lh, rh, q, rd) in enumerate(mms):
                    first, last = i_ == 0, i_ == len(mms) - 1
                    if q is None:
                        F.op("pe", lambda e: e.matmul(py[:], lhsT=lh, rhs=rh, start=first, stop=last), reads=rd, writes=[bpy], signal=last)
                    else:
                        F.op("pe", lambda e: e.matmul(py[32 * q:32 * q + 32, :], lhsT=lh, rhs=rh, start=first, stop=last, tile_position=(0, 32 * q)),
                             reads=rd, writes=[bpy], signal=last)
                y = yt[:, s_ % 2, :]
                by = b_y[s_ % 2]
                A(lambda e: e.activation(out=y, in_=py[:], func=AF.Copy), [bpy], [by])
                if yT_dbg is not None:
                    F.dma("sp", lambda e: e.dma_start(out=yT_dbg[blk, s_], in_=y), reads=[by], writes=[Buf()])
                V(lambda e: e.tensor_tensor(out=sgt[:], in0=y, in1=y, op=ALU.mult), [by], TM)
                V(lambda e: e.tensor_scalar(out=sgt[:], in0=sgt[:], scalar1=0.044715, scalar2=1.0, op0=ALU.mult, op1=ALU.add), TM, TM)
                V(lambda e: e.tensor_tensor(out=sgt[:], in0=sgt[:], in1=y, op=ALU.mult), [by, b_tmp], TM)
                A(lambda e: e.activation(out=sgt[:], in_=sgt[:], func=AF.Sigmoid, scale=1.5957691216057308), TM, TM)
                V(lambda e: e.tensor_tensor(out=gt[:, s_:4096:8], in0=y, in1=sgt[:], op=ALU.mult), [by, b_tmp], [b_g])
            F.dma("sp", lambda e: e.dma_start(out=gT_d[blk], in_=gt[:]), reads=[b_g], writes=[b_gTd])
        F.barrier()
    es_us.close()
    with nc.sbuf_tensor("s5wg", [128, 4, 512], BF16) as wglu, nc.sbuf_tensor("s5bg", [128, 4], F32) as bglu, \
            nc.sbuf_tensor("s5gl", [128, 4, 512], BF16) as gl, nc.sbuf_tensor("s5sg2", [128, 512], F32) as sgt, \
            nc.sbuf_tensor("s5so", [128, 2, 512], BF16) as sot:
        b_par, b_gl, b_tmp = Buf("par2"), Buf("gl"), Buf("tmp2")

        def V(fn, reads, writes):
            F.op("dve", fn, reads=reads, writes=writes)

        def A(fn, reads, writes):
            F.op("act", fn, reads=reads, writes=writes)
        F.dma("sp", lambda e: e.dma_start(out=bglu[:], in_=I["b_gluT"]), writes=[b_par])
        F.dma("pool", lambda e: e.dma_start(out=wglu[:], in_=I["w_glu"].rearrange("(k p) n -> p k n", p=128)), writes=[b_par])
        b_so = [Buf("so0"), Buf("so1")]
        for n in range(8):
            F.dma("sp", lambda e: e.dma_start(out=gl[:], in_=gT_d[:, :, n * 512:(n + 1) * 512].rearrange("k p t -> p k t")), reads=[b_gTd], writes=[b_gl])
            for m in range(4):
                pg = ps[m % 2]
                bpg = bps[m % 2]
                for k in range(4):
                    F.op("pe", lambda e: e.matmul(pg[:], lhsT=wglu[:, k, m * 128:(m + 1) * 128], rhs=gl[:, k, :], start=(k == 0), stop=(k == 3)),
                         reads=[b_par, b_gl], writes=[bpg], signal=(k == 3))
                A(lambda e: e.activation(out=sgt[:], in_=pg[:], func=AF.Sigmoid, bias=bglu[:, m:m + 1], scale=1.0), [bpg, b_par, b_tmp], [b_tmp])
                so = sot[:, m % 2, :]
                V(lambda e: e.tensor_tensor(out=so, in0=gl[:, m, :], in1=sgt[:], op=ALU.mult), [b_gl, b_tmp], [b_so[m % 2]])
                F.dma("act", lambda e: e.dma_start(out=sT_d[m][:, n * 512:(n + 1) * 512], in_=so), reads=[b_so[m % 2]], writes=[b_sTd])
        F.barrier()
    if stop_after == "A3":
        F.barrier()
        return nc

    with ExitStack() as es:
        T = lambda nm, shp, dt: es.enter_context(nc.sbuf_tensor(nm, shp, dt))
        wf = [T("wpf%d" % i, [128, 4096], F32) for i in range(2)]
        wb = [T("wpb%d" % i, [128, 4096], BF16) for i in range(2)]
        bwf = [Buf("wpf%d" % i) for i in range(2)]
        bwb = [Buf("wpb%d" % i) for i in range(2)]
        jobs = []
        for k in range(16):
            jobs.append((I["w_out"][k * 128:(k + 1) * 128, :], 2048, [(Wo_d[:, :, k, :].rearrange("n p c -> p n c"), 0, 2048)], b_Wo))
        g1w = T("wp_g1", [128, D], F32)
        bg1w = Buf("wp_g1")
        F.dma("sp", lambda e: e.dma_start(out=g1w[:], in_=gates_d[2:3, :].partition_broadcast(128)), reads=[b_gates], writes=[bg1w])
        for ji, (src, n, dsts, bd) in enumerate(jobs):
            p = ji % 2
            F.dma("sp", lambda e: e.dma_start(out=wf[p][:, 0:n], in_=src), writes=[bwf[p]])
            eng = ("act", "dve", "pool")[ji % 3]
            if bd is b_Wo:
                F.op("dve", lambda e: e.tensor_tensor(out=wb[p][:, 0:n], in0=wf[p][:, 0:n], in1=g1w[:, 0:n], op=ALU.mult), reads=[bwf[p], bg1w], writes=[bwb[p]])
            elif eng == "act":
                F.op("act", lambda e: e.activation(out=wb[p][:, 0:n], in_=wf[p][:, 0:n], func=AF.Copy), reads=[bwf[p]], writes=[bwb[p]])
            else:
                F.op(eng, lambda e: e.tensor_copy(out=wb[p][:, 0:n], in_=wf[p][:, 0:n]), reads=[bwf[p]], writes=[bwb[p]])
            for (dst, a_, b__) in dsts:
                if len(dst.shape) == 3:
                    F.dma("act", lambda e: e.dma_start(out=dst, in_=wb[p][:, a_:b__].rearrange("p (oc c) -> p oc c", c=dst.shape[2])), reads=[bwb[p]], writes=[bd])
                else:
                    F.dma("act", lambda e: e.dma_start(out=dst, in_=wb[p][:, a_:b__]), reads=[bwb[p]], writes=[bd])
        F.barrier()

    xmid_d = scratch("xmid_d", [TOWN, D], F32)
    xn2_d = scratch("xn2_d", [TOWN, D], BF16)
    b_xmid, b_xn2 = Buf("xmid_d"), Buf("xn2_d")
    lg_all = sb([128, 32, 36], F32, "lg_all")
    b_lg = Buf("lg_all")
    with ExitStack() as es:
        T = lambda nm, shp, dt: es.enter_context(nc.sbuf_tensor(nm, shp, dt))
        vt = [T("a4v%d" % i, [128, D], F32) for i in range(4)]
        bvt = [Buf("a4v%d" % i) for i in range(4)]
        xn = [T("a4xn%d" % i, [128, D], BF16) for i in range(2)]
        bxn = [Buf("a4xn%d" % i) for i in range(2)]
        xf = T("a4xf", [128, D], F32)
        bxf = Buf("a4xf")
        hTbs = [T("a4hT%d" % i, [128, 16, 512], BF16) for i in range(2)]
        bhTs = [Buf("a4hT%d" % i) for i in range(2)]
        fTb, sTb = T("a4fT", [128, 8, 512], BF16), T("a4sT", [128, 4, 512], BF16)
        bfs = Buf("a4fs")
        wm = [T("a4wm%d" % i, [128, 44, 128], BF16) for i in range(2)]
        bwm = [Buf("a4wm%d" % i) for i in range(2)]
        mT = T("a4mT", [128, 16, 512], BF16)
        bmT = Buf("a4mT")
        wo = [T("a4wo%d" % i, [128, 16, 256], BF16) for i in range(2)]
        xn2b = T("a4xn2b", [128, D], BF16)
        bxn2b = Buf("xn2b")
        bwo = [Buf("a4wo%d" % i) for i in range(2)]
        sA, sB = T("a4sA", [128, 512], F32), T("a4sB", [128, 512], F32)
        bsA, bsB = Buf("sA"), Buf("sB")
        lngb, lnbb = T("a4lg", [128, D], F32), T("a4lb", [128, D], F32)
        bbc = Buf("a4bc")
        h2T = T("a4h2T", [128, 16, 128], F32)
        bh2T = Buf("a4h2T")
        wr = T("a4wr", [128, 16, 36], F32)
        brt = T("a4brt", [128, 36], F32)
        bwr = Buf("a4wr")
        st = [T("a4st%d" % i, [128, 24], F32) for i in range(2)]
        sm = [T("a4sm%d" % i, [128, 4], F32) for i in range(2)]
        bst = [Buf("a4st%d" % i) for i in range(2)]
        F.dma("sp", lambda e: e.dma_start(out=lngb[:], in_=I["lnrows"][0:1, :].partition_broadcast(128)), writes=[bbc])
        F.dma("sp", lambda e: e.dma_start(out=lnbb[:], in_=I["lnrows"][1:2, :].partition_broadcast(128)), writes=[bbc])
        F.dma("sp", lambda e: e.dma_start(out=wr[:], in_=I["w_rt"]), writes=[bwr])
        F.dma("sp", lambda e: e.dma_start(out=brt[:], in_=I["b_rt"].partition_broadcast(128)), writes=[bwr])
        stc = [0]
        xa = [T("a4xa%d" % i, [128, D], F32) for i in range(2)]
        bxa = [Buf("a4xa%d" % i) for i in range(2)]

        def stage_a0(tb):
            t0 = tb * 512
            F.dma("act", lambda e: e.dma_start(out=fTb[:], in_=fT_d[:, :, t0:t0 + 512].rearrange("k p t -> p k t")), reads=[b_fTd], writes=[bfs])
            F.dma("act", lambda e: e.dma_start(out=sTb[:], in_=sT_d[:, :, t0:t0 + 512].rearrange("k p t -> p k t")), reads=[b_sTd], writes=[bfs])

        def stage_a1(tb, i):
            r0 = tb * 512 + i * 128
            xx, bxx = xa[i % 2], bxa[i % 2]
            F.dma("sp", lambda e: e.dma_start(out=xx[:], in_=I["x"][r0:r0 + 128, :]), writes=[bxx])
            p = i % 2
            mv, rs, nmr = sma[p][:, 0:2], sma[p][:, 2:3], sma[p][:, 3:4]
            ln_stats(xx, bxx, sta[p], mv, rs, nmr, bsta[p])
            F.op("act", lambda e: e.activation(out=xn[p][:], in_=xx[:], func=AF.Identity, bias=nmr, scale=rs),
                 reads=[bxx, bsta[p]], writes=[bxn[p]])

        def stage_a2(tb, i):
            p = i % 2
            hTb, bhT = hTbs[tb % 2], bhTs[tb % 2]
            for hb in range(2):
                for jj in range(8):
                    j = hb * 8 + jj
                    F.op("pe", lambda e: e.transpose(out=psT[hb][:, jj * 128:(jj + 1) * 128], in_=xn[p][:, j * 128:(j + 1) * 128], identity=ident_bf[:]),
                         reads=[bxn[p], b_identbf], writes=[bpsT[hb]], signal=(jj == 7))
                for jj in range(8):
                    j = hb * 8 + jj
                    if jj % 2 == 0:
                        F.op("act", lambda e: e.activation(out=hTb[:, j, i * 128:(i + 1) * 128], in_=psT[hb][:, jj * 128:(jj + 1) * 128], func=AF.Identity,
                                                           bias=sh1[:, j:j + 1], scale=sc1p[:, j:j + 1]), reads=[bpsT[hb], b_modc], writes=[bhT])
                    else:
                        F.op("dve", lambda e: e.tensor_scalar(out=hTb[:, j, i * 128:(i + 1) * 128], in0=psT[hb][:, jj * 128:(jj + 1) * 128],
                                                              scalar1=sc1p[:, j:j + 1], scalar2=sh1[:, j:j + 1], op0=ALU.mult, op1=ALU.add),
                             reads=[bpsT[hb], b_modc], writes=[bhT])

        def stage_b(tb, oc):
            w = wm[oc % 2]
            bw = bwm[oc % 2]
            hTb, bhT = hTbs[tb % 2], bhTs[tb % 2]
            F.dma("sp", lambda e: e.dma_start(out=w[:], in_=Wmix_d[oc]), reads=[b_Wmix], writes=[bw])
            for k in range(16):
                F.op("pe", lambda e: e.matmul(ps[0][:], lhsT=w[:, k, :], rhs=hTb[:, k, :], start=(k == 0), stop=(k == 15)),
                     reads=[bw, bhT], writes=[bps[0]], signal=(k == 15))
            for k in range(16):
                F.op("pe", lambda e: e.matmul(ps[1][:], lhsT=w[:, 16 + k, :], rhs=hTb[:, k, :], start=(k == 0), stop=(k == 15)),
                     reads=[bw, bhT], writes=[bps[1]], signal=(k == 15))
            for k in range(8):
                F.op("pe", lambda e: e.matmul(ps[2][:], lhsT=w[:, 32 + k, :], rhs=fTb[:, k, :], start=(k == 0), stop=(k == 7)),
                     reads=[bw, bfs], writes=[bps[2]], signal=(k == 7))
            for k in range(4):
                F.op("pe", lambda e: e.matmul(ps[3][:], lhsT=w[:, 40 + k, :], rhs=sTb[:, k, :], start=(k == 0), stop=(k == 3)),
                     reads=[bw, bfs], writes=[bps[3]], signal=(k == 3))
            F.op("act", lambda e: e.activation(out=sA[:], in_=ps[0][:], func=AF.Sigmoid), reads=[bps[0]], writes=[bsA])
            F.op("act", lambda e: e.activation(out=sB[:], in_=ps[1][:], func=AF.Sigmoid), reads=[bps[1]], writes=[bsB])
            F.op("dve", lambda e: e.tensor_tensor(out=sA[:], in0=sA[:], in1=ps[2][:], op=ALU.mult), reads=[bsA, bps[2]], writes=[bsA])
            F.op("dve", lambda e: e.tensor_tensor(out=sB[:], in0=sB[:], in1=ps[3][:], op=ALU.mult), reads=[bsB, bps[3]], writes=[bsB])
            F.op("dve", lambda e: e.tensor_tensor(out=mT[:, oc, :], in0=sA[:], in1=sB[:], op=ALU.add), reads=[bsA, bsB], writes=[bmT])

        def stage_c(tb):
            t0 = tb * 512
            for i in range(4):
                r0 = t0 + i * 128
                F.dma("act", lambda e: e.dma_start(out=vt[i][:], in_=I["x"][r0:r0 + 128, :]), writes=[bvt[i]])
            for n in range(8):
                wv = wo[n % 2]
                bwv = bwo[n % 2]
                F.dma("sp", lambda e: e.dma_start(out=wv[:], in_=Wo_d[n]), reads=[b_Wo], writes=[bwv])
                for i in range(4):
                    pp = ps[4 + i % 2]
                    bpp = bps[4 + i % 2]
                    for mc in range(16):
                        F.op("pe", lambda e: e.matmul(pp[:, 0:256], lhsT=mT[:, mc, i * 128:(i + 1) * 128], rhs=wv[:, mc, :], start=(mc == 0), stop=(mc == 15)),
                             reads=[bmT, bwv], writes=[bpp], signal=(mc == 15))
                    F.op("dve", lambda e: e.scalar_tensor_tensor(out=vt[i][:, n * 256:(n + 1) * 256], in0=vt[i][:, n * 256:(n + 1) * 256], scalar=ALPHA,
                                                                 in1=pp[:, 0:256], op0=ALU.mult, op1=ALU.add), reads=[bvt[i], bpp], writes=[bvt[i]])

        def stage_d1(tb, i):
            r0 = tb * 512 + i * 128
            p = i % 2
            mv, rs, nmr = sm[p][:, 0:2], sm[p][:, 2:3], sm[p][:, 3:4]
            ln_stats(vt[i], bvt[i], st[p], mv, rs, nmr, bst[p])
            F.op("act", lambda e: e.activation(out=vt[i][:], in_=vt[i][:], func=AF.Identity, bias=nmr, scale=rs), reads=[bvt[i], bst[p]], writes=[bvt[i]])
            F.op("pool", lambda e: e.tensor_tensor(out=vt[i][:], in0=vt[i][:], in1=lngb[:], op=ALU.mult), reads=[bvt[i], bbc], writes=[bvt[i]])
            F.op("pool", lambda e: e.tensor_tensor(out=vt[i][:], in0=vt[i][:], in1=lnbb[:], op=ALU.add), reads=[bvt[i], bbc], writes=[bvt[i]])
            F.dma("pool", lambda e: e.dma_start(out=xmid_d[r0:r0 + 128, :], in_=vt[i][:]), reads=[bvt[i]], writes=[b_xmid])
            ln_stats(vt[i], bvt[i], st[p], mv, rs, nmr, bst[p])
            F.op("act", lambda e: e.activation(out=xf[:], in_=vt[i][:], func=AF.Identity, bias=nmr, scale=rs), reads=[bvt[i], bst[p]], writes=[bxf])
            F.op("pool", lambda e: e.tensor_copy(out=xn2b[:], in_=xf[:]), reads=[bxf], writes=[bxn2b])
            F.dma("pool", lambda e: e.dma_start(out=xn2_d[r0:r0 + 128, :], in_=xn2b[:]), reads=[bxn2b], writes=[b_xn2])

        def stage_d2(tb, i):
            tix = tb * 4 + i
            for g4 in range(4):
                pt = ps[4 + g4 % 2]
                bpt = bps[4 + g4 % 2]
                for jj in range(4):
                    j = g4 * 4 + jj
                    F.op("pe", lambda e: e.transpose(out=pt[:, jj * 128:(jj + 1) * 128], in_=xf[:, j * 128:(j + 1) * 128], identity=ident_f[:]),
                         reads=[bxf, b_identf], writes=[bpt], signal=(jj == 3))
                for jj in range(4):
                    j = g4 * 4 + jj
                    if jj % 2 == 0:
                        F.op("act", lambda e: e.activation(out=h2T[:, j, :], in_=pt[:, jj * 128:(jj + 1) * 128], func=AF.Identity,
                                                           bias=sh2[:, j:j + 1], scale=sc2p[:, j:j + 1]), reads=[bpt, b_modc], writes=[bh2T])
                    else:
                        F.op("dve", lambda e: e.tensor_scalar(out=h2T[:, j, :], in0=pt[:, jj * 128:(jj + 1) * 128],
                                                              scalar1=sc2p[:, j:j + 1], scalar2=sh2[:, j:j + 1], op0=ALU.mult, op1=ALU.add),
                             reads=[bpt, b_modc], writes=[bh2T])
            for k in range(16):
                F.op("pe", lambda e: e.matmul(ps[4][:, 0:36], lhsT=h2T[:, k, :], rhs=wr[:, k, :], start=(k == 0), stop=(k == 15)),
                     reads=[bh2T, bwr], writes=[bps[4]], signal=(k == 15))
            F.op("dve", lambda e: e.tensor_tensor(out=lg_all[:, tix, :], in0=ps[4][:, 0:36], in1=brt[:], op=ALU.add), reads=[bps[4], bwr], writes=[b_lg])

        sta = [T("a4sta%d" % i, [128, 24], F32) for i in range(2)]
        sma = [T("a4sma%d" % i, [128, 4], F32) for i in range(2)]
        bsta = [Buf("a4sta%d" % i) for i in range(2)]
        for i in range(4):
            stage_a1(0, i)
            stage_a2(0, i)
        for tb in range(9):
            if tb < 8:
                stage_a0(tb)
            for oc in range(16):
                i4, r4 = oc // 4, oc % 4
                if r4 == 0 and tb >= 1:
                    stage_d1(tb - 1, i4)
                if r4 == 1 and tb + 1 < 8:
                    stage_a1(tb + 1, i4)
                if tb < 8:
                    stage_b(tb, oc)
                if r4 == 2 and tb + 1 < 8:
                    stage_a2(tb + 1, i4)
                if r4 == 3 and tb >= 1:
                    stage_d2(tb - 1, i4)
            if tb < 8:
                stage_c(tb)
        if "lg_d" in dbg:
            ld = scratch("lg_d", [128, 32, 36], F32)
            F.dma("sp", lambda e: e.dma_start(out=ld, in_=lg_all[:]), reads=[b_lg], writes=[Buf()])
        F.barrier()
    if stop_after == "A4":
        F.barrier()
        return nc

    NBLK = 96
    NROW = NBLK * 128
    rowinfo_d = scratch("rowinfo_d", [NROW, 1], I32)
    roww_d = scratch("roww_d", [NROW, 1], F32)
    ybuf_d = scratch("ybuf_d", [2 * TOWN, D], BF16)
    b_rowinfo, b_roww, b_ybuf = Buf("rowinfo_d"), Buf("roww_d"), Buf("ybuf_d")
    idxw = sb([128, NBLK, 4], I32, "idxw")
    b_bexp = Buf("bexp")
    with ExitStack() as es:
        T = lambda nm, shp, dt: es.enter_context(nc.sbuf_tensor(nm, shp, dt))
        R_ = [Buf("rt")]
        V = lambda fn, rd=R_, wr=R_: F.op("dve", fn, reads=rd, writes=wr)
        A = lambda fn, rd=R_, wr=R_: F.op("act", fn, reads=rd, writes=wr)
        tri, ones_bf = T("r_tri", [128, 128], BF16), T("r_ones", [128, 128], BF16)
        thr, blkrow = T("r_thr", [128, 32], F32), T("r_blkrow", [128, NBLK], F32)
        tokid = T("r_tokid", [128, 64], I32)
        for t_, nm in ((tri, "tri"), (ones_bf, "ones_bf"), (thr, "thr"), (blkrow, "blkrow"), (tokid, "tokid")):
            F.dma("sp", lambda e: e.dma_start(out=t_[:], in_=I[nm]), writes=R_)
        gmax, gsum, gtop = T("r_gmax", [128, 32], F32), T("r_gsum", [128, 32], F32), T("r_gtop", [128, 32], F32)
        ohg, exg = T("r_ohg", [128, 32, 4], F32), T("r_exg", [128, 32, 4], F32)
        msk = T("r_msk", [128, 32, 32], F32)
        m8 = T("r_m8", [128, 32, 8], F32)
        oh = [T("r_oh%d" % k, [128, 32, 32], F32) for k in range(2)]
        cntb = T("r_cnt", [128, 32, 32], BF16)
        pf = T("r_pf", [128, 32, 32], F32)
        tot, nbv, pendb, pst_ = T("r_tot", [128, 32], F32), T("r_nb", [128, 32], F32), T("r_pend", [128, 32], F32), T("r_pst", [128, 32], F32)
        cmp_ = T("r_cmp", [128, NBLK, 32], F32)
        onesf = T("r_onesf", [128, 32], F32)
        wk = [T("r_w%d" % k, [128, 32], F32) for k in range(2)]
        dst = [T("r_dst%d" % k, [128, 32], F32) for k in range(2)]
        dsti = [T("r_dsti%d" % k, [128, 32], I32) for k in range(2)]
        bef, bef2 = T("r_bef", [128, NBLK], F32), T("r_bef2", [128, NBLK], F32)
        oobt = T("r_oob", [128, NBLK], I32)
        lgp, lep = lg_all[:, :, 0:4], lg_all[:, :, 4:36]
        V(lambda e: e.tensor_reduce(out=gmax[:], in_=lgp, axis=AX.X, op=ALU.max), [b_lg], R_)
        V(lambda e: e.tensor_tensor(out=exg[:], in0=lgp, in1=gmax[:].unsqueeze(2).to_broadcast([128, 32, 4]), op=ALU.subtract), [b_lg] + R_, R_)
        V(lambda e: e.tensor_single_scalar(out=ohg[:], in_=exg[:], scalar=0.0, op=ALU.is_ge))
        A(lambda e: e.activation(out=exg[:], in_=exg[:], func=AF.Exp))
        V(lambda e: e.tensor_reduce(out=gsum[:], in_=exg[:], axis=AX.X, op=ALU.add))
        V(lambda e: e.reciprocal(out=gtop[:], in_=gsum[:]))
        V(lambda e: e.tensor_scalar(out=ohg[:], in0=ohg[:], scalar1=-1.0, scalar2=1e30, op0=ALU.add, op1=ALU.mult))
        V(lambda e: e.tensor_tensor(out=msk[:].rearrange("p t (g x) -> p t g x", g=4), in0=lep.rearrange("p t (g x) -> p t g x", g=4),
                                    in1=ohg[:].unsqueeze(3).to_broadcast([128, 32, 4, 8]), op=ALU.add), [b_lg] + R_, R_)
        for j in range(32):
            V(lambda e: e.max(out=m8[:, j, :], in_=msk[:, j, :]))
        for k in range(2):
            V(lambda e: e.tensor_tensor(out=oh[k][:], in0=msk[:], in1=m8[:, :, k:k + 1].to_broadcast([128, 32, 32]), op=ALU.is_equal))
        V(lambda e: e.tensor_tensor(out=wk[1][:], in0=m8[:, :, 1], in1=m8[:, :, 0], op=ALU.subtract))
        A(lambda e: e.activation(out=wk[1][:], in_=wk[1][:], func=AF.Exp))
        V(lambda e: e.tensor_scalar(out=wk[1][:], in0=wk[1][:], scalar1=1.0, scalar2=None, op0=ALU.add))
        V(lambda e: e.reciprocal(out=wk[1][:], in_=wk[1][:]))
        V(lambda e: e.tensor_tensor(out=wk[0][:], in0=gtop[:], in1=wk[1][:], op=ALU.mult))
        V(lambda e: e.tensor_tensor(out=wk[1][:], in0=gtop[:], in1=wk[0][:], op=ALU.subtract))
        V(lambda e: e.tensor_tensor(out=cntb[:], in0=oh[0][:], in1=oh[1][:], op=ALU.add))
        for hb in range(2):
            pp = ps[hb]
            for jj in range(16):
                j = hb * 16 + jj
                n_mm = 1 + j
                F.op("pe", lambda e: e.matmul(pp[:, jj * 32:(jj + 1) * 32], lhsT=tri[:], rhs=cntb[:, j, :], start=True, stop=(n_mm == 1)),
                     reads=R_, writes=[bps[hb]], signal=(n_mm == 1 and jj == 15))
                for j2 in range(j):
                    F.op("pe", lambda e: e.matmul(pp[:, jj * 32:(jj + 1) * 32], lhsT=ones_bf[:], rhs=cntb[:, j2, :], start=False, stop=(j2 == j - 1)),
                         reads=R_, writes=[bps[hb]], signal=(j2 == j - 1 and jj == 15))
            V(lambda e: e.tensor_copy(out=pf[:, hb * 16:(hb + 1) * 16, :], in_=pp[:].rearrange("p (t x) -> p t x", t=16)), [bps[hb]] + R_, R_)
        for j in range(32):
            F.op("pe", lambda e: e.matmul(ps[2][:, 0:32], lhsT=ones_bf[:], rhs=cntb[:, j, :], start=(j == 0), stop=(j == 31)),
                 reads=R_, writes=[bps[2]], signal=(j == 31))
        V(lambda e: e.tensor_copy(out=tot[:], in_=ps[2][:, 0:32]), [bps[2]] + R_, R_)
        V(lambda e: e.tensor_tensor(out=cmp_[:, 0:32, :], in0=tot[:].unsqueeze(2).to_broadcast([128, 32, 32]),
                                    in1=thr[:].unsqueeze(1).to_broadcast([128, 32, 32]), op=ALU.is_gt))
        V(lambda e: e.tensor_reduce(out=nbv[:], in_=cmp_[:, 0:32, :], axis=AX.X, op=ALU.add))
        V(lambda e: e.memset(onesf[:], 1.0))
        V(lambda e: e.tensor_tensor_scan(out=pendb[:], data0=onesf[:], data1=nbv[:], initial=0.0, op0=ALU.mult, op1=ALU.add))
        V(lambda e: e.tensor_tensor(out=pst_[:], in0=pendb[:], in1=nbv[:], op=ALU.subtract))
        V(lambda e: e.tensor_scalar(out=pst_[:], in0=pst_[:], scalar1=128.0, scalar2=None, op0=ALU.mult))
        V(lambda e: e.tensor_tensor(out=pf[:], in0=pf[:], in1=pst_[:].unsqueeze(1).to_broadcast([128, 32, 32]), op=ALU.add))
        for k in range(2):
            V(lambda e: e.tensor_tensor(out=oh[k][:], in0=oh[k][:], in1=pf[:], op=ALU.mult))
            V(lambda e: e.tensor_reduce(out=dst[k][:], in_=oh[k][:], axis=AX.X, op=ALU.add))
            V(lambda e: e.tensor_copy(out=dsti[k][:], in_=dst[k][:]))
        V(lambda e: e.tensor_tensor(out=cmp_[:], in0=pendb[:].unsqueeze(1).to_broadcast([128, NBLK, 32]),
                                    in1=blkrow[:].unsqueeze(2).to_broadcast([128, NBLK, 32]), op=ALU.is_le))
        V(lambda e: e.tensor_reduce(out=bef[:], in_=cmp_[:], axis=AX.X, op=ALU.add))
        V(lambda e: e.tensor_scalar(out=bef[:], in0=bef[:], scalar1=31.0, scalar2=None, op0=ALU.min))
        V(lambda e: e.memset(bef2[:], 1.0))
        V(lambda e: e.tensor_tensor(out=bef2[:, 1:NBLK], in0=bef[:, 1:NBLK], in1=bef[:, 0:NBLK - 1], op=ALU.not_equal))
        pg4 = T("r_pg4", [128, 4], F32)
        idxf = T("r_idxf", [128, NBLK, 4], F32)
        F.dma("sp", lambda e: e.dma_start(out=pg4[:], in_=I["pg4"]), writes=R_)
        V(lambda e: e.tensor_scalar(out=bef[:], in0=bef[:], scalar1=-64.0, scalar2=None, op0=ALU.add))
        V(lambda e: e.tensor_tensor(out=bef[:], in0=bef[:], in1=bef2[:], op=ALU.mult))
        V(lambda e: e.tensor_scalar(out=bef[:], in0=bef[:], scalar1=64.0, scalar2=512.0, op0=ALU.add, op1=ALU.mult))
        V(lambda e: e.tensor_tensor(out=idxf[:], in0=bef[:].unsqueeze(2).to_broadcast([128, NBLK, 4]),
                                    in1=pg4[:].unsqueeze(1).to_broadcast([128, NBLK, 4]), op=ALU.add))
        V(lambda e: e.tensor_copy(out=idxw[:], in_=idxf[:]), R_, [b_bexp])
        V(lambda e: e.memset(oobt[:], 1 << 20))
        F.dma("sp", lambda e: e.dma_start(out=rowinfo_d.rearrange("(p a) o -> p (a o)", p=128), in_=oobt[:]), reads=R_, writes=[b_rowinfo])
        F.dma("sp", lambda e: e.dma_start(out=roww_d.rearrange("(p a) o -> p (a o)", p=128), in_=bef[:]), reads=R_, writes=[b_roww])
        for k in range(2):
            for j in range(32):
                F.dma("pool", lambda e: e.indirect_dma_start(out=rowinfo_d, out_offset=bass.IndirectOffsetOnAxis(ap=dsti[k][:, j:j + 1], axis=0),
                                                             in_=tokid[:, k * 32 + j:k * 32 + j + 1], in_offset=None), reads=R_, writes=[b_rowinfo])
                F.dma("pool", lambda e: e.indirect_dma_start(out=roww_d, out_offset=bass.IndirectOffsetOnAxis(ap=dsti[k][:, j:j + 1], axis=0),
                                                             in_=wk[k][:, j:j + 1], in_offset=None), reads=R_, writes=[b_roww])
        if "rt_d" in dbg:
            rd = scratch("rt_d", [128, 6, 32], F32)
            rt = T("r_dbg", [128, 6, 32], F32)
            V(lambda e: e.tensor_copy(out=rt[:, 0, :], in_=dst[0][:]))
            V(lambda e: e.tensor_copy(out=rt[:, 1, :], in_=dst[1][:]))
            V(lambda e: e.tensor_copy(out=rt[:, 2, :], in_=wk[0][:]))
            V(lambda e: e.tensor_copy(out=rt[:, 3, :], in_=wk[1][:]))
            V(lambda e: e.tensor_copy(out=rt[:, 4, :], in_=tot[:]))
            V(lambda e: e.tensor_copy(out=rt[:, 5, :], in_=bef[:, 0:32]))
            F.dma("sp", lambda e: e.dma_start(out=rd, in_=rt[:]), reads=R_, writes=[Buf()])
        F.barrier()
    if stop_after == "R":
        F.barrier()
        return nc

    with ExitStack() as es:
        T = lambda nm, shp, dt: es.enter_context(nc.sbuf_tensor(nm, shp, dt))
        W1, W3, W2 = T("m_w1", [128, 16, 1024], BF16), T("m_w3", [128, 16, 1024], BF16), T("m_w2", [128, 8, D], BF16)
        bW1 = [Buf("w1_%d" % i) for i in range(4)]
        bW3 = [Buf("w3_%d" % i) for i in range(4)]
        bW2 = [Buf("w2_%d" % i) for i in range(4)]
        X = [T("m_x%d" % i, [128, D], BF16) for i in range(2)]
        XT = [T("m_xt%d" % i, [128, 16, 128], BF16) for i in range(2)]
        sl = [T("m_sl%d" % i, [128, 512], F32) for i in range(2)]
        h1 = [T("m_h1%d" % i, [128, 1024], BF16) for i in range(2)]
        h1T = [T("m_h1T%d" % i, [128, 8, 128], BF16) for i in range(2)]
        ysb = [T("m_y%d" % i, [128, D], BF16) for i in range(2)]
        ri = [T("m_ri%d" % i, [128, 2], I32) for i in range(2)]
        rw = [T("m_rw%d" % i, [128, 1], F32) for i in range(2)]
        bX, bXT, bsl, bh1, bh1T, bys, bri = ([Buf("mx%d" % i) for i in range(2)], [Buf("mxt%d" % i) for i in range(2)], [Buf("msl%d" % i) for i in range(2)],
                                             [Buf("mh1%d" % i) for i in range(2)], [Buf("mh1T%d" % i) for i in range(2)], [Buf("my%d" % i) for i in range(2)],
                                             [Buf("mri%d" % i) for i in range(2)])
        w1v = I["w1"].rearrange("e (p g k) n -> (e p g) (k n)", p=128, g=4, k=4)
        w3v = I["w3"].rearrange("e (p g k) n -> (e p g) (k n)", p=128, g=4, k=4)
        w2v = I["w2"].rearrange("e (p g k) n -> (e p g) (k n)", p=128, g=4, k=2)
        sc2k, sh2k = T("m_sc2k", [128, 16], F32), T("m_sh2k", [128, 16], F32)
        b_m2k = Buf("m2k")
        F.dma("sp", lambda e: e.dma_start(out=sh2k[:], in_=gates_d[3].rearrange("(p k) -> p k", k=16)), reads=[b_gates], writes=[b_m2k])
        F.dma("sp", lambda e: e.dma_start(out=sc2k[:], in_=gates_d[4].rearrange("(p k) -> p k", k=16)), reads=[b_gates], writes=[b_m2k])

        rb_w = nc.gpsimd.alloc_register("rb_w")
        nc.gpsimd.reg_mov(rb_w, 32 * 512 - 1)
        rb_y = nc.gpsimd.alloc_register("rb_y")
        nc.gpsimd.reg_mov(rb_y, 2 * TOWN - 1)

        def wload(i, dst_ap, src, g, bw):
            F.dma("pool", lambda e: e.indirect_dma_start(out=dst_ap, out_offset=None, in_=src,
                                                         in_offset=bass.IndirectOffsetOnAxis(ap=idxw[:, i, g:g + 1], axis=0),
                                                         bounds_check=rb_w, oob_is_err=False), reads=[b_bexp], writes=[bw])
        def rows(i):
            p = i % 2
            F.dma("sp", lambda e: e.dma_start(out=ri[p][:, 0:1], in_=rowinfo_d[i * 128:(i + 1) * 128, :]), reads=[b_rowinfo], writes=[bri[p]])
            F.op("dve", lambda e: e.tensor_single_scalar(out=ri[p][:, 1:2], in_=ri[p][:, 0:1], scalar=4095, op=ALU.bitwise_and), reads=[bri[p]], writes=[bri[p]])
            F.dma("pool", lambda e: e.indirect_dma_start(out=X[p][:], out_offset=None, in_=xn2_d, in_offset=bass.IndirectOffsetOnAxis(ap=ri[p][:, 1:2], axis=0)),
                  reads=[bri[p], b_xn2], writes=[bX[p]])

        def T1(i):
            p = i % 2
            for hb in range(2):
                for jj in range(8):
                    j = hb * 8 + jj
                    F.op("pe", lambda e: e.transpose(out=psT[hb][:, jj * 128:(jj + 1) * 128], in_=X[p][:, j:D:16], identity=ident_bf[:]),
                         reads=[bX[p], b_identbf], writes=[bpsT[hb]], signal=(jj == 7))

        def E1(i):
            p = i % 2
            for hb in range(2):
                for jj in range(8):
                    j = hb * 8 + jj
                    if jj % 2 == 0:
                        F.op("act", lambda e: e.activation(out=XT[p][:, j, :], in_=psT[hb][:, jj * 128:(jj + 1) * 128], func=AF.Identity,
                                                           bias=sh2k[:, j:j + 1], scale=sc2k[:, j:j + 1]), reads=[bpsT[hb], b_m2k], writes=[bXT[p]])
                    else:
                        F.op("dve", lambda e: e.tensor_scalar(out=XT[p][:, j, :], in0=psT[hb][:, jj * 128:(jj + 1) * 128],
                                                              scalar1=sc2k[:, j:j + 1], scalar2=sh2k[:, j:j + 1], op0=ALU.mult, op1=ALU.add),
                             reads=[bpsT[hb], b_m2k], writes=[bXT[p]])

        def W13(i):
            for kg in range(4):
                wload(i, W1[:, 4 * kg:4 * kg + 4, :].rearrange("p k n -> p (k n)"), w1v, kg, bW1[kg])
                wload(i, W3[:, 4 * kg:4 * kg + 4, :].rearrange("p k n -> p (k n)"), w3v, kg, bW3[kg])

        def W2l(i):
            for kg in range(4):
                wload(i, W2[:, 2 * kg:2 * kg + 2, :].rearrange("p k n -> p (k n)"), w2v, kg, bW2[kg])

        def H(i):
            p = i % 2
            for k in range(16):
                for n in range(2):
                    F.op("pe", lambda e: e.matmul(ps[2 * n][:], lhsT=XT[p][:, k, :], rhs=W1[:, k, n * 512:(n + 1) * 512], start=(k == 0), stop=(k == 15)),
                         reads=[bXT[p], bW1[k // 4]], writes=[bps[2 * n]], signal=(k % 4 == 3))
                    F.op("pe", lambda e: e.matmul(ps[2 * n + 1][:], lhsT=XT[p][:, k, :], rhs=W3[:, k, n * 512:(n + 1) * 512], start=(k == 0), stop=(k == 15)),
                         reads=[bXT[p], bW3[k // 4]], writes=[bps[2 * n + 1]], signal=(k % 4 == 3))
            for n in range(2):
                F.op("act", lambda e: e.activation(out=sl[n][:], in_=ps[2 * n][:], func=AF.Silu), reads=[bps[2 * n]], writes=[bsl[n]])
                F.op("dve", lambda e: e.tensor_tensor(out=h1[p][:, n * 512:(n + 1) * 512], in0=sl[n][:], in1=ps[2 * n + 1][:], op=ALU.mult),
                     reads=[bsl[n], bps[2 * n + 1]], writes=[bh1[p]])

        ps5b = ps[5][:].bitcast(BF16)

        def T2(i):
            p = i % 2
            for jj in range(8):
                F.op("pe", lambda e: e.transpose(out=ps5b[:, jj * 128:(jj + 1) * 128], in_=h1[p][:, jj:1024:8], identity=ident_bf[:]),
                     reads=[bh1[p], b_identbf], writes=[bps[5]], signal=(jj == 7))
            F.op("act", lambda e: e.activation(out=h1T[p][:, 0:4, :], in_=ps5b[:, 0:512].rearrange("p (k t) -> p k t", k=4), func=AF.Copy),
                 reads=[bps[5]], writes=[bh1T[p]])
            F.op("dve", lambda e: e.tensor_copy(out=h1T[p][:, 4:8, :], in_=ps5b[:, 512:1024].rearrange("p (k t) -> p k t", k=4)),
                 reads=[bps[5]], writes=[bh1T[p]])

        def Y(i):
            p = i % 2
            for n in range(4):
                py = ps[4 + n % 2]
                for k in range(8):
                    F.op("pe", lambda e: e.matmul(py[:], lhsT=h1T[p][:, k, :], rhs=W2[:, k, n * 512:(n + 1) * 512], start=(k == 0), stop=(k == 7)),
                         reads=[bh1T[p], bW2[k // 2]], writes=[bps[4 + n % 2]], signal=(k == 7))
                if n % 2 == 0:
                    F.op("act", lambda e: e.activation(out=ysb[p][:, n * 512:(n + 1) * 512], in_=py[:], func=AF.Identity, scale=rw3[i % 3][:, 0:1]),
                         reads=[bps[4 + n % 2], bri3[i % 3]], writes=[bys[p]])
                else:
                    F.op("dve", lambda e: e.tensor_scalar(out=ysb[p][:, n * 512:(n + 1) * 512], in0=py[:], scalar1=rw3[i % 3][:, 0:1], scalar2=None, op0=ALU.mult),
                         reads=[bps[4 + n % 2], bri3[i % 3]], writes=[bys[p]])

        def SC(i):
            p = i % 2
            F.dma("pool", lambda e: e.indirect_dma_start(out=ybuf_d, out_offset=bass.IndirectOffsetOnAxis(ap=ri3[i % 3][:, 0:1], axis=0), in_=ysb[p][:], in_offset=None,
                                                         bounds_check=rb_y, oob_is_err=False), reads=[bys[p], bri3[i % 3]], writes=[b_ybuf])
        ri3 = [T("m_ri3%d" % k, [128, 1], I32) for k in range(3)]
        bri3 = [Buf("mri3%d" % k) for k in range(3)]
        rw3 = [T("m_rw3%d" % k, [128, 1], F32) for k in range(3)]

        def rows3(i):
            F.dma("sp", lambda e: e.dma_start(out=ri3[i % 3][:], in_=rowinfo_d[i * 128:(i + 1) * 128, :]), reads=[b_rowinfo], writes=[bri3[i % 3]])
            F.dma("sp", lambda e: e.dma_start(out=rw3[i % 3][:], in_=roww_d[i * 128:(i + 1) * 128, :]), reads=[b_roww], writes=[bri3[i % 3]])
        W13(0)
        W2l(0)
        rows(0)
        rows3(0)
        rows(1)
        rows3(1)
        T1(0)
        E1(0)
        for i in range(NBLK):
            if i + 2 < NBLK:
                rows3(i + 2)
                rows(i + 2)
            H(i)
            if i + 1 < NBLK:
                W13(i + 1)
                T1(i + 1)
            T2(i)
            if i + 1 < NBLK:
                E1(i + 1)
            Y(i)
            if i + 1 < NBLK:
                W2l(i + 1)
            SC(i)
        F.barrier()
    if stop_after == "MOE":
        F.barrier()
        return nc

    with ExitStack() as es:
        T = lambda nm, shp, dt: es.enter_context(nc.sbuf_tensor(nm, shp, dt))
        g2b, lg2, lb2 = T("f_g2", [128, D], F32), T("f_lg", [128, D], F32), T("f_lb", [128, D], F32)
        bbc = Buf("f_bc")
        F.dma("sp", lambda e: e.dma_start(out=g2b[:], in_=gates_d[5:6, :].partition_broadcast(128)), reads=[b_gates], writes=[bbc])
        F.dma("sp", lambda e: e.dma_start(out=lg2[:], in_=I["lnrows"][2:3, :].partition_broadcast(128)), writes=[bbc])
        F.dma("sp", lambda e: e.dma_start(out=lb2[:], in_=I["lnrows"][3:4, :].partition_broadcast(128)), writes=[bbc])
        NB_ = 4
        xm = [T("f_xm%d" % i, [128, D], F32) for i in range(NB_)]
        y0 = [T("f_y0%d" % i, [128, D], BF16) for i in range(NB_)]
        y1 = [T("f_y1%d" % i, [128, D], BF16) for i in range(NB_)]
        ys = [T("f_ys%d" % i, [128, D], F32) for i in range(2)]
        bys_ = [Buf("fys%d" % i) for i in range(2)]
        st = [T("f_st%d" % i, [128, 24], F32) for i in range(NB_)]
        sm = [T("f_sm%d" % i, [128, 4], F32) for i in range(NB_)]
        bxm, by0, by1, bst = ([Buf("fxm%d" % i) for i in range(NB_)], [Buf("fy0%d" % i) for i in range(NB_)], [Buf("fy1%d" % i) for i in range(NB_)],
                              [Buf("fst%d" % i) for i in range(NB_)])
        def f_loads(ti):
            p = ti % NB_
            r0 = ti * 128
            F.dma("sp", lambda e: e.dma_start(out=xm[p][:], in_=xmid_d[r0:r0 + 128, :]), reads=[b_xmid], writes=[bxm[p]])
            F.dma("sp", lambda e: e.dma_start(out=y0[p][:], in_=ybuf_d[r0:r0 + 128, :]), reads=[b_ybuf], writes=[by0[p]])
            F.dma("sp", lambda e: e.dma_start(out=y1[p][:], in_=ybuf_d[TOWN + r0:TOWN + r0 + 128, :]), reads=[b_ybuf], writes=[by1[p]])
        for ti in range(3):
            f_loads(ti)
        for ti in range(32):
            p = ti % NB_
            r0 = ti * 128
            if ti + 3 < 32:
                f_loads(ti + 3)
            q = ti % 2
            F.op("pool", lambda e: e.tensor_tensor(out=ys[q][:], in0=y0[p][:], in1=y1[p][:], op=ALU.add), reads=[by0[p], by1[p]], writes=[bys_[q]])
            F.op("pool", lambda e: e.tensor_tensor(out=ys[q][:], in0=ys[q][:], in1=g2b[:], op=ALU.mult), reads=[bys_[q], bbc], writes=[bys_[q]])
            F.op("dve", lambda e: e.scalar_tensor_tensor(out=xm[p][:], in0=xm[p][:], scalar=ALPHA, in1=ys[q][:], op0=ALU.mult, op1=ALU.add),
                 reads=[bxm[p], bys_[q]], writes=[bxm[p]])
            mv, rs, nmr = sm[p][:, 0:2], sm[p][:, 2:3], sm[p][:, 3:4]
            ln_stats(xm[p], bxm[p], st[p], mv, rs, nmr, bst[p])
            F.op("act", lambda e: e.activation(out=xm[p][:], in_=xm[p][:], func=AF.Identity, bias=nmr, scale=rs), reads=[bxm[p], bst[p]], writes=[bxm[p]])
            F.op("dve", lambda e: e.tensor_tensor(out=xm[p][:], in0=xm[p][:], in1=lg2[:], op=ALU.mult), reads=[bxm[p], bbc], writes=[bxm[p]])
            F.op("dve", lambda e: e.tensor_tensor(out=xm[p][:], in0=xm[p][:], in1=lb2[:], op=ALU.add), reads=[bxm[p], bbc], writes=[bxm[p]])
            F.dma("act", lambda e: e.dma_start(out=out_ap[r0:r0 + 128, :], in_=xm[p][:]), reads=[bxm[p]], writes=[b_out])
        F.barrier()
    F.finish([b_out], "sp")
    return nc


def kernel(**inputs):
    inp = {k: np.asarray(v) for k, v in inputs.items()}
    nc = build()
    in_maps = []
    for core in range(8):
        b, half = core // 2, core % 2
        in_maps.append(host_layout(inp, b, half))
    res = run_bass_kernel_spmd(nc, in_maps, core_ids=list(range(8)))
    out = np.zeros((4, TALL, D), np.float32)
    for core in range(8):
        b, half = core // 2, core % 2
        out[b, half * TOWN:(half + 1) * TOWN] = res.results[core]["out"]
    return out
```

```python
import os
from contextlib import ExitStack
import numpy as np
import ml_dtypes
import concourse.bass as bass
import concourse.mybir as mybir
from concourse.bass_utils import run_bass_kernel_spmd

F32 = mybir.dt.float32
BF16 = mybir.dt.bfloat16
I32 = mybir.dt.int32
ALU = mybir.AluOpType
AF = mybir.ActivationFunctionType
AX = mybir.AxisListType
NPBF = ml_dtypes.bfloat16

D = 2048
TOWN = 4096
TALL = 8192
ALPHA = 2.0 ** 0.25
EPS = 1e-5
PI = float(np.pi)


class Buf:
    __slots__ = ("name", "w", "r")

    def __init__(self, name=""):
        self.name = name
        self.w = None
        self.r = []


class Flow:
    def __init__(self, nc, n_dma_sems=56):
        self.nc = nc
        self.engs = {"pe": nc.tensor, "dve": nc.vector, "act": nc.scalar, "pool": nc.gpsimd, "sp": nc.sync}
        self.sem = {k: nc.alloc_semaphore("s_" + k) for k in self.engs}
        self.cnt = {k: 0 for k in self.engs}
        self.waited = {k: {} for k in self.engs}
        self.pending = {k: [] for k in self.engs}
        self.dsems = [nc.alloc_semaphore("d%d" % i) for i in range(n_dma_sems)]
        self.dcnt = [0] * n_dma_sems
        self.dnext = 0
        self.semobj = {}
        for k, s in self.sem.items():
            self.semobj[id(s)] = s
        for s in self.dsems:
            self.semobj[id(s)] = s

    def _need(self, reads, writes):
        need = {}

        def add(p):
            if p is None:
                return
            s, v = p
            k = id(s)
            if need.get(k, 0) < v:
                need[k] = v
        for b in reads:
            add(b.w)
        for b in writes:
            add(b.w)
            for p in b.r:
                add(p)
        return need

    def _emit_waits(self, e, need):
        eng = self.engs[e]
        own = id(self.sem[e])
        for k, v in need.items():
            if k == own and e == "pe":
                continue
            if self.waited[e].get(k, 0) >= v:
                continue
            eng.wait_ge(self.semobj[k], v)
            self.waited[e][k] = v

    def _commit(self, reads, writes, tag):
        for b in writes:
            b.w = tag
            b.r = []
        for b in reads:
            if b.w is not tag:
                b.r.append(tag)
                if len(b.r) > 48:
                    best = {}
                    for (s, v) in b.r:
                        if best.get(id(s), (None, 0))[1] < v:
                            best[id(s)] = (s, v)
                    b.r = list(best.values())

    def op(self, e, fn, reads=(), writes=(), signal=True):
        reads = list(reads)
        writes = list(writes)
        need = self._need(reads, writes)
        self._emit_waits(e, need)
        ins = fn(self.engs[e])
        if signal:
            self.cnt[e] += 1
            ins.then_inc(self.sem[e], 1)
            tag = (self.sem[e], self.cnt[e])
            pr = [b for (b, w) in self.pending[e] if not w] + reads
            pw = [b for (b, w) in self.pending[e] if w] + writes
            self.pending[e] = []
            self._commit(pr, pw, tag)
        else:
            assert e == "pe"
            for b in reads:
                self.pending[e].append((b, False))
            for b in writes:
                self.pending[e].append((b, True))
        return ins

    def dma(self, q, fn, reads=(), writes=()):
        reads = list(reads)
        writes = list(writes)
        need = self._need(reads, writes)
        i = self.dnext
        self.dnext = (self.dnext + 1) % len(self.dsems)
        s = self.dsems[i]
        if self.dcnt[i] > 0:
            need[id(s)] = max(need.get(id(s), 0), self.dcnt[i])
        self._emit_waits(q, need)
        ins = fn(self.engs[q])
        self.dcnt[i] += 16
        ins.then_inc(s, 16)
        tag = (s, self.dcnt[i])
        self._commit(reads, writes, tag)
        return ins

    def barrier(self):
        need = {}
        for k, s in self.sem.items():
            if self.cnt[k] > 0:
                need[id(s)] = self.cnt[k]
        for i, s in enumerate(self.dsems):
            if self.dcnt[i] > 0:
                need[id(s)] = self.dcnt[i]
        for e in self.engs:
            assert not self.pending[e]
            self._emit_waits(e, dict(need))

    def finish(self, bufs, e="sp"):
        need = self._need(bufs, [])
        self._emit_waits(e, need)


def host_consts(half):
    c = {}
    c["ident_bf"] = np.eye(128, dtype=np.float32).astype(NPBF)
    c["ident_f"] = np.eye(128, dtype=np.float32)
    rv = np.arange(128)
    rt = (rv + 64 * half) % 128
    kt = np.arange(64) + 64 * half
    ang = 2 * np.pi * np.outer(rt, kt) / 128.0
    s = 1.0 / np.sqrt(128.0)
    c["rowdft"] = np.concatenate([np.cos(ang) * s, -np.sin(ang) * s], axis=1).astype(NPBF)
    ch = np.arange(256)
    ang = 2 * np.pi * np.outer(ch, ch) / 256.0
    C = np.cos(ang) / 16.0
    S = np.sin(ang) / 16.0
    c["chA"] = np.concatenate([C, -S], axis=1).reshape(2, 128, 512).transpose(1, 0, 2).copy().astype(NPBF)
    c["chB"] = np.concatenate([S, C], axis=1).reshape(2, 128, 512).transpose(1, 0, 2).copy().astype(NPBF)
    cc = np.arange(64)
    ang = 2 * np.pi * np.outer(cc, cc) / 64.0
    CC = np.zeros((128, 128), np.float32)
    CS = np.zeros((128, 128), np.float32)
    CC[0:64, 0:64] = np.cos(ang) / 8.0
    CS[0:64, 0:64] = np.sin(ang) / 8.0
    c["colC"] = CC.astype(NPBF)
    c["colS"] = CS.astype(NPBF)
    c["halfcol"] = np.full((128, 1), float(half), np.float32)
    c["jrow"] = np.tile(np.arange(9, dtype=np.float32)[None, :], (128, 1))
    c["crow"] = np.tile(np.arange(544, dtype=np.float32)[None, :], (128, 1))
    m = np.zeros((4, 32, 4, 32), np.float32)
    for q in range(4):
        m[q, :, q, :] = 1.0
    c["blkmask"] = m.reshape(128, 128)
    tri = (np.arange(128)[:, None] < np.arange(128)[None, :]).astype(np.float32)
    c["tri"] = tri.astype(NPBF)
    c["ones_bf"] = np.ones((128, 128), np.float32).astype(NPBF)
    c["thr"] = np.tile((128.0 * np.arange(32, dtype=np.float32))[None, :], (128, 1))
    c["blkrow"] = np.tile((1.0 * np.arange(96, dtype=np.float32))[None, :], (128, 1))
    c["pidx"] = np.arange(128, dtype=np.float32).reshape(128, 1)
    c["pg4"] = (4.0 * np.arange(128, dtype=np.float32)[:, None] + np.arange(4, dtype=np.float32)[None, :])
    tk = (np.arange(128)[:, None] + 128 * np.arange(32)[None, :]).astype(np.int32)
    c["tokid"] = np.concatenate([tk, tk + TOWN], axis=1).astype(np.int32)
    return c


CONST_SPECS = {
    "ident_bf": ([128, 128], BF16), "ident_f": ([128, 128], F32), "rowdft": ([128, 128], BF16),
    "chA": ([128, 2, 512], BF16), "chB": ([128, 2, 512], BF16), "colC": ([128, 128], BF16), "colS": ([128, 128], BF16),
    "halfcol": ([128, 1], F32), "jrow": ([128, 9], F32), "crow": ([128, 544], F32), "blkmask": ([128, 128], F32),
    "tri": ([128, 128], BF16), "ones_bf": ([128, 128], BF16), "thr": ([128, 32], F32), "blkrow": ([128, 96], F32),
    "pidx": ([128, 1], F32), "tokid": ([128, 64], I32), "pg4": ([128, 4], F32),
}


def host_layout(inp, b, half):
    m = {}
    xb = inp["x"][b]
    m["x"] = np.ascontiguousarray(np.concatenate([xb[half * TOWN:(half + 1) * TOWN], xb[(1 - half) * TOWN:(2 - half) * TOWN]], axis=0))
    m["ctx"] = np.ascontiguousarray(inp["ctx"][b])
    cv = np.stack([inp["c"][b], inp["c_ctx"]], axis=-1)
    m["cvT"] = np.ascontiguousarray(cv.reshape(16, 128, 2).transpose(1, 0, 2))
    m["w_ada"] = inp["w_ada"][0]
    m["b_adaT"] = np.ascontiguousarray(inp["b_ada"][0].reshape(96, 128).T)
    m["w_in"] = inp["w_in"][0]
    m["w_f_proj"] = inp["w_f_proj"][0]
    m["w_s_proj"] = inp["w_s_proj"][0]
    m["w_out"] = inp["w_out"][0]
    m["w_glu"] = inp["w_glu"][0]
    m["b_gluT"] = np.ascontiguousarray(inp["b_glu"][0].reshape(4, 128).T)
    m["lnrows"] = np.ascontiguousarray(np.stack([inp["ln1_g"][0], inp["ln1_b"][0], inp["ln2_g"][0], inp["ln2_b"][0]], axis=0))

    def pd(a):
        return np.ascontiguousarray(a.reshape(2, 16, 2, 64).transpose(2, 3, 0, 1).reshape(128, 32))
    m["lam_re"] = pd(inp["lam_re"][0])
    m["lam_im"] = pd(inp["lam_im"][0])
    m["log_dt"] = pd(np.broadcast_to(inp["log_dt"][0][:, :, None], (2, 32, 64)))

    def bpad(a):
        o = np.zeros((2, 64, 2, 16, 2, 16), np.float32)
        a6 = a.reshape(2, 16, 2, 64, 16)
        for g2 in range(2):
            o[g2, :, :, :, g2, :] = a6[:, :, g2].transpose(2, 0, 1, 3)
        return np.ascontiguousarray(o.reshape(128, 32, 32))
    m["b_re"] = bpad(inp["b_re"][0])
    m["b_im"] = bpad(inp["b_im"][0])

    def cpad(a):
        o = np.zeros((2, 64, 2, 16, 2, 16), np.float32)
        a6 = a.reshape(2, 16, 2, 16, 64)
        for g2 in range(2):
            o[g2, :, :, :, g2, :] = a6[:, :, g2].transpose(3, 0, 1, 2)
        return np.ascontiguousarray(o.reshape(128, 32, 32))
    m["c_re"] = cpad(inp["c_re"][0])
    m["c_im"] = cpad(inp["c_im"][0])
    m["d_skipT"] = np.ascontiguousarray(inp["d_skip"][0].reshape(4, 128).T)
    wr = np.concatenate([inp["w_group"][0], inp["w_expert"][0]], axis=1)
    m["w_rt"] = np.ascontiguousarray(wr.reshape(16, 128, 36).transpose(1, 0, 2))
    m["b_rt"] = np.ascontiguousarray(np.concatenate([inp["b_group"][0], inp["b_expert"][0]])[None, :])
    m["w1"] = inp["w1"][0]
    m["w3"] = inp["w3"][0]
    m["w2"] = inp["w2"][0]
    m.update(host_consts(half))
    return m


IN_SPECS = {
    "x": ([TALL, D], F32), "ctx": ([256, D], F32), "cvT": ([128, 16, 2], F32), "w_ada": ([D, 6 * D], F32),
    "b_adaT": ([128, 96], F32), "w_in": ([D, 5632], F32), "w_f_proj": ([1024, D], F32), "w_s_proj": ([512, D], F32),
    "w_out": ([D, D], F32), "w_glu": ([512, 512], F32), "b_gluT": ([128, 4], F32), "lnrows": ([4, D], F32),
    "lam_re": ([128, 32], F32), "lam_im": ([128, 32], F32), "log_dt": ([128, 32], F32),
    "b_re": ([128, 32, 32], F32), "b_im": ([128, 32, 32], F32), "c_re": ([128, 32, 32], F32), "c_im": ([128, 32, 32], F32),
    "d_skipT": ([128, 4], F32), "w_rt": ([128, 16, 36], F32), "b_rt": ([1, 36], F32),
    "w1": ([32, D, 1024], F32), "w3": ([32, D, 1024], F32), "w2": ([32, 1024, D], F32),
}


def build(stop_after=None, dbg=()):
    nc = bass.Bass("TRN2", target_bir_lowering=False)
    F = Flow(nc)
    I = {}
    early = stop_after in ("P0", "A1", "A2", "A3", "A4", "R")
    for k, (shp, dt) in list(IN_SPECS.items()) + list(CONST_SPECS.items()):
        if early and k in ("w1", "w2", "w3"):
            continue
        I[k] = nc.dram_tensor(k, shp, dt, kind="ExternalInput").ap()
    out_ap = nc.dram_tensor("out", [TOWN, D], F32, kind="ExternalOutput").ap()
    b_out = Buf("out")

    def scratch(name, shape, dt):
        kind = "ExternalOutput" if name in dbg else "Internal"
        return nc.dram_tensor(name, shape, dt, kind=kind).ap()

    _n = [0]

    def sb(shape, dt, name=None):
        _n[0] += 1
        return nc.alloc_sbuf_tensor(name or ("t%d" % _n[0]), shape, dt)

    psT = [nc.alloc_psum_tensor("psT%d" % i, [128, 1024], BF16) for i in range(2)]
    bpsT = [Buf("psT%d" % i) for i in range(2)]
    ps = [nc.alloc_psum_tensor("ps%d" % i, [128, 512], F32) for i in range(6)]
    bps = [Buf("ps%d" % i) for i in range(6)]

    def load_const(name, q="sp"):
        shp, dt = CONST_SPECS[name]
        t = sb(shp, dt, "c_" + name)
        b = Buf(name)
        F.dma(q, lambda e: e.dma_start(out=t[:], in_=I[name]), writes=[b])
        return t, b
    ident_bf, b_identbf = load_const("ident_bf")
    ident_f, b_identf = load_const("ident_f")
    halfcol, b_half = load_const("halfcol")
    epscol = sb([128, 1], F32, "epscol")
    b_eps = Buf("eps")
    F.op("dve", lambda e: e.memset(epscol[:], EPS), writes=[b_eps])

    modc = sb([128, 96], F32, "modc")
    modx = sb([128, 32], F32, "modx")
    b_modc, b_modx = Buf("modc"), Buf("modx")
    gates_d = scratch("gates_d", [6, D], F32)
    b_gates = Buf("gates_d")
    with nc.sbuf_tensor("cc", [128, 16, 2], F32) as cc, nc.sbuf_tensor("badaT", [128, 96], F32) as badaT, \
            nc.sbuf_tensor("gcol", [128, 6, 16], F32) as gcol, ExitStack() as es_p0:
        b_cc, b_bada = Buf("cc"), Buf("bada")
        F.dma("sp", lambda e: e.dma_start(out=cc[:], in_=I["cvT"]), writes=[b_cc])
        F.dma("sp", lambda e: e.dma_start(out=badaT[:], in_=I["b_adaT"]), writes=[b_bada])
        F.op("act", lambda e: e.activation(out=cc[:], in_=cc[:], func=AF.Silu), reads=[b_cc], writes=[b_cc])
        rowbuf = es_p0.enter_context(nc.sbuf_tensor("p0row", [2, 6 * D], F32))
        b_row = Buf("p0row")
        NWB = 6
        wts = [es_p0.enter_context(nc.sbuf_tensor("p0w%d" % i, [128, 2048], F32)) for i in range(NWB)]
        bwts = [Buf("p0w%d" % i) for i in range(NWB)]
        it = 0
        for cg in range(6):
            for k in range(16):
                wt, bw = wts[it % NWB], bwts[it % NWB]
                F.dma("sp" if it % 2 else "act", lambda e: e.dma_start(out=wt[:], in_=I["w_ada"][k * 128:(k + 1) * 128, cg * 2048:(cg + 1) * 2048]), writes=[bw])
                it += 1
                for nt in range(4):
                    F.op("pe", lambda e: e.matmul(ps[nt][0:2, :], lhsT=cc[:, k, :], rhs=wt[:, nt * 512:(nt + 1) * 512], start=(k == 0), stop=(k == 15)),
                         reads=[bw, b_cc], writes=[bps[nt]], signal=(nt == 3 or k == 15))
            for nt in range(4):
                c0 = cg * 2048 + nt * 512
                if nt % 2 == 0:
                    F.op("act", lambda e: e.activation(out=rowbuf[0:2, c0:c0 + 512], in_=ps[nt][0:2, :], func=AF.Copy), reads=[bps[nt]], writes=[b_row])
                else:
                    F.op("dve", lambda e: e.tensor_copy(out=rowbuf[0:2, c0:c0 + 512], in_=ps[nt][0:2, :]), reads=[bps[nt]], writes=[b_row])
        pacc = ps[4]
        pv = pacc[:, 0:192].rearrange("p (m t) -> p m t", t=2)
        for m in range(96):
            F.op("pe", lambda e: e.transpose(out=pv[:, m, :], in_=rowbuf[0:2, m * 128:(m + 1) * 128], identity=ident_f[0:2, 0:2]),
                 reads=[b_row, b_identf], writes=[bps[4]], signal=(m == 95))
        F.op("dve", lambda e: e.tensor_tensor(out=modc[:], in0=pv[:, :, 0], in1=badaT[:], op=ALU.add),
             reads=[bps[4], b_bada], writes=[b_modc])
        F.op("dve", lambda e: e.tensor_tensor(out=modx[:], in0=pv[:, 0:32, 1], in1=badaT[:, 0:32], op=ALU.add),
             reads=[bps[4], b_bada], writes=[b_modx])
        F.op("dve", lambda e: e.tensor_scalar(out=modc[:, 16:32], in0=modc[:, 16:32], scalar1=1.0, scalar2=None, op0=ALU.add),
             reads=[b_modc], writes=[b_modc])
        F.op("dve", lambda e: e.tensor_scalar(out=modc[:, 64:80], in0=modc[:, 64:80], scalar1=1.0, scalar2=None, op0=ALU.add),
             reads=[b_modc], writes=[b_modc])
        F.op("dve", lambda e: e.tensor_scalar(out=modx[:, 16:32], in0=modx[:, 16:32], scalar1=1.0, scalar2=None, op0=ALU.add),
             reads=[b_modx], writes=[b_modx])
        b_gcol = Buf("gcol")
        F.op("dve", lambda e: e.tensor_copy(out=gcol[:].rearrange("p g j -> p (g j)"), in_=modc[:]), reads=[b_modc], writes=[b_gcol])
        F.dma("sp", lambda e: e.dma_start(out=gates_d.rearrange("g (j p) -> p g j", p=128), in_=gcol[:],
                                          allow_slow_non_contiguous=True), reads=[b_gcol], writes=[b_gates])
        if "modc_d" in dbg:
            md = scratch("modc_d", [128, 96], F32)
            F.dma("sp", lambda e: e.dma_start(out=md, in_=modc[:]), reads=[b_modc], writes=[Buf()])
        F.barrier()
    sh1, sc1p, sh2, sc2p = modc[:, 0:16], modc[:, 16:32], modc[:, 48:64], modc[:, 64:80]
    shx, scxp = modx[:, 0:16], modx[:, 16:32]
    if stop_after == "P0":
        F.barrier()
        return nc

    Wmix_d = scratch("Wmix_d", [16, 128, 44, 128], BF16)
    Wo_d = scratch("Wo_d", [128, 16, D], BF16)
    b_Wmix, b_Wo = Buf("Wmix_d"), Buf("Wo_d")
    Wmix_v = Wmix_d.rearrange("oc p k c -> p oc k c")
    wp_jobs = []
    for k in range(16):
        wp_jobs.append((I["w_in"][k * 128:(k + 1) * 128, 1536:3584], Wmix_v[:, :, k, :]))
        wp_jobs.append((I["w_in"][k * 128:(k + 1) * 128, 3584:5632], Wmix_v[:, :, 16 + k, :]))
    for k in range(8):
        wp_jobs.append((I["w_f_proj"][k * 128:(k + 1) * 128, :], Wmix_v[:, :, 32 + k, :]))
    for k in range(4):
        wp_jobs.append((I["w_s_proj"][k * 128:(k + 1) * 128, :], Wmix_v[:, :, 40 + k, :]))
    F1d = scratch("F1d", [8, 128, 64, 128], BF16)
    b_F1d = Buf("F1d")
    es_us = ExitStack()
    usT = es_us.enter_context(nc.sbuf_tensor("usT", [128, 4, 8704], BF16))
    b_usT = Buf("usT")

    def ln_stats(xt, bx, st, mv, rs, nmr, bst):
        for i in range(4):
            F.op("dve", lambda e, i=i: e.bn_stats(out=st[:, 6 * i:6 * i + 6], in_=xt[:, i * 512:(i + 1) * 512]), reads=[bx], writes=[bst])
        F.op("dve", lambda e: e.bn_aggr(out=mv[:], in_=st[:]), reads=[bst], writes=[bst])
        F.op("act", lambda e: e.activation(out=rs[:], in_=mv[:, 1:2], func=AF.Sqrt, bias=epscol[:, 0:1], scale=1.0),
             reads=[bst, b_eps], writes=[bst])
        F.op("dve", lambda e: e.reciprocal(out=rs[:], in_=rs[:]), reads=[bst], writes=[bst])
        F.op("dve", lambda e: e.scalar_tensor_tensor(out=nmr[:], in0=mv[:, 0:1], scalar=-1.0, in1=rs[:], op0=ALU.mult, op1=ALU.mult),
             reads=[bst], writes=[bst])

    with nc.sbuf_tensor("Win", [128, 16, 1536], BF16) as Win, nc.sbuf_tensor("s_rowdft", [128, 128], BF16) as rowdft, \
            nc.sbuf_tensor("xt0", [128, D], F32) as xt0, nc.sbuf_tensor("xt1", [128, D], F32) as xt1, \
            nc.sbuf_tensor("xn0", [128, D], BF16) as xn0, nc.sbuf_tensor("xn1", [128, D], BF16) as xn1, \
            nc.sbuf_tensor("hT0", [128, 16, 128], BF16) as hT0, nc.sbuf_tensor("hT1", [128, 16, 128], BF16) as hT1, \
            nc.sbuf_tensor("uf0", [128, 1024], BF16) as uf0, nc.sbuf_tensor("uf1", [128, 1024], BF16) as uf1, \
            nc.sbuf_tensor("f10", [128, 8, 128], BF16) as f10, nc.sbuf_tensor("f11", [128, 8, 128], BF16) as f11, \
            nc.sbuf_tensor("st0", [128, 24], F32) as st0, nc.sbuf_tensor("st1", [128, 24], F32) as st1, \
            nc.sbuf_tensor("sm0", [128, 4], F32) as sm0, nc.sbuf_tensor("sm1", [128, 4], F32) as sm1:
        b_Win = Buf("Win")
        b_rd = Buf("rowdft")
        es_wp = ExitStack()
        F.dma("sp", lambda e: e.dma_start(out=rowdft[:], in_=I["rowdft"]), writes=[b_rd])
        for kg in range(4):
            F.dma("pool", lambda e, kg=kg: e.dma_start(
                out=Win[:, kg * 4:(kg + 1) * 4, :],
                in_=I["w_in"][kg * 512:(kg + 1) * 512, 0:1536].rearrange("(k p) n -> p k n", p=128)), writes=[b_Win])
        xts, xns, hTs, ufs, f1s, sts, sms = [xt0, xt1], [xn0, xn1], [hT0, hT1], [uf0, uf1], [f10, f11], [st0, st1], [sm0, sm1]
        bxt, bxn, bhT, buf_, bf1, bst = ([Buf("xt%d" % i) for i in range(2)], [Buf("xn%d" % i) for i in range(2)],
                                         [Buf("hT%d" % i) for i in range(2)], [Buf("uf%d" % i) for i in range(2)],
                                         [Buf("f1%d" % i) for i in range(2)], [Buf("st%d" % i) for i in range(2)])
        xcol = I["x"].rearrange("(r c) d -> c r d", c=64)
        ntile = 64 + 2

        def S1(ti):
            p = ti % 2
            xt, xn, st, sm = xts[p], xns[p], sts[p], sms[p]
            src = xcol[ti] if ti < 64 else I["ctx"][(ti - 64) * 128:(ti - 63) * 128, :]
            F.dma("sp", lambda e: e.dma_start(out=xt[:], in_=src), writes=[bxt[p]])
            mv, rs, nmr = sm[:, 0:2], sm[:, 2:3], sm[:, 3:4]
            ln_stats(xt, bxt[p], st, mv, rs, nmr, bst[p])
            F.op("act", lambda e: e.activation(out=xn[:], in_=xt[:], func=AF.Identity, bias=nmr, scale=rs),
                 reads=[bxt[p], bst[p]], writes=[bxn[p]])

        def S2pe(ti):
            p = ti % 2
            for hb in range(2):
                for jj in range(8):
                    j = hb * 8 + jj
                    F.op("pe", lambda e: e.transpose(out=psT[hb][:, jj * 128:(jj + 1) * 128], in_=xns[p][:, j * 128:(j + 1) * 128], identity=ident_bf[:]),
                         reads=[bxn[p], b_identbf], writes=[bpsT[hb]], signal=(jj == 7))

        def S2ev(ti):
            p = ti % 2
            hT = hTs[p]
            scp, shf, bmod = (sc1p, sh1, b_modc) if ti < 64 else (scxp, shx, b_modx)
            for hb in range(2):
                for jj in range(8):
                    j = hb * 8 + jj
                    if jj % 2 == 0:
                        F.op("act", lambda e: e.activation(out=hT[:, j, :], in_=psT[hb][:, jj * 128:(jj + 1) * 128], func=AF.Identity,
                                                           bias=shf[:, j:j + 1], scale=scp[:, j:j + 1]), reads=[bpsT[hb], bmod], writes=[bhT[p]])
                    else:
                        F.op("dve", lambda e: e.tensor_scalar(out=hT[:, j, :], in0=psT[hb][:, jj * 128:(jj + 1) * 128], scalar1=scp[:, j:j + 1],
                                                              scalar2=shf[:, j:j + 1], op0=ALU.mult, op1=ALU.add), reads=[bpsT[hb], bmod], writes=[bhT[p]])

        def S3pe(ti):
            p = ti % 2
            hT = hTs[p]
            for m in range(4):
                for k in range(16):
                    F.op("pe", lambda e: e.matmul(ps[2][:, m * 128:(m + 1) * 128], lhsT=Win[:, k, 1024 + m * 128:1024 + (m + 1) * 128], rhs=hT[:, k, :],
                                                  start=(k == 0), stop=(k == 15)), reads=[b_Win, bhT[p]], writes=[bps[2]], signal=(k == 15 and m == 3))
            if ti < 64:
                for n in range(2):
                    for k in range(16):
                        F.op("pe", lambda e: e.matmul(ps[n][:], lhsT=hT[:, k, :], rhs=Win[:, k, n * 512:(n + 1) * 512], start=(k == 0), stop=(k == 15)),
                             reads=[b_Win, bhT[p]], writes=[bps[n]], signal=(k == 15))

        def S3ev(ti):
            p = ti % 2
            uf = ufs[p]
            pc = ps[2][:].rearrange("p (m t) -> p m t", m=4)
            if ti < 64:
                c = ti
                F.op("act", lambda e: e.activation(out=usT[:, :, c:4096:64], in_=pc[:, :, 0:64], func=AF.Copy), reads=[bps[2]], writes=[b_usT])
                F.op("dve", lambda e: e.tensor_copy(out=usT[:, :, 4352 + c:8448:64], in_=pc[:, :, 64:128]), reads=[bps[2]], writes=[b_usT])
                F.op("act", lambda e: e.activation(out=uf[:, 0:512], in_=ps[0][:], func=AF.Copy), reads=[bps[0]], writes=[buf_[p]])
                F.op("dve", lambda e: e.tensor_copy(out=uf[:, 512:1024], in_=ps[1][:]), reads=[bps[1]], writes=[buf_[p]])
            else:
                t0 = (ti - 64) * 128
                F.op("act", lambda e: e.activation(out=usT[:, :, 4096 + t0:4096 + t0 + 128], in_=pc, func=AF.Copy), reads=[bps[2]], writes=[b_usT])
                F.op("dve", lambda e: e.tensor_copy(out=usT[:, :, 8448 + t0:8448 + t0 + 128], in_=pc), reads=[bps[2]], writes=[b_usT])

        def S4pe(ti):
            p = ti % 2
            for m in range(8):
                F.op("pe", lambda e: e.matmul(ps[3 + m // 4][:, (m % 4) * 128:(m % 4 + 1) * 128], lhsT=ufs[p][:, m * 128:(m + 1) * 128], rhs=rowdft[:],
                                              start=True, stop=True), reads=[buf_[p], b_rd], writes=[bps[3 + m // 4]], signal=(m % 4 == 3))

        def S4ev(ti):
            p = ti % 2
            f1 = f1s[p]
            F.op("act", lambda e: e.activation(out=f1[:, 0:4, :], in_=ps[3][:].rearrange("p (m t) -> p m t", m=4), func=AF.Copy), reads=[bps[3]], writes=[bf1[p]])
            F.op("dve", lambda e: e.tensor_copy(out=f1[:, 4:8, :], in_=ps[4][:].rearrange("p (m t) -> p m t", m=4)), reads=[bps[4]], writes=[bf1[p]])
            F.dma("sp", lambda e: e.dma_start(out=F1d.rearrange("m ch c k -> ch m c k")[:, :, ti, :], in_=f1[:]), reads=[bf1[p]], writes=[b_F1d])

        wpf = [es_wp.enter_context(nc.sbuf_tensor("wpA%d" % i, [128, 2048], F32)) for i in range(2)]
        wpb = [es_wp.enter_context(nc.sbuf_tensor("wpB%d" % i, [128, 2048], BF16)) for i in range(2)]
        bwpf = [Buf("wpA%d" % i) for i in range(2)]
        bwpb = [Buf("wpB%d" % i) for i in range(2)]

        def wprep_piece(ji):
            src, dst = wp_jobs[ji]
            p = ji % 2
            F.dma("pool", lambda e: e.dma_start(out=wpf[p][:], in_=src), writes=[bwpf[p]])
            F.op("pool", lambda e: e.tensor_copy(out=wpb[p][:], in_=wpf[p][:]), reads=[bwpf[p]], writes=[bwpb[p]])
            F.dma("pool", lambda e: e.dma_start(out=dst, in_=wpb[p][:].rearrange("p (oc c) -> p oc c", c=128)), reads=[bwpb[p]], writes=[b_Wmix])

        ok = lambda t: 0 <= t < ntile
        S1(0)
        S1(1)
        S2pe(0)
        S2ev(0)
        for n in range(0, ntile + 1):
            if ok(n + 2):
                S1(n + 2)
            if ok(n + 1):
                S2pe(n + 1)
            if ok(n):
                S3pe(n)
            if ok(n - 1) and n - 1 < 64:
                S4pe(n - 1)
            if ok(n + 1):
                S2ev(n + 1)
            if ok(n):
                S3ev(n)
            if ok(n - 1) and n - 1 < 64:
                S4ev(n - 1)
            if n < len(wp_jobs):
                wprep_piece(n)
        if "usT_d" in dbg:
            ud = scratch("usT_d", [128, 4, 8704], BF16)
            F.dma("sp", lambda e: e.dma_start(out=ud, in_=usT[:]), reads=[b_usT], writes=[Buf()])
        F.barrier()
        es_wp.close()
    if stop_after == "A1":
        F.barrier()
        return nc

    fT_d = scratch("fT_d", [8, 128, TOWN], BF16)
    b_fTd = Buf("fT_d")
    with nc.sbuf_tensor("s_chA", [128, 2, 512], BF16) as chA, nc.sbuf_tensor("s_chB", [128, 2, 512], BF16) as chB, \
            nc.sbuf_tensor("s_colC", [128, 128], BF16) as colC, nc.sbuf_tensor("s_colS", [128, 128], BF16) as colS, \
            nc.sbuf_tensor("F1s0", [128, 2, 64, 128], BF16) as F1s0, nc.sbuf_tensor("F1s1", [128, 2, 64, 128], BF16) as F1s1, \
            nc.sbuf_tensor("G0", [128, 512], BF16) as G0, nc.sbuf_tensor("G1", [128, 512], BF16) as G1, \
            nc.sbuf_tensor("fo0", [128, 2, TOWN], BF16) as fo0, nc.sbuf_tensor("fo1", [128, 2, TOWN], BF16) as fo1:
        b_tabs = Buf("dfttabs")
        for t, nm in ((chA, "chA"), (chB, "chB"), (colC, "colC"), (colS, "colS")):
            F.dma("sp", lambda e, t=t, nm=nm: e.dma_start(out=t[:], in_=I[nm]), writes=[b_tabs])
        F1ss, Gs, fos = [F1s0, F1s1], [G0, G1], [fo0, fo1]
        bF1s, bG, bfo = [Buf("F1s0"), Buf("F1s1")], [Buf("G0"), Buf("G1")], [Buf("fo0"), Buf("fo1")]
        for gi in range(4):
            F1s, fo = F1ss[gi % 2], fos[gi % 2]
            for kc in range(2):
                F.dma("sp" if kc == 0 else "act", lambda e, F1s=F1s, kc=kc, gi=gi: e.dma_start(out=F1s[:, kc, :, :], in_=F1d[2 * gi + kc]),
                      reads=[b_F1d], writes=[bF1s[gi % 2]])
            def step2(kr):
                pa, bpa = ps[kr % 2], bps[kr % 2]
                n = 0
                for kc in range(2):
                    for (off, tab) in ((0, chA), (64, chB)):
                        F.op("pe", lambda e: e.matmul(pa[0:64, :], lhsT=F1s[:, kc, :, off + kr], rhs=tab[:, kc, :], start=(n == 0), stop=(n == 3)),
                             reads=[bF1s[gi % 2], b_tabs], writes=[bpa], signal=(n == 3))
                        n += 1
                G = Gs[kr % 2]
                if kr % 2 == 0:
                    F.op("act", lambda e: e.activation(out=G[0:64, :], in_=pa[0:64, :], func=AF.Copy), reads=[bpa], writes=[bG[kr % 2]])
                else:
                    F.op("dve", lambda e: e.tensor_copy(out=G[0:64, :], in_=pa[0:64, :]), reads=[bpa], writes=[bG[kr % 2]])

            def step3(kr):
                G = Gs[kr % 2]
                g4 = kr // 4
                pb, bpb = ps[2 + g4 % 2], bps[2 + g4 % 2]
                for q in range(2):
                    c0 = (q * 4 + kr % 4) * 64
                    F.op("pe", lambda e: e.matmul(pb[:, c0:c0 + 64], lhsT=G[0:64, q * 128:(q + 1) * 128], rhs=colC[0:64, 0:64],
                                                  start=True, stop=False), reads=[bG[kr % 2], b_tabs], writes=[bpb], signal=False)
                    F.op("pe", lambda e: e.matmul(pb[:, c0:c0 + 64], lhsT=G[0:64, 256 + q * 128:256 + (q + 1) * 128], rhs=colS[0:64, 0:64],
                                                  start=False, stop=True), reads=[bG[kr % 2], b_tabs], writes=[bpb], signal=(q == 1))
                if kr % 4 == 3:
                    kr0 = kr - 3
                    if g4 % 2 == 0:
                        F.op("dve", lambda e: e.tensor_copy(out=fo[:, :, kr0 * 64:(kr0 + 4) * 64], in_=pb[:].rearrange("p (q t) -> p q t", q=2)),
                             reads=[bpb], writes=[bfo[gi % 2]])
                    else:
                        F.op("act", lambda e: e.activation(out=fo[:, :, kr0 * 64:(kr0 + 4) * 64], in_=pb[:].rearrange("p (q t) -> p q t", q=2), func=AF.Copy),
                             reads=[bpb], writes=[bfo[gi % 2]])
            step2(0)
            for kr in range(64):
                if kr + 1 < 64:
                    step2(kr + 1)
                step3(kr)
            F.dma("sp", lambda e, fo=fo, gi=gi: e.dma_start(out=fT_d[2 * gi:2 * gi + 2].rearrange("q p t -> p q t"), in_=fo[:]),
                  reads=[bfo[gi % 2]], writes=[b_fTd])
        F.barrier()
    if stop_after == "A2":
        F.barrier()
        return nc

    sT_d = scratch("sT_d", [4, 128, TOWN], BF16)
    b_sTd = Buf("sT_d")
    gT_d = scratch("gT_d", [4, 128, TOWN], BF16)
    b_gTd = Buf("gT_d")
    yT_dbg = scratch("yT_d", [4, 8, 128, 512], F32) if "yT_d" in dbg else None
    PIS = 3.141592
    with ExitStack() as es_a3:
        par = es_a3.enter_context(nc.sbuf_tensor("s5par", [128, 32 * 12], F32))
        pw = es_a3.enter_context(nc.sbuf_tensor("s5pw", [128, 32 * 9 * 8], F32))
        pwi = es_a3.enter_context(nc.sbuf_tensor("s5pi", [128, 32 * 9], I32))
        Bt = es_a3.enter_context(nc.sbuf_tensor("s5b", [128, 2, 1024], F32))
        Ct = es_a3.enter_context(nc.sbuf_tensor("s5c", [128, 2, 1024], F32))
        Btmp = es_a3.enter_context(nc.sbuf_tensor("s5bt", [128, 2, 1024], F32))
        jrow = es_a3.enter_context(nc.sbuf_tensor("s5jrow", [128, 9], F32))
        crow = es_a3.enter_context(nc.sbuf_tensor("s5crow", [128, 544], F32))
        blkmask = es_a3.enter_context(nc.sbuf_tensor("s5mask", [128, 128], F32))
        dsk = es_a3.enter_context(nc.sbuf_tensor("s5dsk", [128, 4], F32))
        Wt = es_a3.enter_context(nc.sbuf_tensor("s5W", [128, 2, 1056], F32))
        QWt = es_a3.enter_context(nc.sbuf_tensor("s5QW", [128, 2 * 8 * 2 * 128], BF16))
        KWt = es_a3.enter_context(nc.sbuf_tensor("s5KW", [128, 2 * 8 * 128], BF16))
        NWt = es_a3.enter_context(nc.sbuf_tensor("s5NW", [128, 2 * 2 * 1024], BF16))
        tab = es_a3.enter_context(nc.sbuf_tensor("s5tab", [128, 2, 544], F32))
        tf = es_a3.enter_context(nc.sbuf_tensor("s5tf", [128, 544], F32))
        ti_ = es_a3.enter_context(nc.sbuf_tensor("s5ti", [128, 544], I32))
        dm = es_a3.enter_context(nc.sbuf_tensor("s5d", [128, 6, 544], F32))
        cr_ = es_a3.enter_context(nc.sbuf_tensor("s5cr", [128, 16], F32))
        St = es_a3.enter_context(nc.sbuf_tensor("s5S", [128, 8 * 2 * 520], BF16))
        yt = es_a3.enter_context(nc.sbuf_tensor("s5y", [128, 2, 512], F32))
        gt = es_a3.enter_context(nc.sbuf_tensor("s5g", [128, TOWN], BF16))
        sgt = es_a3.enter_context(nc.sbuf_tensor("s5sg", [128, 512], F32))
        b_par, b_W, b_QW, b_KW, b_NW = Buf("par"), Buf("W"), Buf("QW"), Buf("KW"), Buf("NW")
        Zt = Wt
        dmflat = dm[:].rearrange("p a c -> p (a c)")
        b_Z = b_W
        b_tmp = b_W
        b_tab, b_S, b_y, b_g, b_gl = Buf("tab"), Buf("S"), [Buf("y0"), Buf("y1")], Buf("g"), Buf("gl")
        dq = ["sp", "act"]
        lre, lim, ldt = par[:, 0:32], par[:, 32:64], par[:, 64:96]
        F.dma("sp", lambda e: e.dma_start(out=lre, in_=I["lam_re"]), writes=[b_par])
        F.dma("sp", lambda e: e.dma_start(out=lim, in_=I["lam_im"]), writes=[b_par])
        F.dma("sp", lambda e: e.dma_start(out=ldt, in_=I["log_dt"]), writes=[b_par])
        F.dma("sp", lambda e: e.dma_start(out=Bt[:, 0, :], in_=I["b_re"].rearrange("p a b -> p (a b)")), writes=[b_par])
        F.dma("sp", lambda e: e.dma_start(out=Bt[:, 1, :], in_=I["b_im"].rearrange("p a b -> p (a b)")), writes=[b_par])
        F.dma("act", lambda e: e.dma_start(out=Ct[:, 0, :], in_=I["c_re"].rearrange("p a b -> p (a b)")), writes=[b_par])
        F.dma("act", lambda e: e.dma_start(out=Ct[:, 1, :], in_=I["c_im"].rearrange("p a b -> p (a b)")), writes=[b_par])
        F.dma("sp", lambda e: e.dma_start(out=jrow[:], in_=I["jrow"]), writes=[b_par])
        F.dma("sp", lambda e: e.dma_start(out=crow[:], in_=I["crow"]), writes=[b_par])
        F.dma("sp", lambda e: e.dma_start(out=blkmask[:], in_=I["blkmask"]), writes=[b_par])
        F.dma("sp", lambda e: e.dma_start(out=dsk[:], in_=I["d_skipT"]), writes=[b_par])

        def V(fn, reads, writes):
            F.op("dve", fn, reads=reads, writes=writes)

        def A(fn, reads, writes):
            F.op("act", fn, reads=reads, writes=writes)
        P_ = [b_par]
        dtc, aa, th = par[:, 96:128], par[:, 128:160], par[:, 160:192]
        A(lambda e: e.activation(out=dtc, in_=ldt, func=AF.Exp), P_, P_)
        V(lambda e: e.tensor_tensor(out=aa, in0=lre, in1=dtc, op=ALU.mult), P_, P_)
        V(lambda e: e.tensor_tensor(out=th, in0=lim, in1=dtc, op=ALU.mult), P_, P_)
        def p3(i):
            return pw[:, i * 288:(i + 1) * 288].rearrange("p (a j) -> p a j", j=9)
        MAG, ANG, RED, SIN, COS, PR, PI_, TMP = [p3(i) for i in range(8)]
        pwi3 = pwi[:].rearrange("p (a j) -> p a j", j=9)
        jb = jrow[:].unsqueeze(1).to_broadcast([128, 32, 9])
        V(lambda e: e.tensor_tensor(out=MAG, in0=aa.unsqueeze(2).to_broadcast([128, 32, 9]), in1=jb, op=ALU.mult), P_, P_)
        A(lambda e: e.activation(out=MAG, in_=MAG, func=AF.Exp), P_, P_)
        V(lambda e: e.tensor_tensor(out=ANG, in0=th.unsqueeze(2).to_broadcast([128, 32, 9]), in1=jb, op=ALU.mult), P_, P_)

        def range_reduce(dst, src, tmpf, tmpi, shift, R, Wr):
            V(lambda e: e.tensor_scalar(out=tmpf, in0=src, scalar1=shift, scalar2=1.0 / (2 * PI), op0=ALU.add, op1=ALU.mult), R, Wr)
            V(lambda e: e.tensor_copy(out=tmpi, in_=tmpf), Wr, Wr)
            V(lambda e: e.tensor_copy(out=tmpf, in_=tmpi), Wr, Wr)
            V(lambda e: e.scalar_tensor_tensor(out=tmpf, in0=tmpf, scalar=-2 * PI, in1=src, op0=ALU.mult, op1=ALU.add), R + Wr, Wr)
            V(lambda e: e.tensor_scalar(out=dst, in0=tmpf, scalar1=shift, scalar2=-PIS, op0=ALU.add, op1=ALU.max), Wr, Wr)
            V(lambda e: e.tensor_scalar(out=dst, in0=dst, scalar1=PIS, scalar2=None, op0=ALU.min), Wr, Wr)
        range_reduce(RED, ANG, TMP, pwi3, 0.0, P_, P_)
        A(lambda e: e.activation(out=SIN, in_=RED, func=AF.Sin), P_, P_)
        range_reduce(COS, ANG, TMP, pwi3, PI / 2, P_, P_)
        A(lambda e: e.activation(out=COS, in_=COS, func=AF.Sin), P_, P_)
        V(lambda e: e.tensor_tensor(out=PR, in0=MAG, in1=COS, op=ALU.mult), P_, P_)
        V(lambda e: e.tensor_tensor(out=PI_, in0=MAG, in1=SIN, op=ALU.mult), P_, P_)
        nr, ni, den, cr, ci, t1c, t2c = [par[:, 192 + 32 * i:224 + 32 * i] for i in range(6)] + [par[:, 352:384]]
        V(lambda e: e.tensor_scalar(out=nr, in0=PR[:, :, 1], scalar1=-1.0, scalar2=None, op0=ALU.add), P_, P_)
        V(lambda e: e.tensor_copy(out=ni, in_=PI_[:, :, 1]), P_, P_)
        V(lambda e: e.tensor_tensor(out=den, in0=lre, in1=lre, op=ALU.mult), P_, P_)
        V(lambda e: e.tensor_tensor(out=t1c, in0=lim, in1=lim, op=ALU.mult), P_, P_)
        V(lambda e: e.tensor_tensor(out=den, in0=den, in1=t1c, op=ALU.add), P_, P_)
        V(lambda e: e.reciprocal(out=den, in_=den), P_, P_)
        V(lambda e: e.tensor_tensor(out=cr, in0=nr, in1=lre, op=ALU.mult), P_, P_)
        V(lambda e: e.tensor_tensor(out=t1c, in0=ni, in1=lim, op=ALU.mult), P_, P_)
        V(lambda e: e.tensor_tensor(out=cr, in0=cr, in1=t1c, op=ALU.add), P_, P_)
        V(lambda e: e.tensor_tensor(out=cr, in0=cr, in1=den, op=ALU.mult), P_, P_)
        V(lambda e: e.tensor_tensor(out=ci, in0=ni, in1=lre, op=ALU.mult), P_, P_)
        V(lambda e: e.tensor_tensor(out=t1c, in0=nr, in1=lim, op=ALU.mult), P_, P_)
        V(lambda e: e.tensor_tensor(out=ci, in0=ci, in1=t1c, op=ALU.subtract), P_, P_)
        V(lambda e: e.tensor_tensor(out=ci, in0=ci, in1=den, op=ALU.mult), P_, P_)
        B3 = lambda i: Bt[:, i, :].rearrange("p (a h) -> p a h", h=32)
        T3 = lambda i: Btmp[:, i, :].rearrange("p (a h) -> p a h", h=32)
        crb = cr.unsqueeze(2).to_broadcast([128, 32, 32])
        cib = ci.unsqueeze(2).to_broadcast([128, 32, 32])
        V(lambda e: e.tensor_tensor(out=T3(0), in0=B3(0), in1=crb, op=ALU.mult), P_, P_)
        V(lambda e: e.tensor_tensor(out=T3(1), in0=B3(1), in1=cib, op=ALU.mult), P_, P_)
        V(lambda e: e.tensor_tensor(out=T3(0), in0=T3(0), in1=T3(1), op=ALU.subtract), P_, P_)
        V(lambda e: e.tensor_tensor(out=T3(1), in0=B3(1), in1=crb, op=ALU.mult), P_, P_)
        V(lambda e: e.tensor_tensor(out=B3(1), in0=B3(0), in1=cib, op=ALU.mult), P_, P_)
        V(lambda e: e.tensor_tensor(out=T3(1), in0=T3(1), in1=B3(1), op=ALU.add), P_, P_)
        BB = Btmp
        CN = Bt[:, 0, :]
        V(lambda e: e.tensor_scalar(out=CN, in0=Ct[:, 1, :], scalar1=-1.0, scalar2=None, op0=ALU.mult), P_, P_)

        def bview(t2d, pd0):
            return t2d[:, pd0 * 32:(pd0 + 4) * 32].rearrange("p (q h) -> p q h", q=4).unsqueeze(1).to_broadcast([128, 8, 4, 32])

        def pview(T, pd0, j0):
            return T[:, pd0:pd0 + 4, j0:j0 + 8].rearrange("p q j -> p j q").unsqueeze(3).to_broadcast([128, 8, 4, 32])

        for blk in range(4):
            for d in range(2):
                pd0 = d * 16 + blk * 4
                W4 = lambda i: Wt[:, i, 0:1024].rearrange("p (j q h) -> p j q h", j=8, q=4)
                X4 = lambda i: dmflat[:, i * 1024:(i + 1) * 1024].rearrange("p (j q h) -> p j q h", j=8, q=4)
                RW = [b_par, b_W]
                V(lambda e: e.tensor_tensor(out=W4(0), in0=bview(BB[:, 0, :], pd0), in1=pview(PR, pd0, 0), op=ALU.mult), [b_par], [b_W])
                V(lambda e: e.tensor_tensor(out=X4(0), in0=bview(BB[:, 1, :], pd0), in1=pview(PI_, pd0, 0), op=ALU.mult), [b_par], [b_W])
                V(lambda e: e.tensor_tensor(out=W4(0), in0=W4(0), in1=X4(0), op=ALU.subtract), RW, [b_W])
                V(lambda e: e.tensor_tensor(out=W4(1), in0=bview(BB[:, 1, :], pd0), in1=pview(PR, pd0, 0), op=ALU.mult), [b_par], [b_W])
                V(lambda e: e.tensor_tensor(out=X4(1), in0=bview(BB[:, 0, :], pd0), in1=pview(PI_, pd0, 0), op=ALU.mult), [b_par], [b_W])
                V(lambda e: e.tensor_tensor(out=W4(1), in0=W4(1), in1=X4(1), op=ALU.add), RW, [b_W])
                for reim in range(2):
                    for jh in range(2):
                        pst = ps[(reim * 2 + jh) % 4]
                        bpst = bps[(reim * 2 + jh) % 4]
                        for jj in range(4):
                            j = jh * 4 + jj
                            F.op("pe", lambda e: e.transpose(out=pst[:, jj * 128:(jj + 1) * 128], in_=Wt[:, reim, j * 128:(j + 1) * 128], identity=ident_f[:]),
                                 reads=[b_W, b_identf], writes=[bpst], signal=(jj == 3))
                        dst = QWt[:].rearrange("p (d j r c) -> p d j r c", d=2, j=8, r=2)[:, d, jh * 4:(jh + 1) * 4, reim, :]
                        A(lambda e: e.activation(out=dst, in_=pst[:].rearrange("p (j c) -> p j c", j=4), func=AF.Copy), [bpst], [b_QW])
                for jh in range(2):
                    pst = ps[4 + jh]
                    bpst = bps[4 + jh]
                    for jj in range(4):
                        j = jh * 4 + jj
                        F.op("pe", lambda e: e.matmul(pst[:, jj * 128:(jj + 1) * 128], lhsT=Wt[:, 0, j * 128:(j + 1) * 128],
                                                      rhs=Ct[:, 0, pd0 * 32:(pd0 + 4) * 32], start=True, stop=False),
                             reads=[b_W, b_par], writes=[bpst], signal=False)
                        F.op("pe", lambda e: e.matmul(pst[:, jj * 128:(jj + 1) * 128], lhsT=Wt[:, 1, j * 128:(j + 1) * 128],
                                                      rhs=CN[:, pd0 * 32:(pd0 + 4) * 32], start=False, stop=True),
                             reads=[b_W, b_par], writes=[bpst], signal=(jj == 3))
                    dst = KWt[:].rearrange("p (d j c) -> p d j c", d=2, j=8)[:, d, jh * 4:(jh + 1) * 4, :]
                    V(lambda e: e.tensor_tensor(out=dst, in0=pst[:].rearrange("p (j c) -> p j c", j=4),
                                                in1=blkmask[:].unsqueeze(1).to_broadcast([128, 4, 128]), op=ALU.mult), [bpst, b_par], [b_KW])
                if d == 0:
                    k00 = KWt[:, 0:128]
                    V(lambda e: e.scalar_tensor_tensor(out=k00, in0=ident_f[:], scalar=dsk[:, blk:blk + 1], in1=k00, op0=ALU.mult, op1=ALU.add),
                      [b_identf, b_par, b_KW], [b_KW])
                N4 = lambda r: NWt[:].rearrange("p (d r x) -> p d r x", d=2, r=2)[:, d, r, :].rearrange("p (j q h) -> p j q h", j=8, q=4)
                V(lambda e: e.tensor_tensor(out=X4(0), in0=bview(Ct[:, 0, :], pd0), in1=pview(PR, pd0, 1), op=ALU.mult), [b_par, b_W], [b_W])
                V(lambda e: e.tensor_tensor(out=X4(1), in0=bview(CN, pd0), in1=pview(PI_, pd0, 1), op=ALU.mult), [b_par, b_W], [b_W])
                V(lambda e: e.tensor_tensor(out=N4(0), in0=X4(0), in1=X4(1), op=ALU.add), [b_W], [b_NW])
                V(lambda e: e.tensor_tensor(out=X4(0), in0=bview(Ct[:, 0, :], pd0), in1=pview(PI_, pd0, 1), op=ALU.mult), [b_par, b_W], [b_W])
                V(lambda e: e.tensor_tensor(out=X4(1), in0=bview(CN, pd0), in1=pview(PR, pd0, 1), op=ALU.mult), [b_par, b_W], [b_W])
                V(lambda e: e.tensor_tensor(out=N4(1), in0=X4(1), in1=X4(0), op=ALU.subtract), [b_W], [b_NW])
            QW5 = QWt[:].rearrange("p (d j r c) -> p d j r c", d=2, j=8, r=2)
            KW4 = KWt[:].rearrange("p (d j c) -> p d j c", d=2, j=8)
            NW6 = NWt[:].rearrange("p (d r j q h) -> p d r j q h", d=2, r=2, j=8, q=4)
            S5v = St[:].rearrange("p (a r c) -> p a r c", a=8, r=2)
            for d in range(2):
                for q in range(4):
                    pdl = d * 4 + q
                    pd = d * 16 + blk * 4 + q
                    rows = slice(32 * q, 32 * q + 32)
                    if d == 0:
                        segs = [(4096, 512, 0, False), (8192, 32, 512, False), (0, 512, 544, False)]
                    else:
                        segs = [(4352, 512, 32, True), (8448, 32, 0, True), (0, 512, 544, True)]
                    for reim in range(2):
                        for gi_, (base, L, zo, rev) in enumerate(segs):
                            pz = ps[(reim * 3 + gi_) % 6]
                            bpz = bps[(reim * 3 + gi_) % 6]
                            for s_ in range(8):
                                j = 7 - s_ if d == 0 else s_
                                F.op("pe", lambda e: e.matmul(pz[:, 0:L], lhsT=QW5[rows, d, j, reim, :],
                                                              rhs=usT[rows, blk, base + s_:base + s_ + 8 * (L - 1) + 1:8],
                                                              start=(s_ == 0), stop=(s_ == 7), tile_position=(32 * q, 0)),
                                     reads=[b_QW, b_usT], writes=[bpz], signal=(s_ == 7))
                            zs = Zt[:, reim, zo:zo + L]
                            if rev:
                                zs = zs[:, ::-1]
                            if reim == 0:
                                A(lambda e: e.activation(out=zs, in_=pz[:, 0:L], func=AF.Copy), [bpz], [b_Z])
                            else:
                                V(lambda e: e.tensor_copy(out=zs, in_=pz[:, 0:L]), [bpz], [b_Z])
                    r8, th8, c8, s8 = MAG[:, pd, 8:9], RED[:, pd, 8:9], COS[:, pd, 8:9], SIN[:, pd, 8:9]
                    TB = [b_tab]
                    V(lambda e: e.tensor_scalar(out=dm[:, 0, :], in0=crow[:], scalar1=th8, scalar2=None, op0=ALU.mult), [b_par, b_tmp], [b_tmp])
                    range_reduce(tab[:, 0, :], dm[:, 0, :], tf[:], ti_[:], 0.0, [b_tmp], [b_tab])
                    A(lambda e: e.activation(out=tab[:, 0, :], in_=tab[:, 0, :], func=AF.Sin), TB, TB)
                    range_reduce(tab[:, 1, :], dm[:, 0, :], tf[:], ti_[:], PI / 2, [b_tmp], [b_tab])
                    A(lambda e: e.activation(out=tab[:, 1, :], in_=tab[:, 1, :], func=AF.Sin), TB, TB)
                    sinT, cosT = tab[:, 0, :], tab[:, 1, :]
                    TM = [b_tmp]

                    def demod(zo, L):
                        zr, zi = Zt[:, 0, zo:zo + L], Zt[:, 1, zo:zo + L]
                        V(lambda e: e.tensor_tensor(out=dm[:, 0, 0:L], in0=zr, in1=cosT[:, 0:L], op=ALU.mult), [b_Z, b_tab, b_tmp], TM)
                        V(lambda e: e.tensor_tensor(out=dm[:, 1, 0:L], in0=zi, in1=sinT[:, 0:L], op=ALU.mult), [b_Z, b_tab, b_tmp], TM)
                        V(lambda e: e.tensor_tensor(out=dm[:, 2, 0:L], in0=dm[:, 0, 0:L], in1=dm[:, 1, 0:L], op=ALU.add), TM, TM)
                        V(lambda e: e.tensor_tensor(out=dm[:, 0, 0:L], in0=zi, in1=cosT[:, 0:L], op=ALU.mult), [b_Z, b_tab, b_tmp], TM)
                        V(lambda e: e.tensor_tensor(out=dm[:, 1, 0:L], in0=zr, in1=sinT[:, 0:L], op=ALU.mult), [b_Z, b_tab, b_tmp], TM)
                        V(lambda e: e.tensor_tensor(out=dm[:, 3, 0:L], in0=dm[:, 0, 0:L], in1=dm[:, 1, 0:L], op=ALU.subtract), TM, TM)

                    def scan(L, ire, iim):
                        V(lambda e: e.tensor_tensor_scan(out=dm[:, 4, 0:L], data0=r8.to_broadcast([128, L]), data1=dm[:, 2, 0:L],
                                                         initial=ire, op0=ALU.mult, op1=ALU.add), [b_par, b_tmp], TM)
                        V(lambda e: e.tensor_tensor_scan(out=dm[:, 5, 0:L], data0=r8.to_broadcast([128, L]), data1=dm[:, 3, 0:L],
                                                         initial=iim, op0=ALU.mult, op1=ALU.add), [b_par, b_tmp], TM)
                    demod(0, 544)
                    scan(544, 0.0, 0.0)
                    sre2, sim2 = dm[:, 4, 31:544:512], dm[:, 5, 31:544:512]
                    cs2, sn2 = tab[:, 1, 31:544:512], tab[:, 0, 31:544:512]
                    C_ = lambda i, n=1: cr_[:, i:i + n]
                    V(lambda e: e.tensor_tensor(out=C_(4, 2), in0=sre2, in1=cs2, op=ALU.mult), [b_tmp, b_tab], TM)
                    V(lambda e: e.tensor_tensor(out=C_(6, 2), in0=sim2, in1=sn2, op=ALU.mult), [b_tmp, b_tab], TM)
                    V(lambda e: e.tensor_tensor(out=C_(0, 2), in0=C_(4, 2), in1=C_(6, 2), op=ALU.subtract), TM, TM)
                    V(lambda e: e.tensor_tensor(out=C_(4, 2), in0=sim2, in1=cs2, op=ALU.mult), [b_tmp, b_tab], TM)
                    V(lambda e: e.tensor_tensor(out=C_(6, 2), in0=sre2, in1=sn2, op=ALU.mult), [b_tmp, b_tab], TM)
                    V(lambda e: e.tensor_tensor(out=C_(2, 2), in0=C_(4, 2), in1=C_(6, 2), op=ALU.add), TM, TM)
                    for (o, src) in ((8, 0), (9, 2)):
                        a_, b__ = (C_(src), C_(src + 1)) if d == 0 else (C_(src + 1), C_(src))
                        V(lambda e: e.tensor_tensor(out=C_(4), in0=b__, in1=a_, op=ALU.subtract), TM, TM)
                        V(lambda e: e.scalar_tensor_tensor(out=C_(o), in0=C_(4), scalar=halfcol[:, 0:1], in1=a_, op0=ALU.mult, op1=ALU.add),
                          [b_tmp, b_half], TM)
                    V(lambda e: e.tensor_tensor(out=C_(4), in0=C_(9), in1=s8, op=ALU.mult), [b_tmp, b_par], TM)
                    V(lambda e: e.scalar_tensor_tensor(out=C_(10), in0=C_(8), scalar=c8, in1=C_(4), op0=ALU.mult, op1=ALU.subtract), [b_tmp, b_par], TM)
                    V(lambda e: e.tensor_tensor(out=C_(4), in0=C_(9), in1=c8, op=ALU.mult), [b_tmp, b_par], TM)
                    V(lambda e: e.scalar_tensor_tensor(out=C_(11), in0=C_(8), scalar=s8, in1=C_(4), op0=ALU.mult, op1=ALU.add), [b_tmp, b_par], TM)
                    ccol = 0 if d == 0 else 512
                    V(lambda e: e.tensor_copy(out=S5v[:, pdl, 0, ccol:ccol + 1], in_=C_(8)), TM, [b_S])
                    V(lambda e: e.tensor_copy(out=S5v[:, pdl, 1, ccol:ccol + 1], in_=C_(9)), TM, [b_S])
                    demod(544, 512)
                    scan(512, C_(10), C_(11))
                    if d == 0:
                        ore, oim = S5v[:, pdl, 0, 1:513], S5v[:, pdl, 1, 1:513]
                    else:
                        ore, oim = S5v[:, pdl, 0, 0:512][:, ::-1], S5v[:, pdl, 1, 0:512][:, ::-1]
                    V(lambda e: e.tensor_tensor(out=dm[:, 0, 0:512], in0=dm[:, 4, 0:512], in1=cosT[:, 0:512], op=ALU.mult), [b_tmp, b_tab], TM)
                    V(lambda e: e.tensor_tensor(out=dm[:, 1, 0:512], in0=dm[:, 5, 0:512], in1=sinT[:, 0:512], op=ALU.mult), [b_tmp, b_tab], TM)
                    V(lambda e: e.tensor_tensor(out=ore, in0=dm[:, 0, 0:512], in1=dm[:, 1, 0:512], op=ALU.subtract), TM, [b_S])
                    V(lambda e: e.tensor_tensor(out=dm[:, 0, 0:512], in0=dm[:, 5, 0:512], in1=cosT[:, 0:512], op=ALU.mult), [b_tmp, b_tab], TM)
                    V(lambda e: e.tensor_tensor(out=dm[:, 1, 0:512], in0=dm[:, 4, 0:512], in1=sinT[:, 0:512], op=ALU.mult), [b_tmp, b_tab], TM)
                    V(lambda e: e.tensor_tensor(out=oim, in0=dm[:, 0, 0:512], in1=dm[:, 1, 0:512], op=ALU.add), TM, [b_S])
            for s_ in range(8):
                py = ps[s_ % 2]
                bpy = bps[s_ % 2]
                mms = []
                for j in range(s_ + 1):
                    mms.append((KW4[:, 0, j, :], usT[:, blk, s_ - j:s_ - j + 4089:8], None, [b_KW, b_usT]))
                for j in range(8 - s_):
                    mms.append((KW4[:, 1, j, :], usT[:, blk, s_ + j:s_ + j + 4089:8], None, [b_KW, b_usT]))
                for q in range(4):
                    mms.append((NW6[:, 0, 0, s_, q, :], S5v[:, q, 0, 0:512], q, [b_NW, b_S]))
                    mms.append((NW6[:, 0, 1, s_, q, :], S5v[:, q, 1, 0:512], q, [b_NW, b_S]))
                    mms.append((NW6[:, 1, 0, 7 - s_, q, :], S5v[:, 4 + q, 0, 1:513], q, [b_NW, b_S]))
                    mms.append((NW6[:, 1, 1, 7 - s_, q, :], S5v[:, 4 + q, 1, 1:513], q, [b_NW, b_S]))
                for i_, (lh, rh, q, rd) in enumerate(mms):
                    first, last = i_ == 0, i_ == len(mms) - 1
                    if q is None:
                        F.op("pe", lambda e: e.matmul(py[:], lhsT=lh, rhs=rh, start=first, stop=last), reads=rd, writes=[bpy], signal=last)
                    else:
                        F.op("pe", lambda e: e.matmul(py[32 * q:32 * q + 32, :], lhsT=lh, rhs=rh, start=first, stop=last, tile_position=(0, 32 * q)),
                             reads=rd, writes=[bpy], signal=last)
                y = yt[:, s_ % 2, :]
                by = b_y[s_ % 2]
                A(lambda e: e.activation(out=y, in_=py[:], func=AF.Copy), [bpy], [by])
                if yT_dbg is not None:
                    F.dma("sp", lambda e: e.dma_start(out=yT_dbg[blk, s_], in_=y), reads=[by], writes=[Buf()])
                V(lambda e: e.tensor_tensor(out=sgt[:], in0=y, in1=y, op=ALU.mult), [by], TM)
                V(lambda e: e.tensor_scalar(out=sgt[:], in0=sgt[:], scalar1=0.044715, scalar2=1.0, op0=ALU.mult, op1=ALU.add), TM, TM)
                V(lambda e: e.tensor_tensor(out=sgt[:], in0=sgt[:], in1=y, op=ALU.mult), [by, b_tmp], TM)
                A(lambda e: e.activation(out=sgt[:], in_=sgt[:], func=AF.Sigmoid, scale=1.5957691216057308), TM, TM)
                V(lambda e: e.tensor_tensor(out=gt[:, s_:4096:8], in0=y, in1=sgt[:], op=ALU.mult), [by, b_tmp], [b_g])
            F.dma("sp", lambda e: e.dma_start(out=gT_d[blk], in_=gt[:]), reads=[b_g], writes=[b_gTd])
        F.barrier()
    es_us.close()
    with nc.sbuf_tensor("s5wg", [128, 4, 512], BF16) as wglu, nc.sbuf_tensor("s5bg", [128, 4], F32) as bglu, \
            nc.sbuf_tensor("s5gl", [128, 4, 512], BF16) as gl, nc.sbuf_tensor("s5sg2", [128, 512], F32) as sgt, \
            nc.sbuf_tensor("s5so", [128, 2, 512], BF16) as sot:
        b_par, b_gl, b_tmp = Buf("par2"), Buf("gl"), Buf("tmp2")

        def V(fn, reads, writes):
            F.op("dve", fn, reads=reads, writes=writes)

        def A(fn, reads, writes):
            F.op("act", fn, reads=reads, writes=writes)
        F.dma("sp", lambda e: e.dma_start(out=bglu[:], in_=I["b_gluT"]), writes=[b_par])
        F.dma("pool", lambda e: e.dma_start(out=wglu[:], in_=I["w_glu"].rearrange("(k p) n -> p k n", p=128)), writes=[b_par])
        b_so = [Buf("so0"), Buf("so1")]
        for n in range(8):
            F.dma("sp", lambda e: e.dma_start(out=gl[:], in_=gT_d[:, :, n * 512:(n + 1) * 512].rearrange("k p t -> p k t")), reads=[b_gTd], writes=[b_gl])
            for m in range(4):
                pg = ps[m % 2]
                bpg = bps[m % 2]
                for k in range(4):
                    F.op("pe", lambda e: e.matmul(pg[:], lhsT=wglu[:, k, m * 128:(m + 1) * 128], rhs=gl[:, k, :], start=(k == 0), stop=(k == 3)),
                         reads=[b_par, b_gl], writes=[bpg], signal=(k == 3))
                A(lambda e: e.activation(out=sgt[:], in_=pg[:], func=AF.Sigmoid, bias=bglu[:, m:m + 1], scale=1.0), [bpg, b_par, b_tmp], [b_tmp])
                so = sot[:, m % 2, :]
                V(lambda e: e.tensor_tensor(out=so, in0=gl[:, m, :], in1=sgt[:], op=ALU.mult), [b_gl, b_tmp], [b_so[m % 2]])
                F.dma("act", lambda e: e.dma_start(out=sT_d[m][:, n * 512:(n + 1) * 512], in_=so), reads=[b_so[m % 2]], writes=[b_sTd])
        F.barrier()
    if stop_after == "A3":
        F.barrier()
        return nc

    with ExitStack() as es:
        T = lambda nm, shp, dt: es.enter_context(nc.sbuf_tensor(nm, shp, dt))
        wf = [T("wpf%d" % i, [128, 4096], F32) for i in range(2)]
        wb = [T("wpb%d" % i, [128, 4096], BF16) for i in range(2)]
        bwf = [Buf("wpf%d" % i) for i in range(2)]
        bwb = [Buf("wpb%d" % i) for i in range(2)]
        jobs = []
        for k in range(16):
            jobs.append((I["w_out"][k * 128:(k + 1) * 128, :], 2048, [(Wo_d[:, k, :], 0, 2048)], b_Wo))
        g1w = T("wp_g1", [128, D], F32)
        bg1w = Buf("wp_g1")
        F.dma("sp", lambda e: e.dma_start(out=g1w[:], in_=gates_d[2:3, :].partition_broadcast(128)), reads=[b_gates], writes=[bg1w])
        for ji, (src, n, dsts, bd) in enumerate(jobs):
            p = ji % 2
            F.dma("sp", lambda e: e.dma_start(out=wf[p][:, 0:n], in_=src), writes=[bwf[p]])
            eng = ("act", "dve", "pool")[ji % 3]
            if bd is b_Wo:
                F.op("dve", lambda e: e.tensor_tensor(out=wb[p][:, 0:n], in0=wf[p][:, 0:n], in1=g1w[:, 0:n], op=ALU.mult), reads=[bwf[p], bg1w], writes=[bwb[p]])
            elif eng == "act":
                F.op("act", lambda e: e.activation(out=wb[p][:, 0:n], in_=wf[p][:, 0:n], func=AF.Copy), reads=[bwf[p]], writes=[bwb[p]])
            else:
                F.op(eng, lambda e: e.tensor_copy(out=wb[p][:, 0:n], in_=wf[p][:, 0:n]), reads=[bwf[p]], writes=[bwb[p]])
            for (dst, a_, b__) in dsts:
                if len(dst.shape) == 3:
                    F.dma("act", lambda e: e.dma_start(out=dst, in_=wb[p][:, a_:b__].rearrange("p (oc c) -> p oc c", c=128)), reads=[bwb[p]], writes=[bd])
                else:
                    F.dma("act", lambda e: e.dma_start(out=dst, in_=wb[p][:, a_:b__]), reads=[bwb[p]], writes=[bd])
        F.barrier()

    xmid_d = scratch("xmid_d", [TOWN, D], F32)
    xn2_d = scratch("xn2_d", [TOWN, D], BF16)
    b_xmid, b_xn2 = Buf("xmid_d"), Buf("xn2_d")
    lg_all = sb([128, 32, 36], F32, "lg_all")
    b_lg = Buf("lg_all")
    with ExitStack() as es:
        T = lambda nm, shp, dt: es.enter_context(nc.sbuf_tensor(nm, shp, dt))
        vt = [T("a4v%d" % i, [128, D], F32) for i in range(4)]
        bvt = [Buf("a4v%d" % i) for i in range(4)]
        xn = [T("a4xn%d" % i, [128, D], BF16) for i in range(2)]
        bxn = [Buf("a4xn%d" % i) for i in range(2)]
        xf = T("a4xf", [128, D], F32)
        bxf = Buf("a4xf")
        hTbs = [T("a4hT%d" % i, [128, 16, 512], BF16) for i in range(2)]
        bhTs = [Buf("a4hT%d" % i) for i in range(2)]
        fTb, sTb = T("a4fT", [128, 8, 512], BF16), T("a4sT", [128, 4, 512], BF16)
        bfs = Buf("a4fs")
        wm = [T("a4wm%d" % i, [128, 44, 128], BF16) for i in range(2)]
        bwm = [Buf("a4wm%d" % i) for i in range(2)]
        mT = T("a4mT", [128, 16, 512], BF16)
        bmT = Buf("a4mT")
        wo = [T("a4wo%d" % i, [128, 16, 256], BF16) for i in range(2)]
        xn2b = T("a4xn2b", [128, D], BF16)
        bxn2b = Buf("xn2b")
        bwo = [Buf("a4wo%d" % i) for i in range(2)]
        sA, sB = T("a4sA", [128, 512], F32), T("a4sB", [128, 512], F32)
        bsA, bsB = Buf("sA"), Buf("sB")
        lngb, lnbb = T("a4lg", [128, D], F32), T("a4lb", [128, D], F32)
        bbc = Buf("a4bc")
        h2T = T("a4h2T", [128, 16, 128], F32)
        bh2T = Buf("a4h2T")
        wr = T("a4wr", [128, 16, 36], F32)
        brt = T("a4brt", [128, 36], F32)
        bwr = Buf("a4wr")
        st = [T("a4st%d" % i, [128, 24], F32) for i in range(2)]
        sm = [T("a4sm%d" % i, [128, 4], F32) for i in range(2)]
        bst = [Buf("a4st%d" % i) for i in range(2)]
        F.dma("sp", lambda e: e.dma_start(out=lngb[:], in_=I["lnrows"][0:1, :].partition_broadcast(128)), writes=[bbc])
        F.dma("sp", lambda e: e.dma_start(out=lnbb[:], in_=I["lnrows"][1:2, :].partition_broadcast(128)), writes=[bbc])
        F.dma("sp", lambda e: e.dma_start(out=wr[:], in_=I["w_rt"]), writes=[bwr])
        F.dma("sp", lambda e: e.dma_start(out=brt[:], in_=I["b_rt"].partition_broadcast(128)), writes=[bwr])
        stc = [0]
        xa = [T("a4xa%d" % i, [128, D], F32) for i in range(2)]
        bxa = [Buf("a4xa%d" % i) for i in range(2)]

        def stage_a0(tb):
            t0 = tb * 512
            F.dma("act", lambda e: e.dma_start(out=fTb[:], in_=fT_d[:, :, t0:t0 + 512].rearrange("k p t -> p k t")), reads=[b_fTd], writes=[bfs])
            F.dma("act", lambda e: e.dma_start(out=sTb[:], in_=sT_d[:, :, t0:t0 + 512].rearrange("k p t -> p k t")), reads=[b_sTd], writes=[bfs])

        def stage_a1(tb, i):
            r0 = tb * 512 + i * 128
            xx, bxx = xa[i % 2], bxa[i % 2]
            F.dma("sp", lambda e: e.dma_start(out=xx[:], in_=I["x"][r0:r0 + 128, :]), writes=[bxx])
            p = i % 2
            mv, rs, nmr = sma[p][:, 0:2], sma[p][:, 2:3], sma[p][:, 3:4]
            ln_stats(xx, bxx, sta[p], mv, rs, nmr, bsta[p])
            F.op("act", lambda e: e.activation(out=xn[p][:], in_=xx[:], func=AF.Identity, bias=nmr, scale=rs),
                 reads=[bxx, bsta[p]], writes=[bxn[p]])

        def stage_a2(tb, i):
            p = i % 2
            hTb, bhT = hTbs[tb % 2], bhTs[tb % 2]
            for hb in range(2):
                for jj in range(8):
                    j = hb * 8 + jj
                    F.op("pe", lambda e: e.transpose(out=psT[hb][:, jj * 128:(jj + 1) * 128], in_=xn[p][:, j * 128:(j + 1) * 128], identity=ident_bf[:]),
                         reads=[bxn[p], b_identbf], writes=[bpsT[hb]], signal=(jj == 7))
                for jj in range(8):
                    j = hb * 8 + jj
                    if jj % 2 == 0:
                        F.op("act", lambda e: e.activation(out=hTb[:, j, i * 128:(i + 1) * 128], in_=psT[hb][:, jj * 128:(jj + 1) * 128], func=AF.Identity,
                                                           bias=sh1[:, j:j + 1], scale=sc1p[:, j:j + 1]), reads=[bpsT[hb], b_modc], writes=[bhT])
                    else:
                        F.op("dve", lambda e: e.tensor_scalar(out=hTb[:, j, i * 128:(i + 1) * 128], in0=psT[hb][:, jj * 128:(jj + 1) * 128],
                                                              scalar1=sc1p[:, j:j + 1], scalar2=sh1[:, j:j + 1], op0=ALU.mult, op1=ALU.add),
                             reads=[bpsT[hb], b_modc], writes=[bhT])

        def stage_b(tb, oc):
            w = wm[oc % 2]
            bw = bwm[oc % 2]
            hTb, bhT = hTbs[tb % 2], bhTs[tb % 2]
            F.dma("sp", lambda e: e.dma_start(out=w[:], in_=Wmix_d[oc]), reads=[b_Wmix], writes=[bw])
            for k in range(16):
                F.op("pe", lambda e: e.matmul(ps[0][:], lhsT=w[:, k, :], rhs=hTb[:, k, :], start=(k == 0), stop=(k == 15)),
                     reads=[bw, bhT], writes=[bps[0]], signal=(k == 15))
            for k in range(16):
                F.op("pe", lambda e: e.matmul(ps[1][:], lhsT=w[:, 16 + k, :], rhs=hTb[:, k, :], start=(k == 0), stop=(k == 15)),
                     reads=[bw, bhT], writes=[bps[1]], signal=(k == 15))
            for k in range(8):
                F.op("pe", lambda e: e.matmul(ps[2][:], lhsT=w[:, 32 + k, :], rhs=fTb[:, k, :], start=(k == 0), stop=(k == 7)),
                     reads=[bw, bfs], writes=[bps[2]], signal=(k == 7))
            for k in range(4):
                F.op("pe", lambda e: e.matmul(ps[3][:], lhsT=w[:, 40 + k, :], rhs=sTb[:, k, :], start=(k == 0), stop=(k == 3)),
                     reads=[bw, bfs], writes=[bps[3]], signal=(k == 3))
            F.op("act", lambda e: e.activation(out=sA[:], in_=ps[0][:], func=AF.Sigmoid), reads=[bps[0]], writes=[bsA])
            F.op("act", lambda e: e.activation(out=sB[:], in_=ps[1][:], func=AF.Sigmoid), reads=[bps[1]], writes=[bsB])
            F.op("dve", lambda e: e.tensor_tensor(out=sA[:], in0=sA[:], in1=ps[2][:], op=ALU.mult), reads=[bsA, bps[2]], writes=[bsA])
            F.op("dve", lambda e: e.tensor_tensor(out=sB[:], in0=sB[:], in1=ps[3][:], op=ALU.mult), reads=[bsB, bps[3]], writes=[bsB])
            F.op("dve", lambda e: e.tensor_tensor(out=mT[:, oc, :], in0=sA[:], in1=sB[:], op=ALU.add), reads=[bsA, bsB], writes=[bmT])

        def stage_c(tb):
            t0 = tb * 512
            for i in range(4):
                r0 = t0 + i * 128
                F.dma("act", lambda e: e.dma_start(out=vt[i][:], in_=I["x"][r0:r0 + 128, :]), writes=[bvt[i]])
            for n in range(8):
                wv = wo[n % 2]
                bwv = bwo[n % 2]
                F.dma("sp", lambda e: e.dma_start(out=wv[:], in_=Wo_d[:, :, n * 256:(n + 1) * 256]), reads=[b_Wo], writes=[bwv])
                for i in range(4):
                    pp = ps[4 + i % 2]
                    bpp = bps[4 + i % 2]
                    for mc in range(16):
                        F.op("pe", lambda e: e.matmul(pp[:, 0:256], lhsT=mT[:, mc, i * 128:(i + 1) * 128], rhs=wv[:, mc, :], start=(mc == 0), stop=(mc == 15)),
                             reads=[bmT, bwv], writes=[bpp], signal=(mc == 15))
                    F.op("dve", lambda e: e.scalar_tensor_tensor(out=vt[i][:, n * 256:(n + 1) * 256], in0=vt[i][:, n * 256:(n + 1) * 256], scalar=ALPHA,
                                                                 in1=pp[:, 0:256], op0=ALU.mult, op1=ALU.add), reads=[bvt[i], bpp], writes=[bvt[i]])

        def stage_d1(tb, i):
            r0 = tb * 512 + i * 128
            p = i % 2
            mv, rs, nmr = sm[p][:, 0:2], sm[p][:, 2:3], sm[p][:, 3:4]
            ln_stats(vt[i], bvt[i], st[p], mv, rs, nmr, bst[p])
            F.op("act", lambda e: e.activation(out=vt[i][:], in_=vt[i][:], func=AF.Identity, bias=nmr, scale=rs), reads=[bvt[i], bst[p]], writes=[bvt[i]])
            F.op("pool", lambda e: e.tensor_tensor(out=vt[i][:], in0=vt[i][:], in1=lngb[:], op=ALU.mult), reads=[bvt[i], bbc], writes=[bvt[i]])
            F.op("pool", lambda e: e.tensor_tensor(out=vt[i][:], in0=vt[i][:], in1=lnbb[:], op=ALU.add), reads=[bvt[i], bbc], writes=[bvt[i]])
            F.dma("pool", lambda e: e.dma_start(out=xmid_d[r0:r0 + 128, :], in_=vt[i][:]), reads=[bvt[i]], writes=[b_xmid])
            ln_stats(vt[i], bvt[i], st[p], mv, rs, nmr, bst[p])
            F.op("act", lambda e: e.activation(out=xf[:], in_=vt[i][:], func=AF.Identity, bias=nmr, scale=rs), reads=[bvt[i], bst[p]], writes=[bxf])
            F.op("pool", lambda e: e.tensor_copy(out=xn2b[:], in_=xf[:]), reads=[bxf], writes=[bxn2b])
            F.dma("pool", lambda e: e.dma_start(out=xn2_d[r0:r0 + 128, :], in_=xn2b[:]), reads=[bxn2b], writes=[b_xn2])

        def stage_d2(tb, i):
            tix = tb * 4 + i
            for g4 in range(4):
                pt = ps[4 + g4 % 2]
                bpt = bps[4 + g4 % 2]
                for jj in range(4):
                    j = g4 * 4 + jj
                    F.op("pe", lambda e: e.transpose(out=pt[:, jj * 128:(jj + 1) * 128], in_=xf[:, j * 128:(j + 1) * 128], identity=ident_f[:]),
                         reads=[bxf, b_identf], writes=[bpt], signal=(jj == 3))
                for jj in range(4):
                    j = g4 * 4 + jj
                    if jj % 2 == 0:
                        F.op("act", lambda e: e.activation(out=h2T[:, j, :], in_=pt[:, jj * 128:(jj + 1) * 128], func=AF.Identity,
                                                           bias=sh2[:, j:j + 1], scale=sc2p[:, j:j + 1]), reads=[bpt, b_modc], writes=[bh2T])
                    else:
                        F.op("dve", lambda e: e.tensor_scalar(out=h2T[:, j, :], in0=pt[:, jj * 128:(jj + 1) * 128],
                                                              scalar1=sc2p[:, j:j + 1], scalar2=sh2[:, j:j + 1], op0=ALU.mult, op1=ALU.add),
                             reads=[bpt, b_modc], writes=[bh2T])
            for k in range(16):
                F.op("pe", lambda e: e.matmul(ps[4][:, 0:36], lhsT=h2T[:, k, :], rhs=wr[:, k, :], start=(k == 0), stop=(k == 15)),
                     reads=[bh2T, bwr], writes=[bps[4]], signal=(k == 15))
            F.op("dve", lambda e: e.tensor_tensor(out=lg_all[:, tix, :], in0=ps[4][:, 0:36], in1=brt[:], op=ALU.add), reads=[bps[4], bwr], writes=[b_lg])

        sta = [T("a4sta%d" % i, [128, 24], F32) for i in range(2)]
        sma = [T("a4sma%d" % i, [128, 4], F32) for i in range(2)]
        bsta = [Buf("a4sta%d" % i) for i in range(2)]
        for i in range(4):
            stage_a1(0, i)
            stage_a2(0, i)
        for tb in range(9):
            if tb < 8:
                stage_a0(tb)
            for oc in range(16):
                i4, r4 = oc // 4, oc % 4
                if r4 == 0 and tb >= 1:
                    stage_d1(tb - 1, i4)
                if r4 == 1 and tb + 1 < 8:
                    stage_a1(tb + 1, i4)
                if tb < 8:
                    stage_b(tb, oc)
                if r4 == 2 and tb + 1 < 8:
                    stage_a2(tb + 1, i4)
                if r4 == 3 and tb >= 1:
                    stage_d2(tb - 1, i4)
            if tb < 8:
                stage_c(tb)
        if "lg_d" in dbg:
            ld = scratch("lg_d", [128, 32, 36], F32)
            F.dma("sp", lambda e: e.dma_start(out=ld, in_=lg_all[:]), reads=[b_lg], writes=[Buf()])
        F.barrier()
    if stop_after == "A4":
        F.barrier()
        return nc

    NBLK = 96
    NROW = NBLK * 128
    rowinfo_d = scratch("rowinfo_d", [NROW, 1], I32)
    roww_d = scratch("roww_d", [NROW, 1], F32)
    ybuf_d = scratch("ybuf_d", [2 * TOWN, D], BF16)
    b_rowinfo, b_roww, b_ybuf = Buf("rowinfo_d"), Buf("roww_d"), Buf("ybuf_d")
    idxw = sb([128, NBLK, 4], I32, "idxw")
    b_bexp = Buf("bexp")
    with ExitStack() as es:
        T = lambda nm, shp, dt: es.enter_context(nc.sbuf_tensor(nm, shp, dt))
        R_ = [Buf("rt")]
        V = lambda fn, rd=R_, wr=R_: F.op("dve", fn, reads=rd, writes=wr)
        A = lambda fn, rd=R_, wr=R_: F.op("act", fn, reads=rd, writes=wr)
        tri, ones_bf = T("r_tri", [128, 128], BF16), T("r_ones", [128, 128], BF16)
        thr, blkrow = T("r_thr", [128, 32], F32), T("r_blkrow", [128, NBLK], F32)
        tokid = T("r_tokid", [128, 64], I32)
        for t_, nm in ((tri, "tri"), (ones_bf, "ones_bf"), (thr, "thr"), (blkrow, "blkrow"), (tokid, "tokid")):
            F.dma("sp", lambda e: e.dma_start(out=t_[:], in_=I[nm]), writes=R_)
        gmax, gsum, gtop = T("r_gmax", [128, 32], F32), T("r_gsum", [128, 32], F32), T("r_gtop", [128, 32], F32)
        ohg, exg = T("r_ohg", [128, 32, 4], F32), T("r_exg", [128, 32, 4], F32)
        msk = T("r_msk", [128, 32, 32], F32)
        m8 = T("r_m8", [128, 32, 8], F32)
        oh = [T("r_oh%d" % k, [128, 32, 32], F32) for k in range(2)]
        cntb = T("r_cnt", [128, 32, 32], BF16)
        pf = T("r_pf", [128, 32, 32], F32)
        tot, nbv, pendb, pst_ = T("r_tot", [128, 32], F32), T("r_nb", [128, 32], F32), T("r_pend", [128, 32], F32), T("r_pst", [128, 32], F32)
        cmp_ = T("r_cmp", [128, NBLK, 32], F32)
        onesf = T("r_onesf", [128, 32], F32)
        wk = [T("r_w%d" % k, [128, 32], F32) for k in range(2)]
        dst = [T("r_dst%d" % k, [128, 32], F32) for k in range(2)]
        dsti = [T("r_dsti%d" % k, [128, 32], I32) for k in range(2)]
        bef, bef2 = T("r_bef", [128, NBLK], F32), T("r_bef2", [128, NBLK], F32)
        oobt = T("r_oob", [128, NBLK], I32)
        lgp, lep = lg_all[:, :, 0:4], lg_all[:, :, 4:36]
        V(lambda e: e.tensor_reduce(out=gmax[:], in_=lgp, axis=AX.X, op=ALU.max), [b_lg], R_)
        V(lambda e: e.tensor_tensor(out=exg[:], in0=lgp, in1=gmax[:].unsqueeze(2).to_broadcast([128, 32, 4]), op=ALU.subtract), [b_lg] + R_, R_)
        V(lambda e: e.tensor_single_scalar(out=ohg[:], in_=exg[:], scalar=0.0, op=ALU.is_ge))
        A(lambda e: e.activation(out=exg[:], in_=exg[:], func=AF.Exp))
        V(lambda e: e.tensor_reduce(out=gsum[:], in_=exg[:], axis=AX.X, op=ALU.add))
        V(lambda e: e.reciprocal(out=gtop[:], in_=gsum[:]))
        V(lambda e: e.tensor_scalar(out=ohg[:], in0=ohg[:], scalar1=-1.0, scalar2=1e30, op0=ALU.add, op1=ALU.mult))
        V(lambda e: e.tensor_tensor(out=msk[:].rearrange("p t (g x) -> p t g x", g=4), in0=lep.rearrange("p t (g x) -> p t g x", g=4),
                                    in1=ohg[:].unsqueeze(3).to_broadcast([128, 32, 4, 8]), op=ALU.add), [b_lg] + R_, R_)
        for j in range(32):
            V(lambda e: e.max(out=m8[:, j, :], in_=msk[:, j, :]))
        for k in range(2):
            V(lambda e: e.tensor_tensor(out=oh[k][:], in0=msk[:], in1=m8[:, :, k:k + 1].to_broadcast([128, 32, 32]), op=ALU.is_equal))
        V(lambda e: e.tensor_tensor(out=wk[1][:], in0=m8[:, :, 1], in1=m8[:, :, 0], op=ALU.subtract))
        A(lambda e: e.activation(out=wk[1][:], in_=wk[1][:], func=AF.Exp))
        V(lambda e: e.tensor_scalar(out=wk[1][:], in0=wk[1][:], scalar1=1.0, scalar2=None, op0=ALU.add))
        V(lambda e: e.reciprocal(out=wk[1][:], in_=wk[1][:]))
        V(lambda e: e.tensor_tensor(out=wk[0][:], in0=gtop[:], in1=wk[1][:], op=ALU.mult))
        V(lambda e: e.tensor_tensor(out=wk[1][:], in0=gtop[:], in1=wk[0][:], op=ALU.subtract))
        V(lambda e: e.tensor_tensor(out=cntb[:], in0=oh[0][:], in1=oh[1][:], op=ALU.add))
        for hb in range(2):
            pp = ps[hb]
            for jj in range(16):
                j = hb * 16 + jj
                n_mm = 1 + j
                F.op("pe", lambda e: e.matmul(pp[:, jj * 32:(jj + 1) * 32], lhsT=tri[:], rhs=cntb[:, j, :], start=True, stop=(n_mm == 1)),
                     reads=R_, writes=[bps[hb]], signal=(n_mm == 1 and jj == 15))
                for j2 in range(j):
                    F.op("pe", lambda e: e.matmul(pp[:, jj * 32:(jj + 1) * 32], lhsT=ones_bf[:], rhs=cntb[:, j2, :], start=False, stop=(j2 == j - 1)),
                         reads=R_, writes=[bps[hb]], signal=(j2 == j - 1 and jj == 15))
            V(lambda e: e.tensor_copy(out=pf[:, hb * 16:(hb + 1) * 16, :], in_=pp[:].rearrange("p (t x) -> p t x", t=16)), [bps[hb]] + R_, R_)
        for j in range(32):
            F.op("pe", lambda e: e.matmul(ps[2][:, 0:32], lhsT=ones_bf[:], rhs=cntb[:, j, :], start=(j == 0), stop=(j == 31)),
                 reads=R_, writes=[bps[2]], signal=(j == 31))
        V(lambda e: e.tensor_copy(out=tot[:], in_=ps[2][:, 0:32]), [bps[2]] + R_, R_)
        V(lambda e: e.tensor_tensor(out=cmp_[:, 0:32, :], in0=tot[:].unsqueeze(2).to_broadcast([128, 32, 32]),
                                    in1=thr[:].unsqueeze(1).to_broadcast([128, 32, 32]), op=ALU.is_gt))
        V(lambda e: e.tensor_reduce(out=nbv[:], in_=cmp_[:, 0:32, :], axis=AX.X, op=ALU.add))
        V(lambda e: e.memset(onesf[:], 1.0))
        V(lambda e: e.tensor_tensor_scan(out=pendb[:], data0=onesf[:], data1=nbv[:], initial=0.0, op0=ALU.mult, op1=ALU.add))
        V(lambda e: e.tensor_tensor(out=pst_[:], in0=pendb[:], in1=nbv[:], op=ALU.subtract))
        V(lambda e: e.tensor_scalar(out=pst_[:], in0=pst_[:], scalar1=128.0, scalar2=None, op0=ALU.mult))
        V(lambda e: e.tensor_tensor(out=pf[:], in0=pf[:], in1=pst_[:].unsqueeze(1).to_broadcast([128, 32, 32]), op=ALU.add))
        for k in range(2):
            V(lambda e: e.tensor_tensor(out=oh[k][:], in0=oh[k][:], in1=pf[:], op=ALU.mult))
            V(lambda e: e.tensor_reduce(out=dst[k][:], in_=oh[k][:], axis=AX.X, op=ALU.add))
            V(lambda e: e.tensor_copy(out=dsti[k][:], in_=dst[k][:]))
        V(lambda e: e.tensor_tensor(out=cmp_[:], in0=pendb[:].unsqueeze(1).to_broadcast([128, NBLK, 32]),
                                    in1=blkrow[:].unsqueeze(2).to_broadcast([128, NBLK, 32]), op=ALU.is_le))
        V(lambda e: e.tensor_reduce(out=bef[:], in_=cmp_[:], axis=AX.X, op=ALU.add))
        V(lambda e: e.tensor_scalar(out=bef[:], in0=bef[:], scalar1=31.0, scalar2=None, op0=ALU.min))
        V(lambda e: e.memset(bef2[:], 1.0))
        V(lambda e: e.tensor_tensor(out=bef2[:, 1:NBLK], in0=bef[:, 1:NBLK], in1=bef[:, 0:NBLK - 1], op=ALU.not_equal))
        pg4 = T("r_pg4", [128, 4], F32)
        idxf = T("r_idxf", [128, NBLK, 4], F32)
        F.dma("sp", lambda e: e.dma_start(out=pg4[:], in_=I["pg4"]), writes=R_)
        V(lambda e: e.tensor_scalar(out=bef[:], in0=bef[:], scalar1=-64.0, scalar2=None, op0=ALU.add))
        V(lambda e: e.tensor_tensor(out=bef[:], in0=bef[:], in1=bef2[:], op=ALU.mult))
        V(lambda e: e.tensor_scalar(out=bef[:], in0=bef[:], scalar1=64.0, scalar2=512.0, op0=ALU.add, op1=ALU.mult))
        V(lambda e: e.tensor_tensor(out=idxf[:], in0=bef[:].unsqueeze(2).to_broadcast([128, NBLK, 4]),
                                    in1=pg4[:].unsqueeze(1).to_broadcast([128, NBLK, 4]), op=ALU.add))
        V(lambda e: e.tensor_copy(out=idxw[:], in_=idxf[:]), R_, [b_bexp])
        V(lambda e: e.memset(oobt[:], 1 << 20))
        F.dma("sp", lambda e: e.dma_start(out=rowinfo_d.rearrange("(p a) o -> p (a o)", p=128), in_=oobt[:]), reads=R_, writes=[b_rowinfo])
        F.dma("sp", lambda e: e.dma_start(out=roww_d.rearrange("(p a) o -> p (a o)", p=128), in_=bef[:]), reads=R_, writes=[b_roww])
        for k in range(2):
            for j in range(32):
                F.dma("pool", lambda e: e.indirect_dma_start(out=rowinfo_d, out_offset=bass.IndirectOffsetOnAxis(ap=dsti[k][:, j:j + 1], axis=0),
                                                             in_=tokid[:, k * 32 + j:k * 32 + j + 1], in_offset=None), reads=R_, writes=[b_rowinfo])
                F.dma("pool", lambda e: e.indirect_dma_start(out=roww_d, out_offset=bass.IndirectOffsetOnAxis(ap=dsti[k][:, j:j + 1], axis=0),
                                                             in_=wk[k][:, j:j + 1], in_offset=None), reads=R_, writes=[b_roww])
        if "rt_d" in dbg:
            rd = scratch("rt_d", [128, 6, 32], F32)
            rt = T("r_dbg", [128, 6, 32], F32)
            V(lambda e: e.tensor_copy(out=rt[:, 0, :], in_=dst[0][:]))
            V(lambda e: e.tensor_copy(out=rt[:, 1, :], in_=dst[1][:]))
            V(lambda e: e.tensor_copy(out=rt[:, 2, :], in_=wk[0][:]))
            V(lambda e: e.tensor_copy(out=rt[:, 3, :], in_=wk[1][:]))
            V(lambda e: e.tensor_copy(out=rt[:, 4, :], in_=tot[:]))
            V(lambda e: e.tensor_copy(out=rt[:, 5, :], in_=bef[:, 0:32]))
            F.dma("sp", lambda e: e.dma_start(out=rd, in_=rt[:]), reads=R_, writes=[Buf()])
        F.barrier()
    if stop_after == "R":
        F.barrier()
        return nc

    with ExitStack() as es:
        T = lambda nm, shp, dt: es.enter_context(nc.sbuf_tensor(nm, shp, dt))
        W1, W3, W2 = T("m_w1", [128, 16, 1024], BF16), T("m_w3", [128, 16, 1024], BF16), T("m_w2", [128, 8, D], BF16)
        bW1 = [Buf("w1_%d" % i) for i in range(4)]
        bW3 = [Buf("w3_%d" % i) for i in range(4)]
        bW2 = [Buf("w2_%d" % i) for i in range(4)]
        X = [T("m_x%d" % i, [128, D], BF16) for i in range(2)]
        XT = [T("m_xt%d" % i, [128, 16, 128], BF16) for i in range(2)]
        sl = [T("m_sl%d" % i, [128, 512], F32) for i in range(2)]
        h1 = [T("m_h1%d" % i, [128, 1024], BF16) for i in range(2)]
        h1T = [T("m_h1T%d" % i, [128, 8, 128], BF16) for i in range(2)]
        ysb = [T("m_y%d" % i, [128, D], BF16) for i in range(2)]
        ri = [T("m_ri%d" % i, [128, 2], I32) for i in range(2)]
        rw = [T("m_rw%d" % i, [128, 1], F32) for i in range(2)]
        bX, bXT, bsl, bh1, bh1T, bys, bri = ([Buf("mx%d" % i) for i in range(2)], [Buf("mxt%d" % i) for i in range(2)], [Buf("msl%d" % i) for i in range(2)],
                                             [Buf("mh1%d" % i) for i in range(2)], [Buf("mh1T%d" % i) for i in range(2)], [Buf("my%d" % i) for i in range(2)],
                                             [Buf("mri%d" % i) for i in range(2)])
        w1v = I["w1"].rearrange("e (p g k) n -> (e p g) (k n)", p=128, g=4, k=4)
        w3v = I["w3"].rearrange("e (p g k) n -> (e p g) (k n)", p=128, g=4, k=4)
        w2v = I["w2"].rearrange("e (p g k) n -> (e p g) (k n)", p=128, g=4, k=2)
        sc2k, sh2k = T("m_sc2k", [128, 16], F32), T("m_sh2k", [128, 16], F32)
        b_m2k = Buf("m2k")
        F.dma("sp", lambda e: e.dma_start(out=sh2k[:], in_=gates_d[3].rearrange("(p k) -> p k", k=16)), reads=[b_gates], writes=[b_m2k])
        F.dma("sp", lambda e: e.dma_start(out=sc2k[:], in_=gates_d[4].rearrange("(p k) -> p k", k=16)), reads=[b_gates], writes=[b_m2k])

        rb_w = nc.gpsimd.alloc_register("rb_w")
        nc.gpsimd.reg_mov(rb_w, 32 * 512 - 1)
        rb_y = nc.gpsimd.alloc_register("rb_y")
        nc.gpsimd.reg_mov(rb_y, 2 * TOWN - 1)

        def wload(i, dst_ap, src, g, bw):
            F.dma("pool", lambda e: e.indirect_dma_start(out=dst_ap, out_offset=None, in_=src,
                                                         in_offset=bass.IndirectOffsetOnAxis(ap=idxw[:, i, g:g + 1], axis=0),
                                                         bounds_check=rb_w, oob_is_err=False), reads=[b_bexp], writes=[bw])
        def rows(i):
            p = i % 2
            F.dma("sp", lambda e: e.dma_start(out=ri[p][:, 0:1], in_=rowinfo_d[i * 128:(i + 1) * 128, :]), reads=[b_rowinfo], writes=[bri[p]])
            F.op("dve", lambda e: e.tensor_single_scalar(out=ri[p][:, 1:2], in_=ri[p][:, 0:1], scalar=4095, op=ALU.bitwise_and), reads=[bri[p]], writes=[bri[p]])
            F.dma("pool", lambda e: e.indirect_dma_start(out=X[p][:], out_offset=None, in_=xn2_d, in_offset=bass.IndirectOffsetOnAxis(ap=ri[p][:, 1:2], axis=0)),
                  reads=[bri[p], b_xn2], writes=[bX[p]])

        def T1(i):
            p = i % 2
            for hb in range(2):
                for jj in range(8):
                    j = hb * 8 + jj
                    F.op("pe", lambda e: e.transpose(out=psT[hb][:, jj * 128:(jj + 1) * 128], in_=X[p][:, j:D:16], identity=ident_bf[:]),
                         reads=[bX[p], b_identbf], writes=[bpsT[hb]], signal=(jj == 7))

        def E1(i):
            p = i % 2
            for hb in range(2):
                for jj in range(8):
                    j = hb * 8 + jj
                    if jj % 2 == 0:
                        F.op("act", lambda e: e.activation(out=XT[p][:, j, :], in_=psT[hb][:, jj * 128:(jj + 1) * 128], func=AF.Identity,
                                                           bias=sh2k[:, j:j + 1], scale=sc2k[:, j:j + 1]), reads=[bpsT[hb], b_m2k], writes=[bXT[p]])
                    else:
                        F.op("dve", lambda e: e.tensor_scalar(out=XT[p][:, j, :], in0=psT[hb][:, jj * 128:(jj + 1) * 128],
                                                              scalar1=sc2k[:, j:j + 1], scalar2=sh2k[:, j:j + 1], op0=ALU.mult, op1=ALU.add),
                             reads=[bpsT[hb], b_m2k], writes=[bXT[p]])

        def W13(i):
            for kg in range(4):
                wload(i, W1[:, 4 * kg:4 * kg + 4, :].rearrange("p k n -> p (k n)"), w1v, kg, bW1[kg])
                wload(i, W3[:, 4 * kg:4 * kg + 4, :].rearrange("p k n -> p (k n)"), w3v, kg, bW3[kg])

        def W2l(i):
            for kg in range(4):
                wload(i, W2[:, 2 * kg:2 * kg + 2, :].rearrange("p k n -> p (k n)"), w2v, kg, bW2[kg])

        def H(i):
            p = i % 2
            for k in range(16):
                for n in range(2):
                    F.op("pe", lambda e: e.matmul(ps[2 * n][:], lhsT=XT[p][:, k, :], rhs=W1[:, k, n * 512:(n + 1) * 512], start=(k == 0), stop=(k == 15)),
                         reads=[bXT[p], bW1[k // 4]], writes=[bps[2 * n]], signal=(k % 4 == 3))
                    F.op("pe", lambda e: e.matmul(ps[2 * n + 1][:], lhsT=XT[p][:, k, :], rhs=W3[:, k, n * 512:(n + 1) * 512], start=(k == 0), stop=(k == 15)),
                         reads=[bXT[p], bW3[k // 4]], writes=[bps[2 * n + 1]], signal=(k % 4 == 3))
            for n in range(2):
                F.op("act", lambda e: e.activation(out=sl[n][:], in_=ps[2 * n][:], func=AF.Silu), reads=[bps[2 * n]], writes=[bsl[n]])
                F.op("dve", lambda e: e.tensor_tensor(out=h1[p][:, n * 512:(n + 1) * 512], in0=sl[n][:], in1=ps[2 * n + 1][:], op=ALU.mult),
                     reads=[bsl[n], bps[2 * n + 1]], writes=[bh1[p]])

        ps5b = ps[5][:].bitcast(BF16)

        def T2(i):
            p = i % 2
            for jj in range(8):
                F.op("pe", lambda e: e.transpose(out=ps5b[:, jj * 128:(jj + 1) * 128], in_=h1[p][:, jj:1024:8], identity=ident_bf[:]),
                     reads=[bh1[p], b_identbf], writes=[bps[5]], signal=(jj == 7))
            F.op("act", lambda e: e.activation(out=h1T[p][:, 0:4, :], in_=ps5b[:, 0:512].rearrange("p (k t) -> p k t", k=4), func=AF.Copy),
                 reads=[bps[5]], writes=[bh1T[p]])
            F.op("dve", lambda e: e.tensor_copy(out=h1T[p][:, 4:8, :], in_=ps5b[:, 512:1024].rearrange("p (k t) -> p k t", k=4)),
                 reads=[bps[5]], writes=[bh1T[p]])

        def Y(i):
            p = i % 2
            for n in range(4):
                py = ps[4 + n % 2]
                for k in range(8):
                    F.op("pe", lambda e: e.matmul(py[:], lhsT=h1T[p][:, k, :], rhs=W2[:, k, n * 512:(n + 1) * 512], start=(k == 0), stop=(k == 7)),
                         reads=[bh1T[p], bW2[k // 2]], writes=[bps[4 + n % 2]], signal=(k == 7))
                if n % 2 == 0:
                    F.op("act", lambda e: e.activation(out=ysb[p][:, n * 512:(n + 1) * 512], in_=py[:], func=AF.Identity, scale=rw3[i % 3][:, 0:1]),
                         reads=[bps[4 + n % 2], bri3[i % 3]], writes=[bys[p]])
                else:
                    F.op("dve", lambda e: e.tensor_scalar(out=ysb[p][:, n * 512:(n + 1) * 512], in0=py[:], scalar1=rw3[i % 3][:, 0:1], scalar2=None, op0=ALU.mult),
                         reads=[bps[4 + n % 2], bri3[i % 3]], writes=[bys[p]])

        def SC(i):
            p = i % 2
            F.dma("pool", lambda e: e.indirect_dma_start(out=ybuf_d, out_offset=bass.IndirectOffsetOnAxis(ap=ri3[i % 3][:, 0:1], axis=0), in_=ysb[p][:], in_offset=None,
                                                         bounds_check=rb_y, oob_is_err=False), reads=[bys[p], bri3[i % 3]], writes=[b_ybuf])
        ri3 = [T("m_ri3%d" % k, [128, 1], I32) for k in range(3)]
        bri3 = [Buf("mri3%d" % k) for k in range(3)]
        rw3 = [T("m_rw3%d" % k, [128, 1], F32) for k in range(3)]

        def rows3(i):
            F.dma("sp", lambda e: e.dma_start(out=ri3[i % 3][:], in_=rowinfo_d[i * 128:(i + 1) * 128, :]), reads=[b_rowinfo], writes=[bri3[i % 3]])
            F.dma("sp", lambda e: e.dma_start(out=rw3[i % 3][:], in_=roww_d[i * 128:(i + 1) * 128, :]), reads=[b_roww], writes=[bri3[i % 3]])
        W13(0)
        W2l(0)
        rows(0)
        rows3(0)
        rows(1)
        rows3(1)
        T1(0)
        E1(0)
        for i in range(NBLK):
            if i + 2 < NBLK:
                rows3(i + 2)
                rows(i + 2)
            H(i)
            if i + 1 < NBLK:
                W13(i + 1)
                T1(i + 1)
            T2(i)
            if i + 1 < NBLK:
                E1(i + 1)
            Y(i)
            if i + 1 < NBLK:
                W2l(i + 1)
            SC(i)
        F.barrier()
    if stop_after == "MOE":
        F.barrier()
        return nc

    with ExitStack() as es:
        T = lambda nm, shp, dt: es.enter_context(nc.sbuf_tensor(nm, shp, dt))
        g2b, lg2, lb2 = T("f_g2", [128, D], F32), T("f_lg", [128, D], F32), T("f_lb", [128, D], F32)
        bbc = Buf("f_bc")
        F.dma("sp", lambda e: e.dma_start(out=g2b[:], in_=gates_d[5:6, :].partition_broadcast(128)), reads=[b_gates], writes=[bbc])
        F.dma("sp", lambda e: e.dma_start(out=lg2[:], in_=I["lnrows"][2:3, :].partition_broadcast(128)), writes=[bbc])
        F.dma("sp", lambda e: e.dma_start(out=lb2[:], in_=I["lnrows"][3:4, :].partition_broadcast(128)), writes=[bbc])
        NB_ = 4
        xm = [T("f_xm%d" % i, [128, D], F32) for i in range(NB_)]
        y0 = [T("f_y0%d" % i, [128, D], BF16) for i in range(NB_)]
        y1 = [T("f_y1%d" % i, [128, D], BF16) for i in range(NB_)]
        ys = [T("f_ys%d" % i, [128, D], F32) for i in range(2)]
        bys_ = [Buf("fys%d" % i) for i in range(2)]
        st = [T("f_st%d" % i, [128, 24], F32) for i in range(NB_)]
        sm = [T("f_sm%d" % i, [128, 4], F32) for i in range(NB_)]
        bxm, by0, by1, bst = ([Buf("fxm%d" % i) for i in range(NB_)], [Buf("fy0%d" % i) for i in range(NB_)], [Buf("fy1%d" % i) for i in range(NB_)],
                              [Buf("fst%d" % i) for i in range(NB_)])
        def f_loads(ti):
            p = ti % NB_
            r0 = ti * 128
            F.dma("sp", lambda e: e.dma_start(out=xm[p][:], in_=xmid_d[r0:r0 + 128, :]), reads=[b_xmid], writes=[bxm[p]])
            F.dma("sp", lambda e: e.dma_start(out=y0[p][:], in_=ybuf_d[r0:r0 + 128, :]), reads=[b_ybuf], writes=[by0[p]])
            F.dma("sp", lambda e: e.dma_start(out=y1[p][:], in_=ybuf_d[TOWN + r0:TOWN + r0 + 128, :]), reads=[b_ybuf], writes=[by1[p]])
        for ti in range(3):
            f_loads(ti)
        for ti in range(32):
            p = ti % NB_
            r0 = ti * 128
            if ti + 3 < 32:
                f_loads(ti + 3)
            q = ti % 2
            F.op("pool", lambda e: e.tensor_tensor(out=ys[q][:], in0=y0[p][:], in1=y1[p][:], op=ALU.add), reads=[by0[p], by1[p]], writes=[bys_[q]])
            F.op("pool", lambda e: e.tensor_tensor(out=ys[q][:], in0=ys[q][:], in1=g2b[:], op=ALU.mult), reads=[bys_[q], bbc], writes=[bys_[q]])
            F.op("dve", lambda e: e.scalar_tensor_tensor(out=xm[p][:], in0=xm[p][:], scalar=ALPHA, in1=ys[q][:], op0=ALU.mult, op1=ALU.add),
                 reads=[bxm[p], bys_[q]], writes=[bxm[p]])
            mv, rs, nmr = sm[p][:, 0:2], sm[p][:, 2:3], sm[p][:, 3:4]
            ln_stats(xm[p], bxm[p], st[p], mv, rs, nmr, bst[p])
            F.op("act", lambda e: e.activation(out=xm[p][:], in_=xm[p][:], func=AF.Identity, bias=nmr, scale=rs), reads=[bxm[p], bst[p]], writes=[bxm[p]])
            F.op("dve", lambda e: e.tensor_tensor(out=xm[p][:], in0=xm[p][:], in1=lg2[:], op=ALU.mult), reads=[bxm[p], bbc], writes=[bxm[p]])
            F.op("dve", lambda e: e.tensor_tensor(out=xm[p][:], in0=xm[p][:], in1=lb2[:], op=ALU.add), reads=[bxm[p], bbc], writes=[bxm[p]])
            F.dma("act", lambda e: e.dma_start(out=out_ap[r0:r0 + 128, :], in_=xm[p][:]), reads=[bxm[p]], writes=[b_out])
        F.barrier()
    F.finish([b_out], "sp")
    return nc


def kernel(**inputs):
    inp = {k: np.asarray(v) for k, v in inputs.items()}
    nc = build()
    in_maps = []
    for core in range(8):
        b, half = core // 2, core % 2
        in_maps.append(host_layout(inp, b, half))
    res = run_bass_kernel_spmd(nc, in_maps, core_ids=list(range(8)))
    out = np.zeros((4, TALL, D), np.float32)
    for core in range(8):
        b, half = core // 2, core % 2
        out[b, half * TOWN:(half + 1) * TOWN] = res.results[core]["out"]
    return out
```
